# Optimizing a Trainium2 kernel written in Bass

```python
import math
import jax
import jax.numpy as jnp
from jax import lax
import numpy as np


D_MODEL = 1024
BATCH = 8
SEQ = 8192
DEPTH = 2

GRID_W = 64
CTX_LEN = 256
EPS = 1e-6

MIX_WIDTH = 1024
GMLP_CHUNK = 128
GMLP_GROUPS = 4
GMLP_GROUP_DIM = 128
GMLP_WIDTH = GMLP_GROUPS * GMLP_GROUP_DIM
HGRN_HEADS = 4
HGRN_EXPAND = 128
HGRN_HEAD_V = 128
HGRN_WIDTH = HGRN_HEADS * HGRN_HEAD_V
EVEN_IN = 2 * GMLP_WIDTH + 5 * HGRN_WIDTH
GDN_HEADS = 4
GDN_HEAD_DIM = 128
GDN_WIDTH = GDN_HEADS * GDN_HEAD_DIM
GDN_CONV = 5
MLA_HEADS = 8
MLA_NOPE = 64
MLA_ROPE = 32
MLA_V = 64
MLA_Q_RANK = 256
MLA_KV_RANK = 128
MLA_WIDTH = MLA_HEADS * MLA_V
MLA_SCALE = (MLA_NOPE + MLA_ROPE) ** -0.5
ODD_IN = 4 * GDN_WIDTH + 4 * GDN_HEADS + MLA_Q_RANK + MLA_KV_RANK + MLA_ROPE
ROPE_THETA = 10000.0
ATTN_BLOCK = 128
SCAN_CHUNK = 64
N_GROUPS = 4
EXPERTS_PER_GROUP = 8
N_EXPERTS = N_GROUPS * EXPERTS_PER_GROUP
TOP_K = 2
EXPERT_FF = 512
MOE_BLOCK = 256

kernel_name = 'hybrid_gmlp_hgrn2_gdn_mla_hmoe_trunk'


def rms_norm(x, w):
    xf = x.astype(jnp.float32)
    y = xf * lax.rsqrt(jnp.mean(xf * xf, axis=-1, keepdims=True) + EPS)
    return (y * w.astype(jnp.float32)).astype(x.dtype)


def l2_norm(x):
    xf = x.astype(jnp.float32)
    return (xf * lax.rsqrt(jnp.sum(xf * xf, axis=-1, keepdims=True) + EPS)).astype(x.dtype)


def modulate(h, shift, scale):
    return h * (1 + scale) + shift


def to_heads(t, n_heads):
    b, l, _ = t.shape
    return t.reshape(b, l, n_heads, -1).transpose(0, 2, 1, 3)


def from_heads(t):
    b, n, l, d = t.shape
    return t.transpose(0, 2, 1, 3).reshape(b, l, n * d)


def flip(t):
    return jnp.flip(t, axis=2)


def axial_rope(t):
    l = t.shape[1]
    rows = l // GRID_W
    row = jnp.repeat(jnp.arange(rows), GRID_W)
    col = jnp.tile(jnp.arange(GRID_W), rows)
    half = MLA_ROPE // 2
    quarter = half // 2
    freqs = ROPE_THETA ** (-jnp.arange(quarter, dtype=jnp.float32) / quarter)

    def rot(u, pos):
        ang = pos.astype(jnp.float32)[:, None] * freqs
        cos = jnp.cos(ang)[:, None, :].astype(u.dtype)
        sin = jnp.sin(ang)[:, None, :].astype(u.dtype)
        u1, u2 = u[..., :quarter], u[..., quarter:]
        return jnp.concatenate([u1 * cos - u2 * sin, u2 * cos + u1 * sin], axis=-1)

    return jnp.concatenate([rot(t[..., :half], row), rot(t[..., half:], col)], axis=-1)


def short_conv(t, w):
    pad = (GDN_CONV - 1) // 2
    return lax.conv_general_dilated(t, w[:, None, :].astype(t.dtype), window_strides=(1,),
                                    padding=[(pad, pad)], dimension_numbers=('NWC', 'WIO', 'NWC'),
                                    feature_group_count=t.shape[-1])


def gla_chunked(q, k, v, log_f, s0):
    out_dtype = v.dtype
    b, h, l, _ = q.shape
    dv = v.shape[-1]
    n = l // SCAN_CHUNK

    def chunks(t):
        return jnp.moveaxis(t.astype(jnp.float32).reshape(b, h, n, SCAN_CHUNK, t.shape[-1]), 2, 0)

    causal = jnp.tril(jnp.ones((SCAN_CHUNK, SCAN_CHUNK), bool))

    def step(s, inp):
        qc, kc, vc, lf = inp
        g = jnp.cumsum(lf, axis=-2)
        diff = g[..., :, None, :] - g[..., None, :, :]
        decay = jnp.exp(jnp.where(causal[:, :, None], diff, -jnp.inf))
        scores = jnp.einsum('bhid,bhjd,bhijd->bhij', qc, kc, decay)
        o = scores @ vc + (qc * jnp.exp(g)) @ s
        g_last = g[..., -1:, :]
        s_new = jnp.exp(g_last[..., 0, :])[..., None] * s + jnp.einsum('bhjd,bhje->bhde', kc * jnp.exp(g_last - g), vc)
        return s_new, o

    s_fin, o = lax.scan(step, s0.astype(jnp.float32), (chunks(q), chunks(k), chunks(v), chunks(log_f)))
    return jnp.moveaxis(o, 0, 2).reshape(b, h, l, dv).astype(out_dtype), s_fin


def gated_delta_chunked(q, k, v, log_a, beta, s0):
    out_dtype = v.dtype
    b, h, l, dk = q.shape
    dv = v.shape[-1]
    cs = SCAN_CHUNK
    n = l // cs
    qc = q.astype(jnp.float32).reshape(b, h, n, cs, dk)
    kc = k.astype(jnp.float32).reshape(b, h, n, cs, dk)
    vc = v.astype(jnp.float32).reshape(b, h, n, cs, dv)
    bc = beta.astype(jnp.float32).reshape(b, h, n, cs)
    g = jnp.cumsum(log_a.astype(jnp.float32).reshape(b, h, n, cs), axis=-1)
    incl = jnp.tril(jnp.ones((cs, cs), bool))
    strict = jnp.tril(jnp.ones((cs, cs), bool), -1)
    gamma = jnp.exp(jnp.where(incl, g[..., :, None] - g[..., None, :], -jnp.inf))
    kk = jnp.einsum('bhnid,bhnjd->bhnij', kc, kc)
    tri_mat = jnp.where(strict, bc[..., :, None] * kk * gamma, 0.0) + jnp.eye(cs, dtype=jnp.float32)

    def solve(rhs):
        return lax.linalg.triangular_solve(tri_mat, rhs, left_side=True, lower=True)

    u = solve(bc[..., None] * vc)
    w = solve(bc[..., None] * kc * jnp.exp(g)[..., None])
    qk = jnp.where(incl, jnp.einsum('bhnid,bhnjd->bhnij', qc, kc) * gamma, 0.0)
    qg = qc * jnp.exp(g)[..., None]
    kg = kc * jnp.exp(g[..., -1:] - g)[..., None]
    a_last = jnp.exp(g[..., -1])

    def step(s, inp):
        u_c, w_c, qk_c, qg_c, kg_c, a_c = inp
        v_new = u_c - w_c @ s
        o = qg_c @ s + qk_c @ v_new
        s_new = a_c[..., None, None] * s + jnp.swapaxes(kg_c, -1, -2) @ v_new
        return s_new, o

    xs = tuple(jnp.moveaxis(t, 2, 0) for t in (u, w, qk, qg, kg, a_last))
    s_fin, o = lax.scan(step, s0.astype(jnp.float32), xs)
    return jnp.moveaxis(o, 0, 2).reshape(b, h, l, dv).astype(out_dtype), s_fin


def two_way(scan_fn, ctx_fwd, lat_fwd, ctx_bwd, lat_bwd, s0):
    oc_f, sc_f = scan_fn(*ctx_fwd, s0)
    ox_f, _ = scan_fn(*lat_fwd, sc_f)
    oc_b, sc_b = scan_fn(*[flip(t) for t in ctx_bwd], s0)
    ox_b, _ = scan_fn(*[flip(t) for t in lat_bwd], sc_b)
    return oc_f + flip(oc_b), ox_f + flip(ox_b)


def lower_bound(logits, layer):
    return jnp.cumsum(jax.nn.softmax(logits.astype(jnp.float32), axis=0), axis=0)[layer]


def hgrn_gates(f_raw, lb):
    f = lb + (1.0 - lb) * jax.nn.sigmoid(f_raw.astype(jnp.float32))
    return jnp.log(f), 1.0 - f


def chunk_gmlp(p, norm_w, ws, bs):
    b, l, _ = p.shape
    z = jax.nn.gelu(p)
    u = z[..., :GMLP_WIDTH]
    v = rms_norm(z[..., GMLP_WIDTH:], norm_w)
    v = v.reshape(b, l // GMLP_CHUNK, GMLP_CHUNK, GMLP_GROUPS, GMLP_GROUP_DIM)
    s = jnp.einsum('gpq,bnqgc->bnpgc', ws, v) + bs.T[:, :, None]
    return u * s.reshape(b, l, GMLP_WIDTH)


def attend(q, k, v):
    s = jnp.einsum('bhqd,bhkd->bhqk', q, k, preferred_element_type=jnp.float32) * MLA_SCALE
    p = jax.nn.softmax(s, axis=-1)
    return jnp.einsum('bhqk,bhkd->bhqd', p.astype(v.dtype), v)


def blocked_attend(q, k, v):
    b, h, l, d = q.shape
    nb = l // ATTN_BLOCK
    qb = jnp.moveaxis(q.reshape(b, h, nb, ATTN_BLOCK, d), 2, 0)
    ob = lax.map(lambda qq: attend(qq, k, v), qb)
    return jnp.moveaxis(ob, 0, 2).reshape(b, h, l, v.shape[-1])


def mla_queries(cq, norm_w, w_up, qk_w, rotary):
    b, l, _ = cq.shape
    q = (rms_norm(cq, norm_w) @ w_up).reshape(b, l, MLA_HEADS, MLA_NOPE + MLA_ROPE)
    q = rms_norm(q, qk_w)
    if rotary:
        q = jnp.concatenate([q[..., :MLA_NOPE], axial_rope(q[..., MLA_NOPE:])], axis=-1)
    return q.transpose(0, 2, 1, 3)


def mla_keys_values(ckv, k_rope, norm_w, w_up, qk_w, rotary):
    b, l, _ = ckv.shape
    kv = (rms_norm(ckv, norm_w) @ w_up).reshape(b, l, MLA_HEADS, MLA_NOPE + MLA_V)
    k_shared = jnp.broadcast_to(k_rope[:, :, None, :], (b, l, MLA_HEADS, MLA_ROPE))
    k = rms_norm(jnp.concatenate([kv[..., :MLA_NOPE], k_shared], axis=-1), qk_w)
    if rotary:
        k = jnp.concatenate([k[..., :MLA_NOPE], axial_rope(k[..., MLA_NOPE:])], axis=-1)
    return k.transpose(0, 2, 1, 3), kv[..., MLA_NOPE:].transpose(0, 2, 1, 3)


def even_mixer(hx, hc, layer, need_ctx, w_in, w_out, norm_w, ws, bs, lb_logits, out_norm_w):
    b = hx.shape[0]
    px = hx @ w_in
    pc = hc @ w_in
    lb_f = lower_bound(lb_logits[0], layer)
    lb_b = lower_bound(lb_logits[1], layer)
    a_cols = 2 * GMLP_WIDTH

    def hgrn_inputs(p):
        q, i, f_f, f_b, g = jnp.split(p[..., a_cols:], 5, axis=-1)
        logf_f, k_f = hgrn_gates(f_f, lb_f)
        logf_b, k_b = hgrn_gates(f_b, lb_b)
        qh = to_heads(q, HGRN_HEADS)
        ih = to_heads(i, HGRN_HEADS)
        fwd = (qh, to_heads(k_f, HGRN_HEADS), ih, to_heads(logf_f, HGRN_HEADS))
        bwd = (qh, to_heads(k_b, HGRN_HEADS), ih, to_heads(logf_b, HGRN_HEADS))
        return fwd, bwd, g

    cf, cb, gc = hgrn_inputs(pc)
    xf, xb, gx = hgrn_inputs(px)
    s0 = jnp.zeros((b, HGRN_HEADS, HGRN_EXPAND, HGRN_HEAD_V), jnp.float32)
    oc, ox = two_way(gla_chunked, cf, xf, cb, xb, s0)

    def merge(p, o, g):
        rec = rms_norm(o.transpose(0, 2, 1, 3), out_norm_w.reshape(HGRN_HEADS, HGRN_HEAD_V))
        rec = rec.reshape(g.shape) * jax.nn.silu(g)
        mix = chunk_gmlp(p[..., :a_cols], norm_w, ws, bs)
        return jnp.concatenate([mix, rec], axis=-1) @ w_out

    yx = merge(px, ox, gx)
    yc = merge(pc, oc, gc) if need_ctx else None
    return yx, yc


def odd_mixer(hx, hc, need_ctx, w_in, w_out, conv_w, a_log, dt_bias, gdn_norm_w,
              q_norm_w, wq_up, kv_norm_w, wkv_up, qk_q, qk_k):
    b = hx.shape[0]
    wd, nh = GDN_WIDTH, GDN_HEADS
    o_gate = 3 * wd
    o_beta = 4 * wd
    o_decay = o_beta + 2 * nh
    o_cq = o_decay + 2 * nh
    o_ckv = o_cq + MLA_Q_RANK
    o_kr = o_ckv + MLA_KV_RANK
    px = hx @ w_in
    pc = hc @ w_in
    a_neg = -jnp.exp(a_log.astype(jnp.float32)).reshape(2 * nh)
    dt_b = dt_bias.astype(jnp.float32).reshape(2 * nh)

    def gdn_inputs(p):
        qkv = jax.nn.silu(short_conv(p[..., :3 * wd], conv_w))
        q = l2_norm(to_heads(qkv[..., :wd], nh)) * (GDN_HEAD_DIM ** -0.5)
        k = l2_norm(to_heads(qkv[..., wd:2 * wd], nh))
        v = to_heads(qkv[..., 2 * wd:], nh)
        beta = jax.nn.sigmoid(p[..., o_beta:o_decay].astype(jnp.float32)).transpose(0, 2, 1)
        log_a = (a_neg * jax.nn.softplus(p[..., o_decay:o_cq].astype(jnp.float32) + dt_b)).transpose(0, 2, 1)
        fwd = (q, k, v, log_a[:, :nh], beta[:, :nh])
        bwd = (q, k, v, log_a[:, nh:], beta[:, nh:])
        return fwd, bwd

    cf, cb = gdn_inputs(pc)
    xf, xb = gdn_inputs(px)
    s0 = jnp.zeros((b, nh, GDN_HEAD_DIM, GDN_HEAD_DIM), jnp.float32)
    oc, ox = two_way(gated_delta_chunked, cf, xf, cb, xb, s0)

    def gdn_out(o, p):
        z = p[..., o_gate:o_beta]
        return rms_norm(o.transpose(0, 2, 1, 3), gdn_norm_w).reshape(z.shape) * jax.nn.silu(z)

    kx, vx = mla_keys_values(px[..., o_ckv:o_kr], px[..., o_kr:], kv_norm_w, wkv_up, qk_k, True)
    kc, vc = mla_keys_values(pc[..., o_ckv:o_kr], pc[..., o_kr:], kv_norm_w, wkv_up, qk_k, False)
    qx = mla_queries(px[..., o_cq:o_ckv], q_norm_w, wq_up, qk_q, True)
    k_all = jnp.concatenate([kc, kx], axis=2)
    v_all = jnp.concatenate([vc, vx], axis=2)
    att_x = blocked_attend(qx, k_all, v_all)
    yx = jnp.concatenate([gdn_out(ox, px), from_heads(att_x)], axis=-1) @ w_out
    if not need_ctx:
        return yx, None
    qc = mla_queries(pc[..., o_cq:o_ckv], q_norm_w, wq_up, qk_q, False)
    att_c = attend(qc, kc, vc)
    yc = jnp.concatenate([gdn_out(oc, pc), from_heads(att_c)], axis=-1) @ w_out
    return yx, yc


def hier_moe(h, wg_r, bg_r, we_r, be_r, w_gate, w_up, w_down):
    t, d = h.shape
    hf = h.astype(jnp.float32)
    pg = jax.nn.softmax(hf @ wg_r.astype(jnp.float32) + bg_r.astype(jnp.float32), axis=-1)
    pg_top, g_idx = lax.top_k(pg, 1)
    le = (hf @ we_r.astype(jnp.float32) + be_r.astype(jnp.float32)).reshape(t, N_GROUPS, EXPERTS_PER_GROUP)
    le = jnp.take_along_axis(le, g_idx[:, :, None], axis=1)[:, 0]
    pe_top, e_local = lax.top_k(jax.nn.softmax(le, axis=-1), TOP_K)
    wts = pg_top * pe_top / jnp.sum(pe_top, axis=-1, keepdims=True)
    e_idx = g_idx * EXPERTS_PER_GROUP + e_local
    m = t * TOP_K
    e_flat = e_idx.reshape(m)
    w_flat = wts.reshape(m)
    tok_flat = jnp.repeat(jnp.arange(t, dtype=jnp.int32), TOP_K)
    order = jnp.argsort(e_flat, stable=True)
    e_s, tok_s, w_s = e_flat[order], tok_flat[order], w_flat[order]
    counts = jnp.bincount(e_flat, length=N_EXPERTS)
    padded = (counts + MOE_BLOCK - 1) // MOE_BLOCK * MOE_BLOCK
    start = jnp.cumsum(counts) - counts
    pend = jnp.cumsum(padded)
    pstart = pend - padded
    dest = pstart[e_s] + jnp.arange(m, dtype=jnp.int32) - start[e_s]
    n_blk = -(-m // MOE_BLOCK) + N_EXPERTS
    slots = n_blk * MOE_BLOCK
    slot_tok = jnp.full((slots,), t, jnp.int32).at[dest].set(tok_s)
    slot_w = jnp.zeros((slots,), jnp.float32).at[dest].set(w_s)
    blk_e = jnp.minimum(jnp.searchsorted(pend, jnp.arange(n_blk, dtype=jnp.int32) * MOE_BLOCK, side='right'),
                        N_EXPERTS - 1)
    xs = jnp.concatenate([h, jnp.zeros((1, d), h.dtype)], axis=0)[slot_tok].reshape(n_blk, MOE_BLOCK, d)

    def expert(args):
        xb, e = args
        return (jax.nn.silu(xb @ w_gate[e]) * (xb @ w_up[e])) @ w_down[e]

    ys = lax.map(expert, (xs, blk_e)).reshape(slots, d)
    ys = ys * slot_w[:, None].astype(ys.dtype)
    return jax.ops.segment_sum(ys, slot_tok, num_segments=t + 1)[:t]


def setup_inputs(seed: int = 0) -> dict:
    key = jax.random.key(seed)
    ks = iter(jax.random.split(key, 48))
    d = D_MODEL
    ne = (DEPTH + 1) // 2
    no = DEPTH // 2

    def nrm(shape, scale):
        return jax.random.normal(next(ks), shape, jnp.float32) * scale

    dt = jnp.exp(jax.random.uniform(next(ks), (no, 2, GDN_HEADS), jnp.float32,
                                    minval=math.log(1e-3), maxval=math.log(1e-1)))
    a_init = jax.random.uniform(next(ks), (no, 2, GDN_HEADS), jnp.float32, minval=1.0, maxval=16.0)
    return {
        'x': nrm((BATCH, SEQ, d), 1.0),
        'c': nrm((BATCH, d), 1.0),
        'ctx': nrm((BATCH, CTX_LEN, d), 1.0),
        'c_ctx': nrm((d,), 1.0),
        'ada_w': nrm((DEPTH, d, 6 * d), 0.5 * d ** -0.5),
        'ada_b': nrm((DEPTH, 6 * d), 0.01),
        'norm_mix_w': 1.0 + nrm((DEPTH, d), 0.01),
        'norm_ffn_w': 1.0 + nrm((DEPTH, d), 0.01),
        'even_w_in': nrm((ne, d, EVEN_IN), d ** -0.5),
        'even_w_out': nrm((ne, MIX_WIDTH, d), MIX_WIDTH ** -0.5),
        'gmlp_norm_w': 1.0 + nrm((ne, GMLP_WIDTH), 0.01),
        'gmlp_ws': nrm((ne, GMLP_GROUPS, GMLP_CHUNK, GMLP_CHUNK), GMLP_CHUNK ** -0.5),
        'gmlp_bs': 1.0 + nrm((ne, GMLP_GROUPS, GMLP_CHUNK), 0.01),
        'hgrn_lb_logits': nrm((2, DEPTH + 1, HGRN_WIDTH), 0.5),
        'hgrn_norm_w': 1.0 + nrm((ne, HGRN_WIDTH), 0.01),
        'odd_w_in': nrm((no, d, ODD_IN), d ** -0.5),
        'odd_w_out': nrm((no, MIX_WIDTH, d), MIX_WIDTH ** -0.5),
        'gdn_conv_w': nrm((no, GDN_CONV, 3 * GDN_WIDTH), GDN_CONV ** -0.5),
        'gdn_a_log': jnp.log(a_init),
        'gdn_dt_bias': dt + jnp.log(-jnp.expm1(-dt)),
        'gdn_norm_w': 1.0 + nrm((no, GDN_HEAD_DIM), 0.01),
        'mla_q_norm_w': 1.0 + nrm((no, MLA_Q_RANK), 0.01),
        'mla_wq_up': nrm((no, MLA_Q_RANK, MLA_HEADS * (MLA_NOPE + MLA_ROPE)), MLA_Q_RANK ** -0.5),
        'mla_kv_norm_w': 1.0 + nrm((no, MLA_KV_RANK), 0.01),
        'mla_wkv_up': nrm((no, MLA_KV_RANK, MLA_HEADS * (MLA_NOPE + MLA_V)), MLA_KV_RANK ** -0.5),
        'mla_qk_norm_q': 1.0 + nrm((no, MLA_NOPE + MLA_ROPE), 0.01),
        'mla_qk_norm_k': 1.0 + nrm((no, MLA_NOPE + MLA_ROPE), 0.01),
        'router_group_w': nrm((DEPTH, d, N_GROUPS), d ** -0.5),
        'router_group_b': nrm((DEPTH, N_GROUPS), 0.01),
        'router_expert_w': nrm((DEPTH, d, N_EXPERTS), d ** -0.5),
        'router_expert_b': nrm((DEPTH, N_EXPERTS), 0.01),
        'moe_w_gate': nrm((DEPTH, N_EXPERTS, d, EXPERT_FF), d ** -0.5),
        'moe_w_up': nrm((DEPTH, N_EXPERTS, d, EXPERT_FF), d ** -0.5),
        'moe_w_down': nrm((DEPTH, N_EXPERTS, EXPERT_FF, d), EXPERT_FF ** -0.5),
    }


def reference(x, c, ctx, c_ctx, ada_w, ada_b, norm_mix_w, norm_ffn_w,
              even_w_in, even_w_out, gmlp_norm_w, gmlp_ws, gmlp_bs, hgrn_lb_logits, hgrn_norm_w,
              odd_w_in, odd_w_out, gdn_conv_w, gdn_a_log, gdn_dt_bias, gdn_norm_w,
              mla_q_norm_w, mla_wq_up, mla_kv_norm_w, mla_wkv_up, mla_qk_norm_q, mla_qk_norm_k,
              router_group_w, router_group_b, router_expert_w, router_expert_b,
              moe_w_gate, moe_w_up, moe_w_down):
    b, n_lat, d = x.shape
    xc = ctx
    silu_c = jax.nn.silu(c)
    silu_cc = jax.nn.silu(c_ctx)
    for layer in range(DEPTH):
        need_ctx = layer < DEPTH - 1
        j = layer // 2
        mx = jnp.split((silu_c @ ada_w[layer] + ada_b[layer])[:, None, :], 6, axis=-1)
        mc = jnp.split(silu_cc @ ada_w[layer] + ada_b[layer], 6, axis=-1)
        hx = modulate(rms_norm(x, norm_mix_w[layer]), mx[0], mx[1])
        hc = modulate(rms_norm(xc, norm_mix_w[layer]), mc[0], mc[1])
        if layer % 2 == 0:
            yx, yc = even_mixer(hx, hc, layer, need_ctx, even_w_in[j], even_w_out[j], gmlp_norm_w[j],
                                gmlp_ws[j], gmlp_bs[j], hgrn_lb_logits, hgrn_norm_w[j])
        else:
            yx, yc = odd_mixer(hx, hc, need_ctx, odd_w_in[j], odd_w_out[j], gdn_conv_w[j], gdn_a_log[j],
                               gdn_dt_bias[j], gdn_norm_w[j], mla_q_norm_w[j], mla_wq_up[j],
                               mla_kv_norm_w[j], mla_wkv_up[j], mla_qk_norm_q[j], mla_qk_norm_k[j])
        x = x + mx[2] * yx
        moe_params = (router_group_w[layer], router_group_b[layer], router_expert_w[layer],
                      router_expert_b[layer], moe_w_gate[layer], moe_w_up[layer], moe_w_down[layer])
        hx2 = modulate(rms_norm(x, norm_ffn_w[layer]), mx[3], mx[4])
        if need_ctx:
            xc = xc + mc[2] * yc
            hc2 = modulate(rms_norm(xc, norm_ffn_w[layer]), mc[3], mc[4])
            tokens = jnp.concatenate([hx2.reshape(-1, d), hc2.reshape(-1, d)], axis=0)
            y = hier_moe(tokens, *moe_params)
            x = x + mx[5] * y[:b * n_lat].reshape(x.shape)
            xc = xc + mc[5] * y[b * n_lat:].reshape(xc.shape)
        else:
            x = x + mx[5] * hier_moe(hx2.reshape(-1, d), *moe_params).reshape(x.shape)
    return x
```

```python
import os
import numpy as np
import concourse.bass as bass
import concourse.mybir as mybir
from concourse.bass_utils import run_bass_kernel_spmd
from contextlib import ExitStack

F32 = mybir.dt.float32
BF16 = mybir.dt.bfloat16
I32 = mybir.dt.int32
AF = mybir.ActivationFunctionType
ALU = mybir.AluOpType
AX = mybir.AxisListType
EPS = 1e-6
D = 1024
CTX = 256


class Buf:
    __slots__ = ("t", "w", "r", "name", "psum")

    def __init__(self, t, name="", psum=False):
        self.t = t
        self.w = None
        self.r = {}
        self.name = name
        self.psum = psum

    def __getitem__(self, k):
        return self.t[k]


class Bv:
    def __init__(self, base, ap):
        self.base = base
        self.ap = ap

    @property
    def psum(self):
        return self.base.psum

    def __getitem__(self, kk):
        return self.ap[kk]

    @property
    def w(self):
        return self.base.w

    @w.setter
    def w(self, v):
        self.base.w = v

    @property
    def r(self):
        return self.base.r

    @r.setter
    def r(self, v):
        self.base.r = v


class Pool:
    def __init__(self, bufs):
        self.bufs = bufs
        self.i = 0

    def next(self):
        b = self.bufs[self.i % len(self.bufs)]
        self.i += 1
        return b


class KB:
    NDMA = 8

    def __init__(self, nc):
        self.nc = nc
        self.es = ExitStack()
        self.eng = {"pe": nc.tensor, "dve": nc.vector, "act": nc.scalar, "pool": nc.gpsimd, "sp": nc.sync}
        self.sems = {}
        self.cnt = {}
        self.seen = {e: {} for e in self.eng}
        for e in self.eng:
            self.sems[e] = self.es.enter_context(nc.semaphore("c_" + e))
            self.cnt[e] = 0
        self.dsem = {}
        self.dcnt = {}
        for q in ("sp", "pool", "act"):
            self.dsem[q] = [self.es.enter_context(nc.semaphore("d_%s%d" % (q, i))) for i in range(self.NDMA)]
            self.dcnt[q] = 0
        self.semobj = {}
        for e in self.eng:
            self.semobj[("c", e)] = self.sems[e]
        for q in self.dsem:
            for i, s in enumerate(self.dsem[q]):
                self.semobj[("d", q, i)] = s
        self.nbuf = 0

    def sb(self, name, shape, dtype, es=None):
        self.nbuf += 1
        t = (es or self.es).enter_context(self.nc.sbuf_tensor("%s_%d" % (name, self.nbuf), list(shape), dtype))
        return Buf(t, name)

    def ps(self, name, shape, dtype, es=None):
        self.nbuf += 1
        t = (es or self.es).enter_context(self.nc.psum_tensor("%s_%d" % (name, self.nbuf), list(shape), dtype))
        return Buf(t, name, psum=True)

    def sbpool(self, name, shape, dtype, n, es=None):
        return Pool([self.sb(name, shape, dtype, es) for _ in range(n)])

    def pspool(self, name, shape, dtype, n, es=None):
        return Pool([self.ps(name, shape, dtype, es) for _ in range(n)])

    def dram(self, name, shape, dtype, kind="Internal"):
        return self.nc.dram_tensor(name, list(shape), dtype, kind=kind).ap()

    def _deps(self, reads, writes, e=None):
        deps = []
        for b in reads:
            if b.w is not None:
                deps.append(b.w)
            if b.psum:
                for kk, v in b.r.items():
                    if kk != ("c", e):
                        deps.append((kk, v))
        for b in writes:
            if b.w is not None:
                deps.append(b.w)
            for kk, v in b.r.items():
                deps.append((kk, v))
        return deps

    def _wait(self, e, deps):
        seen = self.seen[e]
        need = {}
        for kk, v in deps:
            if e == "pe" and kk == ("c", "pe"):
                continue
            if seen.get(kk, 0) >= v:
                continue
            if need.get(kk, 0) < v:
                need[kk] = v
        for kk, v in need.items():
            self.eng[e].wait_ge(self.semobj[kk], v)
            seen[kk] = v

    def _mark(self, tok, reads, writes):
        kk, v = tok
        for b in reads:
            if b.r.get(kk, 0) < v:
                b.r[kk] = v
        for b in writes:
            b.w = tok
            b.r = {}

    def op(self, e, fn, reads=(), writes=()):
        self._wait(e, self._deps(reads, writes, e))
        ins = fn(self.eng[e])
        self.cnt[e] += 1
        ins.then_inc(self.sems[e], 1)
        tok = (("c", e), self.cnt[e])
        self._mark(tok, reads, writes)
        return tok

    def dma(self, q, out, in_, reads=(), writes=(), **kw):
        i = self.dcnt[q]
        slot = i % self.NDMA
        kk = ("d", q, slot)
        deps = self._deps(reads, writes, q)
        prev = 16 * (i // self.NDMA)
        if prev > 0:
            deps.append((kk, prev))
        self._wait(q, deps)
        ins = self.eng[q].dma_start(out=out, in_=in_, **kw)
        ins.then_inc(self.dsem[q][slot], 16)
        self.dcnt[q] += 1
        tok = (kk, prev + 16)
        self._mark(tok, reads, writes)
        return tok

    def all_tokens(self):
        deps = []
        for e in self.eng:
            if self.cnt[e] > 0:
                deps.append((("c", e), self.cnt[e]))
        for q in self.dsem:
            n = self.dcnt[q]
            for slot in range(self.NDMA):
                c = (n - slot + self.NDMA - 1) // self.NDMA
                if c > 0:
                    deps.append((("d", q, slot), 16 * c))
        return deps

    def barrier(self):
        deps = self.all_tokens()
        for e in self.eng:
            self._wait(e, deps)

    def drain_all(self):
        self._wait("sp", self.all_tokens())


def build(L, stop_after="all", dbg=False):
    nc = bass.Bass("TRN2", target_bir_lowering=False)
    k = KB(nc)
    NT = (CTX + L) // 128
    NL = L // 128
    T = CTX + L

    def din(name, shape):
        return nc.dram_tensor(name, list(shape), F32, kind="ExternalInput").ap()

    x_in = din("x", [L, D])
    ctx_in = din("ctx", [CTX, D])
    c_in = din("c", [D])
    cctx_in = din("c_ctx", [D])
    ada_w = din("ada_w", [2, D, 6 * D])
    ada_b = din("ada_b", [2, 6 * D])
    norm_mix_w = din("norm_mix_w", [2, D])
    norm_ffn_w = din("norm_ffn_w", [2, D])
    even_w_in = din("even_w_in", [1, D, 3584])
    even_w_out = din("even_w_out", [1, D, D])
    gmlp_norm_w = din("gmlp_norm_w", [1, 512])
    gmlp_ws = din("gmlp_ws", [1, 4, 128, 128])
    gmlp_bs = din("gmlp_bs", [1, 4, 128])
    hgrn_lb = din("hgrn_lb_logits", [2, 3, 512])
    hgrn_norm_w = din("hgrn_norm_w", [1, 512])
    router_group_w = din("router_group_w", [2, D, 4])
    router_group_b = din("router_group_b", [2, 4])
    router_expert_w = din("router_expert_w", [2, D, 32])
    router_expert_b = din("router_expert_b", [2, 32])
    moe_w_gate = din("moe_w_gate", [2, 32, D, 512])
    moe_w_up = din("moe_w_up", [2, 32, D, 512])
    moe_w_down = din("moe_w_down", [2, 32, 512, D])
    odd_w_in = din("odd_w_in", [1, D, 2480])
    odd_w_out = din("odd_w_out", [1, D, D])
    gdn_conv_w = din("gdn_conv_w", [1, 5, 1536])
    gdn_a_log = din("gdn_a_log", [1, 2, 4])
    gdn_dt_bias = din("gdn_dt_bias", [1, 2, 4])
    gdn_norm_w = din("gdn_norm_w", [1, 128])
    mla_q_norm_w = din("mla_q_norm_w", [1, 256])
    mla_wq_up = din("mla_wq_up", [1, 256, 768])
    mla_kv_norm_w = din("mla_kv_norm_w", [1, 128])
    mla_wkv_up = din("mla_wkv_up", [1, 128, 1024])
    mla_qk_norm_q = din("mla_qk_norm_q", [1, 96])
    mla_qk_norm_k = din("mla_qk_norm_k", [1, 96])
    rope_cos = din("rope_cos", [L, 16])
    rope_sin = din("rope_sin", [L, 16])

    okind = "ExternalOutput"
    y_out = k.dram("y", [L, D], F32, okind)
    ikind = "ExternalOutput" if dbg else "Internal"
    modrow = k.dram("modrow", [2, 2, 6 * D], F32, ikind)
    of_scr = k.dram("of_scr", [T, 512], F32, ikind)
    x1 = k.dram("x1", [T, D], F32, ikind)
    x2 = k.dram("x2", [T, D], F32, ikind)
    d_modrow = Buf(None, "modrow")
    d_of = [Buf(None, "of%d" % t) for t in range(NT)]
    d_x1 = [Buf(None, "x1_%d" % t) for t in range(NT)]
    d_x2 = [Buf(None, "x2_%d" % t) for t in range(NT)]
    x3 = k.dram("x3", [T, D], F32, ikind)
    d_x3 = [Buf(None, "x3_%d" % t) for t in range(NT)]
    ob_scr = k.dram("ob_scr", [T, 512], F32, ikind)
    QT = k.dram("QT", [8, 96, L], BF16, ikind)
    KT = k.dram("KT", [8, 96, T], BF16, ikind)
    VX = k.dram("VX", [8, NT, 128, 80], BF16, ikind)
    attT = k.dram("attT", [512, L], BF16, ikind)
    d_QT = Buf(None, "QT")
    d_KT = Buf(None, "KT")
    d_VX = Buf(None, "VX")
    d_att = Buf(None, "att")

    def rows(t):
        if t < 2:
            return ctx_in[t * 128:(t + 1) * 128, :]
        return x_in[(t - 2) * 128:(t - 1) * 128, :]

    with k.es:
        ident = k.sb("ident", [128, 128], F32)
        identb = k.sb("identb", [128, 128], BF16)
        ones = k.sb("ones", [128, 128], F32)
        zeros_bf = k.sb("zeros_bf", [128, 512], BF16)
        k.op("pool", lambda e: e.memset(ones[:], 1.0), writes=[ones])
        k.op("pool", lambda e: e.memset(zeros_bf[:], 0.0), writes=[zeros_bf])
        k.op("pool", lambda e: e.affine_select(ident[:], ones[:], pattern=[[-1, 128]], compare_op=ALU.is_equal,
                                               fill=0.0, base=0, channel_multiplier=1), reads=[ones], writes=[ident])
        k.op("dve", lambda e: e.tensor_copy(out=identb[:], in_=ident[:]), reads=[ident], writes=[identb])

        def blockmask(name, lo_keep, es=None):
            m = k.sb(name, [128, 128], F32, es)
            k.op("pool", lambda e: e.memset(m[:], 0.0), writes=[m])
            for cb in range(2):
                sl = m[cb * 64:(cb + 1) * 64, cb * 64:(cb + 1) * 64]
                src = ones[cb * 64:(cb + 1) * 64, cb * 64:(cb + 1) * 64]
                if lo_keep == "all":
                    k.op("pool", lambda e: e.tensor_copy(out=sl, in_=src), reads=[ones], writes=[m])
                else:
                    cm, st = (-1, 1) if lo_keep == "le" else (1, -1)
                    k.op("pool", lambda e: e.affine_select(sl, src, pattern=[[st, 64]], compare_op=ALU.is_ge,
                                                           fill=0.0, base=0, channel_multiplier=cm),
                         reads=[ones], writes=[m])
            return m

        with ExitStack() as es:
            ccol = k.sb("ccol", [128, 8, 2], F32, es)
            scol = k.sb("scol", [128, 8, 2], F32, es)
            k.dma("sp", ccol[:, :, 0], c_in.rearrange("(kc p) -> p kc", p=128), writes=[ccol],
                  allow_slow_non_contiguous=True)
            k.dma("sp", ccol[:, :, 1], cctx_in.rearrange("(kc p) -> p kc", p=128), writes=[ccol],
                  allow_slow_non_contiguous=True)
            k.op("act", lambda e: e.activation(out=scol[:], in_=ccol[:], func=AF.Silu), reads=[ccol], writes=[scol])
            wst = k.sbpool("adaw", [128, 8, 512], F32, 2, es)
            brow = k.sbpool("adab", [2, 512], F32, 2, es)
            orow = k.sbpool("adao", [2, 512], F32, 2, es)
            pp = k.pspool("adap", [2, 512], F32, 2, es)
            for layer in range(2):
                for cb in range(12):
                    w = wst.next()
                    k.dma("sp", w[:], ada_w[layer, :, cb * 512:(cb + 1) * 512].rearrange("(kc p) n -> p kc n", p=128),
                          writes=[w])
                    b = brow.next()
                    k.dma("sp", b[:], ada_b[layer, cb * 512:(cb + 1) * 512].partition_broadcast(2), writes=[b])
                    p = pp.next()
                    for kc in range(8):
                        k.op("pe", lambda e: e.matmul(p[:], lhsT=scol[:, kc, :], rhs=w[:, kc, :], start=(kc == 0),
                                                      stop=(kc == 7)), reads=[scol, w], writes=[p])
                    o = orow.next()
                    k.op("dve", lambda e: e.tensor_tensor(out=o[:], in0=p[:], in1=b[:], op=ALU.add), reads=[p, b],
                         writes=[o])
                    k.dma("sp", modrow[layer, :, cb * 512:(cb + 1) * 512], o[:], reads=[o], writes=[d_modrow])
        k.barrier()
        if stop_after == "prologue":
            k.drain_all()
            return nc

        def rsqrt(buf, out_ap, in_ap):
            k.op("act", lambda e: e.activation(out=out_ap, in_=in_ap, func=AF.Sqrt), reads=[buf], writes=[buf])
            k.op("dve", lambda e: e.reciprocal(out=out_ap, in_=out_ap), reads=[buf], writes=[buf])

        def load_modcols(es, layer, normw_ap, which):
            mc = k.sb("mc", [128, 2, 48], F32, es)
            for s in range(2):
                k.dma("sp", mc[:, s, :], modrow[layer, s, :].rearrange("(c p) -> p c", p=128), reads=[d_modrow],
                      writes=[mc], allow_slow_non_contiguous=True)
            nw = k.sb("nw", [128, 8], F32, es)
            k.dma("sp", nw[:], normw_ap.rearrange("(c p) -> p c", p=128), writes=[nw], allow_slow_non_contiguous=True)
            A = k.sb("A", [128, 2, 8], F32, es)
            S = k.sb("S", [128, 2, 8], F32, es)
            o = which * 24
            for s in range(2):
                k.op("dve", lambda e: e.scalar_tensor_tensor(out=A[:, s, :], in0=mc[:, s, o + 8:o + 16], scalar=1.0,
                                                             in1=nw[:], op0=ALU.add, op1=ALU.mult),
                     reads=[mc, nw], writes=[A])
                k.op("dve", lambda e: e.tensor_copy(out=S[:, s, :], in_=mc[:, s, o:o + 8]), reads=[mc], writes=[S])
            return A, S

        def norm_T(xt, A, S, s, rpool, xnpool, pst, out_bf=None, out_f32=None):
            junk = xnpool["junk"].next()
            ss = rpool.next()
            k.op("act", lambda e: e.activation(out=junk[:], in_=xt[:], func=AF.Square, accum_out=ss[:, 0:1]),
                 reads=[xt], writes=[junk, ss])
            k.op("dve", lambda e: e.tensor_scalar(out=ss[:, 1:2], in0=ss[:, 0:1], scalar1=1.0 / D, scalar2=EPS,
                                                  op0=ALU.mult, op1=ALU.add), reads=[ss], writes=[ss])
            rsqrt(ss, ss[:, 2:3], ss[:, 1:2])
            if out_f32 is None:
                xn = xnpool["bf"].next()
                k.op("act", lambda e: e.activation(out=xn[:], in_=xt[:], func=AF.Copy, scale=ss[:, 2:3]),
                     reads=[xt, ss], writes=[xn])
                for fc in range(8):
                    k.op("pe", lambda e: e.transpose(pst[:, fc, :], xn[:, fc * 128:(fc + 1) * 128], identb[:]),
                         reads=[xn, identb], writes=[pst])
                k.op("dve", lambda e: e.tensor_tensor(out=out_bf[:], in0=pst[:],
                                                      in1=A[:, s, :].unsqueeze(2).to_broadcast([128, 8, 128]),
                                                      op=ALU.mult), reads=[pst, A], writes=[out_bf])
                k.op("dve", lambda e: e.tensor_tensor(out=out_bf[:], in0=out_bf[:],
                                                      in1=S[:, s, :].unsqueeze(2).to_broadcast([128, 8, 128]),
                                                      op=ALU.add), reads=[out_bf, S], writes=[out_bf])
            else:
                xn = xnpool["f32"].next()
                k.op("act", lambda e: e.activation(out=xn[:], in_=xt[:], func=AF.Copy, scale=ss[:, 2:3]),
                     reads=[xt, ss], writes=[xn])
                for half in range(2):
                    p = pst[half]
                    for f4 in range(4):
                        fc = half * 4 + f4
                        k.op("pe", lambda e: e.transpose(p[:, f4, :], xn[:, fc * 128:(fc + 1) * 128], ident[:]),
                             reads=[xn, ident], writes=[p])
                    sl = slice(half * 4, half * 4 + 4)
                    k.op("dve", lambda e: e.tensor_tensor(out=out_f32[:, sl, :], in0=p[:],
                                                          in1=A[:, s, sl].unsqueeze(2).to_broadcast([128, 4, 128]),
                                                          op=ALU.mult), reads=[p, A], writes=[out_f32])
                    k.op("dve", lambda e: e.tensor_tensor(out=out_f32[:, sl, :], in0=out_f32[:, sl, :],
                                                          in1=S[:, s, sl].unsqueeze(2).to_broadcast([128, 4, 128]),
                                                          op=ALU.add), reads=[out_f32, S], writes=[out_f32])
                k.op("act", lambda e: e.copy(out=out_bf[:], in_=out_f32[:]), reads=[out_f32], writes=[out_bf])

        def load_w_bf16(es, dst, src_ap, nkc, ncols, stage_pool, colblk=512, engs=("dve", "act")):
            i = 0
            for c0 in range(0, ncols, colblk):
                c1 = min(ncols, c0 + colblk)
                st = stage_pool.next()
                k.dma("sp", st[:, 0:nkc, 0:c1 - c0], src_ap[:, c0:c1].rearrange("(kc p) n -> p kc n", p=128),
                      writes=[st])
                eng = engs[i % len(engs)]
                i += 1
                if eng == "act":
                    k.op("act", lambda e: e.copy(out=dst[:, :, c0:c1], in_=st[:, 0:nkc, 0:c1 - c0]), reads=[st],
                         writes=[dst])
                else:
                    k.op(eng, lambda e: e.tensor_copy(out=dst[:, :, c0:c1], in_=st[:, 0:nkc, 0:c1 - c0]), reads=[st],
                         writes=[dst])

        def even_pass(direction):
            with ExitStack() as es:
                fwd = direction == 0
                A1, S1 = load_modcols(es, 0, norm_mix_w[0], 0)
                win = k.sb("win", [128, 8, 3584], BF16, es)
                rhsF = k.sb("rhsF", [128, 256], F32, es)
                lrem = k.sb("lrem", [128, 128], F32, es)
                maski = k.sb("maski", [128, 4, 128], I32, es)
                oml_bc = k.sb("oml_bc", [128, 512], F32, es)
                oml_col = k.sb("oml_col", [128, 4], F32, es)
                if not fwd:
                    wout_g = [k.sb("wout_g", [128, 8, 1024], BF16, es) for _ in range(2)]
                    gnw_bc = k.sb("gnw_bc", [128, 512], F32, es)
                    hnw_bc = k.sb("hnw_bc", [128, 512], F32, es)
                    bs_col = k.sb("bs_col", [128, 4], F32, es)
                    wsT = k.sb("wsT", [128, 4, 128], BF16, es)
                with ExitStack() as es2:
                    stage = k.sbpool("stage", [128, 8, 512], F32, 2, es2)
                    load_w_bf16(es2, win, even_w_in[0], 8, 3584, stage)
                    tri = blockmask("tri", "le" if fwd else "ge", es2)
                    allb = blockmask("allb", "all", es2)
                    mid = k.sb("mid", [128, 128], F32, es2)
                    k.op("pool", lambda e: e.memset(mid[:], 0.0), writes=[mid])
                    for cb in range(2):
                        r0 = cb * 64 + (0 if fwd else 32)
                        k.op("pool", lambda e: e.tensor_copy(out=mid[r0:r0 + 32, cb * 64:(cb + 1) * 64],
                                                             in_=ones[r0:r0 + 32, cb * 64:(cb + 1) * 64]),
                             reads=[ones], writes=[mid])
                    k.op("dve", lambda e: e.tensor_tensor(out=rhsF[:, 0:128], in0=tri[:], in1=mid[:], op=ALU.subtract),
                         reads=[tri, mid], writes=[rhsF])
                    k.op("dve", lambda e: e.tensor_copy(out=rhsF[:, 128:256], in_=tri[:]), reads=[tri], writes=[rhsF])
                    k.op("dve", lambda e: e.tensor_tensor(out=lrem[:], in0=allb[:], in1=tri[:], op=ALU.subtract),
                         reads=[allb, tri], writes=[lrem])
                    for h in range(4):
                        k.op("dve", lambda e: e.tensor_copy(out=maski[:, h, :], in_=tri[:]), reads=[tri],
                             writes=[maski])
                    lbt = k.sb("lbt", [128, 3, 512], F32, es2)
                    for r in range(3):
                        k.dma("sp", lbt[:, r, :], hgrn_lb[direction, r, :].partition_broadcast(128), writes=[lbt])
                    k.op("act", lambda e: e.activation(out=lbt[:], in_=lbt[:], func=AF.Exp), reads=[lbt], writes=[lbt])
                    tmpb = k.sb("tmpb", [128, 512], F32, es2)
                    k.op("dve", lambda e: e.tensor_tensor(out=tmpb[:], in0=lbt[:, 0, :], in1=lbt[:, 1, :], op=ALU.add),
                         reads=[lbt], writes=[tmpb])
                    k.op("dve", lambda e: e.tensor_tensor(out=tmpb[:], in0=tmpb[:], in1=lbt[:, 2, :], op=ALU.add),
                         reads=[lbt, tmpb], writes=[tmpb])
                    k.op("dve", lambda e: e.reciprocal(out=tmpb[:], in_=tmpb[:]), reads=[tmpb], writes=[tmpb])
                    k.op("dve", lambda e: e.tensor_tensor(out=oml_bc[:], in0=lbt[:, 1, :], in1=lbt[:, 2, :],
                                                          op=ALU.add), reads=[lbt], writes=[oml_bc])
                    k.op("dve", lambda e: e.tensor_tensor(out=oml_bc[:], in0=oml_bc[:], in1=tmpb[:], op=ALU.mult),
                         reads=[oml_bc, tmpb], writes=[oml_bc])
                    lbc = k.sb("lbc", [128, 3, 4], F32, es2)
                    for r in range(3):
                        k.dma("sp", lbc[:, r, :], hgrn_lb[direction, r, :].rearrange("(h p) -> p h", p=128),
                              writes=[lbc], allow_slow_non_contiguous=True)
                    k.op("act", lambda e: e.activation(out=lbc[:], in_=lbc[:], func=AF.Exp), reads=[lbc], writes=[lbc])
                    tmpc = k.sb("tmpc", [128, 4], F32, es2)
                    k.op("dve", lambda e: e.tensor_tensor(out=tmpc[:], in0=lbc[:, 0, :], in1=lbc[:, 1, :], op=ALU.add),
                         reads=[lbc], writes=[tmpc])
                    k.op("dve", lambda e: e.tensor_tensor(out=tmpc[:], in0=tmpc[:], in1=lbc[:, 2, :], op=ALU.add),
                         reads=[lbc, tmpc], writes=[tmpc])
                    k.op("dve", lambda e: e.reciprocal(out=tmpc[:], in_=tmpc[:]), reads=[tmpc], writes=[tmpc])
                    k.op("dve", lambda e: e.tensor_tensor(out=oml_col[:], in0=lbc[:, 1, :], in1=lbc[:, 2, :],
                                                          op=ALU.add), reads=[lbc], writes=[oml_col])
                    k.op("dve", lambda e: e.tensor_tensor(out=oml_col[:], in0=oml_col[:], in1=tmpc[:], op=ALU.mult),
                         reads=[oml_col, tmpc], writes=[oml_col])
                    if not fwd:
                        gbc = k.sb("gbc", [128, 2, 1024], F32, es2)
                        for s in range(2):
                            k.dma("sp", gbc[:, s, :], modrow[0, s, 2 * D:3 * D].partition_broadcast(128),
                                  reads=[d_modrow], writes=[gbc])
                        for c0 in range(0, 1024, 512):
                            st = stage.next()
                            k.dma("sp", st[:], even_w_out[0][:, c0:c0 + 512].rearrange("(kc p) n -> p kc n", p=128),
                                  writes=[st])
                            for s in range(2):
                                for kc in range(8):
                                    k.op("dve" if kc % 2 else "pool",
                                         lambda e: e.tensor_tensor(out=wout_g[s][:, kc, c0:c0 + 512], in0=st[:, kc, :],
                                                                   in1=gbc[:, s, c0:c0 + 512], op=ALU.mult),
                                         reads=[st, gbc], writes=[wout_g[s]])
                        k.dma("sp", gnw_bc[:], gmlp_norm_w[0].partition_broadcast(128), writes=[gnw_bc])
                        k.dma("sp", hnw_bc[:], hgrn_norm_w[0].partition_broadcast(128), writes=[hnw_bc])
                        k.dma("sp", bs_col[:], gmlp_bs[0].rearrange("g p -> p g"), writes=[bs_col],
                              allow_slow_non_contiguous=True)
                        wsf = k.sb("wsf", [128, 4, 128], F32, es2)
                        k.dma("sp", wsf[:], gmlp_ws[0].rearrange("g p q -> p g q"), writes=[wsf])
                        with ExitStack() as es3:
                            ptmp = k.ps("ptmp", [128, 512], F32, es3)
                            for g in range(4):
                                k.op("pe", lambda e: e.transpose(ptmp[:, g * 128:(g + 1) * 128], wsf[:, g, :], ident[:]),
                                     reads=[wsf, ident], writes=[ptmp])
                            k.op("dve", lambda e: e.tensor_copy(out=wsT[:].rearrange("p g q -> p (g q)"), in_=ptmp[:]),
                                 reads=[ptmp], writes=[wsT])
                            k.barrier()
                    k.barrier()

                Sst = k.sb("Sst", [128, 4, 128], F32, es)
                Sbf = k.sbpool("Sbf", [128, 4, 128], BF16, 3, es)
                k.op("pool", lambda e: e.memset(Sst[:], 0.0), writes=[Sst])
                xpool = k.sbpool("xt", [128, 1024], F32, 3, es)
                rpool = k.sbpool("rs", [128, 4], F32, 4, es)
                xnpool = {"junk": k.sbpool("junk", [128, 1024], BF16, 1, es),
                          "bf": k.sbpool("xnb", [128, 1024], BF16, 2, es)}
                hTp = k.sbpool("hT", [128, 8, 128], BF16, 2, es)
                pst = k.ps("pst", [128, 8, 128], BF16, es)
                proj = k.pspool("proj", [128, 512], F32, 2, es)
                big2 = k.ps("big2", [128, 1024], F32, es)
                gs = k.ps("gs", [128, 512], F32, es)
                ops_ = k.ps("ops", [128, 4, 128], F32, es)
                dSp = k.ps("dSp", [128, 4, 128], F32, es)
                t512 = k.sbpool("t512", [128, 512], F32, 10, es)
                b512 = k.sbpool("b512", [128, 512], BF16, 10, es)
                qhp = k.sbpool("qhat", [128, 2, 4, 128], BF16, 2, es)
                for qb in qhp.bufs:
                    k.op("pool", lambda e: e.memset(qb[:], 0.0), writes=[qb])
                if not fwd:
                    catp = k.sbpool("cat", [128, 1024], BF16, 2, es)
                    catTp = k.sbpool("catT", [128, 8, 128], BF16, 2, es)
                    zp = k.sbpool("z", [128, 1024], F32, 1, es)

                order = list(range(NT)) if fwd else [1, 0] + list(range(NT - 1, 1, -1))
                C_Q, C_I, C_FF, C_FB, C_G = 1024, 1536, 2048, 2560, 3072
                C_F = C_FF if fwd else C_FB
                first_rows, second_rows = (slice(0, 64), slice(64, 128)) if fwd else (slice(64, 128), slice(0, 64))
                first_idx, second_idx = (0, 1) if fwd else (1, 0)
                last_first, last_second = (63, 127) if fwd else (64, 0)

                for t in order:
                    s = 0 if t >= 2 else 1
                    xt = xpool.next()
                    k.dma("sp", xt[:], rows(t), writes=[xt])
                    hT = hTp.next()
                    norm_T(xt, A1, S1, s, rpool, xnpool, pst, out_bf=hT)

                    def proj_tok(c0, n=512):
                        p = proj.next()
                        for kc in range(8):
                            k.op("pe", lambda e: e.matmul(p[:, 0:n], lhsT=hT[:, kc, :], rhs=win[:, kc, c0:c0 + n],
                                                          start=(kc == 0), stop=(kc == 7)), reads=[hT, win], writes=[p])
                        return p

                    def proj_feat(c0):
                        p = proj.next()
                        for h in range(4):
                            for kc in range(8):
                                k.op("pe", lambda e: e.matmul(p[:, h * 128:(h + 1) * 128],
                                                              lhsT=win[:, kc, c0 + h * 128:c0 + (h + 1) * 128],
                                                              rhs=hT[:, kc, :], start=(kc == 0), stop=(kc == 7)),
                                     reads=[hT, win], writes=[p])
                        return p

                    pf = proj_tok(C_F)
                    sg = t512.next()
                    k.op("act", lambda e: e.activation(out=sg[:], in_=pf[:], func=AF.Sigmoid, scale=-1.0), reads=[pf],
                         writes=[sg])
                    ktok = t512.next()
                    k.op("dve", lambda e: e.tensor_tensor(out=ktok[:], in0=sg[:], in1=oml_bc[:], op=ALU.mult),
                         reads=[sg, oml_bc], writes=[ktok])
                    lf = t512.next()
                    k.op("act", lambda e: e.activation(out=lf[:], in_=ktok[:], func=AF.Ln, scale=-1.0, bias=1.0),
                         reads=[ktok], writes=[lf])
                    k.op("pe", lambda e: e.matmul(gs[:], lhsT=lrem[:], rhs=lf[:], start=True, stop=True),
                         reads=[lrem, lf], writes=[gs])
                    er = t512.next()
                    k.op("act", lambda e: e.activation(out=er[:], in_=gs[:], func=AF.Exp), reads=[gs], writes=[er])
                    khat = b512.next()
                    k.op("dve", lambda e: e.tensor_tensor(out=khat[:], in0=ktok[:], in1=er[:], op=ALU.mult),
                         reads=[ktok, er], writes=[khat])
                    pi = proj_tok(C_I)
                    vb = b512.next()
                    k.op("act", lambda e: e.copy(out=vb[:], in_=pi[:]), reads=[pi], writes=[vb])
                    for h in range(4):
                        k.op("pe", lambda e: e.matmul(big2[:, h * 256:(h + 1) * 256], lhsT=lf[:, h * 128:(h + 1) * 128],
                                                      rhs=rhsF[:], start=True, stop=True), reads=[lf, rhsF],
                             writes=[big2])
                    b2v = big2[:].rearrange("p (h c) -> p h c", c=256)
                    e1 = t512.next()
                    e2 = t512.next()
                    e3 = t512.next()
                    e1v = e1[:].rearrange("p (h c) -> p h c", c=128)
                    e2v = e2[:].rearrange("p (h c) -> p h c", c=128)
                    e3v = e3[:].rearrange("p (h c) -> p h c", c=128)
                    k.op("act", lambda e: e.activation(out=e1v, in_=b2v[:, :, 0:128], func=AF.Exp), reads=[big2],
                         writes=[e1])
                    k.op("act", lambda e: e.activation(out=e2v, in_=b2v[:, :, 0:128], func=AF.Exp, scale=-1.0),
                         reads=[big2], writes=[e2])
                    k.op("act", lambda e: e.activation(out=e3v, in_=b2v[:, :, 128:256], func=AF.Exp), reads=[big2],
                         writes=[e3])
                    pq = proj_feat(C_Q)
                    qT = t512.next()
                    k.op("act", lambda e: e.copy(out=qT[:], in_=pq[:]), reads=[pq], writes=[qT])
                    pfT = proj_feat(C_F)
                    kT = t512.next()
                    k.op("act", lambda e: e.activation(out=kT[:], in_=pfT[:], func=AF.Sigmoid, scale=-1.0), reads=[pfT],
                         writes=[kT])
                    kTv = kT[:].rearrange("p (h c) -> p h c", c=128)
                    k.op("dve", lambda e: e.tensor_tensor(out=kTv, in0=kTv,
                                                          in1=oml_col[:].unsqueeze(2).to_broadcast([128, 4, 128]),
                                                          op=ALU.mult), reads=[kT, oml_col], writes=[kT])
                    qtl = b512.next()
                    ktl = b512.next()
                    k.op("dve", lambda e: e.tensor_tensor(out=qtl[:], in0=qT[:], in1=e1[:], op=ALU.mult),
                         reads=[qT, e1], writes=[qtl])
                    k.op("pool", lambda e: e.tensor_tensor(out=ktl[:], in0=kT[:], in1=e2[:], op=ALU.mult),
                         reads=[kT, e2], writes=[ktl])
                    qh = qhp.next()
                    qTv = qT[:].rearrange("p (h c) -> p h c", c=128)
                    for cb in range(2):
                        cs = slice(cb * 64, (cb + 1) * 64)
                        k.op("pool", lambda e: e.tensor_tensor(out=qh[:, cb, :, cs], in0=qTv[:, :, cs],
                                                               in1=e3v[:, :, cs], op=ALU.mult),
                             reads=[qT, e3], writes=[qh])
                    for h in range(4):
                        k.op("pe", lambda e: e.matmul(gs[:, h * 128:(h + 1) * 128], lhsT=ktl[:, h * 128:(h + 1) * 128],
                                                      rhs=qtl[:, h * 128:(h + 1) * 128], start=True, stop=True),
                             reads=[ktl, qtl], writes=[gs])
                    scm = b512.next()
                    k.op("pool", lambda e: e.tensor_copy(out=scm[:], in_=zeros_bf[:]), reads=[zeros_bf], writes=[scm])
                    k.op("dve", lambda e: e.copy_predicated(out=scm[:], mask=maski[:].rearrange("p h c -> p (h c)"),
                                                            data=gs[:]), reads=[gs, maski, scm], writes=[scm])
                    S0 = Sbf.next()
                    k.op("act", lambda e: e.copy(out=S0[:], in_=Sst[:]), reads=[Sst], writes=[S0])
                    for h in range(4):
                        hs = slice(h * 128, (h + 1) * 128)
                        k.op("pe", lambda e: e.matmul(dSp[:, h, :], lhsT=khat[first_rows, hs], rhs=vb[first_rows, hs],
                                                      start=True, stop=True), reads=[khat, vb], writes=[dSp])
                    for h in range(4):
                        k.op("dve", lambda e: e.scalar_tensor_tensor(out=Sst[:, h, :], in0=Sst[:, h, :],
                                                                     scalar=e3v[:, h, last_first:last_first + 1],
                                                                     in1=dSp[:, h, :], op0=ALU.mult, op1=ALU.add),
                             reads=[Sst, e3, dSp], writes=[Sst])
                    S1_ = Sbf.next()
                    k.op("act", lambda e: e.copy(out=S1_[:], in_=Sst[:]), reads=[Sst], writes=[S1_])
                    for h in range(4):
                        hs = slice(h * 128, (h + 1) * 128)
                        k.op("pe", lambda e: e.matmul(ops_[:, h, :], lhsT=scm[:, hs], rhs=vb[:, hs], start=True,
                                                      stop=False), reads=[scm, vb], writes=[ops_])
                        k.op("pe", lambda e: e.matmul(ops_[:, h, :], lhsT=qh[:, first_idx, h, :], rhs=S0[:, h, :],
                                                      start=False, stop=False), reads=[qh, S0], writes=[ops_])
                        k.op("pe", lambda e: e.matmul(ops_[:, h, :], lhsT=qh[:, second_idx, h, :], rhs=S1_[:, h, :],
                                                      start=False, stop=True), reads=[qh, S1_], writes=[ops_])
                    for h in range(4):
                        hs = slice(h * 128, (h + 1) * 128)
                        k.op("pe", lambda e: e.matmul(dSp[:, h, :], lhsT=khat[second_rows, hs], rhs=vb[second_rows, hs],
                                                      start=True, stop=True), reads=[khat, vb], writes=[dSp])
                    for h in range(4):
                        k.op("dve", lambda e: e.scalar_tensor_tensor(out=Sst[:, h, :], in0=Sst[:, h, :],
                                                                     scalar=e3v[:, h, last_second:last_second + 1],
                                                                     in1=dSp[:, h, :], op0=ALU.mult, op1=ALU.add),
                             reads=[Sst, e3, dSp], writes=[Sst])
                    osb = t512.next()
                    if fwd:
                        k.op("act", lambda e: e.copy(out=osb[:], in_=ops_[:].rearrange("p h c -> p (h c)")),
                             reads=[ops_], writes=[osb])
                        k.dma("pool", of_scr[t * 128:(t + 1) * 128, :], osb[:], reads=[osb], writes=[d_of[t]])
                        continue
                    ofl = t512.next()
                    k.dma("sp", ofl[:], of_scr[t * 128:(t + 1) * 128, :], reads=[d_of[t]], writes=[ofl])
                    k.op("dve", lambda e: e.tensor_tensor(out=osb[:], in0=ops_[:].rearrange("p h c -> p (h c)"),
                                                          in1=ofl[:], op=ALU.add), reads=[ops_, ofl], writes=[osb])
                    sq = t512.next()
                    k.op("pool", lambda e: e.tensor_tensor(out=sq[:], in0=osb[:], in1=osb[:], op=ALU.mult), reads=[osb],
                         writes=[sq])
                    rs = rpool.next()
                    k.op("dve", lambda e: e.tensor_reduce(out=rs[:], in_=sq[:].rearrange("p (h c) -> p h c", c=128),
                                                          axis=AX.X, op=ALU.add), reads=[sq], writes=[rs])
                    k.op("dve", lambda e: e.tensor_scalar(out=rs[:], in0=rs[:], scalar1=1.0 / 128, scalar2=EPS,
                                                          op0=ALU.mult, op1=ALU.add), reads=[rs], writes=[rs])
                    rsqrt(rs, rs[:], rs[:])
                    k.op("dve", lambda e: e.tensor_tensor(out=osb[:].rearrange("p (h c) -> p h c", c=128),
                                                          in0=osb[:].rearrange("p (h c) -> p h c", c=128),
                                                          in1=rs[:].unsqueeze(2).to_broadcast([128, 4, 128]),
                                                          op=ALU.mult), reads=[osb, rs], writes=[osb])
                    k.op("pool", lambda e: e.tensor_tensor(out=osb[:], in0=osb[:], in1=hnw_bc[:], op=ALU.mult),
                         reads=[osb, hnw_bc], writes=[osb])
                    pg = proj_tok(C_G)
                    sgt = t512.next()
                    k.op("act", lambda e: e.activation(out=sgt[:], in_=pg[:], func=AF.Silu), reads=[pg], writes=[sgt])
                    cat = catp.next()
                    k.op("dve", lambda e: e.tensor_tensor(out=cat[:, 512:1024], in0=osb[:], in1=sgt[:], op=ALU.mult),
                         reads=[osb, sgt], writes=[cat])
                    z = zp.next()
                    pu = proj_tok(0)
                    k.op("act", lambda e: e.activation(out=z[:, 0:512], in_=pu[:], func=AF.Gelu), reads=[pu], writes=[z])
                    pv = proj_tok(512)
                    k.op("act", lambda e: e.activation(out=z[:, 512:1024], in_=pv[:], func=AF.Gelu), reads=[pv],
                         writes=[z])
                    rs2 = rpool.next()
                    sq2 = t512.next()
                    k.op("act", lambda e: e.activation(out=sq2[:], in_=z[:, 512:1024], func=AF.Square,
                                                       accum_out=rs2[:, 0:1]), reads=[z], writes=[sq2, rs2])
                    k.op("dve", lambda e: e.tensor_scalar(out=rs2[:, 1:2], in0=rs2[:, 0:1], scalar1=1.0 / 512,
                                                          scalar2=EPS, op0=ALU.mult, op1=ALU.add), reads=[rs2],
                         writes=[rs2])
                    rsqrt(rs2, rs2[:, 2:3], rs2[:, 1:2])
                    vn = b512.next()
                    k.op("dve", lambda e: e.scalar_tensor_tensor(out=vn[:], in0=z[:, 512:1024], scalar=rs2[:, 2:3],
                                                                 in1=gnw_bc[:], op0=ALU.mult, op1=ALU.mult),
                         reads=[z, rs2, gnw_bc], writes=[vn])
                    psg = proj.next()
                    for g in range(4):
                        gsl = slice(g * 128, (g + 1) * 128)
                        k.op("pe", lambda e: e.matmul(psg[:, gsl], lhsT=wsT[:, g, :], rhs=vn[:, gsl], start=True,
                                                      stop=True), reads=[wsT, vn], writes=[psg])
                    for g in range(4):
                        gsl = slice(g * 128, (g + 1) * 128)
                        k.op("dve", lambda e: e.scalar_tensor_tensor(out=cat[:, gsl], in0=psg[:, gsl],
                                                                     scalar=bs_col[:, g:g + 1], in1=z[:, gsl],
                                                                     op0=ALU.add, op1=ALU.mult),
                             reads=[psg, bs_col, z], writes=[cat])
                    for fc in range(8):
                        k.op("pe", lambda e: e.transpose(pst[:, fc, :], cat[:, fc * 128:(fc + 1) * 128], identb[:]),
                             reads=[cat, identb], writes=[pst])
                    catT = catTp.next()
                    k.op("act", lambda e: e.copy(out=catT[:], in_=pst[:]), reads=[pst], writes=[catT])
                    for half in range(2):
                        for kc in range(8):
                            k.op("pe", lambda e: e.matmul(big2[:, half * 512:(half + 1) * 512], lhsT=catT[:, kc, :],
                                                          rhs=wout_g[s][:, kc, half * 512:(half + 1) * 512],
                                                          start=(kc == 0), stop=(kc == 7)),
                                 reads=[catT, wout_g[s]], writes=[big2])
                    k.op("dve", lambda e: e.tensor_tensor(out=xt[:], in0=big2[:], in1=xt[:], op=ALU.add),
                         reads=[big2, xt], writes=[xt])
                    k.dma("pool", x1[t * 128:(t + 1) * 128, :], xt[:], reads=[xt], writes=[d_x1[t]])
            k.barrier()

        even_pass(0)
        even_pass(1)
        if stop_after == "mix0":
            k.drain_all()
            return nc

        def moe(layer, src, d_src, dst_fn, tiles):
            with ExitStack() as es:
                A2, S2 = load_modcols(es, layer, norm_ffn_w[layer], 1)
                wr = k.sb("wr", [128, 8, 36], F32, es)
                k.dma("sp", wr[:, :, 0:4], router_group_w[layer].rearrange("(kc p) e -> p kc e", p=128), writes=[wr],
                      allow_slow_non_contiguous=True)
                k.dma("sp", wr[:, :, 4:36], router_expert_w[layer].rearrange("(kc p) e -> p kc e", p=128), writes=[wr],
                      allow_slow_non_contiguous=True)
                rb = k.sb("rb", [128, 36], F32, es)
                k.dma("sp", rb[:, 0:4], router_group_b[layer].partition_broadcast(128), writes=[rb])
                k.dma("sp", rb[:, 4:36], router_expert_b[layer].partition_broadcast(128), writes=[rb])
                g2bc = k.sb("g2bc", [128, 2, 1024], F32, es)
                for s in range(2):
                    k.dma("sp", g2bc[:, s, :], modrow[layer, s, 5 * D:6 * D].partition_broadcast(128),
                          reads=[d_modrow], writes=[g2bc])
                NTS = min(8, len(tiles))
                ST = NTS * 128
                hTs = k.sb("hTs", [128, 8, ST], BF16, es)
                hT32p = k.sbpool("hT32", [128, 8, 128], F32, 2, es)
                yacc = k.sb("yacc", [128, NTS, 1024], F32, es)
                wc = k.sb("wc", [128, NTS, 32], F32, es)
                xpool = k.sbpool("xt", [128, 1024], F32, 2, es)
                rpool = k.sbpool("rs", [128, 8], F32, 4, es)
                xnpool = {"junk": k.sbpool("junk", [128, 1024], BF16, 1, es),
                          "f32": k.sbpool("xnf", [128, 1024], F32, 2, es)}
                stg = k.sbpool("wstg", [128, 4, 512], F32, 4, es)
                wgp = k.sbpool("wg", [128, 8, 512], BF16, 2, es)
                wup = k.sbpool("wu", [128, 8, 512], BF16, 2, es)
                wdp = k.sbpool("wd", [128, 4, 1024], BF16, 2, es)
                silp = k.sbpool("sil", [128, 512], F32, 2, es)
                actp = k.sbpool("actT", [128, 4, 512], BF16, 2, es)
                r36 = k.sbpool("r36", [128, 40], F32, 12, es)
                pga = k.pspool("pga", [128, 512], F32, 2, es)
                pup = k.pspool("pup", [128, 512], F32, 2, es)
                pdn = k.pspool("pdn", [128, 1024], F32, 2, es)
                cast_i = [0]

                def cast(dst_ap, dst, st):
                    eng = ("dve", "act", "pool")[cast_i[0] % 3]
                    cast_i[0] += 1
                    if eng == "act":
                        k.op("act", lambda e: e.copy(out=dst_ap, in_=st[:]), reads=[st], writes=[dst])
                    else:
                        k.op(eng, lambda e: e.tensor_copy(out=dst_ap, in_=st[:]), reads=[st], writes=[dst])

                for st0 in range(0, len(tiles), NTS):
                    stiles = tiles[st0:st0 + NTS]
                    n_t = len(stiles)
                    for ti, t in enumerate(stiles):
                        s = 0 if t >= 2 else 1
                        xt = xpool.next()
                        k.dma("sp", xt[:], src[t * 128:(t + 1) * 128, :], reads=[d_src[t]], writes=[xt])
                        h32 = hT32p.next()
                        pstf = [pga.next(), pup.next()]
                        pst_views = [Bv(p, p[:].rearrange("p (f c) -> p f c", c=128)) for p in pstf]
                        hview = Bv(hTs, hTs[:, :, ti * 128:(ti + 1) * 128])
                        norm_T(xt, A2, S2, s, rpool, xnpool, pst_views, out_bf=hview, out_f32=h32)
                        pl = pga.next()
                        for kc in range(8):
                            k.op("pe", lambda e: e.matmul(pl[:, 0:36], lhsT=h32[:, kc, :], rhs=wr[:, kc, :],
                                                          start=(kc == 0), stop=(kc == 7)), reads=[h32, wr], writes=[pl])
                        lg = r36.next()
                        k.op("dve", lambda e: e.tensor_tensor(out=lg[:, 0:36], in0=pl[:, 0:36], in1=rb[:], op=ALU.add),
                             reads=[pl, rb], writes=[lg])
                        m = r36.next()
                        k.op("dve", lambda e: e.tensor_reduce(out=m[:, 0:1], in_=lg[:, 0:4], axis=AX.X, op=ALU.max),
                             reads=[lg], writes=[m])
                        k.op("dve", lambda e: e.tensor_scalar(out=m[:, 1:2], in0=m[:, 0:1], scalar1=-1.0, scalar2=None,
                                                              op0=ALU.mult), reads=[m], writes=[m])
                        eg = r36.next()
                        k.op("act", lambda e: e.activation(out=eg[:, 0:4], in_=lg[:, 0:4], func=AF.Exp, bias=m[:, 1:2],
                                                           accum_out=m[:, 2:3]), reads=[lg, m], writes=[eg, m])
                        k.op("dve", lambda e: e.reciprocal(out=m[:, 3:4], in_=m[:, 2:3]), reads=[m], writes=[m])
                        ohg = r36.next()
                        k.op("dve", lambda e: e.tensor_scalar(out=ohg[:, 0:4], in0=lg[:, 0:4], scalar1=m[:, 0:1],
                                                              scalar2=1e9, op0=ALU.is_lt, op1=ALU.mult),
                             reads=[lg, m], writes=[ohg])
                        lem = r36.next()
                        k.op("dve", lambda e: e.tensor_tensor(
                            out=lem[:, 0:32].rearrange("p (g e) -> p g e", e=8),
                            in0=lg[:, 4:36].rearrange("p (g e) -> p g e", e=8),
                            in1=ohg[:, 0:4].unsqueeze(2).to_broadcast([128, 4, 8]), op=ALU.subtract),
                             reads=[lg, ohg], writes=[lem])
                        k.op("dve", lambda e: e.tensor_reduce(out=m[:, 4:5], in_=lem[:, 0:32], axis=AX.X, op=ALU.max),
                             reads=[lem], writes=[m])
                        oh1 = r36.next()
                        k.op("dve", lambda e: e.tensor_scalar(out=oh1[:, 0:32], in0=lem[:, 0:32], scalar1=m[:, 4:5],
                                                              scalar2=None, op0=ALU.is_ge), reads=[lem, m], writes=[oh1])
                        lem2 = r36.next()
                        k.op("dve", lambda e: e.scalar_tensor_tensor(out=lem2[:, 0:32], in0=oh1[:, 0:32], scalar=-1e9,
                                                                     in1=lem[:, 0:32], op0=ALU.mult, op1=ALU.add),
                             reads=[oh1, lem], writes=[lem2])
                        k.op("dve", lambda e: e.tensor_reduce(out=m[:, 5:6], in_=lem2[:, 0:32], axis=AX.X, op=ALU.max),
                             reads=[lem2], writes=[m])
                        oh2 = r36.next()
                        k.op("dve", lambda e: e.tensor_scalar(out=oh2[:, 0:32], in0=lem2[:, 0:32], scalar1=m[:, 5:6],
                                                              scalar2=None, op0=ALU.is_ge), reads=[lem2, m],
                             writes=[oh2])
                        k.op("dve", lambda e: e.tensor_tensor(out=m[:, 6:7], in0=m[:, 5:6], in1=m[:, 4:5],
                                                              op=ALU.subtract), reads=[m], writes=[m])
                        k.op("act", lambda e: e.activation(out=m[:, 7:8], in_=m[:, 6:7], func=AF.Exp), reads=[m],
                             writes=[m])
                        k.op("dve", lambda e: e.tensor_scalar(out=m[:, 7:8], in0=m[:, 7:8], scalar1=1.0, scalar2=None,
                                                              op0=ALU.add), reads=[m], writes=[m])
                        k.op("dve", lambda e: e.reciprocal(out=m[:, 8:9], in_=m[:, 7:8]), reads=[m], writes=[m])
                        k.op("dve", lambda e: e.tensor_tensor(out=m[:, 9:10], in0=m[:, 8:9], in1=m[:, 3:4], op=ALU.mult),
                             reads=[m], writes=[m])
                        k.op("dve", lambda e: e.tensor_tensor(out=m[:, 10:11], in0=m[:, 3:4], in1=m[:, 9:10],
                                                              op=ALU.subtract), reads=[m], writes=[m])
                        k.op("dve", lambda e: e.tensor_scalar(out=wc[:, ti, :], in0=oh1[:, 0:32], scalar1=m[:, 9:10],
                                                              scalar2=None, op0=ALU.mult), reads=[oh1, m], writes=[wc])
                        k.op("dve", lambda e: e.scalar_tensor_tensor(out=wc[:, ti, :], in0=oh2[:, 0:32],
                                                                     scalar=m[:, 10:11], in1=wc[:, ti, :],
                                                                     op0=ALU.mult, op1=ALU.add),
                             reads=[oh2, m, wc], writes=[wc])
                    ntok = n_t * 128
                    pending = [None]
                    for ex in range(32):
                        wg = wgp.next()
                        wu = wup.next()
                        wd = wdp.next()
                        for (dst, srcw) in ((wg, moe_w_gate[layer, ex]), (wu, moe_w_up[layer, ex])):
                            for half in range(2):
                                st = stg.next()
                                k.dma("sp", st[:], srcw[half * 512:(half + 1) * 512, :].rearrange(
                                    "(kc p) n -> p kc n", p=128), writes=[st])
                                cast(dst[:, half * 4:(half + 1) * 4, :], dst, st)
                        for half in range(2):
                            st = stg.next()
                            k.dma("sp", st[:], moe_w_down[layer, ex][:, half * 512:(half + 1) * 512].rearrange(
                                "(kc p) n -> p kc n", p=128), writes=[st])
                            cast(wd[:, :, half * 512:(half + 1) * 512], wd, st)
                        for b0 in range(0, ntok, 512):
                            nb = min(512, ntok - b0)
                            aT = actp.next()
                            for ffc in range(4):
                                fsl = slice(ffc * 128, (ffc + 1) * 128)
                                pg_ = pga.next()
                                pu_ = pup.next()
                                for kc in range(8):
                                    k.op("pe", lambda e: e.matmul(pg_[:, 0:nb], lhsT=wg[:, kc, fsl],
                                                                  rhs=hTs[:, kc, b0:b0 + nb], start=(kc == 0),
                                                                  stop=(kc == 7)), reads=[wg, hTs], writes=[pg_])
                                for kc in range(8):
                                    k.op("pe", lambda e: e.matmul(pu_[:, 0:nb], lhsT=wu[:, kc, fsl],
                                                                  rhs=hTs[:, kc, b0:b0 + nb], start=(kc == 0),
                                                                  stop=(kc == 7)), reads=[wu, hTs], writes=[pu_])
                                sl_ = silp.next()
                                k.op("act", lambda e: e.activation(out=sl_[:, 0:nb], in_=pg_[:, 0:nb], func=AF.Silu),
                                     reads=[pg_], writes=[sl_])
                                k.op("dve", lambda e: e.tensor_tensor(out=aT[:, ffc, 0:nb], in0=sl_[:, 0:nb],
                                                                      in1=pu_[:, 0:nb], op=ALU.mult),
                                     reads=[sl_, pu_], writes=[aT])
                            def mk_down(aT=aT, wd=wd, ex=ex, b0=b0, nb=nb):
                              for tt in range(nb // 128):
                                ti = b0 // 128 + tt
                                pd = pdn.next()
                                for half in range(2):
                                    for ffc in range(4):
                                        k.op("pe", lambda e: e.matmul(pd[:, half * 512:(half + 1) * 512],
                                                                      lhsT=aT[:, ffc, tt * 128:(tt + 1) * 128],
                                                                      rhs=wd[:, ffc, half * 512:(half + 1) * 512],
                                                                      start=(ffc == 0), stop=(ffc == 3)),
                                             reads=[aT, wd], writes=[pd])
                                if ex == 0:
                                    k.op("dve", lambda e: e.tensor_scalar(out=yacc[:, ti, :], in0=pd[:],
                                                                          scalar1=wc[:, ti, ex:ex + 1], scalar2=None,
                                                                          op0=ALU.mult), reads=[pd, wc], writes=[yacc])
                                else:
                                    k.op("dve", lambda e: e.scalar_tensor_tensor(out=yacc[:, ti, :], in0=pd[:],
                                                                                 scalar=wc[:, ti, ex:ex + 1],
                                                                                 in1=yacc[:, ti, :], op0=ALU.mult,
                                                                                 op1=ALU.add),
                                         reads=[pd, wc, yacc], writes=[yacc])
                            if pending[0] is not None:
                                pending[0]()
                            pending[0] = mk_down
                    if pending[0] is not None:
                        pending[0]()
                        pending[0] = None
                    for ti, t in enumerate(stiles):
                        s = 0 if t >= 2 else 1
                        xt = xpool.next()
                        k.dma("sp", xt[:], src[t * 128:(t + 1) * 128, :], reads=[d_src[t]], writes=[xt])
                        k.op("pool", lambda e: e.tensor_tensor(out=yacc[:, ti, :], in0=yacc[:, ti, :], in1=g2bc[:, s, :],
                                                               op=ALU.mult), reads=[yacc, g2bc], writes=[yacc])
                        k.op("dve", lambda e: e.tensor_tensor(out=xt[:], in0=xt[:], in1=yacc[:, ti, :], op=ALU.add),
                             reads=[xt, yacc], writes=[xt])
                        dap, dbuf = dst_fn(t)
                        k.dma("pool", dap, xt[:], reads=[xt], writes=[dbuf])
            k.barrier()

        if stop_after == "moe0":
            d_y = Buf(None, "y")

            def dst0(t):
                if t >= 2:
                    return y_out[(t - 2) * 128:(t - 1) * 128, :], d_y
                return x2[t * 128:(t + 1) * 128, :], d_x2[t]
            moe(0, x1, d_x1, dst0, [0, 1] + list(range(2, NT)))
            k.drain_all()
            return nc

        moe(0, x1, d_x1, lambda t: (x2[t * 128:(t + 1) * 128, :], d_x2[t]), [0, 1] + list(range(2, NT)))
        MLA_SCALE = 96 ** -0.5
        O_Z, O_BETA = 1536, 2048

        def odd_pass(direction):
            fwd = direction == 0
            with ExitStack() as es:
                A1, S1 = load_modcols(es, 1, norm_mix_w[1], 0)
                win = k.sb("owin", [128, 8, 2480], BF16, es)
                cdiag = k.sb("cdiag", [128, 60, 128], BF16, es)
                tri = k.sb("otri", [128, 128], F32, es)
                stri = k.sb("ostri", [128, 128], F32, es)
                allb = k.sb("oallb", [128, 128], F32, es)
                nallb = k.sb("onallb", [128, 128], F32, es)
                self_ = k.sb("oself", [128, 128], F32, es)
                sels_ = k.sb("osels", [128, 128], F32, es)
                aneg_bc = k.sb("aneg", [128, 8], F32, es)
                dtb_bc = k.sb("dtb", [128, 8], F32, es)
                if fwd:
                    wq = k.sb("wq", [128, 2, 768], BF16, es)
                    wkv = k.sb("wkv", [128, 1, 1024], BF16, es)
                    qnw_bc = k.sb("qnw", [128, 256], F32, es)
                    kvnw_bc = k.sb("kvnw", [128, 128], F32, es)
                    qkq_bc = k.sb("qkq", [128, 96], F32, es)
                    qkk_bc = k.sb("qkk", [128, 96], F32, es)
                else:
                    wout_g = k.sb("owout_g", [128, 8, 1024], BF16, es)
                    gnw_bc = k.sb("ognw", [128, 128], F32, es)
                with ExitStack() as es2:
                    stage = k.sbpool("stage", [128, 8, 512], F32, 2, es2)
                    load_w_bf16(es2, win, odd_w_in[0], 8, 2480, stage)
                    cw = k.sb("cw", [128, 12, 5], F32, es2)
                    for kk in range(5):
                        k.dma("sp", cw[:, :, kk], gdn_conv_w[0, kk, :].rearrange("(c p) -> p c", p=128), writes=[cw],
                              allow_slow_non_contiguous=True)
                    for c in range(12):
                        for kk in range(5):
                            k.op("dve" if (c + kk) % 2 else "pool",
                                 lambda e: e.tensor_scalar(out=cdiag[:, c * 5 + kk, :], in0=ident[:],
                                                           scalar1=cw[:, c, kk:kk + 1], scalar2=None, op0=ALU.mult),
                                 reads=[ident, cw], writes=[cdiag])
                    tr_ = blockmask("tri_", "le" if fwd else "ge", es2)
                    al_ = blockmask("all_", "all", es2)
                    k.op("dve", lambda e: e.tensor_copy(out=tri[:], in_=tr_[:]), reads=[tr_], writes=[tri])
                    k.op("dve", lambda e: e.tensor_tensor(out=stri[:], in0=tr_[:], in1=ident[:], op=ALU.subtract),
                         reads=[tr_, ident], writes=[stri])
                    k.op("dve", lambda e: e.tensor_copy(out=allb[:], in_=al_[:]), reads=[al_], writes=[allb])
                    k.op("dve", lambda e: e.tensor_scalar(out=nallb[:], in0=al_[:], scalar1=-1.0, scalar2=None,
                                                          op0=ALU.mult), reads=[al_], writes=[nallb])
                    k.op("pool", lambda e: e.memset(self_[:], 0.0), writes=[self_])
                    k.op("pool", lambda e: e.memset(sels_[:], 0.0), writes=[sels_])
                    f0, s0_ = (0, 64) if fwd else (64, 0)
                    k.op("pool", lambda e: e.tensor_copy(out=self_[f0:f0 + 64, :], in_=ones[f0:f0 + 64, :]),
                         reads=[ones], writes=[self_])
                    k.op("pool", lambda e: e.tensor_copy(out=sels_[s0_:s0_ + 64, :], in_=ones[s0_:s0_ + 64, :]),
                         reads=[ones], writes=[sels_])
                    k.dma("sp", aneg_bc[:], gdn_a_log[0].rearrange("a b -> (a b)").partition_broadcast(128),
                          writes=[aneg_bc])
                    k.op("act", lambda e: e.activation(out=aneg_bc[:], in_=aneg_bc[:], func=AF.Exp), reads=[aneg_bc],
                         writes=[aneg_bc])
                    k.op("dve", lambda e: e.tensor_scalar(out=aneg_bc[:], in0=aneg_bc[:], scalar1=-1.0, scalar2=None,
                                                          op0=ALU.mult), reads=[aneg_bc], writes=[aneg_bc])
                    k.dma("sp", dtb_bc[:], gdn_dt_bias[0].rearrange("a b -> (a b)").partition_broadcast(128),
                          writes=[dtb_bc])
                    if fwd:
                        st = stage.next()
                        k.dma("sp", st[:, 0:2, 0:512], mla_wq_up[0][:, 0:512].rearrange("(kc p) n -> p kc n", p=128),
                              writes=[st])
                        k.op("dve", lambda e: e.tensor_copy(out=wq[:, :, 0:512], in_=st[:, 0:2, 0:512]), reads=[st],
                             writes=[wq])
                        st = stage.next()
                        k.dma("sp", st[:, 0:2, 0:256], mla_wq_up[0][:, 512:768].rearrange("(kc p) n -> p kc n", p=128),
                              writes=[st])
                        k.op("dve", lambda e: e.tensor_copy(out=wq[:, :, 512:768], in_=st[:, 0:2, 0:256]), reads=[st],
                             writes=[wq])
                        for c0 in (0, 512):
                            st = stage.next()
                            k.dma("sp", st[:, 0:1, 0:512], mla_wkv_up[0][:, c0:c0 + 512].rearrange(
                                "(kc p) n -> p kc n", p=128), writes=[st])
                            k.op("dve", lambda e: e.tensor_copy(out=wkv[:, :, c0:c0 + 512], in_=st[:, 0:1, 0:512]),
                                 reads=[st], writes=[wkv])
                        k.dma("sp", qnw_bc[:], mla_q_norm_w[0].partition_broadcast(128), writes=[qnw_bc])
                        k.dma("sp", kvnw_bc[:], mla_kv_norm_w[0].partition_broadcast(128), writes=[kvnw_bc])
                        k.dma("sp", qkq_bc[:], mla_qk_norm_q[0].partition_broadcast(128), writes=[qkq_bc])
                        k.op("dve", lambda e: e.tensor_scalar(out=qkq_bc[:], in0=qkq_bc[:], scalar1=MLA_SCALE,
                                                              scalar2=None, op0=ALU.mult), reads=[qkq_bc],
                             writes=[qkq_bc])
                        k.dma("sp", qkk_bc[:], mla_qk_norm_k[0].partition_broadcast(128), writes=[qkk_bc])
                    else:
                        gbc = k.sb("gbc", [128, 1024], F32, es2)
                        k.dma("sp", gbc[:], modrow[1, 0, 2 * D:3 * D].partition_broadcast(128), reads=[d_modrow],
                              writes=[gbc])
                        for c0 in range(0, 1024, 512):
                            st = stage.next()
                            k.dma("sp", st[:], odd_w_out[0][:, c0:c0 + 512].rearrange("(kc p) n -> p kc n", p=128),
                                  writes=[st])
                            for kc in range(8):
                                k.op("dve" if kc % 2 else "pool",
                                     lambda e: e.tensor_tensor(out=wout_g[:, kc, c0:c0 + 512], in0=st[:, kc, :],
                                                               in1=gbc[:, c0:c0 + 512], op=ALU.mult),
                                     reads=[st, gbc], writes=[wout_g])
                        k.dma("sp", gnw_bc[:], gdn_norm_w[0].partition_broadcast(128), writes=[gnw_bc])
                    k.barrier()

                Sst = k.sb("oSst", [128, 4, 128], F32, es)
                k.op("pool", lambda e: e.memset(Sst[:], 0.0), writes=[Sst])
                xpool = k.sbpool("oxt", [128, 1024], F32, 2, es)
                rpool = k.sbpool("ors", [128, 8], F32, 6, es)
                xnpool = {"junk": k.sbpool("ojunk", [128, 1024], BF16, 1, es),
                          "bf": k.sbpool("oxnb", [128, 1024], BF16, 2, es)}
                hTp = k.sbpool("ohT", [128, 8, 128], BF16, 4, es)
                hextp = k.sbpool("ohext", [128, 8, 132], BF16, 2, es)
                pTp = k.sbpool("opT", [128, 12, 132], BF16, 2, es)
                qkvTp = k.sbpool("oqkvT", [128, 12, 128], BF16, 2, es)
                qkvtokp = k.sbpool("oqkvtok", [128, 1536], F32, 2, es)
                qkntokp = k.sbpool("oqkntok", [128, 8, 128], BF16, 2, es)
                qknTp = k.sbpool("oqknT", [128, 8, 128], BF16, 2, es)
                hbufs = []
                for h_ in range(4):
                    d_ = {}
                    for nm in ("TL", "Gm", "t1", "t2", "u32", "oa"):
                        d_[nm] = k.sb("h" + nm, [128, 128], F32, es)
                    for nm in ("qkT", "wb", "wT", "kg", "vn", "S0", "S1"):
                        d_[nm] = k.sb("h" + nm, [128, 128], BF16, es)
                    d_["PT"] = [k.sb("hPT", [128, 128], BF16, es) for _ in range(2)]
                    d_["P"] = [k.sb("hP", [128, 128], BF16, es) for _ in range(2)]
                    d_["X32"] = k.sb("hX32", [128, 256], F32, es)
                    d_["Xb"] = k.sb("hXb", [128, 256], BF16, es)
                    hbufs.append(d_)
                s16 = k.sbpool("os16", [128, 4, 8], F32, 6, es)
                osbp = k.sbpool("oosb", [128, 4, 128], F32, 2, es)
                t512 = k.sbpool("ot512", [128, 512], F32, 5, es)
                pst = k.ps("opst", [128, 8, 128], BF16, es)
                ptb = k.ps("optb", [128, 8, 128], BF16, es)
                ptb_s = [Bv(ptb, ptb.t[:, i, :]) for i in range(8)]
                proj = k.pspool("oproj", [128, 512], F32, 2, es)
                pa = k.ps("opa", [128, 512], F32, es)
                pa_s = [Bv(pa, pa.t[:, i * 128:(i + 1) * 128]) for i in range(4)]
                pn = k.ps("opn", [128, 512], F32, es)
                pn_s = [Bv(pn, pn.t[:, i * 128:(i + 1) * 128]) for i in range(4)]
                pb = k.ps("opb", [128, 512], F32, es)
                pb_s = [Bv(pb, pb.t[:, i * 128:(i + 1) * 128]) for i in range(4)]
                pc = k.ps("opc", [128, 512], F32, es)
                pc_s = [Bv(pc, pc.t[:, i * 128:(i + 1) * 128]) for i in range(4)]
                if fwd:
                    qtp = k.sbpool("oqt", [96, 8, 128], BF16, 2, es)
                    vxp = k.sbpool("ovx", [128, 8, 80], BF16, 2, es)
                    for vb_ in vxp.bufs:
                        k.op("pool", lambda e: e.memset(vb_[:], 1.0), writes=[vb_])
                    q96p = k.sbpool("oq96", [128, 8, 96], F32, 2, es)
                    q96bp = k.sbpool("oq96b", [128, 8, 96], BF16, 2, es)
                    ropep = k.sbpool("orope", [128, 2, 16], F32, 2, es)
                    r8p = k.sbpool("or8", [128, 8, 8], F32, 8, es)
                    cqnp = k.sbpool("ocqn", [128, 256], BF16, 2, es)
                    kvsp = k.sbpool("okvs", [128, 512], F32, 2, es)
                    cqnTp = k.sbpool("ocqnT", [128, 3, 128], BF16, 2, es)
                else:
                    catp = k.sbpool("ocat", [128, 512], BF16, 2, es)
                    catTp = k.sbpool("ocatT", [128, 8, 128], BF16, 2, es)

                hcache = {}

                def get_hT(t):
                    if t in hcache:
                        return hcache[t]
                    s = 0 if t >= 2 else 1
                    xt = xpool.next()
                    k.dma("sp", xt[:], x2[t * 128:(t + 1) * 128, :], reads=[d_x2[t]], writes=[xt])
                    hT = hTp.next()
                    for kk_ in [kk_ for kk_, v_ in hcache.items() if v_ is hT]:
                        del hcache[kk_]
                    norm_T(xt, A1, S1, s, rpool, xnpool, pst, out_bf=hT)
                    hcache[t] = hT
                    return hT

                def rope(buf, tabs):
                    for part in range(2):
                        o = 64 + part * 16
                        u1 = buf[:, :, o:o + 8]
                        u2 = buf[:, :, o + 8:o + 16]
                        cs = tabs[:, 0, part * 8:(part + 1) * 8].unsqueeze(1).to_broadcast([128, 8, 8])
                        sn = tabs[:, 1, part * 8:(part + 1) * 8].unsqueeze(1).to_broadcast([128, 8, 8])
                        a = r8p.next(); b = r8p.next(); c_ = r8p.next(); d_ = r8p.next()
                        k.op("dve", lambda e: e.tensor_tensor(out=a[:], in0=u1, in1=cs, op=ALU.mult), reads=[buf, tabs],
                             writes=[a])
                        k.op("dve", lambda e: e.tensor_tensor(out=b[:], in0=u2, in1=sn, op=ALU.mult), reads=[buf, tabs],
                             writes=[b])
                        k.op("dve", lambda e: e.tensor_tensor(out=c_[:], in0=u2, in1=cs, op=ALU.mult), reads=[buf, tabs],
                             writes=[c_])
                        k.op("dve", lambda e: e.tensor_tensor(out=d_[:], in0=u1, in1=sn, op=ALU.mult), reads=[buf, tabs],
                             writes=[d_])
                        k.op("dve", lambda e: e.tensor_tensor(out=u1, in0=a[:], in1=b[:], op=ALU.subtract),
                             reads=[a, b], writes=[buf])
                        k.op("dve", lambda e: e.tensor_tensor(out=u2, in0=c_[:], in1=d_[:], op=ALU.add),
                             reads=[c_, d_], writes=[buf])

                order = list(range(NT)) if fwd else [1, 0] + list(range(NT - 1, 1, -1))
                R1, R2 = (slice(0, 64), slice(64, 128)) if fwd else (slice(64, 128), slice(0, 64))

                for t in order:
                    lat = t >= 2
                    has_prev = t not in (0, 2)
                    has_next = t not in (1, NT - 1)
                    hT = get_hT(t)
                    hprev = get_hT(t - 1) if has_prev else None
                    hnext = get_hT(t + 1) if has_next else None
                    hext = hextp.next()
                    k.op("pool", lambda e: e.tensor_copy(out=hext[:, :, 2:130], in_=hT[:]), reads=[hT], writes=[hext])
                    if has_prev:
                        k.op("pool", lambda e: e.tensor_copy(out=hext[:, :, 0:2], in_=hprev[:, :, 126:128]),
                             reads=[hprev], writes=[hext])
                    else:
                        k.op("pool", lambda e: e.memset(hext[:, :, 0:2], 0.0), writes=[hext])
                    if has_next:
                        k.op("pool", lambda e: e.tensor_copy(out=hext[:, :, 130:132], in_=hnext[:, :, 0:2]),
                             reads=[hnext], writes=[hext])
                    else:
                        k.op("pool", lambda e: e.memset(hext[:, :, 130:132], 0.0), writes=[hext])

                    def proj_tok(c0, n):
                        p = proj.next()
                        for kc in range(8):
                            k.op("pe", lambda e: e.matmul(p[:, 0:n], lhsT=hT[:, kc, :], rhs=win[:, kc, c0:c0 + n],
                                                          start=(kc == 0), stop=(kc == 7)), reads=[hT, win], writes=[p])
                        return p

                    pT = pTp.next()
                    for g3 in range(4):
                        p = proj.next()
                        pv = p[:, 0:396].rearrange("p (c n) -> p c n", n=132)
                        for ci in range(3):
                            c = g3 * 3 + ci
                            for kc in range(8):
                                k.op("pe", lambda e: e.matmul(pv[:, ci, :], lhsT=win[:, kc, c * 128:(c + 1) * 128],
                                                              rhs=hext[:, kc, :], start=(kc == 0), stop=(kc == 7)),
                                     reads=[win, hext], writes=[p])
                        k.op("act", lambda e: e.copy(out=pT[:, g3 * 3:(g3 + 1) * 3, :], in_=pv), reads=[p], writes=[pT])
                    qkvT = qkvTp.next()
                    for g4 in range(3):
                        p = proj.next()
                        for ci in range(4):
                            c = g4 * 4 + ci
                            for kk in range(5):
                                k.op("pe", lambda e: e.matmul(p[:, ci * 128:(ci + 1) * 128], lhsT=cdiag[:, c * 5 + kk, :],
                                                              rhs=pT[:, c, kk:kk + 128], start=(kk == 0), stop=(kk == 4)),
                                     reads=[cdiag, pT], writes=[p])
                        k.op("act", lambda e: e.activation(out=qkvT[:, g4 * 4:(g4 + 1) * 4, :].rearrange("p c n -> p (c n)"),
                                                           in_=p[:], func=AF.Silu), reads=[p], writes=[qkvT])
                    qkvtok = qkvtokp.next()
                    for c in range(8):
                        k.op("pe", lambda e: e.transpose(pst[:, c, :], qkvT[:, c, :], identb[:]), reads=[qkvT, identb],
                             writes=[pst])
                    k.op("act", lambda e: e.copy(out=qkvtok[:, 0:1024], in_=pst[:].rearrange("p c n -> p (c n)")),
                         reads=[pst], writes=[qkvtok])
                    for c in range(4):
                        k.op("pe", lambda e: e.transpose(pst[:, c, :], qkvT[:, 8 + c, :], identb[:]),
                             reads=[qkvT, identb], writes=[pst])
                    k.op("act", lambda e: e.copy(out=qkvtok[:, 1024:1536],
                                                 in_=pst[:, 0:4, :].rearrange("p c n -> p (c n)")), reads=[pst],
                         writes=[qkvtok])
                    sq = qkvtokp.next()
                    k.op("pool", lambda e: e.tensor_tensor(out=sq[:, 0:1024], in0=qkvtok[:, 0:1024],
                                                           in1=qkvtok[:, 0:1024], op=ALU.mult), reads=[qkvtok],
                         writes=[sq])
                    rn = rpool.next()
                    k.op("dve", lambda e: e.tensor_reduce(out=rn[:], in_=sq[:, 0:1024].rearrange("p (h c) -> p h c", c=128),
                                                          axis=AX.X, op=ALU.add), reads=[sq], writes=[rn])
                    k.op("dve", lambda e: e.tensor_scalar(out=rn[:], in0=rn[:], scalar1=EPS, scalar2=None, op0=ALU.add),
                         reads=[rn], writes=[rn])
                    rsqrt(rn, rn[:], rn[:])
                    k.op("dve", lambda e: e.tensor_scalar(out=rn[:, 0:4], in0=rn[:, 0:4], scalar1=128 ** -0.5,
                                                          scalar2=None, op0=ALU.mult), reads=[rn], writes=[rn])
                    qkntok = qkntokp.next()
                    k.op("dve", lambda e: e.tensor_tensor(out=qkntok[:],
                                                          in0=qkvtok[:, 0:1024].rearrange("p (h c) -> p h c", c=128),
                                                          in1=rn[:].unsqueeze(2).to_broadcast([128, 8, 128]),
                                                          op=ALU.mult), reads=[qkvtok, rn], writes=[qkntok])
                    for c in range(8):
                        k.op("pe", lambda e: e.transpose(pst[:, c, :], qkntok[:, c, :], identb[:]),
                             reads=[qkntok, identb], writes=[pst])
                    qknT = qknTp.next()
                    k.op("act", lambda e: e.copy(out=qknT[:], in_=pst[:]), reads=[pst], writes=[qknT])
                    p2p = proj_tok(O_BETA, 432)
                    p2 = t512.next()
                    k.op("act", lambda e: e.copy(out=p2[:, 0:432], in_=p2p[:, 0:432]), reads=[p2p], writes=[p2])
                    bl = s16.next()
                    blv = bl[:].rearrange("p a b -> p (a b)")
                    k.op("act", lambda e: e.activation(out=blv[:, 0:8], in_=p2[:, 0:8], func=AF.Sigmoid), reads=[p2],
                         writes=[bl])
                    sp_ = s16.next()
                    spv = sp_[:].rearrange("p a b -> p (a b)")
                    k.op("dve", lambda e: e.tensor_tensor(out=spv[:, 0:8], in0=p2[:, 8:16], in1=dtb_bc[:], op=ALU.add),
                         reads=[p2, dtb_bc], writes=[sp_])
                    k.op("act", lambda e: e.activation(out=spv[:, 8:16], in_=spv[:, 0:8], func=AF.Abs), reads=[sp_],
                         writes=[sp_])
                    k.op("act", lambda e: e.activation(out=spv[:, 8:16], in_=spv[:, 8:16], func=AF.Exp, scale=-1.0),
                         reads=[sp_], writes=[sp_])
                    k.op("act", lambda e: e.activation(out=spv[:, 8:16], in_=spv[:, 8:16], func=AF.Ln, bias=1.0),
                         reads=[sp_], writes=[sp_])
                    k.op("dve", lambda e: e.tensor_scalar(out=spv[:, 0:8], in0=spv[:, 0:8], scalar1=0.0, scalar2=None,
                                                          op0=ALU.max), reads=[sp_], writes=[sp_])
                    k.op("dve", lambda e: e.tensor_tensor(out=spv[:, 0:8], in0=spv[:, 0:8], in1=spv[:, 8:16],
                                                          op=ALU.add), reads=[sp_], writes=[sp_])
                    k.op("dve", lambda e: e.tensor_tensor(out=blv[:, 8:16], in0=spv[:, 0:8], in1=aneg_bc[:],
                                                          op=ALU.mult), reads=[sp_, aneg_bc], writes=[bl])
                    dsl = slice(direction * 4, direction * 4 + 4)
                    la4 = bl[:, 1, dsl]
                    be4 = bl[:, 0, dsl]
                    pg = pa_s[3]
                    for i_, lh in enumerate((tri, allb, self_, sels_)):
                        k.op("pe", lambda e: e.matmul(pg[:, i_ * 4:(i_ + 1) * 4], lhsT=lh[:], rhs=la4, start=True,
                                                      stop=True), reads=[lh, bl], writes=[pg])
                    E4 = s16.next()
                    k.op("dve", lambda e: e.tensor_copy(out=E4[:, :, 0:4], in_=pg[:, 0:16].rearrange("p (a b) -> p a b", b=4)),
                         reads=[pg], writes=[E4])
                    k.op("dve", lambda e: e.tensor_tensor(out=E4[:, 1, 0:4], in0=E4[:, 1, 0:4], in1=E4[:, 0, 0:4],
                                                          op=ALU.subtract), reads=[E4], writes=[E4])
                    k.op("act", lambda e: e.activation(out=E4[:, :, 0:4], in_=E4[:, :, 0:4], func=AF.Exp), reads=[E4],
                         writes=[E4])
                    osb = osbp.next()
                    HB = hbufs
                    for h in range(4):
                        B_ = HB[h]
                        knT_h = qknT[:, 4 + h, :]
                        qnT_h = qknT[:, h, :]
                        la_c = la4[:, h:h + 1]
                        be_c = be4[:, h:h + 1]
                        eg_c = E4[:, 0, h:h + 1]
                        TL = B_["TL"]
                        k.op("pool", lambda e: e.tensor_scalar(out=TL[:], in0=tri[:], scalar1=la_c, scalar2=None,
                                                               op0=ALU.mult), reads=[tri, bl], writes=[TL])
                        reg = pa_s if h % 2 == 0 else pc_s
                        GT, KK, QK = reg[0], reg[1], reg[2]
                        k.op("pe", lambda e: e.matmul(GT[:], lhsT=allb[:], rhs=TL[:], start=True, stop=False),
                             reads=[allb, TL], writes=[GT])
                        k.op("pe", lambda e: e.matmul(GT[:], lhsT=TL[:], rhs=nallb[:], start=False, stop=True),
                             reads=[nallb, TL], writes=[GT])
                        k.op("pe", lambda e: e.matmul(KK[:], lhsT=knT_h, rhs=knT_h, start=True, stop=True),
                             reads=[qknT], writes=[KK])
                        k.op("pe", lambda e: e.matmul(QK[:], lhsT=knT_h, rhs=qnT_h, start=True, stop=True),
                             reads=[qknT], writes=[QK])
                        Gm = B_["Gm"]
                        k.op("dve", lambda e: e.tensor_scalar(out=Gm[:], in0=GT[:], scalar1=0.0, scalar2=None,
                                                              op0=ALU.min), reads=[GT], writes=[Gm])
                        k.op("act", lambda e: e.activation(out=Gm[:], in_=Gm[:], func=AF.Exp), reads=[Gm], writes=[Gm])
                        t1 = B_["t1"]
                        k.op("dve", lambda e: e.tensor_tensor(out=t1[:], in0=KK[:], in1=Gm[:], op=ALU.mult),
                             reads=[KK, Gm], writes=[t1])
                        PT = B_["PT"][0]
                        k.op("dve", lambda e: e.scalar_tensor_tensor(out=PT[:], in0=t1[:], scalar=be_c, in1=stri[:],
                                                                     op0=ALU.mult, op1=ALU.mult),
                             reads=[t1, bl, stri], writes=[PT])
                        t2 = B_["t2"]
                        k.op("dve", lambda e: e.tensor_tensor(out=t2[:], in0=QK[:], in1=Gm[:], op=ALU.mult),
                             reads=[QK, Gm], writes=[t2])
                        qkT = B_["qkT"]
                        k.op("pool", lambda e: e.tensor_tensor(out=qkT[:], in0=t2[:], in1=tri[:], op=ALU.mult),
                             reads=[t2, tri], writes=[qkT])
                        X32 = B_["X32"]
                        Xb = B_["Xb"]
                        k.op("act", lambda e: e.copy(out=X32[:, 0:128], in_=qkvtok[:, 1024 + h * 128:1024 + (h + 1) * 128]),
                             reads=[qkvtok], writes=[X32])
                        k.op("pool", lambda e: e.tensor_scalar(out=X32[:, 128:256], in0=qkntok[:, 4 + h, :], scalar1=eg_c,
                                                               scalar2=None, op0=ALU.mult), reads=[qkntok, E4],
                             writes=[X32])
                        k.op("act", lambda e: e.copy(out=Xb[:], in_=X32[:]), reads=[X32], writes=[Xb])
                    for h in range(4):
                        B_ = HB[h]
                        ptp = ptb_s[h]
                        k.op("pe", lambda e: e.transpose(ptp[:], B_["PT"][0][:], identb[:]),
                             reads=[B_["PT"][0], identb], writes=[ptp])
                        k.op("act", lambda e: e.copy(out=B_["P"][0][:], in_=ptp[:]), reads=[ptp], writes=[B_["P"][0]])
                    xreg = [(pn_s[0], pn_s[1]), (pn_s[2], pn_s[3]), (pb_s[0], pb_s[1]), (pb_s[2], pb_s[3])]
                    sreg = [(pa_s[0], pc_s[0]), (pa_s[1], pc_s[1]), (pa_s[2], pc_s[2]), (pa_s[3], pc_s[3])]
                    cur = [0, 0, 0, 0]

                    def x_update(h, sub):
                        B_ = HB[h]
                        PT = B_["PT"][cur[h]]
                        xa, xb_ = xreg[h]
                        k.op("pe", lambda e: e.matmul(xa[:], lhsT=PT[:], rhs=B_["Xb"][:, 0:128], start=True, stop=True),
                             reads=[PT, B_["Xb"]], writes=[xa])
                        k.op("pe", lambda e: e.matmul(xb_[:], lhsT=PT[:], rhs=B_["Xb"][:, 128:256], start=True,
                                                      stop=True), reads=[PT, B_["Xb"]], writes=[xb_])

                    def x_apply(h, sub, last):
                        B_ = HB[h]
                        xa, xb_ = xreg[h]
                        op_ = ALU.subtract if sub else ALU.add
                        k.op("dve", lambda e: e.tensor_tensor(out=B_["X32"][:, 0:128], in0=B_["X32"][:, 0:128], in1=xa[:],
                                                              op=op_), reads=[B_["X32"], xa], writes=[B_["X32"]])
                        k.op("dve", lambda e: e.tensor_tensor(out=B_["X32"][:, 128:256], in0=B_["X32"][:, 128:256],
                                                              in1=xb_[:], op=op_), reads=[B_["X32"], xb_],
                             writes=[B_["X32"]])
                        if not last:
                            k.op("act", lambda e: e.copy(out=B_["Xb"][:], in_=B_["X32"][:]), reads=[B_["X32"]],
                                 writes=[B_["Xb"]])

                    for h in range(4):
                        x_update(h, True)
                    for h in range(4):
                        x_apply(h, True, False)
                    for lvl in range(1, 6):
                        for h in range(4):
                            B_ = HB[h]
                            c_ = cur[h]
                            P, PT = B_["P"][c_], B_["PT"][c_]
                            sa, sb_ = sreg[h]
                            k.op("pe", lambda e: e.matmul(sa[:], lhsT=P[:], rhs=PT[:], start=True, stop=True),
                                 reads=[P, PT], writes=[sa])
                            if lvl < 5:
                                k.op("pe", lambda e: e.matmul(sb_[:], lhsT=PT[:], rhs=P[:], start=True, stop=True),
                                     reads=[P, PT], writes=[sb_])
                        for h in range(4):
                            B_ = HB[h]
                            n_ = 1 - cur[h]
                            sa, sb_ = sreg[h]
                            k.op("act", lambda e: e.copy(out=B_["PT"][n_][:], in_=sa[:]), reads=[sa],
                                 writes=[B_["PT"][n_]])
                            if lvl < 5:
                                k.op("dve", lambda e: e.tensor_copy(out=B_["P"][n_][:], in_=sb_[:]), reads=[sb_],
                                     writes=[B_["P"][n_]])
                            cur[h] = n_
                        for h in range(4):
                            x_update(h, False)
                        for h in range(4):
                            x_apply(h, False, lvl == 5)
                    for h in range(4):
                        B_ = HB[h]
                        be_c = be4[:, h:h + 1]
                        egl_c = E4[:, 1, h:h + 1]
                        k.op("dve", lambda e: e.tensor_scalar(out=B_["u32"][:], in0=B_["X32"][:, 0:128], scalar1=be_c,
                                                              scalar2=None, op0=ALU.mult), reads=[B_["X32"], bl],
                             writes=[B_["u32"]])
                        k.op("dve", lambda e: e.tensor_scalar(out=B_["wb"][:], in0=B_["X32"][:, 128:256], scalar1=be_c,
                                                              scalar2=None, op0=ALU.mult), reads=[B_["X32"], bl],
                             writes=[B_["wb"]])
                        k.op("pool", lambda e: e.tensor_scalar(out=B_["kg"][:], in0=qkntok[:, 4 + h, :], scalar1=egl_c,
                                                               scalar2=None, op0=ALU.mult), reads=[qkntok, E4],
                             writes=[B_["kg"]])
                        k.op("act", lambda e: e.copy(out=B_["S0"][:], in_=Sst[:, h, :]), reads=[Sst], writes=[B_["S0"]])
                    for h in range(4):
                        B_ = HB[h]
                        wtp = ptb_s[4 + h]
                        k.op("pe", lambda e: e.transpose(wtp[:], B_["wb"][:], identb[:]), reads=[B_["wb"], identb],
                             writes=[wtp])
                        k.op("act", lambda e: e.copy(out=B_["wT"][:], in_=wtp[:]), reads=[wtp], writes=[B_["wT"]])
                    banks = [pa_s, pn_s, pb_s, pc_s]
                    for h in range(4):
                        B_ = HB[h]
                        r0, r1 = banks[h][0], banks[h][1]
                        k.op("pe", lambda e: e.matmul(r0[:], lhsT=B_["wT"][:], rhs=B_["S0"][:], start=True, stop=True),
                             reads=[B_["wT"], B_["S0"]], writes=[r0])
                    for h in range(4):
                        B_ = HB[h]
                        r0, r1 = banks[h][0], banks[h][1]
                        k.op("dve", lambda e: e.tensor_tensor(out=B_["vn"][R1, :], in0=B_["u32"][R1, :], in1=r0[R1, :],
                                                              op=ALU.subtract), reads=[B_["u32"], r0], writes=[B_["vn"]])
                        k.op("pe", lambda e: e.matmul(r1[:], lhsT=B_["kg"][R1, :], rhs=B_["vn"][R1, :], start=True,
                                                      stop=True), reads=[B_["kg"], B_["vn"]], writes=[r1])
                    for h in range(4):
                        B_ = HB[h]
                        r0, r1 = banks[h][0], banks[h][1]
                        k.op("dve", lambda e: e.scalar_tensor_tensor(out=Sst[:, h, :], in0=Sst[:, h, :],
                                                                     scalar=E4[:, 2, h:h + 1], in1=r1[:],
                                                                     op0=ALU.mult, op1=ALU.add),
                             reads=[Sst, E4, r1], writes=[Sst])
                        k.op("act", lambda e: e.copy(out=B_["S1"][:], in_=Sst[:, h, :]), reads=[Sst], writes=[B_["S1"]])
                        k.op("pe", lambda e: e.matmul(r0[:], lhsT=B_["wT"][:], rhs=B_["S1"][:], start=True, stop=True),
                             reads=[B_["wT"], B_["S1"]], writes=[r0])
                    for h in range(4):
                        B_ = HB[h]
                        r0, r1 = banks[h][0], banks[h][1]
                        k.op("dve", lambda e: e.tensor_tensor(out=B_["vn"][R2, :], in0=B_["u32"][R2, :], in1=r0[R2, :],
                                                              op=ALU.subtract), reads=[B_["u32"], r0], writes=[B_["vn"]])
                        k.op("pe", lambda e: e.matmul(r1[:], lhsT=B_["kg"][R2, :], rhs=B_["vn"][R2, :], start=True,
                                                      stop=True), reads=[B_["kg"], B_["vn"]], writes=[r1])
                    for h in range(4):
                        B_ = HB[h]
                        r0, r1, r2, r3 = banks[h]
                        qnT_h = qknT[:, h, :]
                        k.op("dve", lambda e: e.scalar_tensor_tensor(out=Sst[:, h, :], in0=Sst[:, h, :],
                                                                     scalar=E4[:, 3, h:h + 1], in1=r1[:],
                                                                     op0=ALU.mult, op1=ALU.add),
                             reads=[Sst, E4, r1], writes=[Sst])
                        k.op("pe", lambda e: e.matmul(r0[:], lhsT=B_["qkT"][:], rhs=B_["vn"][:], start=True, stop=True),
                             reads=[B_["qkT"], B_["vn"]], writes=[r0])
                        k.op("pe", lambda e: e.matmul(r2[:], lhsT=qnT_h, rhs=B_["S0"][:], start=True, stop=True),
                             reads=[qknT, B_["S0"]], writes=[r2])
                        k.op("pe", lambda e: e.matmul(r3[:], lhsT=qnT_h, rhs=B_["S1"][:], start=True, stop=True),
                             reads=[qknT, B_["S1"]], writes=[r3])
                    for h in range(4):
                        B_ = HB[h]
                        r0, r1, r2, r3 = banks[h]
                        oa = B_["oa"]
                        k.op("act", lambda e: e.copy(out=oa[:], in_=r0[:]), reads=[r0], writes=[oa])
                        k.op("dve", lambda e: e.scalar_tensor_tensor(out=osb[R1, h, :], in0=r2[R1, :],
                                                                     scalar=E4[R1, 0, h:h + 1], in1=oa[R1, :],
                                                                     op0=ALU.mult, op1=ALU.add),
                             reads=[r2, E4, oa], writes=[osb])
                        k.op("dve", lambda e: e.scalar_tensor_tensor(out=osb[R2, h, :], in0=r3[R2, :],
                                                                     scalar=E4[R2, 0, h:h + 1], in1=oa[R2, :],
                                                                     op0=ALU.mult, op1=ALU.add),
                             reads=[r3, E4, oa], writes=[osb])
                    osbv = osb[:].rearrange("p h c -> p (h c)")
                    if fwd:
                        if lat:
                            k.dma("pool", of_scr[t * 128:(t + 1) * 128, :], osbv, reads=[osb], writes=[d_of[t]])
                        if os.environ.get("SKIP_MLA"):
                            continue
                        tabs = None
                        if lat:
                            tabs = ropep.next()
                            n0 = (t - 2) * 128
                            k.dma("sp", tabs[:, 0, :], rope_cos[n0:n0 + 128, :], writes=[tabs])
                            k.dma("sp", tabs[:, 1, :], rope_sin[n0:n0 + 128, :], writes=[tabs])
                        jk = t512.next()
                        r1 = rpool.next()
                        k.op("act", lambda e: e.activation(out=jk[:, 0:128], in_=p2[:, 272:400], func=AF.Square,
                                                           accum_out=r1[:, 0:1]), reads=[p2], writes=[jk, r1])
                        k.op("dve", lambda e: e.tensor_scalar(out=r1[:, 1:2], in0=r1[:, 0:1], scalar1=1.0 / 128,
                                                              scalar2=EPS, op0=ALU.mult, op1=ALU.add), reads=[r1],
                             writes=[r1])
                        rsqrt(r1, r1[:, 2:3], r1[:, 1:2])
                        cn = cqnp.next()
                        k.op("dve", lambda e: e.scalar_tensor_tensor(out=cn[:, 0:128], in0=p2[:, 272:400],
                                                                     scalar=r1[:, 2:3], in1=kvnw_bc[:], op0=ALU.mult,
                                                                     op1=ALU.mult), reads=[p2, r1, kvnw_bc], writes=[cn])
                        cnT = cqnTp.next()
                        k.op("pe", lambda e: e.transpose(ptb_s[0][:], cn[:, 0:128], identb[:]), reads=[cn, identb],
                             writes=[ptb_s[0]])
                        k.op("act", lambda e: e.copy(out=cnT[:, 2, :], in_=ptb_s[0][:]), reads=[ptb_s[0]], writes=[cnT])
                        MSUB = int(os.environ.get("MLA_SUB", "9"))
                        if MSUB < 2:
                            continue
                        k96 = q96p.next()
                        vx = vxp.next()
                        for half in range(2):
                            pk = proj.next()
                            k.op("pe", lambda e: e.matmul(pk[:], lhsT=cnT[:, 2, :], rhs=wkv[:, 0, half * 512:(half + 1) * 512],
                                                          start=True, stop=True), reads=[cnT, wkv], writes=[pk])
                            pkv = pk[:].rearrange("p (h c) -> p h c", c=128)
                            hs4 = slice(half * 4, half * 4 + 4)
                            kvs = kvsp.next()
                            k.op("act", lambda e: e.copy(out=kvs[:], in_=pk[:]), reads=[pk], writes=[kvs])
                            kvsv = kvs[:].rearrange("p (h c) -> p h c", c=128)
                            k.op("pool", lambda e: e.tensor_copy(out=k96[:, hs4, 0:64], in_=kvsv[:, :, 0:64]), reads=[kvs],
                                 writes=[k96])
                            k.op("pool", lambda e: e.tensor_copy(out=vx[:, hs4, 0:64], in_=kvsv[:, :, 64:128]), reads=[kvs],
                                 writes=[vx])
                        if MSUB < 3:
                            continue
                        k.op("dve", lambda e: e.tensor_copy(out=k96[:, :, 64:96],
                                                            in_=p2[:, 400:432].unsqueeze(1).to_broadcast([128, 8, 32])),
                             reads=[p2], writes=[k96])
                        if MSUB < 4:
                            continue
                        k.dma("sp", VX[:, t, :, :].rearrange("h p c -> p h c"), vx[:], reads=[vx], writes=[d_VX])

                        def headnorm(buf, wbc):
                            sqh = q96p.next()
                            k.op("pool", lambda e: e.tensor_tensor(out=sqh[:], in0=buf[:], in1=buf[:], op=ALU.mult),
                                 reads=[buf], writes=[sqh])
                            r8 = rpool.next()
                            k.op("dve", lambda e: e.tensor_reduce(out=r8[:], in_=sqh[:], axis=AX.X, op=ALU.add),
                                 reads=[sqh], writes=[r8])
                            k.op("dve", lambda e: e.tensor_scalar(out=r8[:], in0=r8[:], scalar1=1.0 / 96, scalar2=EPS,
                                                                  op0=ALU.mult, op1=ALU.add), reads=[r8], writes=[r8])
                            rsqrt(r8, r8[:], r8[:])
                            k.op("dve", lambda e: e.tensor_tensor(out=buf[:], in0=buf[:],
                                                                  in1=r8[:].unsqueeze(2).to_broadcast([128, 8, 96]),
                                                                  op=ALU.mult), reads=[buf, r8], writes=[buf])
                            k.op("pool", lambda e: e.tensor_tensor(out=buf[:], in0=buf[:],
                                                                   in1=wbc[:].unsqueeze(1).to_broadcast([128, 8, 96]),
                                                                   op=ALU.mult), reads=[buf, wbc], writes=[buf])

                        def heads_T(buf, dram_ap, dbuf):
                            bb = q96bp.next()
                            k.op("act", lambda e: e.copy(out=bb[:], in_=buf[:]), reads=[buf], writes=[bb])
                            for hh in range(8):
                                k.op("pe", lambda e: e.transpose(pst[0:96, hh, :], bb[:, hh, :], identb[:]),
                                     reads=[bb, identb], writes=[pst])
                            qt = qtp.next()
                            k.op("act", lambda e: e.copy(out=qt[:], in_=pst[0:96, :, :]), reads=[pst], writes=[qt])
                            k.dma("sp", dram_ap, qt[:], reads=[qt], writes=[dbuf])

                        MST = int(os.environ.get("MLA_STAGE", "9"))
                        if MST < 2:
                            continue
                        headnorm(k96, qkk_bc)
                        if lat:
                            rope(k96, tabs)
                        if MST < 3:
                            continue
                        heads_T(k96, KT[:, :, t * 128:(t + 1) * 128].rearrange("h d n -> d h n"), d_KT)
                        if lat and MST >= 4:
                            r2 = rpool.next()
                            k.op("act", lambda e: e.activation(out=jk[:, 0:256], in_=p2[:, 16:272], func=AF.Square,
                                                               accum_out=r2[:, 0:1]), reads=[p2], writes=[jk, r2])
                            k.op("dve", lambda e: e.tensor_scalar(out=r2[:, 1:2], in0=r2[:, 0:1], scalar1=1.0 / 256,
                                                                  scalar2=EPS, op0=ALU.mult, op1=ALU.add), reads=[r2],
                                 writes=[r2])
                            rsqrt(r2, r2[:, 2:3], r2[:, 1:2])
                            cq = cqnp.next()
                            k.op("dve", lambda e: e.scalar_tensor_tensor(out=cq[:], in0=p2[:, 16:272], scalar=r2[:, 2:3],
                                                                         in1=qnw_bc[:], op0=ALU.mult, op1=ALU.mult),
                                 reads=[p2, r2, qnw_bc], writes=[cq])
                            cqT = cqnTp.next()
                            for c in range(2):
                                k.op("pe", lambda e: e.transpose(ptb_s[1 + c][:], cq[:, c * 128:(c + 1) * 128], identb[:]),
                                     reads=[cq, identb], writes=[ptb_s[1 + c]])
                                k.op("act", lambda e: e.copy(out=cqT[:, c, :], in_=ptb_s[1 + c][:]), reads=[ptb_s[1 + c]],
                                     writes=[cqT])
                            q96 = q96p.next()
                            q96v = q96[:].rearrange("p h c -> p (h c)")
                            for (c0, n) in ((0, 512), (512, 256)):
                                pq = proj.next()
                                for c in range(2):
                                    k.op("pe", lambda e: e.matmul(pq[:, 0:n], lhsT=cqT[:, c, :], rhs=wq[:, c, c0:c0 + n],
                                                                  start=(c == 0), stop=(c == 1)), reads=[cqT, wq],
                                         writes=[pq])
                                k.op("act", lambda e: e.copy(out=q96v[:, c0:c0 + n], in_=pq[:, 0:n]), reads=[pq],
                                     writes=[q96])
                            headnorm(q96, qkq_bc)
                            rope(q96, tabs)
                            n0 = (t - 2) * 128
                            heads_T(q96, QT[:, :, n0:n0 + 128].rearrange("h d n -> d h n"), d_QT)
                        continue
                    if not lat:
                        continue
                    ofl = t512.next()
                    k.dma("sp", ofl[:], of_scr[t * 128:(t + 1) * 128, :], reads=[d_of[t]], writes=[ofl])
                    k.op("dve", lambda e: e.tensor_tensor(out=osbv, in0=osbv, in1=ofl[:], op=ALU.add), reads=[osb, ofl],
                         writes=[osb])
                    if dbg:
                        k.dma("pool", ob_scr[t * 128:(t + 1) * 128, :], osbv, reads=[osb])
                    sq2 = t512.next()
                    k.op("pool", lambda e: e.tensor_tensor(out=sq2[:], in0=osbv, in1=osbv, op=ALU.mult), reads=[osb],
                         writes=[sq2])
                    rs = rpool.next()
                    k.op("dve", lambda e: e.tensor_reduce(out=rs[:, 0:4], in_=sq2[:].rearrange("p (h c) -> p h c", c=128),
                                                          axis=AX.X, op=ALU.add), reads=[sq2], writes=[rs])
                    k.op("dve", lambda e: e.tensor_scalar(out=rs[:, 0:4], in0=rs[:, 0:4], scalar1=1.0 / 128, scalar2=EPS,
                                                          op0=ALU.mult, op1=ALU.add), reads=[rs], writes=[rs])
                    rsqrt(rs, rs[:, 0:4], rs[:, 0:4])
                    k.op("dve", lambda e: e.tensor_tensor(out=osb[:], in0=osb[:],
                                                          in1=rs[:, 0:4].unsqueeze(2).to_broadcast([128, 4, 128]),
                                                          op=ALU.mult), reads=[osb, rs], writes=[osb])
                    k.op("pool", lambda e: e.tensor_tensor(out=osb[:], in0=osb[:],
                                                           in1=gnw_bc[:].unsqueeze(1).to_broadcast([128, 4, 128]),
                                                           op=ALU.mult), reads=[osb, gnw_bc], writes=[osb])
                    pz = proj_tok(O_Z, 512)
                    sgt = t512.next()
                    k.op("act", lambda e: e.activation(out=sgt[:], in_=pz[:], func=AF.Silu), reads=[pz], writes=[sgt])
                    cat = catp.next()
                    k.op("dve", lambda e: e.tensor_tensor(out=cat[:], in0=osbv, in1=sgt[:], op=ALU.mult),
                         reads=[osb, sgt], writes=[cat])
                    for fc in range(4):
                        k.op("pe", lambda e: e.transpose(pst[:, fc, :], cat[:, fc * 128:(fc + 1) * 128], identb[:]),
                             reads=[cat, identb], writes=[pst])
                    catT = catTp.next()
                    k.op("act", lambda e: e.copy(out=catT[:, 0:4, :], in_=pst[:, 0:4, :]), reads=[pst], writes=[catT])
                    n0 = (t - 2) * 128
                    k.dma("sp", catT[:, 4:8, :], attT[:, n0:n0 + 128].rearrange("(c p) n -> p c n", p=128),
                          reads=[d_att], writes=[catT])
                    xt = xpool.next()
                    k.dma("sp", xt[:], x2[t * 128:(t + 1) * 128, :], reads=[d_x2[t]], writes=[xt])
                    for half in range(2):
                        py = proj.next()
                        for kc in range(8):
                            k.op("pe", lambda e: e.matmul(py[:], lhsT=catT[:, kc, :],
                                                          rhs=wout_g[:, kc, half * 512:(half + 1) * 512],
                                                          start=(kc == 0), stop=(kc == 7)), reads=[catT, wout_g],
                                 writes=[py])
                        k.op("dve", lambda e: e.tensor_tensor(out=xt[:, half * 512:(half + 1) * 512], in0=py[:],
                                                              in1=xt[:, half * 512:(half + 1) * 512], op=ALU.add),
                             reads=[py, xt], writes=[xt])
                    k.dma("pool", x3[t * 128:(t + 1) * 128, :], xt[:], reads=[xt], writes=[d_x3[t]])
            k.barrier()

        def attention():
            with ExitStack() as es:
                ktp = k.sbpool("aKT", [96, T], BF16, 2, es)
                vxp = k.sbpool("aVX", [128, NT, 80], BF16, 2, es)
                qp = k.sbpool("aQ", [96, 512], BF16, 2, es)
                ptp_ = k.sbpool("aP", [128, 512], BF16, 4, es)
                rcp = k.sbpool("arc", [65, 512], F32, 2, es)
                otp = k.sbpool("aot", [64, 512], BF16, 2, es)
                nb10 = k.sb("nb10", [128, 1], F32, es)
                k.op("pool", lambda e: e.memset(nb10[:], -10.0), writes=[nb10])
                pss = k.pspool("aS", [128, 512], F32, 4, es)
                pso = k.pspool("aO", [65, 512], F32, 2, es)
                psb = k.pspool("aB", [64, 512], F32, 1, es)
                QB = min(512, L)
                for h in range(8):
                    kt_ = ktp.next()
                    k.dma("sp", kt_[:], KT[h], reads=[d_KT], writes=[kt_])
                    vx = vxp.next()
                    k.dma("sp", vx[:], VX[h].rearrange("t p c -> p t c"), reads=[d_VX], writes=[vx])
                    for q0 in range(0, L, QB):
                        qt = qp.next()
                        k.dma("sp", qt[:, 0:QB], QT[h, :, q0:q0 + QB], reads=[d_QT], writes=[qt])
                        po = pso.next()
                        psq = {}
                        for kt in range(NT + 2):
                            if kt < NT:
                                ps_ = pss.next()
                                k.op("pe", lambda e: e.matmul(ps_[:, 0:QB], lhsT=kt_[:, kt * 128:(kt + 1) * 128],
                                                              rhs=qt[:, 0:QB], start=True, stop=True), reads=[kt_, qt],
                                     writes=[ps_])
                                psq[kt] = ps_
                            k2 = kt - 2
                            if k2 >= 0:
                                ps2 = psq.pop(k2)
                                pt = ptp_.next()
                                k.op("act", lambda e: e.activation(out=pt[:, 0:QB], in_=ps2[:, 0:QB], func=AF.Exp,
                                                                   bias=nb10[:, 0:1]), reads=[ps2, nb10], writes=[pt])
                                k.op("pe", lambda e: e.matmul(po[:, 0:QB], lhsT=vx[:, k2, 0:65], rhs=pt[:, 0:QB],
                                                              start=(k2 == 0), stop=(k2 == NT - 1)), reads=[vx, pt],
                                     writes=[po])
                        rc = rcp.next()
                        k.op("dve", lambda e: e.reciprocal(out=rc[64:65, 0:QB], in_=po[64:65, 0:QB]), reads=[po],
                             writes=[rc])
                        pb_ = psb.next()
                        k.op("pe", lambda e: e.matmul(pb_[:, 0:QB], lhsT=ones[64:65, 0:64], rhs=rc[64:65, 0:QB],
                                                      start=True, stop=True), reads=[ones, rc], writes=[pb_])
                        k.op("act", lambda e: e.copy(out=rc[0:64, 0:QB], in_=pb_[:, 0:QB]), reads=[pb_], writes=[rc])
                        ot = otp.next()
                        k.op("dve", lambda e: e.tensor_tensor(out=ot[:, 0:QB], in0=po[0:64, 0:QB], in1=rc[0:64, 0:QB],
                                                              op=ALU.mult), reads=[po, rc], writes=[ot])
                        k.dma("pool", attT[h * 64:(h + 1) * 64, q0:q0 + QB], ot[:, 0:QB], reads=[ot], writes=[d_att])
            k.barrier()

        odd_pass(0)
        if stop_after == "odd0":
            k.drain_all()
            return nc
        attention()
        if stop_after == "att":
            k.drain_all()
            return nc
        odd_pass(1)
        d_y = Buf(None, "y")
        if stop_after == "mix1":
            k.drain_all()
            return nc
        moe(1, x3, d_x3, lambda t: (y_out[(t - 2) * 128:(t - 1) * 128, :], d_y), list(range(2, NT)))
        k.drain_all()
    return nc


_W_NAMES = ["c_ctx", "ada_w", "ada_b", "norm_mix_w", "norm_ffn_w", "even_w_in", "even_w_out", "gmlp_norm_w", "gmlp_ws",
            "gmlp_bs", "hgrn_lb_logits", "hgrn_norm_w", "odd_w_in", "odd_w_out", "gdn_conv_w", "gdn_a_log",
            "gdn_dt_bias", "gdn_norm_w", "mla_q_norm_w", "mla_wq_up", "mla_kv_norm_w", "mla_wkv_up", "mla_qk_norm_q",
            "mla_qk_norm_k", "router_group_w", "router_group_b", "router_expert_w", "router_expert_b", "moe_w_gate",
            "moe_w_up", "moe_w_down"]


def rope_tables(L):
    n = np.arange(L)
    row = (n // 64).astype(np.float32)
    col = (n % 64).astype(np.float32)
    freqs = (np.float32(10000.0) ** (-np.arange(8, dtype=np.float32) / np.float32(8))).astype(np.float32)
    ang = np.concatenate([row[:, None] * freqs[None, :], col[:, None] * freqs[None, :]], axis=1).astype(np.float32)
    return np.cos(ang).astype(np.float32), np.sin(ang).astype(np.float32)


def make_in_maps(inputs, ncores, L):
    cos, sin = rope_tables(L)
    shared = {n: np.ascontiguousarray(np.asarray(inputs[n], dtype=np.float32)) for n in _W_NAMES}
    shared["rope_cos"] = cos
    shared["rope_sin"] = sin
    maps = []
    for b in range(ncores):
        m = dict(shared)
        m["x"] = np.ascontiguousarray(np.asarray(inputs["x"][b], dtype=np.float32))
        m["ctx"] = np.ascontiguousarray(np.asarray(inputs["ctx"][b], dtype=np.float32))
        m["c"] = np.ascontiguousarray(np.asarray(inputs["c"][b], dtype=np.float32))
        maps.append(m)
    return maps


def kernel(**inputs):
    x = np.asarray(inputs["x"])
    B, L, _ = x.shape
    nc = build(L)
    maps = make_in_maps(inputs, B, L)
    res = run_bass_kernel_spmd(nc, maps, core_ids=list(range(B)))
    return np.stack([np.asarray(r["y"]) for r in res.results], axis=0).astype(np.float32)
```

```python
import os
import numpy as np
import concourse.bass as bass
import concourse.mybir as mybir
from concourse.bass_utils import run_bass_kernel_spmd
from contextlib import ExitStack

F32 = mybir.dt.float32
BF16 = mybir.dt.bfloat16
I32 = mybir.dt.int32
AF = mybir.ActivationFunctionType
ALU = mybir.AluOpType
AX = mybir.AxisListType
EPS = 1e-6
D = 1024
CTX = 256


class Buf:
    __slots__ = ("t", "w", "r", "name", "psum")

    def __init__(self, t, name="", psum=False):
        self.t = t
        self.w = None
        self.r = {}
        self.name = name
        self.psum = psum

    def __getitem__(self, k):
        return self.t[k]


class Bv:
    def __init__(self, base, ap):
        self.base = base
        self.ap = ap

    @property
    def psum(self):
        return self.base.psum

    def __getitem__(self, kk):
        return self.ap[kk]

    @property
    def w(self):
        return self.base.w

    @w.setter
    def w(self, v):
        self.base.w = v

    @property
    def r(self):
        return self.base.r

    @r.setter
    def r(self, v):
        self.base.r = v


class Pool:
    def __init__(self, bufs):
        self.bufs = bufs
        self.i = 0

    def next(self):
        b = self.bufs[self.i % len(self.bufs)]
        self.i += 1
        return b


class KB:
    NDMA = 8

    def __init__(self, nc):
        self.nc = nc
        self.es = ExitStack()
        self.eng = {"pe": nc.tensor, "dve": nc.vector, "act": nc.scalar, "pool": nc.gpsimd, "sp": nc.sync}
        self.sems = {}
        self.cnt = {}
        self.seen = {e: {} for e in self.eng}
        for e in self.eng:
            self.sems[e] = self.es.enter_context(nc.semaphore("c_" + e))
            self.cnt[e] = 0
        self.dsem = {}
        self.dcnt = {}
        for q in ("sp", "pool", "act"):
            self.dsem[q] = [self.es.enter_context(nc.semaphore("d_%s%d" % (q, i))) for i in range(self.NDMA)]
            self.dcnt[q] = 0
        self.semobj = {}
        for e in self.eng:
            self.semobj[("c", e)] = self.sems[e]
        for q in self.dsem:
            for i, s in enumerate(self.dsem[q]):
                self.semobj[("d", q, i)] = s
        self.nbuf = 0

    def sb(self, name, shape, dtype, es=None):
        self.nbuf += 1
        t = (es or self.es).enter_context(self.nc.sbuf_tensor("%s_%d" % (name, self.nbuf), list(shape), dtype))
        return Buf(t, name)

    def ps(self, name, shape, dtype, es=None):
        self.nbuf += 1
        t = (es or self.es).enter_context(self.nc.psum_tensor("%s_%d" % (name, self.nbuf), list(shape), dtype))
        return Buf(t, name, psum=True)

    def sbpool(self, name, shape, dtype, n, es=None):
        return Pool([self.sb(name, shape, dtype, es) for _ in range(n)])

    def pspool(self, name, shape, dtype, n, es=None):
        return Pool([self.ps(name, shape, dtype, es) for _ in range(n)])

    def dram(self, name, shape, dtype, kind="Internal"):
        return self.nc.dram_tensor(name, list(shape), dtype, kind=kind).ap()

    def _deps(self, reads, writes, e=None):
        deps = []
        for b in reads:
            if b.w is not None:
                deps.append(b.w)
            if b.psum:
                for kk, v in b.r.items():
                    if kk != ("c", e):
                        deps.append((kk, v))
        for b in writes:
            if b.w is not None:
                deps.append(b.w)
            for kk, v in b.r.items():
                deps.append((kk, v))
        return deps

    def _wait(self, e, deps):
        seen = self.seen[e]
        need = {}
        for kk, v in deps:
            if e == "pe" and kk == ("c", "pe"):
                continue
            if seen.get(kk, 0) >= v:
                continue
            if need.get(kk, 0) < v:
                need[kk] = v
        for kk, v in need.items():
            self.eng[e].wait_ge(self.semobj[kk], v)
            seen[kk] = v

    def _mark(self, tok, reads, writes):
        kk, v = tok
        for b in reads:
            if b.r.get(kk, 0) < v:
                b.r[kk] = v
        for b in writes:
            b.w = tok
            b.r = {}

    def op(self, e, fn, reads=(), writes=()):
        self._wait(e, self._deps(reads, writes, e))
        ins = fn(self.eng[e])
        self.cnt[e] += 1
        ins.then_inc(self.sems[e], 1)
        tok = (("c", e), self.cnt[e])
        self._mark(tok, reads, writes)
        return tok

    def dma(self, q, out, in_, reads=(), writes=(), **kw):
        i = self.dcnt[q]
        slot = i % self.NDMA
        kk = ("d", q, slot)
        deps = self._deps(reads, writes, q)
        prev = 16 * (i // self.NDMA)
        if prev > 0:
            deps.append((kk, prev))
        self._wait(q, deps)
        ins = self.eng[q].dma_start(out=out, in_=in_, **kw)
        ins.then_inc(self.dsem[q][slot], 16)
        self.dcnt[q] += 1
        tok = (kk, prev + 16)
        self._mark(tok, reads, writes)
        return tok

    def all_tokens(self):
        deps = []
        for e in self.eng:
            if self.cnt[e] > 0:
                deps.append((("c", e), self.cnt[e]))
        for q in self.dsem:
            n = self.dcnt[q]
            for slot in range(self.NDMA):
                c = (n - slot + self.NDMA - 1) // self.NDMA
                if c > 0:
                    deps.append((("d", q, slot), 16 * c))
        return deps

    def barrier(self):
        deps = self.all_tokens()
        for e in self.eng:
            self._wait(e, deps)

    def drain_all(self):
        self._wait("sp", self.all_tokens())


def build(L, stop_after="all", dbg=False):
    nc = bass.Bass("TRN2", target_bir_lowering=False)
    k = KB(nc)
    NT = (CTX + L) // 128
    NL = L // 128
    T = CTX + L

    def din(name, shape):
        return nc.dram_tensor(name, list(shape), F32, kind="ExternalInput").ap()

    x_in = din("x", [L, D])
    ctx_in = din("ctx", [CTX, D])
    c_in = din("c", [D])
    cctx_in = din("c_ctx", [D])
    ada_w = din("ada_w", [2, D, 6 * D])
    ada_b = din("ada_b", [2, 6 * D])
    norm_mix_w = din("norm_mix_w", [2, D])
    norm_ffn_w = din("norm_ffn_w", [2, D])
    even_w_in = din("even_w_in", [1, D, 3584])
    even_w_out = din("even_w_out", [1, D, D])
    gmlp_norm_w = din("gmlp_norm_w", [1, 512])
    gmlp_ws = din("gmlp_ws", [1, 4, 128, 128])
    gmlp_bs = din("gmlp_bs", [1, 4, 128])
    hgrn_lb = din("hgrn_lb_logits", [2, 3, 512])
    hgrn_norm_w = din("hgrn_norm_w", [1, 512])
    router_group_w = din("router_group_w", [2, D, 4])
    router_group_b = din("router_group_b", [2, 4])
    router_expert_w = din("router_expert_w", [2, D, 32])
    router_expert_b = din("router_expert_b", [2, 32])
    moe_w_gate = din("moe_w_gate", [2, 32, D, 512])
    moe_w_up = din("moe_w_up", [2, 32, D, 512])
    moe_w_down = din("moe_w_down", [2, 32, 512, D])
    odd_w_in = din("odd_w_in", [1, D, 2480])
    odd_w_out = din("odd_w_out", [1, D, D])
    gdn_conv_w = din("gdn_conv_w", [1, 5, 1536])
    gdn_a_log = din("gdn_a_log", [1, 2, 4])
    gdn_dt_bias = din("gdn_dt_bias", [1, 2, 4])
    gdn_norm_w = din("gdn_norm_w", [1, 128])
    mla_q_norm_w = din("mla_q_norm_w", [1, 256])
    mla_wq_up = din("mla_wq_up", [1, 256, 768])
    mla_kv_norm_w = din("mla_kv_norm_w", [1, 128])
    mla_wkv_up = din("mla_wkv_up", [1, 128, 1024])
    mla_qk_norm_q = din("mla_qk_norm_q", [1, 96])
    mla_qk_norm_k = din("mla_qk_norm_k", [1, 96])
    rope_cos = din("rope_cos", [L, 16])
    rope_sin = din("rope_sin", [L, 16])

    okind = "ExternalOutput"
    y_out = k.dram("y", [L, D], F32, okind)
    ikind = "ExternalOutput" if dbg else "Internal"
    modrow = k.dram("modrow", [2, 2, 6 * D], F32, ikind)
    of_scr = k.dram("of_scr", [T, 512], F32, ikind)
    x1 = k.dram("x1", [T, D], F32, ikind)
    x2 = k.dram("x2", [T, D], F32, ikind)
    d_modrow = Buf(None, "modrow")
    d_of = [Buf(None, "of%d" % t) for t in range(NT)]
    d_x1 = [Buf(None, "x1_%d" % t) for t in range(NT)]
    d_x2 = [Buf(None, "x2_%d" % t) for t in range(NT)]
    x3 = k.dram("x3", [T, D], F32, ikind)
    d_x3 = [Buf(None, "x3_%d" % t) for t in range(NT)]
    ob_scr = k.dram("ob_scr", [T, 512], F32, ikind)
    QT = k.dram("QT", [8, 96, L], BF16, ikind)
    KT = k.dram("KT", [8, 96, T], BF16, ikind)
    VX = k.dram("VX", [8, NT, 128, 80], BF16, ikind)
    attT = k.dram("attT", [512, L], BF16, ikind)
    d_QT = Buf(None, "QT")
    d_KT = Buf(None, "KT")
    d_VX = Buf(None, "VX")
    d_att = Buf(None, "att")

    def rows(t):
        if t < 2:
            return ctx_in[t * 128:(t + 1) * 128, :]
        return x_in[(t - 2) * 128:(t - 1) * 128, :]

    with k.es:
        ident = k.sb("ident", [128, 128], F32)
        identb = k.sb("identb", [128, 128], BF16)
        ones = k.sb("ones", [128, 128], F32)
        zeros_bf = k.sb("zeros_bf", [128, 512], BF16)
        k.op("pool", lambda e: e.memset(ones[:], 1.0), writes=[ones])
        k.op("pool", lambda e: e.memset(zeros_bf[:], 0.0), writes=[zeros_bf])
        k.op("pool", lambda e: e.affine_select(ident[:], ones[:], pattern=[[-1, 128]], compare_op=ALU.is_equal,
                                               fill=0.0, base=0, channel_multiplier=1), reads=[ones], writes=[ident])
        k.op("dve", lambda e: e.tensor_copy(out=identb[:], in_=ident[:]), reads=[ident], writes=[identb])

        def blockmask(name, lo_keep, es=None):
            m = k.sb(name, [128, 128], F32, es)
            k.op("pool", lambda e: e.memset(m[:], 0.0), writes=[m])
            for cb in range(2):
                sl = m[cb * 64:(cb + 1) * 64, cb * 64:(cb + 1) * 64]
                src = ones[cb * 64:(cb + 1) * 64, cb * 64:(cb + 1) * 64]
                if lo_keep == "all":
                    k.op("pool", lambda e: e.tensor_copy(out=sl, in_=src), reads=[ones], writes=[m])
                else:
                    cm, st = (-1, 1) if lo_keep == "le" else (1, -1)
                    k.op("pool", lambda e: e.affine_select(sl, src, pattern=[[st, 64]], compare_op=ALU.is_ge,
                                                           fill=0.0, base=0, channel_multiplier=cm),
                         reads=[ones], writes=[m])
            return m

        with ExitStack() as es:
            ccol = k.sb("ccol", [128, 8, 2], F32, es)
            scol = k.sb("scol", [128, 8, 2], F32, es)
            k.dma("sp", ccol[:, :, 0], c_in.rearrange("(kc p) -> p kc", p=128), writes=[ccol],
                  allow_slow_non_contiguous=True)
            k.dma("sp", ccol[:, :, 1], cctx_in.rearrange("(kc p) -> p kc", p=128), writes=[ccol],
                  allow_slow_non_contiguous=True)
            k.op("act", lambda e: e.activation(out=scol[:], in_=ccol[:], func=AF.Silu), reads=[ccol], writes=[scol])
            wst = k.sbpool("adaw", [128, 8, 512], F32, 2, es)
            brow = k.sbpool("adab", [2, 512], F32, 2, es)
            orow = k.sbpool("adao", [2, 512], F32, 2, es)
            pp = k.pspool("adap", [2, 512], F32, 2, es)
            for layer in range(2):
                for cb in range(12):
                    w = wst.next()
                    k.dma("sp", w[:], ada_w[layer, :, cb * 512:(cb + 1) * 512].rearrange("(kc p) n -> p kc n", p=128),
                          writes=[w])
                    b = brow.next()
                    k.dma("sp", b[:], ada_b[layer, cb * 512:(cb + 1) * 512].partition_broadcast(2), writes=[b])
                    p = pp.next()
                    for kc in range(8):
                        k.op("pe", lambda e: e.matmul(p[:], lhsT=scol[:, kc, :], rhs=w[:, kc, :], start=(kc == 0),
                                                      stop=(kc == 7)), reads=[scol, w], writes=[p])
                    o = orow.next()
                    k.op("dve", lambda e: e.tensor_tensor(out=o[:], in0=p[:], in1=b[:], op=ALU.add), reads=[p, b],
                         writes=[o])
                    k.dma("sp", modrow[layer, :, cb * 512:(cb + 1) * 512], o[:], reads=[o], writes=[d_modrow])
        k.barrier()
        if stop_after == "prologue":
            k.drain_all()
            return nc

        def rsqrt(buf, out_ap, in_ap):
            k.op("act", lambda e: e.activation(out=out_ap, in_=in_ap, func=AF.Sqrt), reads=[buf], writes=[buf])
            k.op("dve", lambda e: e.reciprocal(out=out_ap, in_=out_ap), reads=[buf], writes=[buf])

        def load_modcols(es, layer, normw_ap, which):
            mc = k.sb("mc", [128, 2, 48], F32, es)
            for s in range(2):
                k.dma("sp", mc[:, s, :], modrow[layer, s, :].rearrange("(c p) -> p c", p=128), reads=[d_modrow],
                      writes=[mc], allow_slow_non_contiguous=True)
            nw = k.sb("nw", [128, 8], F32, es)
            k.dma("sp", nw[:], normw_ap.rearrange("(c p) -> p c", p=128), writes=[nw], allow_slow_non_contiguous=True)
            A = k.sb("A", [128, 2, 8], F32, es)
            S = k.sb("S", [128, 2, 8], F32, es)
            o = which * 24
            for s in range(2):
                k.op("dve", lambda e: e.scalar_tensor_tensor(out=A[:, s, :], in0=mc[:, s, o + 8:o + 16], scalar=1.0,
                                                             in1=nw[:], op0=ALU.add, op1=ALU.mult),
                     reads=[mc, nw], writes=[A])
                k.op("dve", lambda e: e.tensor_copy(out=S[:, s, :], in_=mc[:, s, o:o + 8]), reads=[mc], writes=[S])
            return A, S

        def norm_T(xt, A, S, s, rpool, xnpool, pst, out_bf=None, out_f32=None):
            junk = xnpool["junk"].next()
            ss = rpool.next()
            k.op("act", lambda e: e.activation(out=junk[:], in_=xt[:], func=AF.Square, accum_out=ss[:, 0:1]),
                 reads=[xt], writes=[junk, ss])
            k.op("dve", lambda e: e.tensor_scalar(out=ss[:, 1:2], in0=ss[:, 0:1], scalar1=1.0 / D, scalar2=EPS,
                                                  op0=ALU.mult, op1=ALU.add), reads=[ss], writes=[ss])
            rsqrt(ss, ss[:, 2:3], ss[:, 1:2])
            if out_f32 is None:
                xn = xnpool["bf"].next()
                k.op("act", lambda e: e.activation(out=xn[:], in_=xt[:], func=AF.Copy, scale=ss[:, 2:3]),
                     reads=[xt, ss], writes=[xn])
                for fc in range(8):
                    k.op("pe", lambda e: e.transpose(pst[:, fc, :], xn[:, fc * 128:(fc + 1) * 128], identb[:]),
                         reads=[xn, identb], writes=[pst])
                k.op("dve", lambda e: e.tensor_tensor(out=out_bf[:], in0=pst[:],
                                                      in1=A[:, s, :].unsqueeze(2).to_broadcast([128, 8, 128]),
                                                      op=ALU.mult), reads=[pst, A], writes=[out_bf])
                k.op("dve", lambda e: e.tensor_tensor(out=out_bf[:], in0=out_bf[:],
                                                      in1=S[:, s, :].unsqueeze(2).to_broadcast([128, 8, 128]),
                                                      op=ALU.add), reads=[out_bf, S], writes=[out_bf])
            else:
                xn = xnpool["f32"].next()
                k.op("act", lambda e: e.activation(out=xn[:], in_=xt[:], func=AF.Copy, scale=ss[:, 2:3]),
                     reads=[xt, ss], writes=[xn])
                for half in range(2):
                    p = pst[half]
                    for f4 in range(4):
                        fc = half * 4 + f4
                        k.op("pe", lambda e: e.transpose(p[:, f4, :], xn[:, fc * 128:(fc + 1) * 128], ident[:]),
                             reads=[xn, ident], writes=[p])
                    sl = slice(half * 4, half * 4 + 4)
                    k.op("dve", lambda e: e.tensor_tensor(out=out_f32[:, sl, :], in0=p[:],
                                                          in1=A[:, s, sl].unsqueeze(2).to_broadcast([128, 4, 128]),
                                                          op=ALU.mult), reads=[p, A], writes=[out_f32])
                    k.op("dve", lambda e: e.tensor_tensor(out=out_f32[:, sl, :], in0=out_f32[:, sl, :],
                                                          in1=S[:, s, sl].unsqueeze(2).to_broadcast([128, 4, 128]),
                                                          op=ALU.add), reads=[out_f32, S], writes=[out_f32])
                k.op("act", lambda e: e.copy(out=out_bf[:], in_=out_f32[:]), reads=[out_f32], writes=[out_bf])

        def load_w_bf16(es, dst, src_ap, nkc, ncols, stage_pool, colblk=512, engs=("dve", "act")):
            i = 0
            for c0 in range(0, ncols, colblk):
                c1 = min(ncols, c0 + colblk)
                st = stage_pool.next()
                k.dma("sp", st[:, 0:nkc, 0:c1 - c0], src_ap[:, c0:c1].rearrange("(kc p) n -> p kc n", p=128),
                      writes=[st])
                eng = engs[i % len(engs)]
                i += 1
                if eng == "act":
                    k.op("act", lambda e: e.copy(out=dst[:, :, c0:c1], in_=st[:, 0:nkc, 0:c1 - c0]), reads=[st],
                         writes=[dst])
                else:
                    k.op(eng, lambda e: e.tensor_copy(out=dst[:, :, c0:c1], in_=st[:, 0:nkc, 0:c1 - c0]), reads=[st],
                         writes=[dst])

        def even_pass(direction):
            with ExitStack() as es:
                fwd = direction == 0
                A1, S1 = load_modcols(es, 0, norm_mix_w[0], 0)
                win = k.sb("win", [128, 8, 3584], BF16, es)
                rhsF = k.sb("rhsF", [128, 256], F32, es)
                lrem = k.sb("lrem", [128, 128], F32, es)
                maski = k.sb("maski", [128, 4, 128], I32, es)
                oml_bc = k.sb("oml_bc", [128, 512], F32, es)
                oml_col = k.sb("oml_col", [128, 4], F32, es)
                if not fwd:
                    wout_g = [k.sb("wout_g", [128, 8, 1024], BF16, es) for _ in range(2)]
                    gnw_bc = k.sb("gnw_bc", [128, 512], F32, es)
                    hnw_bc = k.sb("hnw_bc", [128, 512], F32, es)
                    bs_col = k.sb("bs_col", [128, 4], F32, es)
                    wsT = k.sb("wsT", [128, 4, 128], BF16, es)
                with ExitStack() as es2:
                    stage = k.sbpool("stage", [128, 8, 512], F32, 2, es2)
                    load_w_bf16(es2, win, even_w_in[0], 8, 3584, stage)
                    tri = blockmask("tri", "le" if fwd else "ge", es2)
                    allb = blockmask("allb", "all", es2)
                    mid = k.sb("mid", [128, 128], F32, es2)
                    k.op("pool", lambda e: e.memset(mid[:], 0.0), writes=[mid])
                    for cb in range(2):
                        r0 = cb * 64 + (0 if fwd else 32)
                        k.op("pool", lambda e: e.tensor_copy(out=mid[r0:r0 + 32, cb * 64:(cb + 1) * 64],
                                                             in_=ones[r0:r0 + 32, cb * 64:(cb + 1) * 64]),
                             reads=[ones], writes=[mid])
                    k.op("dve", lambda e: e.tensor_tensor(out=rhsF[:, 0:128], in0=tri[:], in1=mid[:], op=ALU.subtract),
                         reads=[tri, mid], writes=[rhsF])
                    k.op("dve", lambda e: e.tensor_copy(out=rhsF[:, 128:256], in_=tri[:]), reads=[tri], writes=[rhsF])
                    k.op("dve", lambda e: e.tensor_tensor(out=lrem[:], in0=allb[:], in1=tri[:], op=ALU.subtract),
                         reads=[allb, tri], writes=[lrem])
                    for h in range(4):
                        k.op("dve", lambda e: e.tensor_copy(out=maski[:, h, :], in_=tri[:]), reads=[tri],
                             writes=[maski])
                    lbt = k.sb("lbt", [128, 3, 512], F32, es2)
                    for r in range(3):
                        k.dma("sp", lbt[:, r, :], hgrn_lb[direction, r, :].partition_broadcast(128), writes=[lbt])
                    k.op("act", lambda e: e.activation(out=lbt[:], in_=lbt[:], func=AF.Exp), reads=[lbt], writes=[lbt])
                    tmpb = k.sb("tmpb", [128, 512], F32, es2)
                    k.op("dve", lambda e: e.tensor_tensor(out=tmpb[:], in0=lbt[:, 0, :], in1=lbt[:, 1, :], op=ALU.add),
                         reads=[lbt], writes=[tmpb])
                    k.op("dve", lambda e: e.tensor_tensor(out=tmpb[:], in0=tmpb[:], in1=lbt[:, 2, :], op=ALU.add),
                         reads=[lbt, tmpb], writes=[tmpb])
                    k.op("dve", lambda e: e.reciprocal(out=tmpb[:], in_=tmpb[:]), reads=[tmpb], writes=[tmpb])
                    k.op("dve", lambda e: e.tensor_tensor(out=oml_bc[:], in0=lbt[:, 1, :], in1=lbt[:, 2, :],
                                                          op=ALU.add), reads=[lbt], writes=[oml_bc])
                    k.op("dve", lambda e: e.tensor_tensor(out=oml_bc[:], in0=oml_bc[:], in1=tmpb[:], op=ALU.mult),
                         reads=[oml_bc, tmpb], writes=[oml_bc])
                    lbc = k.sb("lbc", [128, 3, 4], F32, es2)
                    for r in range(3):
                        k.dma("sp", lbc[:, r, :], hgrn_lb[direction, r, :].rearrange("(h p) -> p h", p=128),
                              writes=[lbc], allow_slow_non_contiguous=True)
                    k.op("act", lambda e: e.activation(out=lbc[:], in_=lbc[:], func=AF.Exp), reads=[lbc], writes=[lbc])
                    tmpc = k.sb("tmpc", [128, 4], F32, es2)
                    k.op("dve", lambda e: e.tensor_tensor(out=tmpc[:], in0=lbc[:, 0, :], in1=lbc[:, 1, :], op=ALU.add),
                         reads=[lbc], writes=[tmpc])
                    k.op("dve", lambda e: e.tensor_tensor(out=tmpc[:], in0=tmpc[:], in1=lbc[:, 2, :], op=ALU.add),
                         reads=[lbc, tmpc], writes=[tmpc])
                    k.op("dve", lambda e: e.reciprocal(out=tmpc[:], in_=tmpc[:]), reads=[tmpc], writes=[tmpc])
                    k.op("dve", lambda e: e.tensor_tensor(out=oml_col[:], in0=lbc[:, 1, :], in1=lbc[:, 2, :],
                                                          op=ALU.add), reads=[lbc], writes=[oml_col])
                    k.op("dve", lambda e: e.tensor_tensor(out=oml_col[:], in0=oml_col[:], in1=tmpc[:], op=ALU.mult),
                         reads=[oml_col, tmpc], writes=[oml_col])
                    if not fwd:
                        gbc = k.sb("gbc", [128, 2, 1024], F32, es2)
                        for s in range(2):
                            k.dma("sp", gbc[:, s, :], modrow[0, s, 2 * D:3 * D].partition_broadcast(128),
                                  reads=[d_modrow], writes=[gbc])
                        for c0 in range(0, 1024, 512):
                            st = stage.next()
                            k.dma("sp", st[:], even_w_out[0][:, c0:c0 + 512].rearrange("(kc p) n -> p kc n", p=128),
                                  writes=[st])
                            for s in range(2):
                                for kc in range(8):
                                    k.op("dve" if kc % 2 else "pool",
                                         lambda e: e.tensor_tensor(out=wout_g[s][:, kc, c0:c0 + 512], in0=st[:, kc, :],
                                                                   in1=gbc[:, s, c0:c0 + 512], op=ALU.mult),
                                         reads=[st, gbc], writes=[wout_g[s]])
                        k.dma("sp", gnw_bc[:], gmlp_norm_w[0].partition_broadcast(128), writes=[gnw_bc])
                        k.dma("sp", hnw_bc[:], hgrn_norm_w[0].partition_broadcast(128), writes=[hnw_bc])
                        k.dma("sp", bs_col[:], gmlp_bs[0].rearrange("g p -> p g"), writes=[bs_col],
                              allow_slow_non_contiguous=True)
                        wsf = k.sb("wsf", [128, 4, 128], F32, es2)
                        k.dma("sp", wsf[:], gmlp_ws[0].rearrange("g p q -> p g q"), writes=[wsf])
                        with ExitStack() as es3:
                            ptmp = k.ps("ptmp", [128, 512], F32, es3)
                            for g in range(4):
                                k.op("pe", lambda e: e.transpose(ptmp[:, g * 128:(g + 1) * 128], wsf[:, g, :], ident[:]),
                                     reads=[wsf, ident], writes=[ptmp])
                            k.op("dve", lambda e: e.tensor_copy(out=wsT[:].rearrange("p g q -> p (g q)"), in_=ptmp[:]),
                                 reads=[ptmp], writes=[wsT])
                            k.barrier()
                    k.barrier()

                Sst = k.sb("Sst", [128, 4, 128], F32, es)
                Sbf = k.sbpool("Sbf", [128, 4, 128], BF16, 3, es)
                k.op("pool", lambda e: e.memset(Sst[:], 0.0), writes=[Sst])
                xpool = k.sbpool("xt", [128, 1024], F32, 3, es)
                rpool = k.sbpool("rs", [128, 4], F32, 4, es)
                xnpool = {"junk": k.sbpool("junk", [128, 1024], BF16, 1, es),
                          "bf": k.sbpool("xnb", [128, 1024], BF16, 2, es)}
                hTp = k.sbpool("hT", [128, 8, 128], BF16, 2, es)
                pst = k.ps("pst", [128, 8, 128], BF16, es)
                proj = k.pspool("proj", [128, 512], F32, 2, es)
                big2 = k.ps("big2", [128, 1024], F32, es)
                gs = k.ps("gs", [128, 512], F32, es)
                ops_ = k.ps("ops", [128, 4, 128], F32, es)
                dSp = k.ps("dSp", [128, 4, 128], F32, es)
                t512 = k.sbpool("t512", [128, 512], F32, 10, es)
                b512 = k.sbpool("b512", [128, 512], BF16, 10, es)
                qhp = k.sbpool("qhat", [128, 2, 4, 128], BF16, 2, es)
                for qb in qhp.bufs:
                    k.op("pool", lambda e: e.memset(qb[:], 0.0), writes=[qb])
                if not fwd:
                    catp = k.sbpool("cat", [128, 1024], BF16, 2, es)
                    catTp = k.sbpool("catT", [128, 8, 128], BF16, 2, es)
                    zp = k.sbpool("z", [128, 1024], F32, 1, es)

                order = list(range(NT)) if fwd else [1, 0] + list(range(NT - 1, 1, -1))
                C_Q, C_I, C_FF, C_FB, C_G = 1024, 1536, 2048, 2560, 3072
                C_F = C_FF if fwd else C_FB
                first_rows, second_rows = (slice(0, 64), slice(64, 128)) if fwd else (slice(64, 128), slice(0, 64))
                first_idx, second_idx = (0, 1) if fwd else (1, 0)
                last_first, last_second = (63, 127) if fwd else (64, 0)

                for t in order:
                    s = 0 if t >= 2 else 1
                    xt = xpool.next()
                    k.dma("sp", xt[:], rows(t), writes=[xt])
                    hT = hTp.next()
                    norm_T(xt, A1, S1, s, rpool, xnpool, pst, out_bf=hT)

                    def proj_tok(c0, n=512):
                        p = proj.next()
                        for kc in range(8):
                            k.op("pe", lambda e: e.matmul(p[:, 0:n], lhsT=hT[:, kc, :], rhs=win[:, kc, c0:c0 + n],
                                                          start=(kc == 0), stop=(kc == 7)), reads=[hT, win], writes=[p])
                        return p

                    def proj_feat(c0):
                        p = proj.next()
                        for h in range(4):
                            for kc in range(8):
                                k.op("pe", lambda e: e.matmul(p[:, h * 128:(h + 1) * 128],
                                                              lhsT=win[:, kc, c0 + h * 128:c0 + (h + 1) * 128],
                                                              rhs=hT[:, kc, :], start=(kc == 0), stop=(kc == 7)),
                                     reads=[hT, win], writes=[p])
                        return p

                    pf = proj_tok(C_F)
                    sg = t512.next()
                    k.op("act", lambda e: e.activation(out=sg[:], in_=pf[:], func=AF.Sigmoid, scale=-1.0), reads=[pf],
                         writes=[sg])
                    ktok = t512.next()
                    k.op("dve", lambda e: e.tensor_tensor(out=ktok[:], in0=sg[:], in1=oml_bc[:], op=ALU.mult),
                         reads=[sg, oml_bc], writes=[ktok])
                    lf = t512.next()
                    k.op("act", lambda e: e.activation(out=lf[:], in_=ktok[:], func=AF.Ln, scale=-1.0, bias=1.0),
                         reads=[ktok], writes=[lf])
                    k.op("pe", lambda e: e.matmul(gs[:], lhsT=lrem[:], rhs=lf[:], start=True, stop=True),
                         reads=[lrem, lf], writes=[gs])
                    er = t512.next()
                    k.op("act", lambda e: e.activation(out=er[:], in_=gs[:], func=AF.Exp), reads=[gs], writes=[er])
                    khat = b512.next()
                    k.op("dve", lambda e: e.tensor_tensor(out=khat[:], in0=ktok[:], in1=er[:], op=ALU.mult),
                         reads=[ktok, er], writes=[khat])
                    pi = proj_tok(C_I)
                    vb = b512.next()
                    k.op("act", lambda e: e.copy(out=vb[:], in_=pi[:]), reads=[pi], writes=[vb])
                    for h in range(4):
                        k.op("pe", lambda e: e.matmul(big2[:, h * 256:(h + 1) * 256], lhsT=lf[:, h * 128:(h + 1) * 128],
                                                      rhs=rhsF[:], start=True, stop=True), reads=[lf, rhsF],
                             writes=[big2])
                    b2v = big2[:].rearrange("p (h c) -> p h c", c=256)
                    e1 = t512.next()
                    e2 = t512.next()
                    e3 = t512.next()
                    e1v = e1[:].rearrange("p (h c) -> p h c", c=128)
                    e2v = e2[:].rearrange("p (h c) -> p h c", c=128)
                    e3v = e3[:].rearrange("p (h c) -> p h c", c=128)
                    k.op("act", lambda e: e.activation(out=e1v, in_=b2v[:, :, 0:128], func=AF.Exp), reads=[big2],
                         writes=[e1])
                    k.op("act", lambda e: e.activation(out=e2v, in_=b2v[:, :, 0:128], func=AF.Exp, scale=-1.0),
                         reads=[big2], writes=[e2])
                    k.op("act", lambda e: e.activation(out=e3v, in_=b2v[:, :, 128:256], func=AF.Exp), reads=[big2],
                         writes=[e3])
                    pq = proj_feat(C_Q)
                    qT = t512.next()
                    k.op("act", lambda e: e.copy(out=qT[:], in_=pq[:]), reads=[pq], writes=[qT])
                    pfT = proj_feat(C_F)
                    kT = t512.next()
                    k.op("act", lambda e: e.activation(out=kT[:], in_=pfT[:], func=AF.Sigmoid, scale=-1.0), reads=[pfT],
                         writes=[kT])
                    kTv = kT[:].rearrange("p (h c) -> p h c", c=128)
                    k.op("dve", lambda e: e.tensor_tensor(out=kTv, in0=kTv,
                                                          in1=oml_col[:].unsqueeze(2).to_broadcast([128, 4, 128]),
                                                          op=ALU.mult), reads=[kT, oml_col], writes=[kT])
                    qtl = b512.next()
                    ktl = b512.next()
                    k.op("dve", lambda e: e.tensor_tensor(out=qtl[:], in0=qT[:], in1=e1[:], op=ALU.mult),
                         reads=[qT, e1], writes=[qtl])
                    k.op("dve", lambda e: e.tensor_tensor(out=ktl[:], in0=kT[:], in1=e2[:], op=ALU.mult),
                         reads=[kT, e2], writes=[ktl])
                    qh = qhp.next()
                    qTv = qT[:].rearrange("p (h c) -> p h c", c=128)
                    for cb in range(2):
                        cs = slice(cb * 64, (cb + 1) * 64)
                        k.op("dve", lambda e: e.tensor_tensor(out=qh[:, cb, :, cs], in0=qTv[:, :, cs],
                                                               in1=e3v[:, :, cs], op=ALU.mult),
                             reads=[qT, e3], writes=[qh])
                    for h in range(4):
                        k.op("pe", lambda e: e.matmul(gs[:, h * 128:(h + 1) * 128], lhsT=ktl[:, h * 128:(h + 1) * 128],
                                                      rhs=qtl[:, h * 128:(h + 1) * 128], start=True, stop=True),
                             reads=[ktl, qtl], writes=[gs])
                    scm = b512.next()
                    k.op("act", lambda e: e.copy(out=scm[:], in_=zeros_bf[:]), reads=[zeros_bf], writes=[scm])
                    k.op("dve", lambda e: e.copy_predicated(out=scm[:], mask=maski[:].rearrange("p h c -> p (h c)"),
                                                            data=gs[:]), reads=[gs, maski, scm], writes=[scm])
                    S0 = Sbf.next()
                    k.op("act", lambda e: e.copy(out=S0[:], in_=Sst[:]), reads=[Sst], writes=[S0])
                    for h in range(4):
                        hs = slice(h * 128, (h + 1) * 128)
                        k.op("pe", lambda e: e.matmul(dSp[:, h, :], lhsT=khat[first_rows, hs], rhs=vb[first_rows, hs],
                                                      start=True, stop=True), reads=[khat, vb], writes=[dSp])
                    for h in range(4):
                        k.op("dve", lambda e: e.scalar_tensor_tensor(out=Sst[:, h, :], in0=Sst[:, h, :],
                                                                     scalar=e3v[:, h, last_first:last_first + 1],
                                                                     in1=dSp[:, h, :], op0=ALU.mult, op1=ALU.add),
                             reads=[Sst, e3, dSp], writes=[Sst])
                    S1_ = Sbf.next()
                    k.op("act", lambda e: e.copy(out=S1_[:], in_=Sst[:]), reads=[Sst], writes=[S1_])
                    for h in range(4):
                        hs = slice(h * 128, (h + 1) * 128)
                        k.op("pe", lambda e: e.matmul(ops_[:, h, :], lhsT=scm[:, hs], rhs=vb[:, hs], start=True,
                                                      stop=False), reads=[scm, vb], writes=[ops_])
                        k.op("pe", lambda e: e.matmul(ops_[:, h, :], lhsT=qh[:, first_idx, h, :], rhs=S0[:, h, :],
                                                      start=False, stop=False), reads=[qh, S0], writes=[ops_])
                        k.op("pe", lambda e: e.matmul(ops_[:, h, :], lhsT=qh[:, second_idx, h, :], rhs=S1_[:, h, :],
                                                      start=False, stop=True), reads=[qh, S1_], writes=[ops_])
                    for h in range(4):
                        hs = slice(h * 128, (h + 1) * 128)
                        k.op("pe", lambda e: e.matmul(dSp[:, h, :], lhsT=khat[second_rows, hs], rhs=vb[second_rows, hs],
                                                      start=True, stop=True), reads=[khat, vb], writes=[dSp])
                    for h in range(4):
                        k.op("dve", lambda e: e.scalar_tensor_tensor(out=Sst[:, h, :], in0=Sst[:, h, :],
                                                                     scalar=e3v[:, h, last_second:last_second + 1],
                                                                     in1=dSp[:, h, :], op0=ALU.mult, op1=ALU.add),
                             reads=[Sst, e3, dSp], writes=[Sst])
                    osb = t512.next()
                    if fwd:
                        k.op("act", lambda e: e.copy(out=osb[:], in_=ops_[:].rearrange("p h c -> p (h c)")),
                             reads=[ops_], writes=[osb])
                        k.dma("pool", of_scr[t * 128:(t + 1) * 128, :], osb[:], reads=[osb], writes=[d_of[t]])
                        continue
                    ofl = t512.next()
                    k.dma("sp", ofl[:], of_scr[t * 128:(t + 1) * 128, :], reads=[d_of[t]], writes=[ofl])
                    k.op("dve", lambda e: e.tensor_tensor(out=osb[:], in0=ops_[:].rearrange("p h c -> p (h c)"),
                                                          in1=ofl[:], op=ALU.add), reads=[ops_, ofl], writes=[osb])
                    sq = t512.next()
                    k.op("act", lambda e: e.activation(out=sq[:], in_=osb[:], func=AF.Square), reads=[osb],
                         writes=[sq])
                    rs = rpool.next()
                    k.op("dve", lambda e: e.tensor_reduce(out=rs[:], in_=sq[:].rearrange("p (h c) -> p h c", c=128),
                                                          axis=AX.X, op=ALU.add), reads=[sq], writes=[rs])
                    k.op("dve", lambda e: e.tensor_scalar(out=rs[:], in0=rs[:], scalar1=1.0 / 128, scalar2=EPS,
                                                          op0=ALU.mult, op1=ALU.add), reads=[rs], writes=[rs])
                    rsqrt(rs, rs[:], rs[:])
                    k.op("dve", lambda e: e.tensor_tensor(out=osb[:].rearrange("p (h c) -> p h c", c=128),
                                                          in0=osb[:].rearrange("p (h c) -> p h c", c=128),
                                                          in1=rs[:].unsqueeze(2).to_broadcast([128, 4, 128]),
                                                          op=ALU.mult), reads=[osb, rs], writes=[osb])
                    k.op("dve", lambda e: e.tensor_tensor(out=osb[:], in0=osb[:], in1=hnw_bc[:], op=ALU.mult),
                         reads=[osb, hnw_bc], writes=[osb])
                    pg = proj_tok(C_G)
                    sgt = t512.next()
                    k.op("act", lambda e: e.activation(out=sgt[:], in_=pg[:], func=AF.Silu), reads=[pg], writes=[sgt])
                    cat = catp.next()
                    k.op("dve", lambda e: e.tensor_tensor(out=cat[:, 512:1024], in0=osb[:], in1=sgt[:], op=ALU.mult),
                         reads=[osb, sgt], writes=[cat])
                    z = zp.next()
                    pu = proj_tok(0)
                    k.op("act", lambda e: e.activation(out=z[:, 0:512], in_=pu[:], func=AF.Gelu), reads=[pu], writes=[z])
                    pv = proj_tok(512)
                    k.op("act", lambda e: e.activation(out=z[:, 512:1024], in_=pv[:], func=AF.Gelu), reads=[pv],
                         writes=[z])
                    rs2 = rpool.next()
                    sq2 = t512.next()
                    k.op("act", lambda e: e.activation(out=sq2[:], in_=z[:, 512:1024], func=AF.Square,
                                                       accum_out=rs2[:, 0:1]), reads=[z], writes=[sq2, rs2])
                    k.op("dve", lambda e: e.tensor_scalar(out=rs2[:, 1:2], in0=rs2[:, 0:1], scalar1=1.0 / 512,
                                                          scalar2=EPS, op0=ALU.mult, op1=ALU.add), reads=[rs2],
                         writes=[rs2])
                    rsqrt(rs2, rs2[:, 2:3], rs2[:, 1:2])
                    vn = b512.next()
                    k.op("dve", lambda e: e.scalar_tensor_tensor(out=vn[:], in0=z[:, 512:1024], scalar=rs2[:, 2:3],
                                                                 in1=gnw_bc[:], op0=ALU.mult, op1=ALU.mult),
                         reads=[z, rs2, gnw_bc], writes=[vn])
                    psg = proj.next()
                    for g in range(4):
                        gsl = slice(g * 128, (g + 1) * 128)
                        k.op("pe", lambda e: e.matmul(psg[:, gsl], lhsT=wsT[:, g, :], rhs=vn[:, gsl], start=True,
                                                      stop=True), reads=[wsT, vn], writes=[psg])
                    for g in range(4):
                        gsl = slice(g * 128, (g + 1) * 128)
                        k.op("dve", lambda e: e.scalar_tensor_tensor(out=cat[:, gsl], in0=psg[:, gsl],
                                                                     scalar=bs_col[:, g:g + 1], in1=z[:, gsl],
                                                                     op0=ALU.add, op1=ALU.mult),
                             reads=[psg, bs_col, z], writes=[cat])
                    for fc in range(8):
                        k.op("pe", lambda e: e.transpose(pst[:, fc, :], cat[:, fc * 128:(fc + 1) * 128], identb[:]),
                             reads=[cat, identb], writes=[pst])
                    catT = catTp.next()
                    k.op("act", lambda e: e.copy(out=catT[:], in_=pst[:]), reads=[pst], writes=[catT])
                    for half in range(2):
                        for kc in range(8):
                            k.op("pe", lambda e: e.matmul(big2[:, half * 512:(half + 1) * 512], lhsT=catT[:, kc, :],
                                                          rhs=wout_g[s][:, kc, half * 512:(half + 1) * 512],
                                                          start=(kc == 0), stop=(kc == 7)),
                                 reads=[catT, wout_g[s]], writes=[big2])
                    k.op("dve", lambda e: e.tensor_tensor(out=xt[:], in0=big2[:], in1=xt[:], op=ALU.add),
                         reads=[big2, xt], writes=[xt])
                    k.dma("pool", x1[t * 128:(t + 1) * 128, :], xt[:], reads=[xt], writes=[d_x1[t]])
            k.barrier()

        even_pass(0)
        even_pass(1)
        if stop_after == "mix0":
            k.drain_all()
            return nc

        def moe(layer, src, d_src, dst_fn, tiles):
            with ExitStack() as es:
                A2, S2 = load_modcols(es, layer, norm_ffn_w[layer], 1)
                wr = k.sb("wr", [128, 8, 36], F32, es)
                k.dma("sp", wr[:, :, 0:4], router_group_w[layer].rearrange("(kc p) e -> p kc e", p=128), writes=[wr],
                      allow_slow_non_contiguous=True)
                k.dma("sp", wr[:, :, 4:36], router_expert_w[layer].rearrange("(kc p) e -> p kc e", p=128), writes=[wr],
                      allow_slow_non_contiguous=True)
                rb = k.sb("rb", [128, 36], F32, es)
                k.dma("sp", rb[:, 0:4], router_group_b[layer].partition_broadcast(128), writes=[rb])
                k.dma("sp", rb[:, 4:36], router_expert_b[layer].partition_broadcast(128), writes=[rb])
                g2bc = k.sb("g2bc", [128, 2, 1024], F32, es)
                for s in range(2):
                    k.dma("sp", g2bc[:, s, :], modrow[layer, s, 5 * D:6 * D].partition_broadcast(128),
                          reads=[d_modrow], writes=[g2bc])
                n_super = -(-len(tiles) // 12)
                NTS = -(-len(tiles) // n_super)
                ST = NTS * 128
                hTs = k.sb("hTs", [128, 8, ST], BF16, es)
                hT32p = k.sbpool("hT32", [128, 8, 128], F32, 1, es)
                yacc = k.sb("yacc", [128, NTS, 1024], F32, es)
                wc = k.sb("wc", [128, NTS, 32], F32, es)
                xpool = k.sbpool("xt", [128, 1024], F32, 2, es)
                rpool = k.sbpool("rs", [128, 8], F32, 4, es)
                xnpool = {"junk": k.sbpool("junk", [128, 1024], BF16, 1, es),
                          "f32": k.sbpool("xnf", [128, 1024], F32, 1, es)}
                stg = k.sbpool("wstg", [128, 4, 512], F32, 4, es)
                wgp = k.sbpool("wg", [128, 8, 512], BF16, 2, es)
                wup = k.sbpool("wu", [128, 8, 512], BF16, 2, es)
                wdp = k.sbpool("wd", [128, 4, 1024], BF16, 2, es)
                silp = k.sbpool("sil", [128, 512], F32, 2, es)
                actp = k.sbpool("actT", [128, 4, 512], BF16, 2, es)
                r36 = k.sbpool("r36", [128, 40], F32, 12, es)
                pga = k.pspool("pga", [128, 512], F32, 2, es)
                pup = k.pspool("pup", [128, 512], F32, 2, es)
                pdn = k.pspool("pdn", [128, 1024], F32, 2, es)
                cast_i = [0]

                def cast(dst_ap, dst, st):
                    eng = ("dve", "act")[cast_i[0] % 2]
                    cast_i[0] += 1
                    if eng == "act":
                        k.op("act", lambda e: e.copy(out=dst_ap, in_=st[:]), reads=[st], writes=[dst])
                    else:
                        k.op(eng, lambda e: e.tensor_copy(out=dst_ap, in_=st[:]), reads=[st], writes=[dst])

                bounds = [round(i * len(tiles) / n_super) for i in range(n_super + 1)]
                for si_ in range(n_super):
                    stiles = tiles[bounds[si_]:bounds[si_ + 1]]
                    n_t = len(stiles)
                    for ti, t in enumerate(stiles):
                        s = 0 if t >= 2 else 1
                        xt = xpool.next()
                        k.dma("sp", xt[:], src[t * 128:(t + 1) * 128, :], reads=[d_src[t]], writes=[xt])
                        h32 = hT32p.next()
                        pstf = [pga.next(), pup.next()]
                        pst_views = [Bv(p, p[:].rearrange("p (f c) -> p f c", c=128)) for p in pstf]
                        hview = Bv(hTs, hTs[:, :, ti * 128:(ti + 1) * 128])
                        norm_T(xt, A2, S2, s, rpool, xnpool, pst_views, out_bf=hview, out_f32=h32)
                        pl = pga.next()
                        for kc in range(8):
                            k.op("pe", lambda e: e.matmul(pl[:, 0:36], lhsT=h32[:, kc, :], rhs=wr[:, kc, :],
                                                          start=(kc == 0), stop=(kc == 7)), reads=[h32, wr], writes=[pl])
                        lg = r36.next()
                        k.op("dve", lambda e: e.tensor_tensor(out=lg[:, 0:36], in0=pl[:, 0:36], in1=rb[:], op=ALU.add),
                             reads=[pl, rb], writes=[lg])
                        m = r36.next()
                        k.op("dve", lambda e: e.tensor_reduce(out=m[:, 0:1], in_=lg[:, 0:4], axis=AX.X, op=ALU.max),
                             reads=[lg], writes=[m])
                        k.op("dve", lambda e: e.tensor_scalar(out=m[:, 1:2], in0=m[:, 0:1], scalar1=-1.0, scalar2=None,
                                                              op0=ALU.mult), reads=[m], writes=[m])
                        eg = r36.next()
                        k.op("act", lambda e: e.activation(out=eg[:, 0:4], in_=lg[:, 0:4], func=AF.Exp, bias=m[:, 1:2],
                                                           accum_out=m[:, 2:3]), reads=[lg, m], writes=[eg, m])
                        k.op("dve", lambda e: e.reciprocal(out=m[:, 3:4], in_=m[:, 2:3]), reads=[m], writes=[m])
                        ohg = r36.next()
                        k.op("dve", lambda e: e.tensor_scalar(out=ohg[:, 0:4], in0=lg[:, 0:4], scalar1=m[:, 0:1],
                                                              scalar2=1e9, op0=ALU.is_lt, op1=ALU.mult),
                             reads=[lg, m], writes=[ohg])
                        lem = r36.next()
                        k.op("dve", lambda e: e.tensor_tensor(
                            out=lem[:, 0:32].rearrange("p (g e) -> p g e", e=8),
                            in0=lg[:, 4:36].rearrange("p (g e) -> p g e", e=8),
                            in1=ohg[:, 0:4].unsqueeze(2).to_broadcast([128, 4, 8]), op=ALU.subtract),
                             reads=[lg, ohg], writes=[lem])
                        k.op("dve", lambda e: e.tensor_reduce(out=m[:, 4:5], in_=lem[:, 0:32], axis=AX.X, op=ALU.max),
                             reads=[lem], writes=[m])
                        oh1 = r36.next()
                        k.op("dve", lambda e: e.tensor_scalar(out=oh1[:, 0:32], in0=lem[:, 0:32], scalar1=m[:, 4:5],
                                                              scalar2=None, op0=ALU.is_ge), reads=[lem, m], writes=[oh1])
                        lem2 = r36.next()
                        k.op("dve", lambda e: e.scalar_tensor_tensor(out=lem2[:, 0:32], in0=oh1[:, 0:32], scalar=-1e9,
                                                                     in1=lem[:, 0:32], op0=ALU.mult, op1=ALU.add),
                             reads=[oh1, lem], writes=[lem2])
                        k.op("dve", lambda e: e.tensor_reduce(out=m[:, 5:6], in_=lem2[:, 0:32], axis=AX.X, op=ALU.max),
                             reads=[lem2], writes=[m])
                        oh2 = r36.next()
                        k.op("dve", lambda e: e.tensor_scalar(out=oh2[:, 0:32], in0=lem2[:, 0:32], scalar1=m[:, 5:6],
                                                              scalar2=None, op0=ALU.is_ge), reads=[lem2, m],
                             writes=[oh2])
                        k.op("dve", lambda e: e.tensor_tensor(out=m[:, 6:7], in0=m[:, 5:6], in1=m[:, 4:5],
                                                              op=ALU.subtract), reads=[m], writes=[m])
                        k.op("act", lambda e: e.activation(out=m[:, 7:8], in_=m[:, 6:7], func=AF.Exp), reads=[m],
                             writes=[m])
                        k.op("dve", lambda e: e.tensor_scalar(out=m[:, 7:8], in0=m[:, 7:8], scalar1=1.0, scalar2=None,
                                                              op0=ALU.add), reads=[m], writes=[m])
                        k.op("dve", lambda e: e.reciprocal(out=m[:, 8:9], in_=m[:, 7:8]), reads=[m], writes=[m])
                        k.op("dve", lambda e: e.tensor_tensor(out=m[:, 9:10], in0=m[:, 8:9], in1=m[:, 3:4], op=ALU.mult),
                             reads=[m], writes=[m])
                        k.op("dve", lambda e: e.tensor_tensor(out=m[:, 10:11], in0=m[:, 3:4], in1=m[:, 9:10],
                                                              op=ALU.subtract), reads=[m], writes=[m])
                        k.op("dve", lambda e: e.tensor_scalar(out=wc[:, ti, :], in0=oh1[:, 0:32], scalar1=m[:, 9:10],
                                                              scalar2=None, op0=ALU.mult), reads=[oh1, m], writes=[wc])
                        k.op("dve", lambda e: e.scalar_tensor_tensor(out=wc[:, ti, :], in0=oh2[:, 0:32],
                                                                     scalar=m[:, 10:11], in1=wc[:, ti, :],
                                                                     op0=ALU.mult, op1=ALU.add),
                             reads=[oh2, m, wc], writes=[wc])
                    ntok = n_t * 128
                    pending = [None]
                    for ex in range(32):
                        wg = wgp.next()
                        wu = wup.next()
                        wd = wdp.next()
                        for (dst, srcw) in ((wg, moe_w_gate[layer, ex]), (wu, moe_w_up[layer, ex])):
                            for half in range(2):
                                st = stg.next()
                                k.dma("sp", st[:], srcw[half * 512:(half + 1) * 512, :].rearrange(
                                    "(kc p) n -> p kc n", p=128), writes=[st])
                                cast(dst[:, half * 4:(half + 1) * 4, :], dst, st)
                        for half in range(2):
                            st = stg.next()
                            k.dma("sp", st[:], moe_w_down[layer, ex][:, half * 512:(half + 1) * 512].rearrange(
                                "(kc p) n -> p kc n", p=128), writes=[st])
                            cast(wd[:, :, half * 512:(half + 1) * 512], wd, st)
                        for b0 in range(0, ntok, 512):
                            nb = min(512, ntok - b0)
                            aT = actp.next()
                            for ffc in range(4):
                                fsl = slice(ffc * 128, (ffc + 1) * 128)
                                pg_ = pga.next()
                                pu_ = pup.next()
                                for kc in range(8):
                                    k.op("pe", lambda e: e.matmul(pg_[:, 0:nb], lhsT=wg[:, kc, fsl],
                                                                  rhs=hTs[:, kc, b0:b0 + nb], start=(kc == 0),
                                                                  stop=(kc == 7)), reads=[wg, hTs], writes=[pg_])
                                for kc in range(8):
                                    k.op("pe", lambda e: e.matmul(pu_[:, 0:nb], lhsT=wu[:, kc, fsl],
                                                                  rhs=hTs[:, kc, b0:b0 + nb], start=(kc == 0),
                                                                  stop=(kc == 7)), reads=[wu, hTs], writes=[pu_])
                                sl_ = silp.next()
                                k.op("act", lambda e: e.activation(out=sl_[:, 0:nb], in_=pg_[:, 0:nb], func=AF.Silu),
                                     reads=[pg_], writes=[sl_])
                                k.op("dve", lambda e: e.tensor_tensor(out=aT[:, ffc, 0:nb], in0=sl_[:, 0:nb],
                                                                      in1=pu_[:, 0:nb], op=ALU.mult),
                                     reads=[sl_, pu_], writes=[aT])
                            def mk_down(aT=aT, wd=wd, ex=ex, b0=b0, nb=nb):
                              for tt in range(nb // 128):
                                ti = b0 // 128 + tt
                                pd = pdn.next()
                                for half in range(2):
                                    for ffc in range(4):
                                        k.op("pe", lambda e: e.matmul(pd[:, half * 512:(half + 1) * 512],
                                                                      lhsT=aT[:, ffc, tt * 128:(tt + 1) * 128],
                                                                      rhs=wd[:, ffc, half * 512:(half + 1) * 512],
                                                                      start=(ffc == 0), stop=(ffc == 3)),
                                             reads=[aT, wd], writes=[pd])
                                if ex == 0:
                                    k.op("dve", lambda e: e.tensor_scalar(out=yacc[:, ti, :], in0=pd[:],
                                                                          scalar1=wc[:, ti, ex:ex + 1], scalar2=None,
                                                                          op0=ALU.mult), reads=[pd, wc], writes=[yacc])
                                else:
                                    k.op("dve", lambda e: e.scalar_tensor_tensor(out=yacc[:, ti, :], in0=pd[:],
                                                                                 scalar=wc[:, ti, ex:ex + 1],
                                                                                 in1=yacc[:, ti, :], op0=ALU.mult,
                                                                                 op1=ALU.add),
                                         reads=[pd, wc, yacc], writes=[yacc])
                            if pending[0] is not None:
                                pending[0]()
                            pending[0] = mk_down
                    if pending[0] is not None:
                        pending[0]()
                        pending[0] = None
                    for ti, t in enumerate(stiles):
                        s = 0 if t >= 2 else 1
                        xt = xpool.next()
                        k.dma("sp", xt[:], src[t * 128:(t + 1) * 128, :], reads=[d_src[t]], writes=[xt])
                        k.op("dve", lambda e: e.tensor_tensor(out=yacc[:, ti, :], in0=yacc[:, ti, :], in1=g2bc[:, s, :],
                                                               op=ALU.mult), reads=[yacc, g2bc], writes=[yacc])
                        k.op("dve", lambda e: e.tensor_tensor(out=xt[:], in0=xt[:], in1=yacc[:, ti, :], op=ALU.add),
                             reads=[xt, yacc], writes=[xt])
                        dap, dbuf = dst_fn(t)
                        k.dma("pool", dap, xt[:], reads=[xt], writes=[dbuf])
            k.barrier()

        if stop_after == "moe0":
            d_y = Buf(None, "y")

            def dst0(t):
                if t >= 2:
                    return y_out[(t - 2) * 128:(t - 1) * 128, :], d_y
                return x2[t * 128:(t + 1) * 128, :], d_x2[t]
            moe(0, x1, d_x1, dst0, [0, 1] + list(range(2, NT)))
            k.drain_all()
            return nc

        moe(0, x1, d_x1, lambda t: (x2[t * 128:(t + 1) * 128, :], d_x2[t]), [0, 1] + list(range(2, NT)))
        MLA_SCALE = 96 ** -0.5
        O_Z, O_BETA = 1536, 2048

        def odd_pass(direction):
            fwd = direction == 0
            with ExitStack() as es:
                A1, S1 = load_modcols(es, 1, norm_mix_w[1], 0)
                win = k.sb("owin", [128, 8, 2480], BF16, es)
                cdiag = k.sb("cdiag", [128, 60, 128], BF16, es)
                tri = k.sb("otri", [128, 128], F32, es)
                stri = k.sb("ostri", [128, 128], F32, es)
                allb = k.sb("oallb", [128, 128], F32, es)
                nallb = k.sb("onallb", [128, 128], F32, es)
                self_ = k.sb("oself", [128, 128], F32, es)
                sels_ = k.sb("osels", [128, 128], F32, es)
                aneg_bc = k.sb("aneg", [128, 8], F32, es)
                dtb_bc = k.sb("dtb", [128, 8], F32, es)
                if fwd:
                    wq = k.sb("wq", [128, 2, 768], BF16, es)
                    wkv = k.sb("wkv", [128, 1, 1024], BF16, es)
                    qnw_bc = k.sb("qnw", [128, 256], F32, es)
                    kvnw_bc = k.sb("kvnw", [128, 128], F32, es)
                    qkq_bc = k.sb("qkq", [128, 96], F32, es)
                    qkk_bc = k.sb("qkk", [128, 96], F32, es)
                else:
                    wout_g = k.sb("owout_g", [128, 8, 1024], BF16, es)
                    gnw_bc = k.sb("ognw", [128, 128], F32, es)
                with ExitStack() as es2:
                    stage = k.sbpool("stage", [128, 8, 512], F32, 2, es2)
                    load_w_bf16(es2, win, odd_w_in[0], 8, 2480, stage)
                    cw = k.sb("cw", [128, 12, 5], F32, es2)
                    for kk in range(5):
                        k.dma("sp", cw[:, :, kk], gdn_conv_w[0, kk, :].rearrange("(c p) -> p c", p=128), writes=[cw],
                              allow_slow_non_contiguous=True)
                    for c in range(12):
                        for kk in range(5):
                            k.op("dve" if (c + kk) % 2 else "pool",
                                 lambda e: e.tensor_scalar(out=cdiag[:, c * 5 + kk, :], in0=ident[:],
                                                           scalar1=cw[:, c, kk:kk + 1], scalar2=None, op0=ALU.mult),
                                 reads=[ident, cw], writes=[cdiag])
                    tr_ = blockmask("tri_", "le" if fwd else "ge", es2)
                    al_ = blockmask("all_", "all", es2)
                    k.op("dve", lambda e: e.tensor_copy(out=tri[:], in_=tr_[:]), reads=[tr_], writes=[tri])
                    k.op("dve", lambda e: e.tensor_tensor(out=stri[:], in0=tr_[:], in1=ident[:], op=ALU.subtract),
                         reads=[tr_, ident], writes=[stri])
                    k.op("dve", lambda e: e.tensor_copy(out=allb[:], in_=al_[:]), reads=[al_], writes=[allb])
                    k.op("dve", lambda e: e.tensor_scalar(out=nallb[:], in0=al_[:], scalar1=-1.0, scalar2=None,
                                                          op0=ALU.mult), reads=[al_], writes=[nallb])
                    k.op("pool", lambda e: e.memset(self_[:], 0.0), writes=[self_])
                    k.op("pool", lambda e: e.memset(sels_[:], 0.0), writes=[sels_])
                    f0, s0_ = (0, 64) if fwd else (64, 0)
                    k.op("pool", lambda e: e.tensor_copy(out=self_[f0:f0 + 64, :], in_=ones[f0:f0 + 64, :]),
                         reads=[ones], writes=[self_])
                    k.op("pool", lambda e: e.tensor_copy(out=sels_[s0_:s0_ + 64, :], in_=ones[s0_:s0_ + 64, :]),
                         reads=[ones], writes=[sels_])
                    k.dma("sp", aneg_bc[:], gdn_a_log[0].rearrange("a b -> (a b)").partition_broadcast(128),
                          writes=[aneg_bc])
                    k.op("act", lambda e: e.activation(out=aneg_bc[:], in_=aneg_bc[:], func=AF.Exp), reads=[aneg_bc],
                         writes=[aneg_bc])
                    k.op("dve", lambda e: e.tensor_scalar(out=aneg_bc[:], in0=aneg_bc[:], scalar1=-1.0, scalar2=None,
                                                          op0=ALU.mult), reads=[aneg_bc], writes=[aneg_bc])
                    k.dma("sp", dtb_bc[:], gdn_dt_bias[0].rearrange("a b -> (a b)").partition_broadcast(128),
                          writes=[dtb_bc])
                    if fwd:
                        st = stage.next()
                        k.dma("sp", st[:, 0:2, 0:512], mla_wq_up[0][:, 0:512].rearrange("(kc p) n -> p kc n", p=128),
                              writes=[st])
                        k.op("dve", lambda e: e.tensor_copy(out=wq[:, :, 0:512], in_=st[:, 0:2, 0:512]), reads=[st],
                             writes=[wq])
                        st = stage.next()
                        k.dma("sp", st[:, 0:2, 0:256], mla_wq_up[0][:, 512:768].rearrange("(kc p) n -> p kc n", p=128),
                              writes=[st])
                        k.op("dve", lambda e: e.tensor_copy(out=wq[:, :, 512:768], in_=st[:, 0:2, 0:256]), reads=[st],
                             writes=[wq])
                        for c0 in (0, 512):
                            st = stage.next()
                            k.dma("sp", st[:, 0:1, 0:512], mla_wkv_up[0][:, c0:c0 + 512].rearrange(
                                "(kc p) n -> p kc n", p=128), writes=[st])
                            k.op("dve", lambda e: e.tensor_copy(out=wkv[:, :, c0:c0 + 512], in_=st[:, 0:1, 0:512]),
                                 reads=[st], writes=[wkv])
                        k.dma("sp", qnw_bc[:], mla_q_norm_w[0].partition_broadcast(128), writes=[qnw_bc])
                        k.dma("sp", kvnw_bc[:], mla_kv_norm_w[0].partition_broadcast(128), writes=[kvnw_bc])
                        k.dma("sp", qkq_bc[:], mla_qk_norm_q[0].partition_broadcast(128), writes=[qkq_bc])
                        k.op("dve", lambda e: e.tensor_scalar(out=qkq_bc[:], in0=qkq_bc[:], scalar1=MLA_SCALE,
                                                              scalar2=None, op0=ALU.mult), reads=[qkq_bc],
                             writes=[qkq_bc])
                        k.dma("sp", qkk_bc[:], mla_qk_norm_k[0].partition_broadcast(128), writes=[qkk_bc])
                    else:
                        gbc = k.sb("gbc", [128, 1024], F32, es2)
                        k.dma("sp", gbc[:], modrow[1, 0, 2 * D:3 * D].partition_broadcast(128), reads=[d_modrow],
                              writes=[gbc])
                        for c0 in range(0, 1024, 512):
                            st = stage.next()
                            k.dma("sp", st[:], odd_w_out[0][:, c0:c0 + 512].rearrange("(kc p) n -> p kc n", p=128),
                                  writes=[st])
                            for kc in range(8):
                                k.op("dve" if kc % 2 else "pool",
                                     lambda e: e.tensor_tensor(out=wout_g[:, kc, c0:c0 + 512], in0=st[:, kc, :],
                                                               in1=gbc[:, c0:c0 + 512], op=ALU.mult),
                                     reads=[st, gbc], writes=[wout_g])
                        k.dma("sp", gnw_bc[:], gdn_norm_w[0].partition_broadcast(128), writes=[gnw_bc])
                    k.barrier()

                Sst = k.sb("oSst", [128, 4, 128], F32, es)
                k.op("pool", lambda e: e.memset(Sst[:], 0.0), writes=[Sst])
                xpool = k.sbpool("oxt", [128, 1024], F32, 2, es)
                rpool = k.sbpool("ors", [128, 8], F32, 12, es)
                xnpool = {"junk": k.sbpool("ojunk", [128, 1024], BF16, 1, es),
                          "bf": k.sbpool("oxnb", [128, 1024], BF16, 2, es)}
                hTp = k.sbpool("ohT", [128, 8, 128], BF16, 5, es)
                hextp = k.sbpool("ohext", [128, 8, 132], BF16, 2, es)
                pTp = k.sbpool("opT", [128, 12, 132], BF16, 2, es)
                qkvTp = k.sbpool("oqkvT", [128, 12, 128], BF16, 2, es)
                qkvtokp = k.sbpool("oqkvtok", [128, 1536], F32, 2, es)
                sqp = k.sbpool("osq", [128, 1024], F32, 1, es)
                p2pool = k.sbpool("op2", [128, 432], F32, 2, es)
                qkntokp = k.sbpool("oqkntok", [128, 8, 128], BF16, 2, es)
                qknTp = k.sbpool("oqknT", [128, 8, 128], BF16, 2, es)
                hbufs = []
                for h_ in range(4):
                    d_ = {}
                    for nm in ("TL", "Gm", "t1", "t2", "u32", "oa"):
                        d_[nm] = k.sb("h" + nm, [128, 128], F32, es)
                    for nm in ("qkT", "wb", "wT", "kg", "vn", "S0", "S1"):
                        d_[nm] = k.sb("h" + nm, [128, 128], BF16, es)
                    d_["PT"] = [k.sb("hPT", [128, 128], BF16, es) for _ in range(2)]
                    d_["P"] = [k.sb("hP", [128, 128], BF16, es) for _ in range(2)]
                    d_["X32"] = k.sb("hX32", [128, 256], F32, es)
                    d_["Xb"] = k.sb("hXb", [128, 256], BF16, es)
                    hbufs.append(d_)
                s16 = k.sbpool("os16", [128, 4, 8], F32, 6, es)
                osbp = k.sbpool("oosb", [128, 4, 128], F32, 2, es)
                t512 = k.sbpool("ot512", [128, 512], F32, 5, es)
                pst = k.ps("opst", [128, 8, 128], BF16, es)
                ptb = k.ps("optb", [128, 8, 128], BF16, es)
                ptb_s = [Bv(ptb, ptb.t[:, i, :]) for i in range(8)]
                proj = k.pspool("oproj", [128, 512], F32, 2, es)
                pa = k.ps("opa", [128, 512], F32, es)
                pa_s = [Bv(pa, pa.t[:, i * 128:(i + 1) * 128]) for i in range(4)]
                pn = k.ps("opn", [128, 512], F32, es)
                pn_s = [Bv(pn, pn.t[:, i * 128:(i + 1) * 128]) for i in range(4)]
                pb = k.ps("opb", [128, 512], F32, es)
                pb_s = [Bv(pb, pb.t[:, i * 128:(i + 1) * 128]) for i in range(4)]
                pc = k.ps("opc", [128, 512], F32, es)
                pc_s = [Bv(pc, pc.t[:, i * 128:(i + 1) * 128]) for i in range(4)]
                if fwd:
                    qtp = k.sbpool("oqt", [96, 8, 128], BF16, 2, es)
                    vxp = k.sbpool("ovx", [128, 8, 80], BF16, 2, es)
                    for vb_ in vxp.bufs:
                        k.op("pool", lambda e: e.memset(vb_[:], 1.0), writes=[vb_])
                    q96p = k.sbpool("oq96", [128, 8, 96], F32, 2, es)
                    q96bp = k.sbpool("oq96b", [128, 8, 96], BF16, 2, es)
                    ropep = k.sbpool("orope", [128, 2, 16], F32, 2, es)
                    r8p = k.sbpool("or8", [128, 8, 8], F32, 8, es)
                    cqnp = k.sbpool("ocqn", [128, 256], BF16, 2, es)
                    kvsp = k.sbpool("okvs", [128, 512], F32, 2, es)
                    cqnTp = k.sbpool("ocqnT", [128, 3, 128], BF16, 2, es)
                else:
                    catp = k.sbpool("ocat", [128, 512], BF16, 2, es)
                    catTp = k.sbpool("ocatT", [128, 8, 128], BF16, 2, es)

                hcache = {}

                def get_hT(t):
                    if t in hcache:
                        return hcache[t]
                    s = 0 if t >= 2 else 1
                    xt = xpool.next()
                    k.dma("sp", xt[:], x2[t * 128:(t + 1) * 128, :], reads=[d_x2[t]], writes=[xt])
                    hT = hTp.next()
                    for kk_ in [kk_ for kk_, v_ in hcache.items() if v_ is hT]:
                        del hcache[kk_]
                    norm_T(xt, A1, S1, s, rpool, xnpool, pst, out_bf=hT)
                    hcache[t] = hT
                    return hT

                def rope(buf, tabs):
                    for part in range(2):
                        o = 64 + part * 16
                        u1 = buf[:, :, o:o + 8]
                        u2 = buf[:, :, o + 8:o + 16]
                        cs = tabs[:, 0, part * 8:(part + 1) * 8].unsqueeze(1).to_broadcast([128, 8, 8])
                        sn = tabs[:, 1, part * 8:(part + 1) * 8].unsqueeze(1).to_broadcast([128, 8, 8])
                        a = r8p.next(); b = r8p.next(); c_ = r8p.next(); d_ = r8p.next()
                        k.op("dve", lambda e: e.tensor_tensor(out=a[:], in0=u1, in1=cs, op=ALU.mult), reads=[buf, tabs],
                             writes=[a])
                        k.op("dve", lambda e: e.tensor_tensor(out=b[:], in0=u2, in1=sn, op=ALU.mult), reads=[buf, tabs],
                             writes=[b])
                        k.op("dve", lambda e: e.tensor_tensor(out=c_[:], in0=u2, in1=cs, op=ALU.mult), reads=[buf, tabs],
                             writes=[c_])
                        k.op("dve", lambda e: e.tensor_tensor(out=d_[:], in0=u1, in1=sn, op=ALU.mult), reads=[buf, tabs],
                             writes=[d_])
                        k.op("dve", lambda e: e.tensor_tensor(out=u1, in0=a[:], in1=b[:], op=ALU.subtract),
                             reads=[a, b], writes=[buf])
                        k.op("dve", lambda e: e.tensor_tensor(out=u2, in0=c_[:], in1=d_[:], op=ALU.add),
                             reads=[c_, d_], writes=[buf])

                order = list(range(NT)) if fwd else [1, 0] + list(range(NT - 1, 1, -1))
                R1, R2 = (slice(0, 64), slice(64, 128)) if fwd else (slice(64, 128), slice(0, 64))

                def tile_A(t, cx):
                    lat = t >= 2
                    has_prev = t not in (0, 2)
                    has_next = t not in (1, NT - 1)
                    hT = get_hT(t)
                    hprev = get_hT(t - 1) if has_prev else None
                    hnext = get_hT(t + 1) if has_next else None
                    hext = hextp.next()
                    k.op("act", lambda e: e.copy(out=hext[:, :, 2:130], in_=hT[:]), reads=[hT], writes=[hext])
                    if has_prev:
                        k.op("dve", lambda e: e.tensor_copy(out=hext[:, :, 0:2], in_=hprev[:, :, 126:128]),
                             reads=[hprev], writes=[hext])
                    else:
                        k.op("dve", lambda e: e.memset(hext[:, :, 0:2], 0.0), writes=[hext])
                    if has_next:
                        k.op("dve", lambda e: e.tensor_copy(out=hext[:, :, 130:132], in_=hnext[:, :, 0:2]),
                             reads=[hnext], writes=[hext])
                    else:
                        k.op("dve", lambda e: e.memset(hext[:, :, 130:132], 0.0), writes=[hext])

                    def proj_tok(c0, n):
                        p = proj.next()
                        for kc in range(8):
                            k.op("pe", lambda e: e.matmul(p[:, 0:n], lhsT=hT[:, kc, :], rhs=win[:, kc, c0:c0 + n],
                                                          start=(kc == 0), stop=(kc == 7)), reads=[hT, win], writes=[p])
                        return p

                    yield
                    pT = pTp.next()
                    for g3 in range(4):
                        p = proj.next()
                        pv = p[:, 0:396].rearrange("p (c n) -> p c n", n=132)
                        for ci in range(3):
                            c = g3 * 3 + ci
                            for kc in range(8):
                                k.op("pe", lambda e: e.matmul(pv[:, ci, :], lhsT=win[:, kc, c * 128:(c + 1) * 128],
                                                              rhs=hext[:, kc, :], start=(kc == 0), stop=(kc == 7)),
                                     reads=[win, hext], writes=[p])
                        k.op("act", lambda e: e.copy(out=pT[:, g3 * 3:(g3 + 1) * 3, :], in_=pv), reads=[p], writes=[pT])
                        yield
                    qkvT = qkvTp.next()
                    yield
                    for g4 in range(3):
                        p = proj.next()
                        for ci in range(4):
                            c = g4 * 4 + ci
                            for kk in range(5):
                                k.op("pe", lambda e: e.matmul(p[:, ci * 128:(ci + 1) * 128], lhsT=cdiag[:, c * 5 + kk, :],
                                                              rhs=pT[:, c, kk:kk + 128], start=(kk == 0), stop=(kk == 4)),
                                     reads=[cdiag, pT], writes=[p])
                        k.op("act", lambda e: e.activation(out=qkvT[:, g4 * 4:(g4 + 1) * 4, :].rearrange("p c n -> p (c n)"),
                                                           in_=p[:], func=AF.Silu), reads=[p], writes=[qkvT])
                        yield
                    qkvtok = qkvtokp.next()
                    for c in range(8):
                        k.op("pe", lambda e: e.transpose(pst[:, c, :], qkvT[:, c, :], identb[:]), reads=[qkvT, identb],
                             writes=[pst])
                    k.op("act", lambda e: e.copy(out=qkvtok[:, 0:1024], in_=pst[:].rearrange("p c n -> p (c n)")),
                         reads=[pst], writes=[qkvtok])
                    for c in range(4):
                        k.op("pe", lambda e: e.transpose(pst[:, c, :], qkvT[:, 8 + c, :], identb[:]),
                             reads=[qkvT, identb], writes=[pst])
                    k.op("act", lambda e: e.copy(out=qkvtok[:, 1024:1536],
                                                 in_=pst[:, 0:4, :].rearrange("p c n -> p (c n)")), reads=[pst],
                         writes=[qkvtok])
                    yield
                    sq = sqp.next()
                    k.op("act", lambda e: e.activation(out=sq[:, 0:1024], in_=qkvtok[:, 0:1024], func=AF.Square),
                         reads=[qkvtok], writes=[sq])
                    rn = rpool.next()
                    k.op("dve", lambda e: e.tensor_reduce(out=rn[:], in_=sq[:, 0:1024].rearrange("p (h c) -> p h c", c=128),
                                                          axis=AX.X, op=ALU.add), reads=[sq], writes=[rn])
                    k.op("dve", lambda e: e.tensor_scalar(out=rn[:], in0=rn[:], scalar1=EPS, scalar2=None, op0=ALU.add),
                         reads=[rn], writes=[rn])
                    rsqrt(rn, rn[:], rn[:])
                    k.op("dve", lambda e: e.tensor_scalar(out=rn[:, 0:4], in0=rn[:, 0:4], scalar1=128 ** -0.5,
                                                          scalar2=None, op0=ALU.mult), reads=[rn], writes=[rn])
                    qkntok = qkntokp.next()
                    yield
                    k.op("dve", lambda e: e.tensor_tensor(out=qkntok[:],
                                                          in0=qkvtok[:, 0:1024].rearrange("p (h c) -> p h c", c=128),
                                                          in1=rn[:].unsqueeze(2).to_broadcast([128, 8, 128]),
                                                          op=ALU.mult), reads=[qkvtok, rn], writes=[qkntok])
                    for c in range(8):
                        k.op("pe", lambda e: e.transpose(pst[:, c, :], qkntok[:, c, :], identb[:]),
                             reads=[qkntok, identb], writes=[pst])
                    qknT = qknTp.next()
                    yield
                    k.op("act", lambda e: e.copy(out=qknT[:], in_=pst[:]), reads=[pst], writes=[qknT])
                    p2p = proj_tok(O_BETA, 432)
                    yield
                    p2 = p2pool.next()
                    k.op("act", lambda e: e.copy(out=p2[:, 0:432], in_=p2p[:, 0:432]), reads=[p2p], writes=[p2])
                    bl = s16.next()
                    blv = bl[:].rearrange("p a b -> p (a b)")
                    k.op("act", lambda e: e.activation(out=blv[:, 0:8], in_=p2[:, 0:8], func=AF.Sigmoid), reads=[p2],
                         writes=[bl])
                    sp_ = s16.next()
                    spv = sp_[:].rearrange("p a b -> p (a b)")
                    k.op("dve", lambda e: e.tensor_tensor(out=spv[:, 0:8], in0=p2[:, 8:16], in1=dtb_bc[:], op=ALU.add),
                         reads=[p2, dtb_bc], writes=[sp_])
                    k.op("act", lambda e: e.activation(out=spv[:, 8:16], in_=spv[:, 0:8], func=AF.Abs), reads=[sp_],
                         writes=[sp_])
                    k.op("act", lambda e: e.activation(out=spv[:, 8:16], in_=spv[:, 8:16], func=AF.Exp, scale=-1.0),
                         reads=[sp_], writes=[sp_])
                    k.op("act", lambda e: e.activation(out=spv[:, 8:16], in_=spv[:, 8:16], func=AF.Ln, bias=1.0),
                         reads=[sp_], writes=[sp_])
                    k.op("dve", lambda e: e.tensor_scalar(out=spv[:, 0:8], in0=spv[:, 0:8], scalar1=0.0, scalar2=None,
                                                          op0=ALU.max), reads=[sp_], writes=[sp_])
                    k.op("dve", lambda e: e.tensor_tensor(out=spv[:, 0:8], in0=spv[:, 0:8], in1=spv[:, 8:16],
                                                          op=ALU.add), reads=[sp_], writes=[sp_])
                    k.op("dve", lambda e: e.tensor_tensor(out=blv[:, 8:16], in0=spv[:, 0:8], in1=aneg_bc[:],
                                                          op=ALU.mult), reads=[sp_, aneg_bc], writes=[bl])
                    dsl = slice(direction * 4, direction * 4 + 4)
                    yield
                    la4 = bl[:, 1, dsl]
                    be4 = bl[:, 0, dsl]
                    pg = pa_s[3]
                    for i_, lh in enumerate((tri, allb, self_, sels_)):
                        k.op("pe", lambda e: e.matmul(pg[:, i_ * 4:(i_ + 1) * 4], lhsT=lh[:], rhs=la4, start=True,
                                                      stop=True), reads=[lh, bl], writes=[pg])
                    E4 = s16.next()
                    k.op("dve", lambda e: e.tensor_copy(out=E4[:, :, 0:4], in_=pg[:, 0:16].rearrange("p (a b) -> p a b", b=4)),
                         reads=[pg], writes=[E4])
                    k.op("dve", lambda e: e.tensor_tensor(out=E4[:, 1, 0:4], in0=E4[:, 1, 0:4], in1=E4[:, 0, 0:4],
                                                          op=ALU.subtract), reads=[E4], writes=[E4])
                    k.op("act", lambda e: e.activation(out=E4[:, :, 0:4], in_=E4[:, :, 0:4], func=AF.Exp), reads=[E4],
                         writes=[E4])
                    cx.update(dict(lat=lat, hT=hT, qkvtok=qkvtok, qkntok=qkntok, qknT=qknT, bl=bl, la4=la4, be4=be4,
                                   E4=E4, p2=p2))
                    yield

                def tile_B(t, cx):
                    lat = cx["lat"]; hT = cx["hT"]; qkvtok = cx["qkvtok"]; qkntok = cx["qkntok"]; qknT = cx["qknT"]
                    bl = cx["bl"]; la4 = cx["la4"]; be4 = cx["be4"]; E4 = cx["E4"]; p2 = cx["p2"]

                    def proj_tok(c0, n):
                        p = proj.next()
                        for kc in range(8):
                            k.op("pe", lambda e: e.matmul(p[:, 0:n], lhsT=hT[:, kc, :], rhs=win[:, kc, c0:c0 + n],
                                                          start=(kc == 0), stop=(kc == 7)), reads=[hT, win], writes=[p])
                        return p

                    osb = osbp.next()
                    HB = hbufs
                    yield
                    for h in range(4):
                        B_ = HB[h]
                        knT_h = qknT[:, 4 + h, :]
                        qnT_h = qknT[:, h, :]
                        la_c = la4[:, h:h + 1]
                        be_c = be4[:, h:h + 1]
                        eg_c = E4[:, 0, h:h + 1]
                        TL = B_["TL"]
                        k.op("dve", lambda e: e.tensor_scalar(out=TL[:], in0=tri[:], scalar1=la_c, scalar2=None,
                                                               op0=ALU.mult), reads=[tri, bl], writes=[TL])
                        reg = pa_s if h % 2 == 0 else pc_s
                        GT, KK, QK = reg[0], reg[1], reg[2]
                        k.op("pe", lambda e: e.matmul(GT[:], lhsT=allb[:], rhs=TL[:], start=True, stop=False),
                             reads=[allb, TL], writes=[GT])
                        k.op("pe", lambda e: e.matmul(GT[:], lhsT=TL[:], rhs=nallb[:], start=False, stop=True),
                             reads=[nallb, TL], writes=[GT])
                        k.op("pe", lambda e: e.matmul(KK[:], lhsT=knT_h, rhs=knT_h, start=True, stop=True),
                             reads=[qknT], writes=[KK])
                        k.op("pe", lambda e: e.matmul(QK[:], lhsT=knT_h, rhs=qnT_h, start=True, stop=True),
                             reads=[qknT], writes=[QK])
                        Gm = B_["Gm"]
                        k.op("dve", lambda e: e.tensor_scalar(out=Gm[:], in0=GT[:], scalar1=0.0, scalar2=None,
                                                              op0=ALU.min), reads=[GT], writes=[Gm])
                        k.op("act", lambda e: e.activation(out=Gm[:], in_=Gm[:], func=AF.Exp), reads=[Gm], writes=[Gm])
                        t1 = B_["t1"]
                        k.op("dve", lambda e: e.tensor_tensor(out=t1[:], in0=KK[:], in1=Gm[:], op=ALU.mult),
                             reads=[KK, Gm], writes=[t1])
                        PT = B_["PT"][0]
                        k.op("dve", lambda e: e.scalar_tensor_tensor(out=PT[:], in0=t1[:], scalar=be_c, in1=stri[:],
                                                                     op0=ALU.mult, op1=ALU.mult),
                             reads=[t1, bl, stri], writes=[PT])
                        t2 = B_["t2"]
                        k.op("dve", lambda e: e.tensor_tensor(out=t2[:], in0=QK[:], in1=Gm[:], op=ALU.mult),
                             reads=[QK, Gm], writes=[t2])
                        qkT = B_["qkT"]
                        k.op("dve", lambda e: e.tensor_tensor(out=qkT[:], in0=t2[:], in1=tri[:], op=ALU.mult),
                             reads=[t2, tri], writes=[qkT])
                        X32 = B_["X32"]
                        Xb = B_["Xb"]
                        k.op("act", lambda e: e.copy(out=X32[:, 0:128], in_=qkvtok[:, 1024 + h * 128:1024 + (h + 1) * 128]),
                             reads=[qkvtok], writes=[X32])
                        k.op("dve", lambda e: e.tensor_scalar(out=X32[:, 128:256], in0=qkntok[:, 4 + h, :], scalar1=eg_c,
                                                               scalar2=None, op0=ALU.mult), reads=[qkntok, E4],
                             writes=[X32])
                        k.op("act", lambda e: e.copy(out=Xb[:], in_=X32[:]), reads=[X32], writes=[Xb])
                    yield
                    for h in range(4):
                        B_ = HB[h]
                        ptp = ptb_s[h]
                        k.op("pe", lambda e: e.transpose(ptp[:], B_["PT"][0][:], identb[:]),
                             reads=[B_["PT"][0], identb], writes=[ptp])
                        k.op("act", lambda e: e.copy(out=B_["P"][0][:], in_=ptp[:]), reads=[ptp], writes=[B_["P"][0]])
                    xreg = [(pn_s[0], pn_s[1]), (pn_s[2], pn_s[3]), (pb_s[0], pb_s[1]), (pb_s[2], pb_s[3])]
                    sreg = [(pa_s[0], pc_s[0]), (pa_s[1], pc_s[1]), (pa_s[2], pc_s[2]), (pa_s[3], pc_s[3])]
                    cur = [0, 0, 0, 0]

                    def x_update(h, sub):
                        B_ = HB[h]
                        PT = B_["PT"][cur[h]]
                        xa, xb_ = xreg[h]
                        k.op("pe", lambda e: e.matmul(xa[:], lhsT=PT[:], rhs=B_["Xb"][:, 0:128], start=True, stop=True),
                             reads=[PT, B_["Xb"]], writes=[xa])
                        k.op("pe", lambda e: e.matmul(xb_[:], lhsT=PT[:], rhs=B_["Xb"][:, 128:256], start=True,
                                                      stop=True), reads=[PT, B_["Xb"]], writes=[xb_])

                    def x_apply(h, sub, last):
                        B_ = HB[h]
                        xa, xb_ = xreg[h]
                        op_ = ALU.subtract if sub else ALU.add
                        k.op("dve", lambda e: e.tensor_tensor(out=B_["X32"][:, 0:128], in0=B_["X32"][:, 0:128], in1=xa[:],
                                                              op=op_), reads=[B_["X32"], xa], writes=[B_["X32"]])
                        k.op("dve", lambda e: e.tensor_tensor(out=B_["X32"][:, 128:256], in0=B_["X32"][:, 128:256],
                                                              in1=xb_[:], op=op_), reads=[B_["X32"], xb_],
                             writes=[B_["X32"]])
                        if not last:
                            k.op("act", lambda e: e.copy(out=B_["Xb"][:], in_=B_["X32"][:]), reads=[B_["X32"]],
                                 writes=[B_["Xb"]])

                    yield
                    for h in range(4):
                        x_update(h, True)
                    yield
                    for h in range(4):
                        x_apply(h, True, False)
                    for lvl in range(1, 6):
                        yield
                        for h in range(4):
                            B_ = HB[h]
                            c_ = cur[h]
                            P, PT = B_["P"][c_], B_["PT"][c_]
                            sa, sb_ = sreg[h]
                            k.op("pe", lambda e: e.matmul(sa[:], lhsT=P[:], rhs=PT[:], start=True, stop=True),
                                 reads=[P, PT], writes=[sa])
                            if lvl < 5:
                                k.op("pe", lambda e: e.matmul(sb_[:], lhsT=PT[:], rhs=P[:], start=True, stop=True),
                                     reads=[P, PT], writes=[sb_])
                        yield
                        for h in range(4):
                            B_ = HB[h]
                            n_ = 1 - cur[h]
                            sa, sb_ = sreg[h]
                            k.op("act", lambda e: e.copy(out=B_["PT"][n_][:], in_=sa[:]), reads=[sa],
                                 writes=[B_["PT"][n_]])
                            if lvl < 5:
                                k.op("dve", lambda e: e.tensor_copy(out=B_["P"][n_][:], in_=sb_[:]), reads=[sb_],
                                     writes=[B_["P"][n_]])
                            cur[h] = n_
                        yield
                        for h in range(4):
                            x_update(h, False)
                        yield
                        for h in range(4):
                            x_apply(h, False, lvl == 5)
                    yield
                    for h in range(4):
                        B_ = HB[h]
                        be_c = be4[:, h:h + 1]
                        egl_c = E4[:, 1, h:h + 1]
                        k.op("dve", lambda e: e.tensor_scalar(out=B_["u32"][:], in0=B_["X32"][:, 0:128], scalar1=be_c,
                                                              scalar2=None, op0=ALU.mult), reads=[B_["X32"], bl],
                             writes=[B_["u32"]])
                        k.op("dve", lambda e: e.tensor_scalar(out=B_["wb"][:], in0=B_["X32"][:, 128:256], scalar1=be_c,
                                                              scalar2=None, op0=ALU.mult), reads=[B_["X32"], bl],
                             writes=[B_["wb"]])
                        k.op("dve", lambda e: e.tensor_scalar(out=B_["kg"][:], in0=qkntok[:, 4 + h, :], scalar1=egl_c,
                                                               scalar2=None, op0=ALU.mult), reads=[qkntok, E4],
                             writes=[B_["kg"]])
                        k.op("act", lambda e: e.copy(out=B_["S0"][:], in_=Sst[:, h, :]), reads=[Sst], writes=[B_["S0"]])
                    yield
                    for h in range(4):
                        B_ = HB[h]
                        wtp = ptb_s[4 + h]
                        k.op("pe", lambda e: e.transpose(wtp[:], B_["wb"][:], identb[:]), reads=[B_["wb"], identb],
                             writes=[wtp])
                        k.op("act", lambda e: e.copy(out=B_["wT"][:], in_=wtp[:]), reads=[wtp], writes=[B_["wT"]])
                    banks = [pa_s, pn_s, pb_s, pc_s]
                    yield
                    for h in range(4):
                        B_ = HB[h]
                        r0, r1 = banks[h][0], banks[h][1]
                        k.op("pe", lambda e: e.matmul(r0[:], lhsT=B_["wT"][:], rhs=B_["S0"][:], start=True, stop=True),
                             reads=[B_["wT"], B_["S0"]], writes=[r0])
                    yield
                    for h in range(4):
                        B_ = HB[h]
                        r0, r1 = banks[h][0], banks[h][1]
                        k.op("dve", lambda e: e.tensor_tensor(out=B_["vn"][R1, :], in0=B_["u32"][R1, :], in1=r0[R1, :],
                                                              op=ALU.subtract), reads=[B_["u32"], r0], writes=[B_["vn"]])
                        k.op("pe", lambda e: e.matmul(r1[:], lhsT=B_["kg"][R1, :], rhs=B_["vn"][R1, :], start=True,
                                                      stop=True), reads=[B_["kg"], B_["vn"]], writes=[r1])
                    yield
                    for h in range(4):
                        B_ = HB[h]
                        r0, r1 = banks[h][0], banks[h][1]
                        k.op("dve", lambda e: e.scalar_tensor_tensor(out=Sst[:, h, :], in0=Sst[:, h, :],
                                                                     scalar=E4[:, 2, h:h + 1], in1=r1[:],
                                                                     op0=ALU.mult, op1=ALU.add),
                             reads=[Sst, E4, r1], writes=[Sst])
                        k.op("act", lambda e: e.copy(out=B_["S1"][:], in_=Sst[:, h, :]), reads=[Sst], writes=[B_["S1"]])
                        k.op("pe", lambda e: e.matmul(r0[:], lhsT=B_["wT"][:], rhs=B_["S1"][:], start=True, stop=True),
                             reads=[B_["wT"], B_["S1"]], writes=[r0])
                    yield
                    for h in range(4):
                        B_ = HB[h]
                        r0, r1 = banks[h][0], banks[h][1]
                        k.op("dve", lambda e: e.tensor_tensor(out=B_["vn"][R2, :], in0=B_["u32"][R2, :], in1=r0[R2, :],
                                                              op=ALU.subtract), reads=[B_["u32"], r0], writes=[B_["vn"]])
                        k.op("pe", lambda e: e.matmul(r1[:], lhsT=B_["kg"][R2, :], rhs=B_["vn"][R2, :], start=True,
                                                      stop=True), reads=[B_["kg"], B_["vn"]], writes=[r1])
                    yield
                    for h in range(4):
                        B_ = HB[h]
                        r0, r1, r2, r3 = banks[h]
                        qnT_h = qknT[:, h, :]
                        k.op("dve", lambda e: e.scalar_tensor_tensor(out=Sst[:, h, :], in0=Sst[:, h, :],
                                                                     scalar=E4[:, 3, h:h + 1], in1=r1[:],
                                                                     op0=ALU.mult, op1=ALU.add),
                             reads=[Sst, E4, r1], writes=[Sst])
                        k.op("pe", lambda e: e.matmul(r0[:], lhsT=B_["qkT"][:], rhs=B_["vn"][:], start=True, stop=True),
                             reads=[B_["qkT"], B_["vn"]], writes=[r0])
                        k.op("pe", lambda e: e.matmul(r2[:], lhsT=qnT_h, rhs=B_["S0"][:], start=True, stop=True),
                             reads=[qknT, B_["S0"]], writes=[r2])
                        k.op("pe", lambda e: e.matmul(r3[:], lhsT=qnT_h, rhs=B_["S1"][:], start=True, stop=True),
                             reads=[qknT, B_["S1"]], writes=[r3])
                    yield
                    for h in range(4):
                        B_ = HB[h]
                        r0, r1, r2, r3 = banks[h]
                        oa = B_["oa"]
                        k.op("act", lambda e: e.copy(out=oa[:], in_=r0[:]), reads=[r0], writes=[oa])
                        k.op("dve", lambda e: e.scalar_tensor_tensor(out=osb[R1, h, :], in0=r2[R1, :],
                                                                     scalar=E4[R1, 0, h:h + 1], in1=oa[R1, :],
                                                                     op0=ALU.mult, op1=ALU.add),
                             reads=[r2, E4, oa], writes=[osb])
                        k.op("dve", lambda e: e.scalar_tensor_tensor(out=osb[R2, h, :], in0=r3[R2, :],
                                                                     scalar=E4[R2, 0, h:h + 1], in1=oa[R2, :],
                                                                     op0=ALU.mult, op1=ALU.add),
                             reads=[r3, E4, oa], writes=[osb])
                    osbv = osb[:].rearrange("p h c -> p (h c)")
                    if fwd:
                        if lat:
                            k.dma("pool", of_scr[t * 128:(t + 1) * 128, :], osbv, reads=[osb], writes=[d_of[t]])
                        if os.environ.get("SKIP_MLA"):
                            return
                        tabs = None
                        if lat:
                            tabs = ropep.next()
                            n0 = (t - 2) * 128
                            k.dma("sp", tabs[:, 0, :], rope_cos[n0:n0 + 128, :], writes=[tabs])
                            k.dma("sp", tabs[:, 1, :], rope_sin[n0:n0 + 128, :], writes=[tabs])
                        jk = t512.next()
                        r1 = rpool.next()
                        k.op("act", lambda e: e.activation(out=jk[:, 0:128], in_=p2[:, 272:400], func=AF.Square,
                                                           accum_out=r1[:, 0:1]), reads=[p2], writes=[jk, r1])
                        k.op("dve", lambda e: e.tensor_scalar(out=r1[:, 1:2], in0=r1[:, 0:1], scalar1=1.0 / 128,
                                                              scalar2=EPS, op0=ALU.mult, op1=ALU.add), reads=[r1],
                             writes=[r1])
                        rsqrt(r1, r1[:, 2:3], r1[:, 1:2])
                        cn = cqnp.next()
                        k.op("dve", lambda e: e.scalar_tensor_tensor(out=cn[:, 0:128], in0=p2[:, 272:400],
                                                                     scalar=r1[:, 2:3], in1=kvnw_bc[:], op0=ALU.mult,
                                                                     op1=ALU.mult), reads=[p2, r1, kvnw_bc], writes=[cn])
                        cnT = cqnTp.next()
                        k.op("pe", lambda e: e.transpose(ptb_s[0][:], cn[:, 0:128], identb[:]), reads=[cn, identb],
                             writes=[ptb_s[0]])
                        k.op("act", lambda e: e.copy(out=cnT[:, 2, :], in_=ptb_s[0][:]), reads=[ptb_s[0]], writes=[cnT])
                        MSUB = int(os.environ.get("MLA_SUB", "9"))
                        if MSUB < 2:
                            return
                        yield
                        k96 = q96p.next()
                        vx = vxp.next()
                        for half in range(2):
                            pk = proj.next()
                            k.op("pe", lambda e: e.matmul(pk[:], lhsT=cnT[:, 2, :], rhs=wkv[:, 0, half * 512:(half + 1) * 512],
                                                          start=True, stop=True), reads=[cnT, wkv], writes=[pk])
                            pkv = pk[:].rearrange("p (h c) -> p h c", c=128)
                            hs4 = slice(half * 4, half * 4 + 4)
                            kvs = kvsp.next()
                            k.op("act", lambda e: e.copy(out=kvs[:], in_=pk[:]), reads=[pk], writes=[kvs])
                            kvsv = kvs[:].rearrange("p (h c) -> p h c", c=128)
                            k.op("act", lambda e: e.copy(out=k96[:, hs4, 0:64], in_=kvsv[:, :, 0:64]), reads=[kvs],
                                 writes=[k96])
                            k.op("dve", lambda e: e.tensor_copy(out=vx[:, hs4, 0:64], in_=kvsv[:, :, 64:128]), reads=[kvs],
                                 writes=[vx])
                        if MSUB < 3:
                            return
                        k.op("dve", lambda e: e.tensor_copy(out=k96[:, :, 64:96],
                                                            in_=p2[:, 400:432].unsqueeze(1).to_broadcast([128, 8, 32])),
                             reads=[p2], writes=[k96])
                        if MSUB < 4:
                            return
                        k.dma("sp", VX[:, t, :, :].rearrange("h p c -> p h c"), vx[:], reads=[vx], writes=[d_VX])

                        def headnorm(buf, wbc):
                            sqh = q96p.next()
                            k.op("act", lambda e: e.activation(out=sqh[:], in_=buf[:], func=AF.Square),
                                 reads=[buf], writes=[sqh])
                            r8 = rpool.next()
                            k.op("dve", lambda e: e.tensor_reduce(out=r8[:], in_=sqh[:], axis=AX.X, op=ALU.add),
                                 reads=[sqh], writes=[r8])
                            k.op("dve", lambda e: e.tensor_scalar(out=r8[:], in0=r8[:], scalar1=1.0 / 96, scalar2=EPS,
                                                                  op0=ALU.mult, op1=ALU.add), reads=[r8], writes=[r8])
                            rsqrt(r8, r8[:], r8[:])
                            k.op("dve", lambda e: e.tensor_tensor(out=buf[:], in0=buf[:],
                                                                  in1=r8[:].unsqueeze(2).to_broadcast([128, 8, 96]),
                                                                  op=ALU.mult), reads=[buf, r8], writes=[buf])
                            k.op("dve", lambda e: e.tensor_tensor(out=buf[:], in0=buf[:],
                                                                   in1=wbc[:].unsqueeze(1).to_broadcast([128, 8, 96]),
                                                                   op=ALU.mult), reads=[buf, wbc], writes=[buf])

                        def heads_T(buf, dram_ap, dbuf):
                            bb = q96bp.next()
                            k.op("act", lambda e: e.copy(out=bb[:], in_=buf[:]), reads=[buf], writes=[bb])
                            for hh in range(8):
                                k.op("pe", lambda e: e.transpose(pst[0:96, hh, :], bb[:, hh, :], identb[:]),
                                     reads=[bb, identb], writes=[pst])
                            qt = qtp.next()
                            k.op("act", lambda e: e.copy(out=qt[:], in_=pst[0:96, :, :]), reads=[pst], writes=[qt])
                            k.dma("sp", dram_ap, qt[:], reads=[qt], writes=[dbuf])

                        MST = int(os.environ.get("MLA_STAGE", "9"))
                        if MST < 2:
                            return
                        yield
                        headnorm(k96, qkk_bc)
                        if lat:
                            rope(k96, tabs)
                        if MST < 3:
                            return
                        heads_T(k96, KT[:, :, t * 128:(t + 1) * 128].rearrange("h d n -> d h n"), d_KT)
                        if lat and MST >= 4:
                            r2 = rpool.next()
                            k.op("act", lambda e: e.activation(out=jk[:, 0:256], in_=p2[:, 16:272], func=AF.Square,
                                                               accum_out=r2[:, 0:1]), reads=[p2], writes=[jk, r2])
                            k.op("dve", lambda e: e.tensor_scalar(out=r2[:, 1:2], in0=r2[:, 0:1], scalar1=1.0 / 256,
                                                                  scalar2=EPS, op0=ALU.mult, op1=ALU.add), reads=[r2],
                                 writes=[r2])
                            rsqrt(r2, r2[:, 2:3], r2[:, 1:2])
                            cq = cqnp.next()
                            k.op("dve", lambda e: e.scalar_tensor_tensor(out=cq[:], in0=p2[:, 16:272], scalar=r2[:, 2:3],
                                                                         in1=qnw_bc[:], op0=ALU.mult, op1=ALU.mult),
                                 reads=[p2, r2, qnw_bc], writes=[cq])
                            cqT = cqnTp.next()
                            for c in range(2):
                                k.op("pe", lambda e: e.transpose(ptb_s[1 + c][:], cq[:, c * 128:(c + 1) * 128], identb[:]),
                                     reads=[cq, identb], writes=[ptb_s[1 + c]])
                                k.op("act", lambda e: e.copy(out=cqT[:, c, :], in_=ptb_s[1 + c][:]), reads=[ptb_s[1 + c]],
                                     writes=[cqT])
                            yield
                            q96 = q96p.next()
                            q96v = q96[:].rearrange("p h c -> p (h c)")
                            for (c0, n) in ((0, 512), (512, 256)):
                                pq = proj.next()
                                for c in range(2):
                                    k.op("pe", lambda e: e.matmul(pq[:, 0:n], lhsT=cqT[:, c, :], rhs=wq[:, c, c0:c0 + n],
                                                                  start=(c == 0), stop=(c == 1)), reads=[cqT, wq],
                                         writes=[pq])
                                k.op("act", lambda e: e.copy(out=q96v[:, c0:c0 + n], in_=pq[:, 0:n]), reads=[pq],
                                     writes=[q96])
                            headnorm(q96, qkq_bc)
                            rope(q96, tabs)
                            n0 = (t - 2) * 128
                            heads_T(q96, QT[:, :, n0:n0 + 128].rearrange("h d n -> d h n"), d_QT)
                        return
                    if not lat:
                        return
                    ofl = t512.next()
                    k.dma("sp", ofl[:], of_scr[t * 128:(t + 1) * 128, :], reads=[d_of[t]], writes=[ofl])
                    k.op("dve", lambda e: e.tensor_tensor(out=osbv, in0=osbv, in1=ofl[:], op=ALU.add), reads=[osb, ofl],
                         writes=[osb])
                    if dbg:
                        k.dma("pool", ob_scr[t * 128:(t + 1) * 128, :], osbv, reads=[osb])
                    sq2 = t512.next()
                    k.op("act", lambda e: e.activation(out=sq2[:], in_=osbv, func=AF.Square), reads=[osb],
                         writes=[sq2])
                    rs = rpool.next()
                    k.op("dve", lambda e: e.tensor_reduce(out=rs[:, 0:4], in_=sq2[:].rearrange("p (h c) -> p h c", c=128),
                                                          axis=AX.X, op=ALU.add), reads=[sq2], writes=[rs])
                    k.op("dve", lambda e: e.tensor_scalar(out=rs[:, 0:4], in0=rs[:, 0:4], scalar1=1.0 / 128, scalar2=EPS,
                                                          op0=ALU.mult, op1=ALU.add), reads=[rs], writes=[rs])
                    rsqrt(rs, rs[:, 0:4], rs[:, 0:4])
                    k.op("dve", lambda e: e.tensor_tensor(out=osb[:], in0=osb[:],
                                                          in1=rs[:, 0:4].unsqueeze(2).to_broadcast([128, 4, 128]),
                                                          op=ALU.mult), reads=[osb, rs], writes=[osb])
                    k.op("dve", lambda e: e.tensor_tensor(out=osb[:], in0=osb[:],
                                                           in1=gnw_bc[:].unsqueeze(1).to_broadcast([128, 4, 128]),
                                                           op=ALU.mult), reads=[osb, gnw_bc], writes=[osb])
                    yield
                    pz = proj_tok(O_Z, 512)
                    sgt = t512.next()
                    k.op("act", lambda e: e.activation(out=sgt[:], in_=pz[:], func=AF.Silu), reads=[pz], writes=[sgt])
                    cat = catp.next()
                    k.op("dve", lambda e: e.tensor_tensor(out=cat[:], in0=osbv, in1=sgt[:], op=ALU.mult),
                         reads=[osb, sgt], writes=[cat])
                    for fc in range(4):
                        k.op("pe", lambda e: e.transpose(pst[:, fc, :], cat[:, fc * 128:(fc + 1) * 128], identb[:]),
                             reads=[cat, identb], writes=[pst])
                    yield
                    catT = catTp.next()
                    k.op("act", lambda e: e.copy(out=catT[:, 0:4, :], in_=pst[:, 0:4, :]), reads=[pst], writes=[catT])
                    n0 = (t - 2) * 128
                    k.dma("sp", catT[:, 4:8, :], attT[:, n0:n0 + 128].rearrange("(c p) n -> p c n", p=128),
                          reads=[d_att], writes=[catT])
                    xt = xpool.next()
                    k.dma("sp", xt[:], x2[t * 128:(t + 1) * 128, :], reads=[d_x2[t]], writes=[xt])
                    for half in range(2):
                        py = proj.next()
                        for kc in range(8):
                            k.op("pe", lambda e: e.matmul(py[:], lhsT=catT[:, kc, :],
                                                          rhs=wout_g[:, kc, half * 512:(half + 1) * 512],
                                                          start=(kc == 0), stop=(kc == 7)), reads=[catT, wout_g],
                                 writes=[py])
                        k.op("dve", lambda e: e.tensor_tensor(out=xt[:, half * 512:(half + 1) * 512], in0=py[:],
                                                              in1=xt[:, half * 512:(half + 1) * 512], op=ALU.add),
                             reads=[py, xt], writes=[xt])
                    k.dma("pool", x3[t * 128:(t + 1) * 128, :], xt[:], reads=[xt], writes=[d_x3[t]])
                    yield

                def interleave(gens):
                    alive = list(gens)
                    while alive:
                        for g in list(alive):
                            try:
                                next(g)
                            except StopIteration:
                                alive.remove(g)

                ctxs = [dict() for _ in order]
                interleave([tile_A(order[0], ctxs[0])])
                for i_, t in enumerate(order):
                    gens = [tile_B(t, ctxs[i_])]
                    if i_ + 1 < len(order):
                        gens.append(tile_A(order[i_ + 1], ctxs[i_ + 1]))
                    interleave(gens)
                    ctxs[i_] = None
            k.barrier()

        def attention():
            with ExitStack() as es:
                ktp = k.sbpool("aKT", [96, T], BF16, 2, es)
                vxp = k.sbpool("aVX", [128, NT, 80], BF16, 2, es)
                qp = k.sbpool("aQ", [96, 512], BF16, 2, es)
                ptp_ = k.sbpool("aP", [128, 512], BF16, 4, es)
                rcp = k.sbpool("arc", [65, 512], F32, 2, es)
                otp = k.sbpool("aot", [64, 512], BF16, 2, es)
                nb10 = k.sb("nb10", [128, 1], F32, es)
                k.op("pool", lambda e: e.memset(nb10[:], -10.0), writes=[nb10])
                pss = k.pspool("aS", [128, 512], F32, 4, es)
                pso = k.pspool("aO", [65, 512], F32, 2, es)
                psb = k.pspool("aB", [64, 512], F32, 1, es)
                QB = min(512, L)
                for h in range(8):
                    kt_ = ktp.next()
                    k.dma("sp", kt_[:], KT[h], reads=[d_KT], writes=[kt_])
                    vx = vxp.next()
                    k.dma("sp", vx[:], VX[h].rearrange("t p c -> p t c"), reads=[d_VX], writes=[vx])
                    for q0 in range(0, L, QB):
                        qt = qp.next()
                        k.dma("sp", qt[:, 0:QB], QT[h, :, q0:q0 + QB], reads=[d_QT], writes=[qt])
                        po = pso.next()
                        psq = {}
                        for kt in range(NT + 2):
                            if kt < NT:
                                ps_ = pss.next()
                                k.op("pe", lambda e: e.matmul(ps_[:, 0:QB], lhsT=kt_[:, kt * 128:(kt + 1) * 128],
                                                              rhs=qt[:, 0:QB], start=True, stop=True), reads=[kt_, qt],
                                     writes=[ps_])
                                psq[kt] = ps_
                            k2 = kt - 2
                            if k2 >= 0:
                                ps2 = psq.pop(k2)
                                pt = ptp_.next()
                                k.op("act", lambda e: e.activation(out=pt[:, 0:QB], in_=ps2[:, 0:QB], func=AF.Exp,
                                                                   bias=nb10[:, 0:1]), reads=[ps2, nb10], writes=[pt])
                                k.op("pe", lambda e: e.matmul(po[:, 0:QB], lhsT=vx[:, k2, 0:65], rhs=pt[:, 0:QB],
                                                              start=(k2 == 0), stop=(k2 == NT - 1)), reads=[vx, pt],
                                     writes=[po])
                        rc = rcp.next()
                        k.op("dve", lambda e: e.reciprocal(out=rc[64:65, 0:QB], in_=po[64:65, 0:QB]), reads=[po],
                             writes=[rc])
                        pb_ = psb.next()
                        k.op("pe", lambda e: e.matmul(pb_[:, 0:QB], lhsT=ones[64:65, 0:64], rhs=rc[64:65, 0:QB],
                                                      start=True, stop=True), reads=[ones, rc], writes=[pb_])
                        k.op("act", lambda e: e.copy(out=rc[0:64, 0:QB], in_=pb_[:, 0:QB]), reads=[pb_], writes=[rc])
                        ot = otp.next()
                        k.op("dve", lambda e: e.tensor_tensor(out=ot[:, 0:QB], in0=po[0:64, 0:QB], in1=rc[0:64, 0:QB],
                                                              op=ALU.mult), reads=[po, rc], writes=[ot])
                        k.dma("pool", attT[h * 64:(h + 1) * 64, q0:q0 + QB], ot[:, 0:QB], reads=[ot], writes=[d_att])
            k.barrier()

        odd_pass(0)
        if stop_after == "odd0":
            k.drain_all()
            return nc
        attention()
        if stop_after == "att":
            k.drain_all()
            return nc
        odd_pass(1)
        d_y = Buf(None, "y")
        if stop_after == "mix1":
            k.drain_all()
            return nc
        moe(1, x3, d_x3, lambda t: (y_out[(t - 2) * 128:(t - 1) * 128, :], d_y), list(range(2, NT)))
        k.drain_all()
    return nc


_W_NAMES = ["c_ctx", "ada_w", "ada_b", "norm_mix_w", "norm_ffn_w", "even_w_in", "even_w_out", "gmlp_norm_w", "gmlp_ws",
            "gmlp_bs", "hgrn_lb_logits", "hgrn_norm_w", "odd_w_in", "odd_w_out", "gdn_conv_w", "gdn_a_log",
            "gdn_dt_bias", "gdn_norm_w", "mla_q_norm_w", "mla_wq_up", "mla_kv_norm_w", "mla_wkv_up", "mla_qk_norm_q",
            "mla_qk_norm_k", "router_group_w", "router_group_b", "router_expert_w", "router_expert_b", "moe_w_gate",
            "moe_w_up", "moe_w_down"]


def rope_tables(L):
    n = np.arange(L)
    row = (n // 64).astype(np.float32)
    col = (n % 64).astype(np.float32)
    freqs = (np.float32(10000.0) ** (-np.arange(8, dtype=np.float32) / np.float32(8))).astype(np.float32)
    ang = np.concatenate([row[:, None] * freqs[None, :], col[:, None] * freqs[None, :]], axis=1).astype(np.float32)
    return np.cos(ang).astype(np.float32), np.sin(ang).astype(np.float32)


def make_in_maps(inputs, ncores, L):
    cos, sin = rope_tables(L)
    shared = {n: np.ascontiguousarray(np.asarray(inputs[n], dtype=np.float32)) for n in _W_NAMES}
    shared["rope_cos"] = cos
    shared["rope_sin"] = sin
    maps = []
    for b in range(ncores):
        m = dict(shared)
        m["x"] = np.ascontiguousarray(np.asarray(inputs["x"][b], dtype=np.float32))
        m["ctx"] = np.ascontiguousarray(np.asarray(inputs["ctx"][b], dtype=np.float32))
        m["c"] = np.ascontiguousarray(np.asarray(inputs["c"][b], dtype=np.float32))
        maps.append(m)
    return maps


def kernel(**inputs):
    x = np.asarray(inputs["x"])
    B, L, _ = x.shape
    nc = build(L)
    maps = make_in_maps(inputs, B, L)
    res = run_bass_kernel_spmd(nc, maps, core_ids=list(range(B)))
    return np.stack([np.asarray(r["y"]) for r in res.results], axis=0).astype(np.float32)
```

```python
import os
import numpy as np
import concourse.bass as bass
import concourse.mybir as mybir
from concourse.bass_utils import run_bass_kernel_spmd
from contextlib import ExitStack

F32 = mybir.dt.float32
BF16 = mybir.dt.bfloat16
I32 = mybir.dt.int32
AF = mybir.ActivationFunctionType
ALU = mybir.AluOpType
AX = mybir.AxisListType
EPS = 1e-6
D = 1024
CTX = 256


class Buf:
    __slots__ = ("t", "w", "r", "name", "psum")

    def __init__(self, t, name="", psum=False):
        self.t = t
        self.w = None
        self.r = {}
        self.name = name
        self.psum = psum

    def __getitem__(self, k):
        return self.t[k]


class Bv:
    def __init__(self, base, ap):
        self.base = base
        self.ap = ap

    @property
    def psum(self):
        return self.base.psum

    def __getitem__(self, kk):
        return self.ap[kk]

    @property
    def w(self):
        return self.base.w

    @w.setter
    def w(self, v):
        self.base.w = v

    @property
    def r(self):
        return self.base.r

    @r.setter
    def r(self, v):
        self.base.r = v


class Pool:
    def __init__(self, bufs):
        self.bufs = bufs
        self.i = 0

    def next(self):
        b = self.bufs[self.i % len(self.bufs)]
        self.i += 1
        return b


class KB:
    NDMA = 8

    def __init__(self, nc):
        self.nc = nc
        self.es = ExitStack()
        self.eng = {"pe": nc.tensor, "dve": nc.vector, "act": nc.scalar, "pool": nc.gpsimd, "sp": nc.sync}
        self.sems = {}
        self.cnt = {}
        self.seen = {e: {} for e in self.eng}
        for e in self.eng:
            self.sems[e] = self.es.enter_context(nc.semaphore("c_" + e))
            self.cnt[e] = 0
        self.dsem = {}
        self.dcnt = {}
        for q in ("sp", "pool", "act"):
            self.dsem[q] = [self.es.enter_context(nc.semaphore("d_%s%d" % (q, i))) for i in range(self.NDMA)]
            self.dcnt[q] = 0
        self.semobj = {}
        for e in self.eng:
            self.semobj[("c", e)] = self.sems[e]
        for q in self.dsem:
            for i, s in enumerate(self.dsem[q]):
                self.semobj[("d", q, i)] = s
        self.nbuf = 0

    def sb(self, name, shape, dtype, es=None):
        self.nbuf += 1
        t = (es or self.es).enter_context(self.nc.sbuf_tensor("%s_%d" % (name, self.nbuf), list(shape), dtype))
        return Buf(t, name)

    def ps(self, name, shape, dtype, es=None):
        self.nbuf += 1
        t = (es or self.es).enter_context(self.nc.psum_tensor("%s_%d" % (name, self.nbuf), list(shape), dtype))
        return Buf(t, name, psum=True)

    def sbpool(self, name, shape, dtype, n, es=None):
        return Pool([self.sb(name, shape, dtype, es) for _ in range(n)])

    def pspool(self, name, shape, dtype, n, es=None):
        return Pool([self.ps(name, shape, dtype, es) for _ in range(n)])

    def dram(self, name, shape, dtype, kind="Internal"):
        return self.nc.dram_tensor(name, list(shape), dtype, kind=kind).ap()

    def _deps(self, reads, writes, e=None):
        deps = []
        for b in reads:
            if b.w is not None:
                deps.append(b.w)
            if b.psum:
                for kk, v in b.r.items():
                    if kk != ("c", e):
                        deps.append((kk, v))
        for b in writes:
            if b.w is not None:
                deps.append(b.w)
            for kk, v in b.r.items():
                deps.append((kk, v))
        return deps

    def _wait(self, e, deps):
        seen = self.seen[e]
        need = {}
        for kk, v in deps:
            if e == "pe" and kk == ("c", "pe"):
                continue
            if seen.get(kk, 0) >= v:
                continue
            if need.get(kk, 0) < v:
                need[kk] = v
        for kk, v in need.items():
            self.eng[e].wait_ge(self.semobj[kk], v)
            seen[kk] = v

    def _mark(self, tok, reads, writes):
        kk, v = tok
        for b in reads:
            if b.r.get(kk, 0) < v:
                b.r[kk] = v
        for b in writes:
            b.w = tok
            b.r = {}

    def op(self, e, fn, reads=(), writes=()):
        deps = self._deps(reads, writes, e)
        if e == "pe":
            self._wait(e, deps)
            ins = fn(self.eng[e])
        else:
            seen = self.seen[e]
            need = {}
            for kk, v in deps:
                if seen.get(kk, 0) >= v:
                    continue
                if need.get(kk, 0) < v:
                    need[kk] = v
            items = list(need.items())
            for kk, v in items[:-1]:
                self.eng[e].wait_ge(self.semobj[kk], v)
                seen[kk] = v
            ins = fn(self.eng[e])
            if items:
                kk, v = items[-1]
                ins._wait_ge(self.semobj[kk], v)
                seen[kk] = v
        self.cnt[e] += 1
        ins.then_inc(self.sems[e], 1)
        tok = (("c", e), self.cnt[e])
        self._mark(tok, reads, writes)
        return tok

    def dma(self, q, out, in_, reads=(), writes=(), **kw):
        i = self.dcnt[q]
        slot = i % self.NDMA
        kk = ("d", q, slot)
        deps = self._deps(reads, writes, q)
        prev = 16 * (i // self.NDMA)
        if prev > 0:
            deps.append((kk, prev))
        self._wait(q, deps)
        ins = self.eng[q].dma_start(out=out, in_=in_, **kw)
        ins.then_inc(self.dsem[q][slot], 16)
        self.dcnt[q] += 1
        tok = (kk, prev + 16)
        self._mark(tok, reads, writes)
        return tok

    def all_tokens(self):
        deps = []
        for e in self.eng:
            if self.cnt[e] > 0:
                deps.append((("c", e), self.cnt[e]))
        for q in self.dsem:
            n = self.dcnt[q]
            for slot in range(self.NDMA):
                c = (n - slot + self.NDMA - 1) // self.NDMA
                if c > 0:
                    deps.append((("d", q, slot), 16 * c))
        return deps

    def barrier(self):
        deps = self.all_tokens()
        for e in self.eng:
            self._wait(e, deps)

    def drain_all(self):
        self._wait("sp", self.all_tokens())


def build(L, stop_after="all", dbg=False):
    nc = bass.Bass("TRN2", target_bir_lowering=False)
    k = KB(nc)
    NT = (CTX + L) // 128
    NL = L // 128
    T = CTX + L

    def din(name, shape):
        return nc.dram_tensor(name, list(shape), F32, kind="ExternalInput").ap()

    x_in = din("x", [L, D])
    ctx_in = din("ctx", [CTX, D])
    c_in = din("c", [D])
    cctx_in = din("c_ctx", [D])
    ada_w = din("ada_w", [2, D, 6 * D])
    ada_b = din("ada_b", [2, 6 * D])
    norm_mix_w = din("norm_mix_w", [2, D])
    norm_ffn_w = din("norm_ffn_w", [2, D])
    even_w_in = din("even_w_in", [1, D, 3584])
    even_w_out = din("even_w_out", [1, D, D])
    gmlp_norm_w = din("gmlp_norm_w", [1, 512])
    gmlp_ws = din("gmlp_ws", [1, 4, 128, 128])
    gmlp_bs = din("gmlp_bs", [1, 4, 128])
    hgrn_lb = din("hgrn_lb_logits", [2, 3, 512])
    hgrn_norm_w = din("hgrn_norm_w", [1, 512])
    router_group_w = din("router_group_w", [2, D, 4])
    router_group_b = din("router_group_b", [2, 4])
    router_expert_w = din("router_expert_w", [2, D, 32])
    router_expert_b = din("router_expert_b", [2, 32])
    moe_w_gate = din("moe_w_gate", [2, 32, D, 512])
    moe_w_up = din("moe_w_up", [2, 32, D, 512])
    moe_w_down = din("moe_w_down", [2, 32, 512, D])
    odd_w_in = din("odd_w_in", [1, D, 2480])
    odd_w_out = din("odd_w_out", [1, D, D])
    gdn_conv_w = din("gdn_conv_w", [1, 5, 1536])
    gdn_a_log = din("gdn_a_log", [1, 2, 4])
    gdn_dt_bias = din("gdn_dt_bias", [1, 2, 4])
    gdn_norm_w = din("gdn_norm_w", [1, 128])
    mla_q_norm_w = din("mla_q_norm_w", [1, 256])
    mla_wq_up = din("mla_wq_up", [1, 256, 768])
    mla_kv_norm_w = din("mla_kv_norm_w", [1, 128])
    mla_wkv_up = din("mla_wkv_up", [1, 128, 1024])
    mla_qk_norm_q = din("mla_qk_norm_q", [1, 96])
    mla_qk_norm_k = din("mla_qk_norm_k", [1, 96])
    rope_cos = din("rope_cos", [L, 16])
    rope_sin = din("rope_sin", [L, 16])

    okind = "ExternalOutput"
    y_out = k.dram("y", [L, D], F32, okind)
    ikind = "ExternalOutput" if dbg else "Internal"
    modrow = k.dram("modrow", [2, 2, 6 * D], F32, ikind)
    of_scr = k.dram("of_scr", [T, 512], F32, ikind)
    x1 = k.dram("x1", [T, D], F32, ikind)
    x2 = k.dram("x2", [T, D], F32, ikind)
    d_modrow = Buf(None, "modrow")
    d_of = [Buf(None, "of%d" % t) for t in range(NT)]
    d_x1 = [Buf(None, "x1_%d" % t) for t in range(NT)]
    d_x2 = [Buf(None, "x2_%d" % t) for t in range(NT)]
    x3 = k.dram("x3", [T, D], F32, ikind)
    d_x3 = [Buf(None, "x3_%d" % t) for t in range(NT)]
    ob_scr = k.dram("ob_scr", [T, 512], F32, ikind)
    QT = k.dram("QT", [8, 96, L], BF16, ikind)
    KT = k.dram("KT", [8, 96, T], BF16, ikind)
    VX = k.dram("VX", [8, NT, 128, 80], BF16, ikind)
    attT = k.dram("attT", [512, L], BF16, ikind)
    d_QT = Buf(None, "QT")
    d_KT = Buf(None, "KT")
    d_VX = Buf(None, "VX")
    d_att = Buf(None, "att")

    def rows(t):
        if t < 2:
            return ctx_in[t * 128:(t + 1) * 128, :]
        return x_in[(t - 2) * 128:(t - 1) * 128, :]

    with k.es:
        ident = k.sb("ident", [128, 128], F32)
        identb = k.sb("identb", [128, 128], BF16)
        ones = k.sb("ones", [128, 128], F32)
        zeros_bf = k.sb("zeros_bf", [128, 512], BF16)
        k.op("pool", lambda e: e.memset(ones[:], 1.0), writes=[ones])
        k.op("pool", lambda e: e.memset(zeros_bf[:], 0.0), writes=[zeros_bf])
        k.op("pool", lambda e: e.affine_select(ident[:], ones[:], pattern=[[-1, 128]], compare_op=ALU.is_equal,
                                               fill=0.0, base=0, channel_multiplier=1), reads=[ones], writes=[ident])
        k.op("dve", lambda e: e.tensor_copy(out=identb[:], in_=ident[:]), reads=[ident], writes=[identb])

        def blockmask(name, lo_keep, es=None):
            m = k.sb(name, [128, 128], F32, es)
            k.op("pool", lambda e: e.memset(m[:], 0.0), writes=[m])
            for cb in range(2):
                sl = m[cb * 64:(cb + 1) * 64, cb * 64:(cb + 1) * 64]
                src = ones[cb * 64:(cb + 1) * 64, cb * 64:(cb + 1) * 64]
                if lo_keep == "all":
                    k.op("pool", lambda e: e.tensor_copy(out=sl, in_=src), reads=[ones], writes=[m])
                else:
                    cm, st = (-1, 1) if lo_keep == "le" else (1, -1)
                    k.op("pool", lambda e: e.affine_select(sl, src, pattern=[[st, 64]], compare_op=ALU.is_ge,
                                                           fill=0.0, base=0, channel_multiplier=cm),
                         reads=[ones], writes=[m])
            return m

        with ExitStack() as es:
            ccol = k.sb("ccol", [128, 8, 2], F32, es)
            scol = k.sb("scol", [128, 8, 2], F32, es)
            k.dma("sp", ccol[:, :, 0], c_in.rearrange("(kc p) -> p kc", p=128), writes=[ccol],
                  allow_slow_non_contiguous=True)
            k.dma("sp", ccol[:, :, 1], cctx_in.rearrange("(kc p) -> p kc", p=128), writes=[ccol],
                  allow_slow_non_contiguous=True)
            k.op("act", lambda e: e.activation(out=scol[:], in_=ccol[:], func=AF.Silu), reads=[ccol], writes=[scol])
            wst = k.sbpool("adaw", [128, 8, 512], F32, 2, es)
            brow = k.sbpool("adab", [2, 512], F32, 2, es)
            orow = k.sbpool("adao", [2, 512], F32, 2, es)
            pp = k.pspool("adap", [2, 512], F32, 2, es)
            for layer in range(2):
                for cb in range(12):
                    w = wst.next()
                    k.dma("sp", w[:], ada_w[layer, :, cb * 512:(cb + 1) * 512].rearrange("(kc p) n -> p kc n", p=128),
                          writes=[w])
                    b = brow.next()
                    k.dma("sp", b[:], ada_b[layer, cb * 512:(cb + 1) * 512].partition_broadcast(2), writes=[b])
                    p = pp.next()
                    for kc in range(8):
                        k.op("pe", lambda e: e.matmul(p[:], lhsT=scol[:, kc, :], rhs=w[:, kc, :], start=(kc == 0),
                                                      stop=(kc == 7)), reads=[scol, w], writes=[p])
                    o = orow.next()
                    k.op("dve", lambda e: e.tensor_tensor(out=o[:], in0=p[:], in1=b[:], op=ALU.add), reads=[p, b],
                         writes=[o])
                    k.dma("sp", modrow[layer, :, cb * 512:(cb + 1) * 512], o[:], reads=[o], writes=[d_modrow])
        k.barrier()
        if stop_after == "prologue":
            k.drain_all()
            return nc

        def rsqrt(buf, out_ap, in_ap):
            k.op("act", lambda e: e.activation(out=out_ap, in_=in_ap, func=AF.Sqrt), reads=[buf], writes=[buf])
            k.op("dve", lambda e: e.reciprocal(out=out_ap, in_=out_ap), reads=[buf], writes=[buf])

        def load_modcols(es, layer, normw_ap, which):
            mc = k.sb("mc", [128, 2, 48], F32, es)
            for s in range(2):
                k.dma("sp", mc[:, s, :], modrow[layer, s, :].rearrange("(c p) -> p c", p=128), reads=[d_modrow],
                      writes=[mc], allow_slow_non_contiguous=True)
            nw = k.sb("nw", [128, 8], F32, es)
            k.dma("sp", nw[:], normw_ap.rearrange("(c p) -> p c", p=128), writes=[nw], allow_slow_non_contiguous=True)
            A = k.sb("A", [128, 2, 8], F32, es)
            S = k.sb("S", [128, 2, 8], F32, es)
            o = which * 24
            for s in range(2):
                k.op("dve", lambda e: e.scalar_tensor_tensor(out=A[:, s, :], in0=mc[:, s, o + 8:o + 16], scalar=1.0,
                                                             in1=nw[:], op0=ALU.add, op1=ALU.mult),
                     reads=[mc, nw], writes=[A])
                k.op("dve", lambda e: e.tensor_copy(out=S[:, s, :], in_=mc[:, s, o:o + 8]), reads=[mc], writes=[S])
            return A, S

        def norm_T(xt, A, S, s, rpool, xnpool, pst, out_bf=None, out_f32=None):
            junk = xnpool["junk"].next()
            ss = rpool.next()
            k.op("act", lambda e: e.activation(out=junk[:], in_=xt[:], func=AF.Square, accum_out=ss[:, 0:1]),
                 reads=[xt], writes=[junk, ss])
            k.op("dve", lambda e: e.tensor_scalar(out=ss[:, 1:2], in0=ss[:, 0:1], scalar1=1.0 / D, scalar2=EPS,
                                                  op0=ALU.mult, op1=ALU.add), reads=[ss], writes=[ss])
            rsqrt(ss, ss[:, 2:3], ss[:, 1:2])
            if out_f32 is None:
                xn = xnpool["bf"].next()
                k.op("act", lambda e: e.activation(out=xn[:], in_=xt[:], func=AF.Copy, scale=ss[:, 2:3]),
                     reads=[xt, ss], writes=[xn])
                for fc in range(8):
                    k.op("pe", lambda e: e.transpose(pst[:, fc, :], xn[:, fc * 128:(fc + 1) * 128], identb[:]),
                         reads=[xn, identb], writes=[pst])
                k.op("dve", lambda e: e.tensor_tensor(out=out_bf[:], in0=pst[:],
                                                      in1=A[:, s, :].unsqueeze(2).to_broadcast([128, 8, 128]),
                                                      op=ALU.mult), reads=[pst, A], writes=[out_bf])
                k.op("dve", lambda e: e.tensor_tensor(out=out_bf[:], in0=out_bf[:],
                                                      in1=S[:, s, :].unsqueeze(2).to_broadcast([128, 8, 128]),
                                                      op=ALU.add), reads=[out_bf, S], writes=[out_bf])
            else:
                xn = xnpool["f32"].next()
                k.op("act", lambda e: e.activation(out=xn[:], in_=xt[:], func=AF.Copy, scale=ss[:, 2:3]),
                     reads=[xt, ss], writes=[xn])
                for half in range(2):
                    p = pst[half]
                    for f4 in range(4):
                        fc = half * 4 + f4
                        k.op("pe", lambda e: e.transpose(p[:, f4, :], xn[:, fc * 128:(fc + 1) * 128], ident[:]),
                             reads=[xn, ident], writes=[p])
                    sl = slice(half * 4, half * 4 + 4)
                    k.op("dve", lambda e: e.tensor_tensor(out=out_f32[:, sl, :], in0=p[:],
                                                          in1=A[:, s, sl].unsqueeze(2).to_broadcast([128, 4, 128]),
                                                          op=ALU.mult), reads=[p, A], writes=[out_f32])
                    k.op("dve", lambda e: e.tensor_tensor(out=out_f32[:, sl, :], in0=out_f32[:, sl, :],
                                                          in1=S[:, s, sl].unsqueeze(2).to_broadcast([128, 4, 128]),
                                                          op=ALU.add), reads=[out_f32, S], writes=[out_f32])
                k.op("act", lambda e: e.copy(out=out_bf[:], in_=out_f32[:]), reads=[out_f32], writes=[out_bf])

        def load_w_bf16(es, dst, src_ap, nkc, ncols, stage_pool, colblk=512, engs=("dve", "act")):
            i = 0
            for c0 in range(0, ncols, colblk):
                c1 = min(ncols, c0 + colblk)
                st = stage_pool.next()
                k.dma("sp", st[:, 0:nkc, 0:c1 - c0], src_ap[:, c0:c1].rearrange("(kc p) n -> p kc n", p=128),
                      writes=[st])
                eng = engs[i % len(engs)]
                i += 1
                if eng == "act":
                    k.op("act", lambda e: e.copy(out=dst[:, :, c0:c1], in_=st[:, 0:nkc, 0:c1 - c0]), reads=[st],
                         writes=[dst])
                else:
                    k.op(eng, lambda e: e.tensor_copy(out=dst[:, :, c0:c1], in_=st[:, 0:nkc, 0:c1 - c0]), reads=[st],
                         writes=[dst])

        def even_pass(direction):
            with ExitStack() as es:
                fwd = direction == 0
                A1, S1 = load_modcols(es, 0, norm_mix_w[0], 0)
                win = k.sb("win", [128, 8, 3584], BF16, es)
                rhsF = k.sb("rhsF", [128, 256], F32, es)
                lrem = k.sb("lrem", [128, 128], F32, es)
                maski = k.sb("maski", [128, 4, 128], I32, es)
                oml_bc = k.sb("oml_bc", [128, 512], F32, es)
                oml_col = k.sb("oml_col", [128, 4], F32, es)
                if not fwd:
                    wout_g = [k.sb("wout_g", [128, 8, 1024], BF16, es) for _ in range(2)]
                    gnw_bc = k.sb("gnw_bc", [128, 512], F32, es)
                    hnw_bc = k.sb("hnw_bc", [128, 512], F32, es)
                    bs_col = k.sb("bs_col", [128, 4], F32, es)
                    wsT = k.sb("wsT", [128, 4, 128], BF16, es)
                with ExitStack() as es2:
                    stage = k.sbpool("stage", [128, 8, 512], F32, 2, es2)
                    load_w_bf16(es2, win, even_w_in[0], 8, 3584, stage)
                    tri = blockmask("tri", "le" if fwd else "ge", es2)
                    allb = blockmask("allb", "all", es2)
                    mid = k.sb("mid", [128, 128], F32, es2)
                    k.op("pool", lambda e: e.memset(mid[:], 0.0), writes=[mid])
                    for cb in range(2):
                        r0 = cb * 64 + (0 if fwd else 32)
                        k.op("pool", lambda e: e.tensor_copy(out=mid[r0:r0 + 32, cb * 64:(cb + 1) * 64],
                                                             in_=ones[r0:r0 + 32, cb * 64:(cb + 1) * 64]),
                             reads=[ones], writes=[mid])
                    k.op("dve", lambda e: e.tensor_tensor(out=rhsF[:, 0:128], in0=tri[:], in1=mid[:], op=ALU.subtract),
                         reads=[tri, mid], writes=[rhsF])
                    k.op("dve", lambda e: e.tensor_copy(out=rhsF[:, 128:256], in_=tri[:]), reads=[tri], writes=[rhsF])
                    k.op("dve", lambda e: e.tensor_tensor(out=lrem[:], in0=allb[:], in1=tri[:], op=ALU.subtract),
                         reads=[allb, tri], writes=[lrem])
                    for h in range(4):
                        k.op("dve", lambda e: e.tensor_copy(out=maski[:, h, :], in_=tri[:]), reads=[tri],
                             writes=[maski])
                    lbt = k.sb("lbt", [128, 3, 512], F32, es2)
                    for r in range(3):
                        k.dma("sp", lbt[:, r, :], hgrn_lb[direction, r, :].partition_broadcast(128), writes=[lbt])
                    k.op("act", lambda e: e.activation(out=lbt[:], in_=lbt[:], func=AF.Exp), reads=[lbt], writes=[lbt])
                    tmpb = k.sb("tmpb", [128, 512], F32, es2)
                    k.op("dve", lambda e: e.tensor_tensor(out=tmpb[:], in0=lbt[:, 0, :], in1=lbt[:, 1, :], op=ALU.add),
                         reads=[lbt], writes=[tmpb])
                    k.op("dve", lambda e: e.tensor_tensor(out=tmpb[:], in0=tmpb[:], in1=lbt[:, 2, :], op=ALU.add),
                         reads=[lbt, tmpb], writes=[tmpb])
                    k.op("dve", lambda e: e.reciprocal(out=tmpb[:], in_=tmpb[:]), reads=[tmpb], writes=[tmpb])
                    k.op("dve", lambda e: e.tensor_tensor(out=oml_bc[:], in0=lbt[:, 1, :], in1=lbt[:, 2, :],
                                                          op=ALU.add), reads=[lbt], writes=[oml_bc])
                    k.op("dve", lambda e: e.tensor_tensor(out=oml_bc[:], in0=oml_bc[:], in1=tmpb[:], op=ALU.mult),
                         reads=[oml_bc, tmpb], writes=[oml_bc])
                    lbc = k.sb("lbc", [128, 3, 4], F32, es2)
                    for r in range(3):
                        k.dma("sp", lbc[:, r, :], hgrn_lb[direction, r, :].rearrange("(h p) -> p h", p=128),
                              writes=[lbc], allow_slow_non_contiguous=True)
                    k.op("act", lambda e: e.activation(out=lbc[:], in_=lbc[:], func=AF.Exp), reads=[lbc], writes=[lbc])
                    tmpc = k.sb("tmpc", [128, 4], F32, es2)
                    k.op("dve", lambda e: e.tensor_tensor(out=tmpc[:], in0=lbc[:, 0, :], in1=lbc[:, 1, :], op=ALU.add),
                         reads=[lbc], writes=[tmpc])
                    k.op("dve", lambda e: e.tensor_tensor(out=tmpc[:], in0=tmpc[:], in1=lbc[:, 2, :], op=ALU.add),
                         reads=[lbc, tmpc], writes=[tmpc])
                    k.op("dve", lambda e: e.reciprocal(out=tmpc[:], in_=tmpc[:]), reads=[tmpc], writes=[tmpc])
                    k.op("dve", lambda e: e.tensor_tensor(out=oml_col[:], in0=lbc[:, 1, :], in1=lbc[:, 2, :],
                                                          op=ALU.add), reads=[lbc], writes=[oml_col])
                    k.op("dve", lambda e: e.tensor_tensor(out=oml_col[:], in0=oml_col[:], in1=tmpc[:], op=ALU.mult),
                         reads=[oml_col, tmpc], writes=[oml_col])
                    if not fwd:
                        gbc = k.sb("gbc", [128, 2, 1024], F32, es2)
                        for s in range(2):
                            k.dma("sp", gbc[:, s, :], modrow[0, s, 2 * D:3 * D].partition_broadcast(128),
                                  reads=[d_modrow], writes=[gbc])
                        for c0 in range(0, 1024, 512):
                            st = stage.next()
                            k.dma("sp", st[:], even_w_out[0][:, c0:c0 + 512].rearrange("(kc p) n -> p kc n", p=128),
                                  writes=[st])
                            for s in range(2):
                                for kc in range(8):
                                    k.op("dve" if kc % 2 else "pool",
                                         lambda e: e.tensor_tensor(out=wout_g[s][:, kc, c0:c0 + 512], in0=st[:, kc, :],
                                                                   in1=gbc[:, s, c0:c0 + 512], op=ALU.mult),
                                         reads=[st, gbc], writes=[wout_g[s]])
                        k.dma("sp", gnw_bc[:], gmlp_norm_w[0].partition_broadcast(128), writes=[gnw_bc])
                        k.dma("sp", hnw_bc[:], hgrn_norm_w[0].partition_broadcast(128), writes=[hnw_bc])
                        k.dma("sp", bs_col[:], gmlp_bs[0].rearrange("g p -> p g"), writes=[bs_col],
                              allow_slow_non_contiguous=True)
                        wsf = k.sb("wsf", [128, 4, 128], F32, es2)
                        k.dma("sp", wsf[:], gmlp_ws[0].rearrange("g p q -> p g q"), writes=[wsf])
                        with ExitStack() as es3:
                            ptmp = k.ps("ptmp", [128, 512], F32, es3)
                            for g in range(4):
                                k.op("pe", lambda e: e.transpose(ptmp[:, g * 128:(g + 1) * 128], wsf[:, g, :], ident[:]),
                                     reads=[wsf, ident], writes=[ptmp])
                            k.op("dve", lambda e: e.tensor_copy(out=wsT[:].rearrange("p g q -> p (g q)"), in_=ptmp[:]),
                                 reads=[ptmp], writes=[wsT])
                            k.barrier()
                    k.barrier()

                Sst = k.sb("Sst", [128, 4, 128], F32, es)
                Sbf = k.sbpool("Sbf", [128, 4, 128], BF16, 3, es)
                k.op("pool", lambda e: e.memset(Sst[:], 0.0), writes=[Sst])
                xpool = k.sbpool("xt", [128, 1024], F32, 3, es)
                rpool = k.sbpool("rs", [128, 4], F32, 4, es)
                xnpool = {"junk": k.sbpool("junk", [128, 1024], BF16, 1, es),
                          "bf": k.sbpool("xnb", [128, 1024], BF16, 2, es)}
                hTp = k.sbpool("hT", [128, 8, 128], BF16, 2, es)
                pst = k.ps("pst", [128, 8, 128], BF16, es)
                proj = k.pspool("proj", [128, 512], F32, 2, es)
                big2 = k.ps("big2", [128, 1024], F32, es)
                gs = k.ps("gs", [128, 512], F32, es)
                ops_ = k.ps("ops", [128, 4, 128], F32, es)
                dSp = k.ps("dSp", [128, 4, 128], F32, es)
                t512 = k.sbpool("t512", [128, 512], F32, 10, es)
                b512 = k.sbpool("b512", [128, 512], BF16, 10, es)
                qhp = k.sbpool("qhat", [128, 2, 4, 128], BF16, 2, es)
                for qb in qhp.bufs:
                    k.op("pool", lambda e: e.memset(qb[:], 0.0), writes=[qb])
                if not fwd:
                    catp = k.sbpool("cat", [128, 1024], BF16, 2, es)
                    catTp = k.sbpool("catT", [128, 8, 128], BF16, 2, es)
                    zp = k.sbpool("z", [128, 1024], F32, 1, es)

                order = list(range(NT)) if fwd else [1, 0] + list(range(NT - 1, 1, -1))
                C_Q, C_I, C_FF, C_FB, C_G = 1024, 1536, 2048, 2560, 3072
                C_F = C_FF if fwd else C_FB
                first_rows, second_rows = (slice(0, 64), slice(64, 128)) if fwd else (slice(64, 128), slice(0, 64))
                first_idx, second_idx = (0, 1) if fwd else (1, 0)
                last_first, last_second = (63, 127) if fwd else (64, 0)

                for t in order:
                    s = 0 if t >= 2 else 1
                    xt = xpool.next()
                    k.dma("sp", xt[:], rows(t), writes=[xt])
                    hT = hTp.next()
                    norm_T(xt, A1, S1, s, rpool, xnpool, pst, out_bf=hT)

                    def proj_tok(c0, n=512):
                        p = proj.next()
                        for kc in range(8):
                            k.op("pe", lambda e: e.matmul(p[:, 0:n], lhsT=hT[:, kc, :], rhs=win[:, kc, c0:c0 + n],
                                                          start=(kc == 0), stop=(kc == 7)), reads=[hT, win], writes=[p])
                        return p

                    def proj_feat(c0):
                        p = proj.next()
                        for h in range(4):
                            for kc in range(8):
                                k.op("pe", lambda e: e.matmul(p[:, h * 128:(h + 1) * 128],
                                                              lhsT=win[:, kc, c0 + h * 128:c0 + (h + 1) * 128],
                                                              rhs=hT[:, kc, :], start=(kc == 0), stop=(kc == 7)),
                                     reads=[hT, win], writes=[p])
                        return p

                    pf = proj_tok(C_F)
                    sg = t512.next()
                    k.op("act", lambda e: e.activation(out=sg[:], in_=pf[:], func=AF.Sigmoid, scale=-1.0), reads=[pf],
                         writes=[sg])
                    ktok = t512.next()
                    k.op("dve", lambda e: e.tensor_tensor(out=ktok[:], in0=sg[:], in1=oml_bc[:], op=ALU.mult),
                         reads=[sg, oml_bc], writes=[ktok])
                    lf = t512.next()
                    k.op("act", lambda e: e.activation(out=lf[:], in_=ktok[:], func=AF.Ln, scale=-1.0, bias=1.0),
                         reads=[ktok], writes=[lf])
                    k.op("pe", lambda e: e.matmul(gs[:], lhsT=lrem[:], rhs=lf[:], start=True, stop=True),
                         reads=[lrem, lf], writes=[gs])
                    er = t512.next()
                    k.op("act", lambda e: e.activation(out=er[:], in_=gs[:], func=AF.Exp), reads=[gs], writes=[er])
                    khat = b512.next()
                    k.op("dve", lambda e: e.tensor_tensor(out=khat[:], in0=ktok[:], in1=er[:], op=ALU.mult),
                         reads=[ktok, er], writes=[khat])
                    pi = proj_tok(C_I)
                    vb = b512.next()
                    k.op("act", lambda e: e.copy(out=vb[:], in_=pi[:]), reads=[pi], writes=[vb])
                    for h in range(4):
                        k.op("pe", lambda e: e.matmul(big2[:, h * 256:(h + 1) * 256], lhsT=lf[:, h * 128:(h + 1) * 128],
                                                      rhs=rhsF[:], start=True, stop=True), reads=[lf, rhsF],
                             writes=[big2])
                    b2v = big2[:].rearrange("p (h c) -> p h c", c=256)
                    e1 = t512.next()
                    e2 = t512.next()
                    e3 = t512.next()
                    e1v = e1[:].rearrange("p (h c) -> p h c", c=128)
                    e2v = e2[:].rearrange("p (h c) -> p h c", c=128)
                    e3v = e3[:].rearrange("p (h c) -> p h c", c=128)
                    k.op("act", lambda e: e.activation(out=e1v, in_=b2v[:, :, 0:128], func=AF.Exp), reads=[big2],
                         writes=[e1])
                    k.op("act", lambda e: e.activation(out=e2v, in_=b2v[:, :, 0:128], func=AF.Exp, scale=-1.0),
                         reads=[big2], writes=[e2])
                    k.op("act", lambda e: e.activation(out=e3v, in_=b2v[:, :, 128:256], func=AF.Exp), reads=[big2],
                         writes=[e3])
                    pq = proj_feat(C_Q)
                    qT = t512.next()
                    k.op("act", lambda e: e.copy(out=qT[:], in_=pq[:]), reads=[pq], writes=[qT])
                    pfT = proj_feat(C_F)
                    kT = t512.next()
                    k.op("act", lambda e: e.activation(out=kT[:], in_=pfT[:], func=AF.Sigmoid, scale=-1.0), reads=[pfT],
                         writes=[kT])
                    kTv = kT[:].rearrange("p (h c) -> p h c", c=128)
                    k.op("dve", lambda e: e.tensor_tensor(out=kTv, in0=kTv,
                                                          in1=oml_col[:].unsqueeze(2).to_broadcast([128, 4, 128]),
                                                          op=ALU.mult), reads=[kT, oml_col], writes=[kT])
                    qtl = b512.next()
                    ktl = b512.next()
                    k.op("dve", lambda e: e.tensor_tensor(out=qtl[:], in0=qT[:], in1=e1[:], op=ALU.mult),
                         reads=[qT, e1], writes=[qtl])
                    k.op("dve", lambda e: e.tensor_tensor(out=ktl[:], in0=kT[:], in1=e2[:], op=ALU.mult),
                         reads=[kT, e2], writes=[ktl])
                    qh = qhp.next()
                    qTv = qT[:].rearrange("p (h c) -> p h c", c=128)
                    for cb in range(2):
                        cs = slice(cb * 64, (cb + 1) * 64)
                        k.op("dve", lambda e: e.tensor_tensor(out=qh[:, cb, :, cs], in0=qTv[:, :, cs],
                                                               in1=e3v[:, :, cs], op=ALU.mult),
                             reads=[qT, e3], writes=[qh])
                    for h in range(4):
                        k.op("pe", lambda e: e.matmul(gs[:, h * 128:(h + 1) * 128], lhsT=ktl[:, h * 128:(h + 1) * 128],
                                                      rhs=qtl[:, h * 128:(h + 1) * 128], start=True, stop=True),
                             reads=[ktl, qtl], writes=[gs])
                    scm = b512.next()
                    k.op("act", lambda e: e.copy(out=scm[:], in_=zeros_bf[:]), reads=[zeros_bf], writes=[scm])
                    k.op("dve", lambda e: e.copy_predicated(out=scm[:], mask=maski[:].rearrange("p h c -> p (h c)"),
                                                            data=gs[:]), reads=[gs, maski, scm], writes=[scm])
                    S0 = Sbf.next()
                    k.op("act", lambda e: e.copy(out=S0[:], in_=Sst[:]), reads=[Sst], writes=[S0])
                    for h in range(4):
                        hs = slice(h * 128, (h + 1) * 128)
                        k.op("pe", lambda e: e.matmul(dSp[:, h, :], lhsT=khat[first_rows, hs], rhs=vb[first_rows, hs],
                                                      start=True, stop=True), reads=[khat, vb], writes=[dSp])
                    for h in range(4):
                        k.op("dve", lambda e: e.scalar_tensor_tensor(out=Sst[:, h, :], in0=Sst[:, h, :],
                                                                     scalar=e3v[:, h, last_first:last_first + 1],
                                                                     in1=dSp[:, h, :], op0=ALU.mult, op1=ALU.add),
                             reads=[Sst, e3, dSp], writes=[Sst])
                    S1_ = Sbf.next()
                    k.op("act", lambda e: e.copy(out=S1_[:], in_=Sst[:]), reads=[Sst], writes=[S1_])
                    for h in range(4):
                        hs = slice(h * 128, (h + 1) * 128)
                        k.op("pe", lambda e: e.matmul(ops_[:, h, :], lhsT=scm[:, hs], rhs=vb[:, hs], start=True,
                                                      stop=False), reads=[scm, vb], writes=[ops_])
                        k.op("pe", lambda e: e.matmul(ops_[:, h, :], lhsT=qh[:, first_idx, h, :], rhs=S0[:, h, :],
                                                      start=False, stop=False), reads=[qh, S0], writes=[ops_])
                        k.op("pe", lambda e: e.matmul(ops_[:, h, :], lhsT=qh[:, second_idx, h, :], rhs=S1_[:, h, :],
                                                      start=False, stop=True), reads=[qh, S1_], writes=[ops_])
                    for h in range(4):
                        hs = slice(h * 128, (h + 1) * 128)
                        k.op("pe", lambda e: e.matmul(dSp[:, h, :], lhsT=khat[second_rows, hs], rhs=vb[second_rows, hs],
                                                      start=True, stop=True), reads=[khat, vb], writes=[dSp])
                    for h in range(4):
                        k.op("dve", lambda e: e.scalar_tensor_tensor(out=Sst[:, h, :], in0=Sst[:, h, :],
                                                                     scalar=e3v[:, h, last_second:last_second + 1],
                                                                     in1=dSp[:, h, :], op0=ALU.mult, op1=ALU.add),
                             reads=[Sst, e3, dSp], writes=[Sst])
                    osb = t512.next()
                    if fwd:
                        k.op("act", lambda e: e.copy(out=osb[:], in_=ops_[:].rearrange("p h c -> p (h c)")),
                             reads=[ops_], writes=[osb])
                        k.dma("pool", of_scr[t * 128:(t + 1) * 128, :], osb[:], reads=[osb], writes=[d_of[t]])
                        continue
                    ofl = t512.next()
                    k.dma("sp", ofl[:], of_scr[t * 128:(t + 1) * 128, :], reads=[d_of[t]], writes=[ofl])
                    k.op("dve", lambda e: e.tensor_tensor(out=osb[:], in0=ops_[:].rearrange("p h c -> p (h c)"),
                                                          in1=ofl[:], op=ALU.add), reads=[ops_, ofl], writes=[osb])
                    sq = t512.next()
                    k.op("act", lambda e: e.activation(out=sq[:], in_=osb[:], func=AF.Square), reads=[osb],
                         writes=[sq])
                    rs = rpool.next()
                    k.op("dve", lambda e: e.tensor_reduce(out=rs[:], in_=sq[:].rearrange("p (h c) -> p h c", c=128),
                                                          axis=AX.X, op=ALU.add), reads=[sq], writes=[rs])
                    k.op("dve", lambda e: e.tensor_scalar(out=rs[:], in0=rs[:], scalar1=1.0 / 128, scalar2=EPS,
                                                          op0=ALU.mult, op1=ALU.add), reads=[rs], writes=[rs])
                    rsqrt(rs, rs[:], rs[:])
                    k.op("dve", lambda e: e.tensor_tensor(out=osb[:].rearrange("p (h c) -> p h c", c=128),
                                                          in0=osb[:].rearrange("p (h c) -> p h c", c=128),
                                                          in1=rs[:].unsqueeze(2).to_broadcast([128, 4, 128]),
                                                          op=ALU.mult), reads=[osb, rs], writes=[osb])
                    k.op("dve", lambda e: e.tensor_tensor(out=osb[:], in0=osb[:], in1=hnw_bc[:], op=ALU.mult),
                         reads=[osb, hnw_bc], writes=[osb])
                    pg = proj_tok(C_G)
                    sgt = t512.next()
                    k.op("act", lambda e: e.activation(out=sgt[:], in_=pg[:], func=AF.Silu), reads=[pg], writes=[sgt])
                    cat = catp.next()
                    k.op("dve", lambda e: e.tensor_tensor(out=cat[:, 512:1024], in0=osb[:], in1=sgt[:], op=ALU.mult),
                         reads=[osb, sgt], writes=[cat])
                    z = zp.next()
                    pu = proj_tok(0)
                    k.op("act", lambda e: e.activation(out=z[:, 0:512], in_=pu[:], func=AF.Gelu), reads=[pu], writes=[z])
                    pv = proj_tok(512)
                    k.op("act", lambda e: e.activation(out=z[:, 512:1024], in_=pv[:], func=AF.Gelu), reads=[pv],
                         writes=[z])
                    rs2 = rpool.next()
                    sq2 = t512.next()
                    k.op("act", lambda e: e.activation(out=sq2[:], in_=z[:, 512:1024], func=AF.Square,
                                                       accum_out=rs2[:, 0:1]), reads=[z], writes=[sq2, rs2])
                    k.op("dve", lambda e: e.tensor_scalar(out=rs2[:, 1:2], in0=rs2[:, 0:1], scalar1=1.0 / 512,
                                                          scalar2=EPS, op0=ALU.mult, op1=ALU.add), reads=[rs2],
                         writes=[rs2])
                    rsqrt(rs2, rs2[:, 2:3], rs2[:, 1:2])
                    vn = b512.next()
                    k.op("dve", lambda e: e.scalar_tensor_tensor(out=vn[:], in0=z[:, 512:1024], scalar=rs2[:, 2:3],
                                                                 in1=gnw_bc[:], op0=ALU.mult, op1=ALU.mult),
                         reads=[z, rs2, gnw_bc], writes=[vn])
                    psg = proj.next()
                    for g in range(4):
                        gsl = slice(g * 128, (g + 1) * 128)
                        k.op("pe", lambda e: e.matmul(psg[:, gsl], lhsT=wsT[:, g, :], rhs=vn[:, gsl], start=True,
                                                      stop=True), reads=[wsT, vn], writes=[psg])
                    for g in range(4):
                        gsl = slice(g * 128, (g + 1) * 128)
                        k.op("dve", lambda e: e.scalar_tensor_tensor(out=cat[:, gsl], in0=psg[:, gsl],
                                                                     scalar=bs_col[:, g:g + 1], in1=z[:, gsl],
                                                                     op0=ALU.add, op1=ALU.mult),
                             reads=[psg, bs_col, z], writes=[cat])
                    for fc in range(8):
                        k.op("pe", lambda e: e.transpose(pst[:, fc, :], cat[:, fc * 128:(fc + 1) * 128], identb[:]),
                             reads=[cat, identb], writes=[pst])
                    catT = catTp.next()
                    k.op("act", lambda e: e.copy(out=catT[:], in_=pst[:]), reads=[pst], writes=[catT])
                    for half in range(2):
                        for kc in range(8):
                            k.op("pe", lambda e: e.matmul(big2[:, half * 512:(half + 1) * 512], lhsT=catT[:, kc, :],
                                                          rhs=wout_g[s][:, kc, half * 512:(half + 1) * 512],
                                                          start=(kc == 0), stop=(kc == 7)),
                                 reads=[catT, wout_g[s]], writes=[big2])
                    k.op("dve", lambda e: e.tensor_tensor(out=xt[:], in0=big2[:], in1=xt[:], op=ALU.add),
                         reads=[big2, xt], writes=[xt])
                    k.dma("pool", x1[t * 128:(t + 1) * 128, :], xt[:], reads=[xt], writes=[d_x1[t]])
            k.barrier()

        even_pass(0)
        even_pass(1)
        if stop_after == "mix0":
            k.drain_all()
            return nc

        def moe(layer, src, d_src, dst_fn, tiles):
            with ExitStack() as es:
                A2, S2 = load_modcols(es, layer, norm_ffn_w[layer], 1)
                wr = k.sb("wr", [128, 8, 36], F32, es)
                k.dma("sp", wr[:, :, 0:4], router_group_w[layer].rearrange("(kc p) e -> p kc e", p=128), writes=[wr],
                      allow_slow_non_contiguous=True)
                k.dma("sp", wr[:, :, 4:36], router_expert_w[layer].rearrange("(kc p) e -> p kc e", p=128), writes=[wr],
                      allow_slow_non_contiguous=True)
                rb = k.sb("rb", [128, 36], F32, es)
                k.dma("sp", rb[:, 0:4], router_group_b[layer].partition_broadcast(128), writes=[rb])
                k.dma("sp", rb[:, 4:36], router_expert_b[layer].partition_broadcast(128), writes=[rb])
                g2bc = k.sb("g2bc", [128, 2, 1024], F32, es)
                for s in range(2):
                    k.dma("sp", g2bc[:, s, :], modrow[layer, s, 5 * D:6 * D].partition_broadcast(128),
                          reads=[d_modrow], writes=[g2bc])
                n_super = -(-len(tiles) // 12)
                NTS = -(-len(tiles) // n_super)
                ST = NTS * 128
                hTs = k.sb("hTs", [128, 8, ST], BF16, es)
                hT32p = k.sbpool("hT32", [128, 8, 128], F32, 1, es)
                yacc = k.sb("yacc", [128, NTS, 1024], F32, es)
                wc = k.sb("wc", [128, NTS, 32], F32, es)
                xpool = k.sbpool("xt", [128, 1024], F32, 2, es)
                rpool = k.sbpool("rs", [128, 8], F32, 4, es)
                xnpool = {"junk": k.sbpool("junk", [128, 1024], BF16, 1, es),
                          "f32": k.sbpool("xnf", [128, 1024], F32, 1, es)}
                stg = k.sbpool("wstg", [128, 4, 512], F32, 4, es)
                wgp = k.sbpool("wg", [128, 8, 512], BF16, 2, es)
                wup = k.sbpool("wu", [128, 8, 512], BF16, 2, es)
                wdp = k.sbpool("wd", [128, 4, 1024], BF16, 2, es)
                silp = k.sbpool("sil", [128, 512], F32, 2, es)
                actp = k.sbpool("actT", [128, 4, 512], BF16, 2, es)
                r36 = k.sbpool("r36", [128, 40], F32, 12, es)
                pga = k.pspool("pga", [128, 512], F32, 2, es)
                pup = k.pspool("pup", [128, 512], F32, 2, es)
                pdn = k.pspool("pdn", [128, 1024], F32, 2, es)
                cast_i = [0]

                def cast(dst_ap, dst, st):
                    eng = ("dve", "act")[cast_i[0] % 2]
                    cast_i[0] += 1
                    if eng == "act":
                        k.op("act", lambda e: e.copy(out=dst_ap, in_=st[:]), reads=[st], writes=[dst])
                    else:
                        k.op(eng, lambda e: e.tensor_copy(out=dst_ap, in_=st[:]), reads=[st], writes=[dst])

                bounds = [round(i * len(tiles) / n_super) for i in range(n_super + 1)]
                for si_ in range(n_super):
                    stiles = tiles[bounds[si_]:bounds[si_ + 1]]
                    n_t = len(stiles)
                    for ti, t in enumerate(stiles):
                        s = 0 if t >= 2 else 1
                        xt = xpool.next()
                        k.dma("sp", xt[:], src[t * 128:(t + 1) * 128, :], reads=[d_src[t]], writes=[xt])
                        h32 = hT32p.next()
                        pstf = [pga.next(), pup.next()]
                        pst_views = [Bv(p, p[:].rearrange("p (f c) -> p f c", c=128)) for p in pstf]
                        hview = Bv(hTs, hTs[:, :, ti * 128:(ti + 1) * 128])
                        norm_T(xt, A2, S2, s, rpool, xnpool, pst_views, out_bf=hview, out_f32=h32)
                        pl = pga.next()
                        for kc in range(8):
                            k.op("pe", lambda e: e.matmul(pl[:, 0:36], lhsT=h32[:, kc, :], rhs=wr[:, kc, :],
                                                          start=(kc == 0), stop=(kc == 7)), reads=[h32, wr], writes=[pl])
                        lg = r36.next()
                        k.op("dve", lambda e: e.tensor_tensor(out=lg[:, 0:36], in0=pl[:, 0:36], in1=rb[:], op=ALU.add),
                             reads=[pl, rb], writes=[lg])
                        m = r36.next()
                        k.op("dve", lambda e: e.tensor_reduce(out=m[:, 0:1], in_=lg[:, 0:4], axis=AX.X, op=ALU.max),
                             reads=[lg], writes=[m])
                        k.op("dve", lambda e: e.tensor_scalar(out=m[:, 1:2], in0=m[:, 0:1], scalar1=-1.0, scalar2=None,
                                                              op0=ALU.mult), reads=[m], writes=[m])
                        eg = r36.next()
                        k.op("act", lambda e: e.activation(out=eg[:, 0:4], in_=lg[:, 0:4], func=AF.Exp, bias=m[:, 1:2],
                                                           accum_out=m[:, 2:3]), reads=[lg, m], writes=[eg, m])
                        k.op("dve", lambda e: e.reciprocal(out=m[:, 3:4], in_=m[:, 2:3]), reads=[m], writes=[m])
                        ohg = r36.next()
                        k.op("dve", lambda e: e.tensor_scalar(out=ohg[:, 0:4], in0=lg[:, 0:4], scalar1=m[:, 0:1],
                                                              scalar2=1e9, op0=ALU.is_lt, op1=ALU.mult),
                             reads=[lg, m], writes=[ohg])
                        lem = r36.next()
                        k.op("dve", lambda e: e.tensor_tensor(
                            out=lem[:, 0:32].rearrange("p (g e) -> p g e", e=8),
                            in0=lg[:, 4:36].rearrange("p (g e) -> p g e", e=8),
                            in1=ohg[:, 0:4].unsqueeze(2).to_broadcast([128, 4, 8]), op=ALU.subtract),
                             reads=[lg, ohg], writes=[lem])
                        k.op("dve", lambda e: e.tensor_reduce(out=m[:, 4:5], in_=lem[:, 0:32], axis=AX.X, op=ALU.max),
                             reads=[lem], writes=[m])
                        oh1 = r36.next()
                        k.op("dve", lambda e: e.tensor_scalar(out=oh1[:, 0:32], in0=lem[:, 0:32], scalar1=m[:, 4:5],
                                                              scalar2=None, op0=ALU.is_ge), reads=[lem, m], writes=[oh1])
                        lem2 = r36.next()
                        k.op("dve", lambda e: e.scalar_tensor_tensor(out=lem2[:, 0:32], in0=oh1[:, 0:32], scalar=-1e9,
                                                                     in1=lem[:, 0:32], op0=ALU.mult, op1=ALU.add),
                             reads=[oh1, lem], writes=[lem2])
                        k.op("dve", lambda e: e.tensor_reduce(out=m[:, 5:6], in_=lem2[:, 0:32], axis=AX.X, op=ALU.max),
                             reads=[lem2], writes=[m])
                        oh2 = r36.next()
                        k.op("dve", lambda e: e.tensor_scalar(out=oh2[:, 0:32], in0=lem2[:, 0:32], scalar1=m[:, 5:6],
                                                              scalar2=None, op0=ALU.is_ge), reads=[lem2, m],
                             writes=[oh2])
                        k.op("dve", lambda e: e.tensor_tensor(out=m[:, 6:7], in0=m[:, 5:6], in1=m[:, 4:5],
                                                              op=ALU.subtract), reads=[m], writes=[m])
                        k.op("act", lambda e: e.activation(out=m[:, 7:8], in_=m[:, 6:7], func=AF.Exp), reads=[m],
                             writes=[m])
                        k.op("dve", lambda e: e.tensor_scalar(out=m[:, 7:8], in0=m[:, 7:8], scalar1=1.0, scalar2=None,
                                                              op0=ALU.add), reads=[m], writes=[m])
                        k.op("dve", lambda e: e.reciprocal(out=m[:, 8:9], in_=m[:, 7:8]), reads=[m], writes=[m])
                        k.op("dve", lambda e: e.tensor_tensor(out=m[:, 9:10], in0=m[:, 8:9], in1=m[:, 3:4], op=ALU.mult),
                             reads=[m], writes=[m])
                        k.op("dve", lambda e: e.tensor_tensor(out=m[:, 10:11], in0=m[:, 3:4], in1=m[:, 9:10],
                                                              op=ALU.subtract), reads=[m], writes=[m])
                        k.op("dve", lambda e: e.tensor_scalar(out=wc[:, ti, :], in0=oh1[:, 0:32], scalar1=m[:, 9:10],
                                                              scalar2=None, op0=ALU.mult), reads=[oh1, m], writes=[wc])
                        k.op("dve", lambda e: e.scalar_tensor_tensor(out=wc[:, ti, :], in0=oh2[:, 0:32],
                                                                     scalar=m[:, 10:11], in1=wc[:, ti, :],
                                                                     op0=ALU.mult, op1=ALU.add),
                             reads=[oh2, m, wc], writes=[wc])
                    ntok = n_t * 128
                    pending = [None]
                    for ex in range(32):
                        wg = wgp.next()
                        wu = wup.next()
                        wd = wdp.next()
                        for (dst, srcw) in ((wg, moe_w_gate[layer, ex]), (wu, moe_w_up[layer, ex])):
                            for half in range(2):
                                st = stg.next()
                                k.dma("sp", st[:], srcw[half * 512:(half + 1) * 512, :].rearrange(
                                    "(kc p) n -> p kc n", p=128), writes=[st])
                                cast(dst[:, half * 4:(half + 1) * 4, :], dst, st)
                        for half in range(2):
                            st = stg.next()
                            k.dma("sp", st[:], moe_w_down[layer, ex][:, half * 512:(half + 1) * 512].rearrange(
                                "(kc p) n -> p kc n", p=128), writes=[st])
                            cast(wd[:, :, half * 512:(half + 1) * 512], wd, st)
                        for b0 in range(0, ntok, 512):
                            nb = min(512, ntok - b0)
                            aT = actp.next()
                            for ffc in range(4):
                                fsl = slice(ffc * 128, (ffc + 1) * 128)
                                pg_ = pga.next()
                                pu_ = pup.next()
                                for kc in range(8):
                                    k.op("pe", lambda e: e.matmul(pg_[:, 0:nb], lhsT=wg[:, kc, fsl],
                                                                  rhs=hTs[:, kc, b0:b0 + nb], start=(kc == 0),
                                                                  stop=(kc == 7)), reads=[wg, hTs], writes=[pg_])
                                for kc in range(8):
                                    k.op("pe", lambda e: e.matmul(pu_[:, 0:nb], lhsT=wu[:, kc, fsl],
                                                                  rhs=hTs[:, kc, b0:b0 + nb], start=(kc == 0),
                                                                  stop=(kc == 7)), reads=[wu, hTs], writes=[pu_])
                                sl_ = silp.next()
                                k.op("act", lambda e: e.activation(out=sl_[:, 0:nb], in_=pg_[:, 0:nb], func=AF.Silu),
                                     reads=[pg_], writes=[sl_])
                                k.op("dve", lambda e: e.tensor_tensor(out=aT[:, ffc, 0:nb], in0=sl_[:, 0:nb],
                                                                      in1=pu_[:, 0:nb], op=ALU.mult),
                                     reads=[sl_, pu_], writes=[aT])
                            def mk_down(aT=aT, wd=wd, ex=ex, b0=b0, nb=nb):
                              for tt in range(nb // 128):
                                ti = b0 // 128 + tt
                                pd = pdn.next()
                                for half in range(2):
                                    for ffc in range(4):
                                        k.op("pe", lambda e: e.matmul(pd[:, half * 512:(half + 1) * 512],
                                                                      lhsT=aT[:, ffc, tt * 128:(tt + 1) * 128],
                                                                      rhs=wd[:, ffc, half * 512:(half + 1) * 512],
                                                                      start=(ffc == 0), stop=(ffc == 3)),
                                             reads=[aT, wd], writes=[pd])
                                if ex == 0:
                                    k.op("dve", lambda e: e.tensor_scalar(out=yacc[:, ti, :], in0=pd[:],
                                                                          scalar1=wc[:, ti, ex:ex + 1], scalar2=None,
                                                                          op0=ALU.mult), reads=[pd, wc], writes=[yacc])
                                else:
                                    k.op("dve", lambda e: e.scalar_tensor_tensor(out=yacc[:, ti, :], in0=pd[:],
                                                                                 scalar=wc[:, ti, ex:ex + 1],
                                                                                 in1=yacc[:, ti, :], op0=ALU.mult,
                                                                                 op1=ALU.add),
                                         reads=[pd, wc, yacc], writes=[yacc])
                            if pending[0] is not None:
                                pending[0]()
                            pending[0] = mk_down
                    if pending[0] is not None:
                        pending[0]()
                        pending[0] = None
                    for ti, t in enumerate(stiles):
                        s = 0 if t >= 2 else 1
                        xt = xpool.next()
                        k.dma("sp", xt[:], src[t * 128:(t + 1) * 128, :], reads=[d_src[t]], writes=[xt])
                        k.op("dve", lambda e: e.tensor_tensor(out=yacc[:, ti, :], in0=yacc[:, ti, :], in1=g2bc[:, s, :],
                                                               op=ALU.mult), reads=[yacc, g2bc], writes=[yacc])
                        k.op("dve", lambda e: e.tensor_tensor(out=xt[:], in0=xt[:], in1=yacc[:, ti, :], op=ALU.add),
                             reads=[xt, yacc], writes=[xt])
                        dap, dbuf = dst_fn(t)
                        k.dma("pool", dap, xt[:], reads=[xt], writes=[dbuf])
            k.barrier()

        if stop_after == "moe0":
            d_y = Buf(None, "y")

            def dst0(t):
                if t >= 2:
                    return y_out[(t - 2) * 128:(t - 1) * 128, :], d_y
                return x2[t * 128:(t + 1) * 128, :], d_x2[t]
            moe(0, x1, d_x1, dst0, [0, 1] + list(range(2, NT)))
            k.drain_all()
            return nc

        moe(0, x1, d_x1, lambda t: (x2[t * 128:(t + 1) * 128, :], d_x2[t]), [0, 1] + list(range(2, NT)))
        MLA_SCALE = 96 ** -0.5
        O_Z, O_BETA = 1536, 2048

        def odd_pass(direction):
            fwd = direction == 0
            with ExitStack() as es:
                A1, S1 = load_modcols(es, 1, norm_mix_w[1], 0)
                win = k.sb("owin", [128, 8, 2480], BF16, es)
                cdiag = k.sb("cdiag", [128, 60, 128], BF16, es)
                tri = k.sb("otri", [128, 128], F32, es)
                stri = k.sb("ostri", [128, 128], F32, es)
                allb = k.sb("oallb", [128, 128], F32, es)
                nallb = k.sb("onallb", [128, 128], F32, es)
                self_ = k.sb("oself", [128, 128], F32, es)
                sels_ = k.sb("osels", [128, 128], F32, es)
                aneg_bc = k.sb("aneg", [128, 8], F32, es)
                dtb_bc = k.sb("dtb", [128, 8], F32, es)
                if fwd:
                    wq = k.sb("wq", [128, 2, 768], BF16, es)
                    wkv = k.sb("wkv", [128, 1, 1024], BF16, es)
                    qnw_bc = k.sb("qnw", [128, 256], F32, es)
                    kvnw_bc = k.sb("kvnw", [128, 128], F32, es)
                    qkq_bc = k.sb("qkq", [128, 96], F32, es)
                    qkk_bc = k.sb("qkk", [128, 96], F32, es)
                else:
                    wout_g = k.sb("owout_g", [128, 8, 1024], BF16, es)
                    gnw_bc = k.sb("ognw", [128, 128], F32, es)
                with ExitStack() as es2:
                    stage = k.sbpool("stage", [128, 8, 512], F32, 2, es2)
                    load_w_bf16(es2, win, odd_w_in[0], 8, 2480, stage)
                    cw = k.sb("cw", [128, 12, 5], F32, es2)
                    for kk in range(5):
                        k.dma("sp", cw[:, :, kk], gdn_conv_w[0, kk, :].rearrange("(c p) -> p c", p=128), writes=[cw],
                              allow_slow_non_contiguous=True)
                    for c in range(12):
                        for kk in range(5):
                            k.op("dve" if (c + kk) % 2 else "pool",
                                 lambda e: e.tensor_scalar(out=cdiag[:, c * 5 + kk, :], in0=ident[:],
                                                           scalar1=cw[:, c, kk:kk + 1], scalar2=None, op0=ALU.mult),
                                 reads=[ident, cw], writes=[cdiag])
                    tr_ = blockmask("tri_", "le" if fwd else "ge", es2)
                    al_ = blockmask("all_", "all", es2)
                    k.op("dve", lambda e: e.tensor_copy(out=tri[:], in_=tr_[:]), reads=[tr_], writes=[tri])
                    k.op("dve", lambda e: e.tensor_tensor(out=stri[:], in0=tr_[:], in1=ident[:], op=ALU.subtract),
                         reads=[tr_, ident], writes=[stri])
                    k.op("dve", lambda e: e.tensor_copy(out=allb[:], in_=al_[:]), reads=[al_], writes=[allb])
                    k.op("dve", lambda e: e.tensor_scalar(out=nallb[:], in0=al_[:], scalar1=-1.0, scalar2=None,
                                                          op0=ALU.mult), reads=[al_], writes=[nallb])
                    k.op("pool", lambda e: e.memset(self_[:], 0.0), writes=[self_])
                    k.op("pool", lambda e: e.memset(sels_[:], 0.0), writes=[sels_])
                    f0, s0_ = (0, 64) if fwd else (64, 0)
                    k.op("pool", lambda e: e.tensor_copy(out=self_[f0:f0 + 64, :], in_=ones[f0:f0 + 64, :]),
                         reads=[ones], writes=[self_])
                    k.op("pool", lambda e: e.tensor_copy(out=sels_[s0_:s0_ + 64, :], in_=ones[s0_:s0_ + 64, :]),
                         reads=[ones], writes=[sels_])
                    k.dma("sp", aneg_bc[:], gdn_a_log[0].rearrange("a b -> (a b)").partition_broadcast(128),
                          writes=[aneg_bc])
                    k.op("act", lambda e: e.activation(out=aneg_bc[:], in_=aneg_bc[:], func=AF.Exp), reads=[aneg_bc],
                         writes=[aneg_bc])
                    k.op("dve", lambda e: e.tensor_scalar(out=aneg_bc[:], in0=aneg_bc[:], scalar1=-1.0, scalar2=None,
                                                          op0=ALU.mult), reads=[aneg_bc], writes=[aneg_bc])
                    k.dma("sp", dtb_bc[:], gdn_dt_bias[0].rearrange("a b -> (a b)").partition_broadcast(128),
                          writes=[dtb_bc])
                    if fwd:
                        st = stage.next()
                        k.dma("sp", st[:, 0:2, 0:512], mla_wq_up[0][:, 0:512].rearrange("(kc p) n -> p kc n", p=128),
                              writes=[st])
                        k.op("dve", lambda e: e.tensor_copy(out=wq[:, :, 0:512], in_=st[:, 0:2, 0:512]), reads=[st],
                             writes=[wq])
                        st = stage.next()
                        k.dma("sp", st[:, 0:2, 0:256], mla_wq_up[0][:, 512:768].rearrange("(kc p) n -> p kc n", p=128),
                              writes=[st])
                        k.op("dve", lambda e: e.tensor_copy(out=wq[:, :, 512:768], in_=st[:, 0:2, 0:256]), reads=[st],
                             writes=[wq])
                        for c0 in (0, 512):
                            st = stage.next()
                            k.dma("sp", st[:, 0:1, 0:512], mla_wkv_up[0][:, c0:c0 + 512].rearrange(
                                "(kc p) n -> p kc n", p=128), writes=[st])
                            k.op("dve", lambda e: e.tensor_copy(out=wkv[:, :, c0:c0 + 512], in_=st[:, 0:1, 0:512]),
                                 reads=[st], writes=[wkv])
                        k.dma("sp", qnw_bc[:], mla_q_norm_w[0].partition_broadcast(128), writes=[qnw_bc])
                        k.dma("sp", kvnw_bc[:], mla_kv_norm_w[0].partition_broadcast(128), writes=[kvnw_bc])
                        k.dma("sp", qkq_bc[:], mla_qk_norm_q[0].partition_broadcast(128), writes=[qkq_bc])
                        k.op("dve", lambda e: e.tensor_scalar(out=qkq_bc[:], in0=qkq_bc[:], scalar1=MLA_SCALE,
                                                              scalar2=None, op0=ALU.mult), reads=[qkq_bc],
                             writes=[qkq_bc])
                        k.dma("sp", qkk_bc[:], mla_qk_norm_k[0].partition_broadcast(128), writes=[qkk_bc])
                    else:
                        gbc = k.sb("gbc", [128, 1024], F32, es2)
                        k.dma("sp", gbc[:], modrow[1, 0, 2 * D:3 * D].partition_broadcast(128), reads=[d_modrow],
                              writes=[gbc])
                        for c0 in range(0, 1024, 512):
                            st = stage.next()
                            k.dma("sp", st[:], odd_w_out[0][:, c0:c0 + 512].rearrange("(kc p) n -> p kc n", p=128),
                                  writes=[st])
                            for kc in range(8):
                                k.op("dve" if kc % 2 else "pool",
                                     lambda e: e.tensor_tensor(out=wout_g[:, kc, c0:c0 + 512], in0=st[:, kc, :],
                                                               in1=gbc[:, c0:c0 + 512], op=ALU.mult),
                                     reads=[st, gbc], writes=[wout_g])
                        k.dma("sp", gnw_bc[:], gdn_norm_w[0].partition_broadcast(128), writes=[gnw_bc])
                    k.barrier()

                Sst = k.sb("oSst", [128, 4, 128], F32, es)
                k.op("pool", lambda e: e.memset(Sst[:], 0.0), writes=[Sst])
                xpool = k.sbpool("oxt", [128, 1024], F32, 2, es)
                rpool = k.sbpool("ors", [128, 8], F32, 12, es)
                xnpool = {"junk": k.sbpool("ojunk", [128, 1024], BF16, 1, es),
                          "bf": k.sbpool("oxnb", [128, 1024], BF16, 2, es)}
                hTp = k.sbpool("ohT", [128, 8, 128], BF16, 5, es)
                hextp = k.sbpool("ohext", [128, 8, 132], BF16, 2, es)
                pTp = k.sbpool("opT", [128, 12, 132], BF16, 2, es)
                qkvTp = k.sbpool("oqkvT", [128, 12, 128], BF16, 2, es)
                qkvtokp = k.sbpool("oqkvtok", [128, 1536], F32, 2, es)
                sqp = k.sbpool("osq", [128, 1024], F32, 1, es)
                p2pool = k.sbpool("op2", [128, 432], F32, 2, es)
                qkntokp = k.sbpool("oqkntok", [128, 8, 128], BF16, 2, es)
                qknTp = k.sbpool("oqknT", [128, 8, 128], BF16, 2, es)
                hbufs = []
                for h_ in range(4):
                    d_ = {}
                    for nm in ("TL", "Gm", "t1", "t2", "u32", "oa"):
                        d_[nm] = k.sb("h" + nm, [128, 128], F32, es)
                    for nm in ("qkT", "wb", "wT", "kg", "vn", "S0", "S1"):
                        d_[nm] = k.sb("h" + nm, [128, 128], BF16, es)
                    d_["PT"] = [k.sb("hPT", [128, 128], BF16, es) for _ in range(2)]
                    d_["P"] = [k.sb("hP", [128, 128], BF16, es) for _ in range(2)]
                    d_["X32"] = k.sb("hX32", [128, 256], F32, es)
                    d_["Xb"] = k.sb("hXb", [128, 256], BF16, es)
                    hbufs.append(d_)
                s16 = k.sbpool("os16", [128, 4, 8], F32, 6, es)
                osbp = k.sbpool("oosb", [128, 4, 128], F32, 2, es)
                t512 = k.sbpool("ot512", [128, 512], F32, 5, es)
                pst = k.ps("opst", [128, 8, 128], BF16, es)
                ptb = k.ps("optb", [128, 8, 128], BF16, es)
                ptb_s = [Bv(ptb, ptb.t[:, i, :]) for i in range(8)]
                proj = k.pspool("oproj", [128, 512], F32, 2, es)
                pa = k.ps("opa", [128, 512], F32, es)
                pa_s = [Bv(pa, pa.t[:, i * 128:(i + 1) * 128]) for i in range(4)]
                pn = k.ps("opn", [128, 512], F32, es)
                pn_s = [Bv(pn, pn.t[:, i * 128:(i + 1) * 128]) for i in range(4)]
                pb = k.ps("opb", [128, 512], F32, es)
                pb_s = [Bv(pb, pb.t[:, i * 128:(i + 1) * 128]) for i in range(4)]
                pc = k.ps("opc", [128, 512], F32, es)
                pc_s = [Bv(pc, pc.t[:, i * 128:(i + 1) * 128]) for i in range(4)]
                if fwd:
                    qtp = k.sbpool("oqt", [96, 8, 128], BF16, 2, es)
                    vxp = k.sbpool("ovx", [128, 8, 80], BF16, 2, es)
                    for vb_ in vxp.bufs:
                        k.op("pool", lambda e: e.memset(vb_[:], 1.0), writes=[vb_])
                    q96p = k.sbpool("oq96", [128, 8, 96], F32, 2, es)
                    q96bp = k.sbpool("oq96b", [128, 8, 96], BF16, 2, es)
                    ropep = k.sbpool("orope", [128, 2, 16], F32, 2, es)
                    r8p = k.sbpool("or8", [128, 8, 8], F32, 8, es)
                    cqnp = k.sbpool("ocqn", [128, 256], BF16, 2, es)
                    kvsp = k.sbpool("okvs", [128, 512], F32, 2, es)
                    cqnTp = k.sbpool("ocqnT", [128, 3, 128], BF16, 2, es)
                else:
                    catp = k.sbpool("ocat", [128, 512], BF16, 2, es)
                    catTp = k.sbpool("ocatT", [128, 8, 128], BF16, 2, es)

                hcache = {}

                def get_hT(t):
                    if t in hcache:
                        return hcache[t]
                    s = 0 if t >= 2 else 1
                    xt = xpool.next()
                    k.dma("sp", xt[:], x2[t * 128:(t + 1) * 128, :], reads=[d_x2[t]], writes=[xt])
                    hT = hTp.next()
                    for kk_ in [kk_ for kk_, v_ in hcache.items() if v_ is hT]:
                        del hcache[kk_]
                    norm_T(xt, A1, S1, s, rpool, xnpool, pst, out_bf=hT)
                    hcache[t] = hT
                    return hT

                def rope(buf, tabs):
                    for part in range(2):
                        o = 64 + part * 16
                        u1 = buf[:, :, o:o + 8]
                        u2 = buf[:, :, o + 8:o + 16]
                        cs = tabs[:, 0, part * 8:(part + 1) * 8].unsqueeze(1).to_broadcast([128, 8, 8])
                        sn = tabs[:, 1, part * 8:(part + 1) * 8].unsqueeze(1).to_broadcast([128, 8, 8])
                        a = r8p.next(); b = r8p.next(); c_ = r8p.next(); d_ = r8p.next()
                        k.op("dve", lambda e: e.tensor_tensor(out=a[:], in0=u1, in1=cs, op=ALU.mult), reads=[buf, tabs],
                             writes=[a])
                        k.op("dve", lambda e: e.tensor_tensor(out=b[:], in0=u2, in1=sn, op=ALU.mult), reads=[buf, tabs],
                             writes=[b])
                        k.op("dve", lambda e: e.tensor_tensor(out=c_[:], in0=u2, in1=cs, op=ALU.mult), reads=[buf, tabs],
                             writes=[c_])
                        k.op("dve", lambda e: e.tensor_tensor(out=d_[:], in0=u1, in1=sn, op=ALU.mult), reads=[buf, tabs],
                             writes=[d_])
                        k.op("dve", lambda e: e.tensor_tensor(out=u1, in0=a[:], in1=b[:], op=ALU.subtract),
                             reads=[a, b], writes=[buf])
                        k.op("dve", lambda e: e.tensor_tensor(out=u2, in0=c_[:], in1=d_[:], op=ALU.add),
                             reads=[c_, d_], writes=[buf])

                order = list(range(NT)) if fwd else [1, 0] + list(range(NT - 1, 1, -1))
                R1, R2 = (slice(0, 64), slice(64, 128)) if fwd else (slice(64, 128), slice(0, 64))

                def tile_A(t, cx):
                    lat = t >= 2
                    has_prev = t not in (0, 2)
                    has_next = t not in (1, NT - 1)
                    hT = get_hT(t)
                    hprev = get_hT(t - 1) if has_prev else None
                    hnext = get_hT(t + 1) if has_next else None
                    hext = hextp.next()
                    k.op("act", lambda e: e.copy(out=hext[:, :, 2:130], in_=hT[:]), reads=[hT], writes=[hext])
                    yield
                    if has_prev:
                        k.op("dve", lambda e: e.tensor_copy(out=hext[:, :, 0:2], in_=hprev[:, :, 126:128]),
                             reads=[hprev], writes=[hext])
                        yield
                    else:
                        k.op("dve", lambda e: e.memset(hext[:, :, 0:2], 0.0), writes=[hext])
                        yield
                    if has_next:
                        k.op("dve", lambda e: e.tensor_copy(out=hext[:, :, 130:132], in_=hnext[:, :, 0:2]),
                             reads=[hnext], writes=[hext])
                        yield
                    else:
                        k.op("dve", lambda e: e.memset(hext[:, :, 130:132], 0.0), writes=[hext])
                        yield

                    def proj_tok(c0, n):
                        p = proj.next()
                        for kc in range(8):
                            k.op("pe", lambda e: e.matmul(p[:, 0:n], lhsT=hT[:, kc, :], rhs=win[:, kc, c0:c0 + n],
                                                          start=(kc == 0), stop=(kc == 7)), reads=[hT, win], writes=[p])
                        return p

                    yield
                    pT = pTp.next()
                    for g3 in range(4):
                        p = proj.next()
                        pv = p[:, 0:396].rearrange("p (c n) -> p c n", n=132)
                        for ci in range(3):
                            c = g3 * 3 + ci
                            for kc in range(8):
                                k.op("pe", lambda e: e.matmul(pv[:, ci, :], lhsT=win[:, kc, c * 128:(c + 1) * 128],
                                                              rhs=hext[:, kc, :], start=(kc == 0), stop=(kc == 7)),
                                     reads=[win, hext], writes=[p])
                                yield
                        k.op("act", lambda e: e.copy(out=pT[:, g3 * 3:(g3 + 1) * 3, :], in_=pv), reads=[p], writes=[pT])
                        yield
                    qkvT = qkvTp.next()
                    yield
                    for g4 in range(3):
                        p = proj.next()
                        for ci in range(4):
                            c = g4 * 4 + ci
                            for kk in range(5):
                                k.op("pe", lambda e: e.matmul(p[:, ci * 128:(ci + 1) * 128], lhsT=cdiag[:, c * 5 + kk, :],
                                                              rhs=pT[:, c, kk:kk + 128], start=(kk == 0), stop=(kk == 4)),
                                     reads=[cdiag, pT], writes=[p])
                                yield
                        k.op("act", lambda e: e.activation(out=qkvT[:, g4 * 4:(g4 + 1) * 4, :].rearrange("p c n -> p (c n)"),
                                                           in_=p[:], func=AF.Silu), reads=[p], writes=[qkvT])
                        yield
                    qkvtok = qkvtokp.next()
                    for c in range(8):
                        k.op("pe", lambda e: e.transpose(pst[:, c, :], qkvT[:, c, :], identb[:]), reads=[qkvT, identb],
                             writes=[pst])
                        yield
                    k.op("act", lambda e: e.copy(out=qkvtok[:, 0:1024], in_=pst[:].rearrange("p c n -> p (c n)")),
                         reads=[pst], writes=[qkvtok])
                    yield
                    for c in range(4):
                        k.op("pe", lambda e: e.transpose(pst[:, c, :], qkvT[:, 8 + c, :], identb[:]),
                             reads=[qkvT, identb], writes=[pst])
                        yield
                    k.op("act", lambda e: e.copy(out=qkvtok[:, 1024:1536],
                                                 in_=pst[:, 0:4, :].rearrange("p c n -> p (c n)")), reads=[pst],
                         writes=[qkvtok])
                    yield
                    yield
                    sq = sqp.next()
                    k.op("act", lambda e: e.activation(out=sq[:, 0:1024], in_=qkvtok[:, 0:1024], func=AF.Square),
                         reads=[qkvtok], writes=[sq])
                    yield
                    rn = rpool.next()
                    k.op("dve", lambda e: e.tensor_reduce(out=rn[:], in_=sq[:, 0:1024].rearrange("p (h c) -> p h c", c=128),
                                                          axis=AX.X, op=ALU.add), reads=[sq], writes=[rn])
                    yield
                    k.op("dve", lambda e: e.tensor_scalar(out=rn[:], in0=rn[:], scalar1=EPS, scalar2=None, op0=ALU.add),
                         reads=[rn], writes=[rn])
                    yield
                    rsqrt(rn, rn[:], rn[:])
                    k.op("dve", lambda e: e.tensor_scalar(out=rn[:, 0:4], in0=rn[:, 0:4], scalar1=128 ** -0.5,
                                                          scalar2=None, op0=ALU.mult), reads=[rn], writes=[rn])
                    yield
                    qkntok = qkntokp.next()
                    yield
                    k.op("dve", lambda e: e.tensor_tensor(out=qkntok[:],
                                                          in0=qkvtok[:, 0:1024].rearrange("p (h c) -> p h c", c=128),
                                                          in1=rn[:].unsqueeze(2).to_broadcast([128, 8, 128]),
                                                          op=ALU.mult), reads=[qkvtok, rn], writes=[qkntok])
                    yield
                    for c in range(8):
                        k.op("pe", lambda e: e.transpose(pst[:, c, :], qkntok[:, c, :], identb[:]),
                             reads=[qkntok, identb], writes=[pst])
                        yield
                    qknT = qknTp.next()
                    yield
                    k.op("act", lambda e: e.copy(out=qknT[:], in_=pst[:]), reads=[pst], writes=[qknT])
                    yield
                    p2p = proj_tok(O_BETA, 432)
                    yield
                    p2 = p2pool.next()
                    k.op("act", lambda e: e.copy(out=p2[:, 0:432], in_=p2p[:, 0:432]), reads=[p2p], writes=[p2])
                    yield
                    bl = s16.next()
                    blv = bl[:].rearrange("p a b -> p (a b)")
                    k.op("act", lambda e: e.activation(out=blv[:, 0:8], in_=p2[:, 0:8], func=AF.Sigmoid), reads=[p2],
                         writes=[bl])
                    yield
                    sp_ = s16.next()
                    spv = sp_[:].rearrange("p a b -> p (a b)")
                    k.op("dve", lambda e: e.tensor_tensor(out=spv[:, 0:8], in0=p2[:, 8:16], in1=dtb_bc[:], op=ALU.add),
                         reads=[p2, dtb_bc], writes=[sp_])
                    yield
                    k.op("act", lambda e: e.activation(out=spv[:, 8:16], in_=spv[:, 0:8], func=AF.Abs), reads=[sp_],
                         writes=[sp_])
                    yield
                    k.op("act", lambda e: e.activation(out=spv[:, 8:16], in_=spv[:, 8:16], func=AF.Exp, scale=-1.0),
                         reads=[sp_], writes=[sp_])
                    yield
                    k.op("act", lambda e: e.activation(out=spv[:, 8:16], in_=spv[:, 8:16], func=AF.Ln, bias=1.0),
                         reads=[sp_], writes=[sp_])
                    yield
                    k.op("dve", lambda e: e.tensor_scalar(out=spv[:, 0:8], in0=spv[:, 0:8], scalar1=0.0, scalar2=None,
                                                          op0=ALU.max), reads=[sp_], writes=[sp_])
                    yield
                    k.op("dve", lambda e: e.tensor_tensor(out=spv[:, 0:8], in0=spv[:, 0:8], in1=spv[:, 8:16],
                                                          op=ALU.add), reads=[sp_], writes=[sp_])
                    yield
                    k.op("dve", lambda e: e.tensor_tensor(out=blv[:, 8:16], in0=spv[:, 0:8], in1=aneg_bc[:],
                                                          op=ALU.mult), reads=[sp_, aneg_bc], writes=[bl])
                    yield
                    dsl = slice(direction * 4, direction * 4 + 4)
                    yield
                    la4 = bl[:, 1, dsl]
                    be4 = bl[:, 0, dsl]
                    pg = pa_s[3]
                    for i_, lh in enumerate((tri, allb, self_, sels_)):
                        k.op("pe", lambda e: e.matmul(pg[:, i_ * 4:(i_ + 1) * 4], lhsT=lh[:], rhs=la4, start=True,
                                                      stop=True), reads=[lh, bl], writes=[pg])
                        yield
                    E4 = s16.next()
                    k.op("dve", lambda e: e.tensor_copy(out=E4[:, :, 0:4], in_=pg[:, 0:16].rearrange("p (a b) -> p a b", b=4)),
                         reads=[pg], writes=[E4])
                    yield
                    k.op("dve", lambda e: e.tensor_tensor(out=E4[:, 1, 0:4], in0=E4[:, 1, 0:4], in1=E4[:, 0, 0:4],
                                                          op=ALU.subtract), reads=[E4], writes=[E4])
                    yield
                    k.op("act", lambda e: e.activation(out=E4[:, :, 0:4], in_=E4[:, :, 0:4], func=AF.Exp), reads=[E4],
                         writes=[E4])
                    yield
                    cx.update(dict(lat=lat, hT=hT, qkvtok=qkvtok, qkntok=qkntok, qknT=qknT, bl=bl, la4=la4, be4=be4,
                                   E4=E4, p2=p2))
                    yield

                def tile_B(t, cx):
                    lat = cx["lat"]; hT = cx["hT"]; qkvtok = cx["qkvtok"]; qkntok = cx["qkntok"]; qknT = cx["qknT"]
                    bl = cx["bl"]; la4 = cx["la4"]; be4 = cx["be4"]; E4 = cx["E4"]; p2 = cx["p2"]

                    def proj_tok(c0, n):
                        p = proj.next()
                        for kc in range(8):
                            k.op("pe", lambda e: e.matmul(p[:, 0:n], lhsT=hT[:, kc, :], rhs=win[:, kc, c0:c0 + n],
                                                          start=(kc == 0), stop=(kc == 7)), reads=[hT, win], writes=[p])
                        return p

                    osb = osbp.next()
                    HB = hbufs
                    yield
                    for h in range(4):
                        B_ = HB[h]
                        knT_h = qknT[:, 4 + h, :]
                        qnT_h = qknT[:, h, :]
                        la_c = la4[:, h:h + 1]
                        be_c = be4[:, h:h + 1]
                        eg_c = E4[:, 0, h:h + 1]
                        TL = B_["TL"]
                        k.op("dve", lambda e: e.tensor_scalar(out=TL[:], in0=tri[:], scalar1=la_c, scalar2=None,
                                                               op0=ALU.mult), reads=[tri, bl], writes=[TL])
                        yield
                        reg = pa_s if h % 2 == 0 else pc_s
                        GT, KK, QK = reg[0], reg[1], reg[2]
                        k.op("pe", lambda e: e.matmul(GT[:], lhsT=allb[:], rhs=TL[:], start=True, stop=False),
                             reads=[allb, TL], writes=[GT])
                        yield
                        k.op("pe", lambda e: e.matmul(GT[:], lhsT=TL[:], rhs=nallb[:], start=False, stop=True),
                             reads=[nallb, TL], writes=[GT])
                        yield
                        k.op("pe", lambda e: e.matmul(KK[:], lhsT=knT_h, rhs=knT_h, start=True, stop=True),
                             reads=[qknT], writes=[KK])
                        yield
                        k.op("pe", lambda e: e.matmul(QK[:], lhsT=knT_h, rhs=qnT_h, start=True, stop=True),
                             reads=[qknT], writes=[QK])
                        yield
                        Gm = B_["Gm"]
                        k.op("dve", lambda e: e.tensor_scalar(out=Gm[:], in0=GT[:], scalar1=0.0, scalar2=None,
                                                              op0=ALU.min), reads=[GT], writes=[Gm])
                        yield
                        k.op("act", lambda e: e.activation(out=Gm[:], in_=Gm[:], func=AF.Exp), reads=[Gm], writes=[Gm])
                        yield
                        t1 = B_["t1"]
                        k.op("dve", lambda e: e.tensor_tensor(out=t1[:], in0=KK[:], in1=Gm[:], op=ALU.mult),
                             reads=[KK, Gm], writes=[t1])
                        yield
                        PT = B_["PT"][0]
                        k.op("dve", lambda e: e.scalar_tensor_tensor(out=PT[:], in0=t1[:], scalar=be_c, in1=stri[:],
                                                                     op0=ALU.mult, op1=ALU.mult),
                             reads=[t1, bl, stri], writes=[PT])
                        yield
                        t2 = B_["t2"]
                        k.op("dve", lambda e: e.tensor_tensor(out=t2[:], in0=QK[:], in1=Gm[:], op=ALU.mult),
                             reads=[QK, Gm], writes=[t2])
                        yield
                        qkT = B_["qkT"]
                        k.op("dve", lambda e: e.tensor_tensor(out=qkT[:], in0=t2[:], in1=tri[:], op=ALU.mult),
                             reads=[t2, tri], writes=[qkT])
                        yield
                        X32 = B_["X32"]
                        Xb = B_["Xb"]
                        k.op("act", lambda e: e.copy(out=X32[:, 0:128], in_=qkvtok[:, 1024 + h * 128:1024 + (h + 1) * 128]),
                             reads=[qkvtok], writes=[X32])
                        yield
                        k.op("dve", lambda e: e.tensor_scalar(out=X32[:, 128:256], in0=qkntok[:, 4 + h, :], scalar1=eg_c,
                                                               scalar2=None, op0=ALU.mult), reads=[qkntok, E4],
                             writes=[X32])
                        yield
                        k.op("act", lambda e: e.copy(out=Xb[:], in_=X32[:]), reads=[X32], writes=[Xb])
                    yield
                    for h in range(4):
                        B_ = HB[h]
                        ptp = ptb_s[h]
                        k.op("pe", lambda e: e.transpose(ptp[:], B_["PT"][0][:], identb[:]),
                             reads=[B_["PT"][0], identb], writes=[ptp])
                        yield
                        k.op("act", lambda e: e.copy(out=B_["P"][0][:], in_=ptp[:]), reads=[ptp], writes=[B_["P"][0]])
                        yield
                    xreg = [(pn_s[0], pn_s[1]), (pn_s[2], pn_s[3]), (pb_s[0], pb_s[1]), (pb_s[2], pb_s[3])]
                    sreg = [(pa_s[0], pc_s[0]), (pa_s[1], pc_s[1]), (pa_s[2], pc_s[2]), (pa_s[3], pc_s[3])]
                    cur = [0, 0, 0, 0]

                    def x_update(h, sub):
                        B_ = HB[h]
                        PT = B_["PT"][cur[h]]
                        xa, xb_ = xreg[h]
                        k.op("pe", lambda e: e.matmul(xa[:], lhsT=PT[:], rhs=B_["Xb"][:, 0:128], start=True, stop=True),
                             reads=[PT, B_["Xb"]], writes=[xa])
                        k.op("pe", lambda e: e.matmul(xb_[:], lhsT=PT[:], rhs=B_["Xb"][:, 128:256], start=True,
                                                      stop=True), reads=[PT, B_["Xb"]], writes=[xb_])

                    def x_apply(h, sub, last):
                        B_ = HB[h]
                        xa, xb_ = xreg[h]
                        op_ = ALU.subtract if sub else ALU.add
                        k.op("dve", lambda e: e.tensor_tensor(out=B_["X32"][:, 0:128], in0=B_["X32"][:, 0:128], in1=xa[:],
                                                              op=op_), reads=[B_["X32"], xa], writes=[B_["X32"]])
                        k.op("dve", lambda e: e.tensor_tensor(out=B_["X32"][:, 128:256], in0=B_["X32"][:, 128:256],
                                                              in1=xb_[:], op=op_), reads=[B_["X32"], xb_],
                             writes=[B_["X32"]])
                        if not last:
                            k.op("act", lambda e: e.copy(out=B_["Xb"][:], in_=B_["X32"][:]), reads=[B_["X32"]],
                                 writes=[B_["Xb"]])

                    yield
                    for h in range(4):
                        x_update(h, True)
                    yield
                    for h in range(4):
                        x_apply(h, True, False)
                    for lvl in range(1, 6):
                        yield
                        for h in range(4):
                            B_ = HB[h]
                            c_ = cur[h]
                            P, PT = B_["P"][c_], B_["PT"][c_]
                            sa, sb_ = sreg[h]
                            k.op("pe", lambda e: e.matmul(sa[:], lhsT=P[:], rhs=PT[:], start=True, stop=True),
                                 reads=[P, PT], writes=[sa])
                            yield
                            if lvl < 5:
                                k.op("pe", lambda e: e.matmul(sb_[:], lhsT=PT[:], rhs=P[:], start=True, stop=True),
                                     reads=[P, PT], writes=[sb_])
                        yield
                        for h in range(4):
                            B_ = HB[h]
                            n_ = 1 - cur[h]
                            sa, sb_ = sreg[h]
                            k.op("act", lambda e: e.copy(out=B_["PT"][n_][:], in_=sa[:]), reads=[sa],
                                 writes=[B_["PT"][n_]])
                            yield
                            if lvl < 5:
                                k.op("dve", lambda e: e.tensor_copy(out=B_["P"][n_][:], in_=sb_[:]), reads=[sb_],
                                     writes=[B_["P"][n_]])
                                yield
                            cur[h] = n_
                        yield
                        for h in range(4):
                            x_update(h, False)
                        yield
                        for h in range(4):
                            x_apply(h, False, lvl == 5)
                    yield
                    for h in range(4):
                        B_ = HB[h]
                        be_c = be4[:, h:h + 1]
                        egl_c = E4[:, 1, h:h + 1]
                        k.op("dve", lambda e: e.tensor_scalar(out=B_["u32"][:], in0=B_["X32"][:, 0:128], scalar1=be_c,
                                                              scalar2=None, op0=ALU.mult), reads=[B_["X32"], bl],
                             writes=[B_["u32"]])
                        yield
                        k.op("dve", lambda e: e.tensor_scalar(out=B_["wb"][:], in0=B_["X32"][:, 128:256], scalar1=be_c,
                                                              scalar2=None, op0=ALU.mult), reads=[B_["X32"], bl],
                             writes=[B_["wb"]])
                        yield
                        k.op("dve", lambda e: e.tensor_scalar(out=B_["kg"][:], in0=qkntok[:, 4 + h, :], scalar1=egl_c,
                                                               scalar2=None, op0=ALU.mult), reads=[qkntok, E4],
                             writes=[B_["kg"]])
                        yield
                        k.op("act", lambda e: e.copy(out=B_["S0"][:], in_=Sst[:, h, :]), reads=[Sst], writes=[B_["S0"]])
                    yield
                    for h in range(4):
                        B_ = HB[h]
                        wtp = ptb_s[4 + h]
                        k.op("pe", lambda e: e.transpose(wtp[:], B_["wb"][:], identb[:]), reads=[B_["wb"], identb],
                             writes=[wtp])
                        yield
                        k.op("act", lambda e: e.copy(out=B_["wT"][:], in_=wtp[:]), reads=[wtp], writes=[B_["wT"]])
                        yield
                    banks = [pa_s, pn_s, pb_s, pc_s]
                    yield
                    for h in range(4):
                        B_ = HB[h]
                        r0, r1 = banks[h][0], banks[h][1]
                        k.op("pe", lambda e: e.matmul(r0[:], lhsT=B_["wT"][:], rhs=B_["S0"][:], start=True, stop=True),
                             reads=[B_["wT"], B_["S0"]], writes=[r0])
                    yield
                    for h in range(4):
                        B_ = HB[h]
                        r0, r1 = banks[h][0], banks[h][1]
                        k.op("dve", lambda e: e.tensor_tensor(out=B_["vn"][R1, :], in0=B_["u32"][R1, :], in1=r0[R1, :],
                                                              op=ALU.subtract), reads=[B_["u32"], r0], writes=[B_["vn"]])
                        yield
                        k.op("pe", lambda e: e.matmul(r1[:], lhsT=B_["kg"][R1, :], rhs=B_["vn"][R1, :], start=True,
                                                      stop=True), reads=[B_["kg"], B_["vn"]], writes=[r1])
                    yield
                    for h in range(4):
                        B_ = HB[h]
                        r0, r1 = banks[h][0], banks[h][1]
                        k.op("dve", lambda e: e.scalar_tensor_tensor(out=Sst[:, h, :], in0=Sst[:, h, :],
                                                                     scalar=E4[:, 2, h:h + 1], in1=r1[:],
                                                                     op0=ALU.mult, op1=ALU.add),
                             reads=[Sst, E4, r1], writes=[Sst])
                        yield
                        k.op("act", lambda e: e.copy(out=B_["S1"][:], in_=Sst[:, h, :]), reads=[Sst], writes=[B_["S1"]])
                        yield
                        k.op("pe", lambda e: e.matmul(r0[:], lhsT=B_["wT"][:], rhs=B_["S1"][:], start=True, stop=True),
                             reads=[B_["wT"], B_["S1"]], writes=[r0])
                    yield
                    for h in range(4):
                        B_ = HB[h]
                        r0, r1 = banks[h][0], banks[h][1]
                        k.op("dve", lambda e: e.tensor_tensor(out=B_["vn"][R2, :], in0=B_["u32"][R2, :], in1=r0[R2, :],
                                                              op=ALU.subtract), reads=[B_["u32"], r0], writes=[B_["vn"]])
                        yield
                        k.op("pe", lambda e: e.matmul(r1[:], lhsT=B_["kg"][R2, :], rhs=B_["vn"][R2, :], start=True,
                                                      stop=True), reads=[B_["kg"], B_["vn"]], writes=[r1])
                    yield
                    for h in range(4):
                        B_ = HB[h]
                        r0, r1, r2, r3 = banks[h]
                        qnT_h = qknT[:, h, :]
                        k.op("dve", lambda e: e.scalar_tensor_tensor(out=Sst[:, h, :], in0=Sst[:, h, :],
                                                                     scalar=E4[:, 3, h:h + 1], in1=r1[:],
                                                                     op0=ALU.mult, op1=ALU.add),
                             reads=[Sst, E4, r1], writes=[Sst])
                        yield
                        k.op("pe", lambda e: e.matmul(r0[:], lhsT=B_["qkT"][:], rhs=B_["vn"][:], start=True, stop=True),
                             reads=[B_["qkT"], B_["vn"]], writes=[r0])
                        yield
                        k.op("pe", lambda e: e.matmul(r2[:], lhsT=qnT_h, rhs=B_["S0"][:], start=True, stop=True),
                             reads=[qknT, B_["S0"]], writes=[r2])
                        yield
                        k.op("pe", lambda e: e.matmul(r3[:], lhsT=qnT_h, rhs=B_["S1"][:], start=True, stop=True),
                             reads=[qknT, B_["S1"]], writes=[r3])
                    yield
                    for h in range(4):
                        B_ = HB[h]
                        r0, r1, r2, r3 = banks[h]
                        oa = B_["oa"]
                        k.op("act", lambda e: e.copy(out=oa[:], in_=r0[:]), reads=[r0], writes=[oa])
                        yield
                        k.op("dve", lambda e: e.scalar_tensor_tensor(out=osb[R1, h, :], in0=r2[R1, :],
                                                                     scalar=E4[R1, 0, h:h + 1], in1=oa[R1, :],
                                                                     op0=ALU.mult, op1=ALU.add),
                             reads=[r2, E4, oa], writes=[osb])
                        yield
                        k.op("dve", lambda e: e.scalar_tensor_tensor(out=osb[R2, h, :], in0=r3[R2, :],
                                                                     scalar=E4[R2, 0, h:h + 1], in1=oa[R2, :],
                                                                     op0=ALU.mult, op1=ALU.add),
                             reads=[r3, E4, oa], writes=[osb])
                        yield
                    osbv = osb[:].rearrange("p h c -> p (h c)")
                    if fwd:
                        if lat:
                            k.dma("pool", of_scr[t * 128:(t + 1) * 128, :], osbv, reads=[osb], writes=[d_of[t]])
                            yield
                        if os.environ.get("SKIP_MLA"):
                            return
                        tabs = None
                        if lat:
                            tabs = ropep.next()
                            n0 = (t - 2) * 128
                            k.dma("sp", tabs[:, 0, :], rope_cos[n0:n0 + 128, :], writes=[tabs])
                            yield
                            k.dma("sp", tabs[:, 1, :], rope_sin[n0:n0 + 128, :], writes=[tabs])
                            yield
                        jk = t512.next()
                        r1 = rpool.next()
                        k.op("act", lambda e: e.activation(out=jk[:, 0:128], in_=p2[:, 272:400], func=AF.Square,
                                                           accum_out=r1[:, 0:1]), reads=[p2], writes=[jk, r1])
                        yield
                        k.op("dve", lambda e: e.tensor_scalar(out=r1[:, 1:2], in0=r1[:, 0:1], scalar1=1.0 / 128,
                                                              scalar2=EPS, op0=ALU.mult, op1=ALU.add), reads=[r1],
                             writes=[r1])
                        yield
                        rsqrt(r1, r1[:, 2:3], r1[:, 1:2])
                        cn = cqnp.next()
                        k.op("dve", lambda e: e.scalar_tensor_tensor(out=cn[:, 0:128], in0=p2[:, 272:400],
                                                                     scalar=r1[:, 2:3], in1=kvnw_bc[:], op0=ALU.mult,
                                                                     op1=ALU.mult), reads=[p2, r1, kvnw_bc], writes=[cn])
                        yield
                        cnT = cqnTp.next()
                        k.op("pe", lambda e: e.transpose(ptb_s[0][:], cn[:, 0:128], identb[:]), reads=[cn, identb],
                             writes=[ptb_s[0]])
                        yield
                        k.op("act", lambda e: e.copy(out=cnT[:, 2, :], in_=ptb_s[0][:]), reads=[ptb_s[0]], writes=[cnT])
                        yield
                        MSUB = int(os.environ.get("MLA_SUB", "9"))
                        if MSUB < 2:
                            return
                        yield
                        k96 = q96p.next()
                        vx = vxp.next()
                        for half in range(2):
                            pk = proj.next()
                            k.op("pe", lambda e: e.matmul(pk[:], lhsT=cnT[:, 2, :], rhs=wkv[:, 0, half * 512:(half + 1) * 512],
                                                          start=True, stop=True), reads=[cnT, wkv], writes=[pk])
                            yield
                            pkv = pk[:].rearrange("p (h c) -> p h c", c=128)
                            hs4 = slice(half * 4, half * 4 + 4)
                            kvs = kvsp.next()
                            k.op("act", lambda e: e.copy(out=kvs[:], in_=pk[:]), reads=[pk], writes=[kvs])
                            yield
                            kvsv = kvs[:].rearrange("p (h c) -> p h c", c=128)
                            k.op("act", lambda e: e.copy(out=k96[:, hs4, 0:64], in_=kvsv[:, :, 0:64]), reads=[kvs],
                                 writes=[k96])
                            yield
                            k.op("dve", lambda e: e.tensor_copy(out=vx[:, hs4, 0:64], in_=kvsv[:, :, 64:128]), reads=[kvs],
                                 writes=[vx])
                            yield
                        if MSUB < 3:
                            return
                        k.op("dve", lambda e: e.tensor_copy(out=k96[:, :, 64:96],
                                                            in_=p2[:, 400:432].unsqueeze(1).to_broadcast([128, 8, 32])),
                             reads=[p2], writes=[k96])
                        yield
                        if MSUB < 4:
                            return
                        k.dma("sp", VX[:, t, :, :].rearrange("h p c -> p h c"), vx[:], reads=[vx], writes=[d_VX])
                        yield

                        def headnorm(buf, wbc):
                            sqh = q96p.next()
                            k.op("act", lambda e: e.activation(out=sqh[:], in_=buf[:], func=AF.Square),
                                 reads=[buf], writes=[sqh])
                            r8 = rpool.next()
                            k.op("dve", lambda e: e.tensor_reduce(out=r8[:], in_=sqh[:], axis=AX.X, op=ALU.add),
                                 reads=[sqh], writes=[r8])
                            k.op("dve", lambda e: e.tensor_scalar(out=r8[:], in0=r8[:], scalar1=1.0 / 96, scalar2=EPS,
                                                                  op0=ALU.mult, op1=ALU.add), reads=[r8], writes=[r8])
                            rsqrt(r8, r8[:], r8[:])
                            k.op("dve", lambda e: e.tensor_tensor(out=buf[:], in0=buf[:],
                                                                  in1=r8[:].unsqueeze(2).to_broadcast([128, 8, 96]),
                                                                  op=ALU.mult), reads=[buf, r8], writes=[buf])
                            k.op("dve", lambda e: e.tensor_tensor(out=buf[:], in0=buf[:],
                                                                   in1=wbc[:].unsqueeze(1).to_broadcast([128, 8, 96]),
                                                                   op=ALU.mult), reads=[buf, wbc], writes=[buf])

                        def heads_T(buf, dram_ap, dbuf):
                            bb = q96bp.next()
                            k.op("act", lambda e: e.copy(out=bb[:], in_=buf[:]), reads=[buf], writes=[bb])
                            for hh in range(8):
                                k.op("pe", lambda e: e.transpose(pst[0:96, hh, :], bb[:, hh, :], identb[:]),
                                     reads=[bb, identb], writes=[pst])
                            qt = qtp.next()
                            k.op("act", lambda e: e.copy(out=qt[:], in_=pst[0:96, :, :]), reads=[pst], writes=[qt])
                            k.dma("sp", dram_ap, qt[:], reads=[qt], writes=[dbuf])

                        MST = int(os.environ.get("MLA_STAGE", "9"))
                        if MST < 2:
                            return
                        yield
                        headnorm(k96, qkk_bc)
                        if lat:
                            rope(k96, tabs)
                        if MST < 3:
                            return
                        heads_T(k96, KT[:, :, t * 128:(t + 1) * 128].rearrange("h d n -> d h n"), d_KT)
                        if lat and MST >= 4:
                            r2 = rpool.next()
                            k.op("act", lambda e: e.activation(out=jk[:, 0:256], in_=p2[:, 16:272], func=AF.Square,
                                                               accum_out=r2[:, 0:1]), reads=[p2], writes=[jk, r2])
                            yield
                            k.op("dve", lambda e: e.tensor_scalar(out=r2[:, 1:2], in0=r2[:, 0:1], scalar1=1.0 / 256,
                                                                  scalar2=EPS, op0=ALU.mult, op1=ALU.add), reads=[r2],
                                 writes=[r2])
                            yield
                            rsqrt(r2, r2[:, 2:3], r2[:, 1:2])
                            cq = cqnp.next()
                            k.op("dve", lambda e: e.scalar_tensor_tensor(out=cq[:], in0=p2[:, 16:272], scalar=r2[:, 2:3],
                                                                         in1=qnw_bc[:], op0=ALU.mult, op1=ALU.mult),
                                 reads=[p2, r2, qnw_bc], writes=[cq])
                            yield
                            cqT = cqnTp.next()
                            for c in range(2):
                                k.op("pe", lambda e: e.transpose(ptb_s[1 + c][:], cq[:, c * 128:(c + 1) * 128], identb[:]),
                                     reads=[cq, identb], writes=[ptb_s[1 + c]])
                                yield
                                k.op("act", lambda e: e.copy(out=cqT[:, c, :], in_=ptb_s[1 + c][:]), reads=[ptb_s[1 + c]],
                                     writes=[cqT])
                            yield
                            q96 = q96p.next()
                            q96v = q96[:].rearrange("p h c -> p (h c)")
                            for (c0, n) in ((0, 512), (512, 256)):
                                pq = proj.next()
                                for c in range(2):
                                    k.op("pe", lambda e: e.matmul(pq[:, 0:n], lhsT=cqT[:, c, :], rhs=wq[:, c, c0:c0 + n],
                                                                  start=(c == 0), stop=(c == 1)), reads=[cqT, wq],
                                         writes=[pq])
                                    yield
                                k.op("act", lambda e: e.copy(out=q96v[:, c0:c0 + n], in_=pq[:, 0:n]), reads=[pq],
                                     writes=[q96])
                                yield
                            headnorm(q96, qkq_bc)
                            rope(q96, tabs)
                            n0 = (t - 2) * 128
                            heads_T(q96, QT[:, :, n0:n0 + 128].rearrange("h d n -> d h n"), d_QT)
                        return
                    if not lat:
                        return
                    ofl = t512.next()
                    k.dma("sp", ofl[:], of_scr[t * 128:(t + 1) * 128, :], reads=[d_of[t]], writes=[ofl])
                    yield
                    k.op("dve", lambda e: e.tensor_tensor(out=osbv, in0=osbv, in1=ofl[:], op=ALU.add), reads=[osb, ofl],
                         writes=[osb])
                    yield
                    if dbg:
                        k.dma("pool", ob_scr[t * 128:(t + 1) * 128, :], osbv, reads=[osb])
                        yield
                    sq2 = t512.next()
                    k.op("act", lambda e: e.activation(out=sq2[:], in_=osbv, func=AF.Square), reads=[osb],
                         writes=[sq2])
                    yield
                    rs = rpool.next()
                    k.op("dve", lambda e: e.tensor_reduce(out=rs[:, 0:4], in_=sq2[:].rearrange("p (h c) -> p h c", c=128),
                                                          axis=AX.X, op=ALU.add), reads=[sq2], writes=[rs])
                    yield
                    k.op("dve", lambda e: e.tensor_scalar(out=rs[:, 0:4], in0=rs[:, 0:4], scalar1=1.0 / 128, scalar2=EPS,
                                                          op0=ALU.mult, op1=ALU.add), reads=[rs], writes=[rs])
                    yield
                    rsqrt(rs, rs[:, 0:4], rs[:, 0:4])
                    k.op("dve", lambda e: e.tensor_tensor(out=osb[:], in0=osb[:],
                                                          in1=rs[:, 0:4].unsqueeze(2).to_broadcast([128, 4, 128]),
                                                          op=ALU.mult), reads=[osb, rs], writes=[osb])
                    yield
                    k.op("dve", lambda e: e.tensor_tensor(out=osb[:], in0=osb[:],
                                                           in1=gnw_bc[:].unsqueeze(1).to_broadcast([128, 4, 128]),
                                                           op=ALU.mult), reads=[osb, gnw_bc], writes=[osb])
                    yield
                    pz = proj_tok(O_Z, 512)
                    sgt = t512.next()
                    k.op("act", lambda e: e.activation(out=sgt[:], in_=pz[:], func=AF.Silu), reads=[pz], writes=[sgt])
                    yield
                    cat = catp.next()
                    k.op("dve", lambda e: e.tensor_tensor(out=cat[:], in0=osbv, in1=sgt[:], op=ALU.mult),
                         reads=[osb, sgt], writes=[cat])
                    yield
                    for fc in range(4):
                        k.op("pe", lambda e: e.transpose(pst[:, fc, :], cat[:, fc * 128:(fc + 1) * 128], identb[:]),
                             reads=[cat, identb], writes=[pst])
                    yield
                    catT = catTp.next()
                    k.op("act", lambda e: e.copy(out=catT[:, 0:4, :], in_=pst[:, 0:4, :]), reads=[pst], writes=[catT])
                    yield
                    n0 = (t - 2) * 128
                    k.dma("sp", catT[:, 4:8, :], attT[:, n0:n0 + 128].rearrange("(c p) n -> p c n", p=128),
                          reads=[d_att], writes=[catT])
                    yield
                    xt = xpool.next()
                    k.dma("sp", xt[:], x2[t * 128:(t + 1) * 128, :], reads=[d_x2[t]], writes=[xt])
                    yield
                    for half in range(2):
                        py = proj.next()
                        for kc in range(8):
                            k.op("pe", lambda e: e.matmul(py[:], lhsT=catT[:, kc, :],
                                                          rhs=wout_g[:, kc, half * 512:(half + 1) * 512],
                                                          start=(kc == 0), stop=(kc == 7)), reads=[catT, wout_g],
                                 writes=[py])
                            yield
                        k.op("dve", lambda e: e.tensor_tensor(out=xt[:, half * 512:(half + 1) * 512], in0=py[:],
                                                              in1=xt[:, half * 512:(half + 1) * 512], op=ALU.add),
                             reads=[py, xt], writes=[xt])
                        yield
                    k.dma("pool", x3[t * 128:(t + 1) * 128, :], xt[:], reads=[xt], writes=[d_x3[t]])
                    yield

                def interleave(gens):
                    alive = list(gens)
                    while alive:
                        for g in list(alive):
                            try:
                                next(g)
                            except StopIteration:
                                alive.remove(g)

                ctxs = [dict() for _ in order]
                interleave([tile_A(order[0], ctxs[0])])
                for i_, t in enumerate(order):
                    gens = [tile_B(t, ctxs[i_])]
                    if i_ + 1 < len(order):
                        gens.append(tile_A(order[i_ + 1], ctxs[i_ + 1]))
                    interleave(gens)
                    ctxs[i_] = None
            k.barrier()

        def attention():
            with ExitStack() as es:
                ktp = k.sbpool("aKT", [96, T], BF16, 2, es)
                vxp = k.sbpool("aVX", [128, NT, 80], BF16, 2, es)
                qp = k.sbpool("aQ", [96, 512], BF16, 2, es)
                ptp_ = k.sbpool("aP", [128, 512], BF16, 4, es)
                rcp = k.sbpool("arc", [65, 512], F32, 2, es)
                otp = k.sbpool("aot", [64, 512], BF16, 2, es)
                nb10 = k.sb("nb10", [128, 1], F32, es)
                k.op("pool", lambda e: e.memset(nb10[:], -10.0), writes=[nb10])
                pss = k.pspool("aS", [128, 512], F32, 4, es)
                pso = k.pspool("aO", [65, 512], F32, 2, es)
                psb = k.pspool("aB", [64, 512], F32, 1, es)
                QB = min(512, L)
                for h in range(8):
                    kt_ = ktp.next()
                    k.dma("sp", kt_[:], KT[h], reads=[d_KT], writes=[kt_])
                    vx = vxp.next()
                    k.dma("sp", vx[:], VX[h].rearrange("t p c -> p t c"), reads=[d_VX], writes=[vx])
                    for q0 in range(0, L, QB):
                        qt = qp.next()
                        k.dma("sp", qt[:, 0:QB], QT[h, :, q0:q0 + QB], reads=[d_QT], writes=[qt])
                        po = pso.next()
                        psq = {}
                        for kt in range(NT + 2):
                            if kt < NT:
                                ps_ = pss.next()
                                k.op("pe", lambda e: e.matmul(ps_[:, 0:QB], lhsT=kt_[:, kt * 128:(kt + 1) * 128],
                                                              rhs=qt[:, 0:QB], start=True, stop=True), reads=[kt_, qt],
                                     writes=[ps_])
                                psq[kt] = ps_
                            k2 = kt - 2
                            if k2 >= 0:
                                ps2 = psq.pop(k2)
                                pt = ptp_.next()
                                k.op("act", lambda e: e.activation(out=pt[:, 0:QB], in_=ps2[:, 0:QB], func=AF.Exp,
                                                                   bias=nb10[:, 0:1]), reads=[ps2, nb10], writes=[pt])
                                k.op("pe", lambda e: e.matmul(po[:, 0:QB], lhsT=vx[:, k2, 0:65], rhs=pt[:, 0:QB],
                                                              start=(k2 == 0), stop=(k2 == NT - 1)), reads=[vx, pt],
                                     writes=[po])
                        rc = rcp.next()
                        k.op("dve", lambda e: e.reciprocal(out=rc[64:65, 0:QB], in_=po[64:65, 0:QB]), reads=[po],
                             writes=[rc])
                        pb_ = psb.next()
                        k.op("pe", lambda e: e.matmul(pb_[:, 0:QB], lhsT=ones[64:65, 0:64], rhs=rc[64:65, 0:QB],
                                                      start=True, stop=True), reads=[ones, rc], writes=[pb_])
                        k.op("act", lambda e: e.copy(out=rc[0:64, 0:QB], in_=pb_[:, 0:QB]), reads=[pb_], writes=[rc])
                        ot = otp.next()
                        k.op("dve", lambda e: e.tensor_tensor(out=ot[:, 0:QB], in0=po[0:64, 0:QB], in1=rc[0:64, 0:QB],
                                                              op=ALU.mult), reads=[po, rc], writes=[ot])
                        k.dma("pool", attT[h * 64:(h + 1) * 64, q0:q0 + QB], ot[:, 0:QB], reads=[ot], writes=[d_att])
            k.barrier()

        odd_pass(0)
        if stop_after == "odd0":
            k.drain_all()
            return nc
        attention()
        if stop_after == "att":
            k.drain_all()
            return nc
        odd_pass(1)
        d_y = Buf(None, "y")
        if stop_after == "mix1":
            k.drain_all()
            return nc
        moe(1, x3, d_x3, lambda t: (y_out[(t - 2) * 128:(t - 1) * 128, :], d_y), list(range(2, NT)))
        k.drain_all()
    return nc


_W_NAMES = ["c_ctx", "ada_w", "ada_b", "norm_mix_w", "norm_ffn_w", "even_w_in", "even_w_out", "gmlp_norm_w", "gmlp_ws",
            "gmlp_bs", "hgrn_lb_logits", "hgrn_norm_w", "odd_w_in", "odd_w_out", "gdn_conv_w", "gdn_a_log",
            "gdn_dt_bias", "gdn_norm_w", "mla_q_norm_w", "mla_wq_up", "mla_kv_norm_w", "mla_wkv_up", "mla_qk_norm_q",
            "mla_qk_norm_k", "router_group_w", "router_group_b", "router_expert_w", "router_expert_b", "moe_w_gate",
            "moe_w_up", "moe_w_down"]


def rope_tables(L):
    n = np.arange(L)
    row = (n // 64).astype(np.float32)
    col = (n % 64).astype(np.float32)
    freqs = (np.float32(10000.0) ** (-np.arange(8, dtype=np.float32) / np.float32(8))).astype(np.float32)
    ang = np.concatenate([row[:, None] * freqs[None, :], col[:, None] * freqs[None, :]], axis=1).astype(np.float32)
    return np.cos(ang).astype(np.float32), np.sin(ang).astype(np.float32)


def make_in_maps(inputs, ncores, L):
    cos, sin = rope_tables(L)
    shared = {n: np.ascontiguousarray(np.asarray(inputs[n], dtype=np.float32)) for n in _W_NAMES}
    shared["rope_cos"] = cos
    shared["rope_sin"] = sin
    maps = []
    for b in range(ncores):
        m = dict(shared)
        m["x"] = np.ascontiguousarray(np.asarray(inputs["x"][b], dtype=np.float32))
        m["ctx"] = np.ascontiguousarray(np.asarray(inputs["ctx"][b], dtype=np.float32))
        m["c"] = np.ascontiguousarray(np.asarray(inputs["c"][b], dtype=np.float32))
        maps.append(m)
    return maps


def kernel(**inputs):
    x = np.asarray(inputs["x"])
    B, L, _ = x.shape
    nc = build(L)
    maps = make_in_maps(inputs, B, L)
    res = run_bass_kernel_spmd(nc, maps, core_ids=list(range(B)))
    return np.stack([np.asarray(r["y"]) for r in res.results], axis=0).astype(np.float32)
```

```python
import os
import numpy as np
import concourse.bass as bass
import concourse.mybir as mybir
from concourse.bass_utils import run_bass_kernel_spmd
from contextlib import ExitStack

F32 = mybir.dt.float32
BF16 = mybir.dt.bfloat16
I32 = mybir.dt.int32
AF = mybir.ActivationFunctionType
ALU = mybir.AluOpType
AX = mybir.AxisListType
EPS = 1e-6
D = 1024
CTX = 256


class Buf:
    __slots__ = ("t", "w", "r", "name", "psum")

    def __init__(self, t, name="", psum=False):
        self.t = t
        self.w = None
        self.r = {}
        self.name = name
        self.psum = psum

    def __getitem__(self, k):
        return self.t[k]


class Bv:
    def __init__(self, base, ap):
        self.base = base
        self.ap = ap

    @property
    def psum(self):
        return self.base.psum

    def __getitem__(self, kk):
        return self.ap[kk]

    @property
    def w(self):
        return self.base.w

    @w.setter
    def w(self, v):
        self.base.w = v

    @property
    def r(self):
        return self.base.r

    @r.setter
    def r(self, v):
        self.base.r = v


class Pool:
    def __init__(self, bufs):
        self.bufs = bufs
        self.i = 0

    def next(self):
        b = self.bufs[self.i % len(self.bufs)]
        self.i += 1
        return b


class KB:
    NDMA = 8

    def __init__(self, nc):
        self.nc = nc
        self.es = ExitStack()
        self.eng = {"pe": nc.tensor, "dve": nc.vector, "act": nc.scalar, "pool": nc.gpsimd, "sp": nc.sync}
        self.sems = {}
        self.cnt = {}
        self.seen = {e: {} for e in self.eng}
        for e in self.eng:
            self.sems[e] = self.es.enter_context(nc.semaphore("c_" + e))
            self.cnt[e] = 0
        self.dsem = {}
        self.dcnt = {}
        for q in ("sp", "pool", "act"):
            self.dsem[q] = [self.es.enter_context(nc.semaphore("d_%s%d" % (q, i))) for i in range(self.NDMA)]
            self.dcnt[q] = 0
        self.semobj = {}
        for e in self.eng:
            self.semobj[("c", e)] = self.sems[e]
        for q in self.dsem:
            for i, s in enumerate(self.dsem[q]):
                self.semobj[("d", q, i)] = s
        self.nbuf = 0

    def sb(self, name, shape, dtype, es=None):
        self.nbuf += 1
        t = (es or self.es).enter_context(self.nc.sbuf_tensor("%s_%d" % (name, self.nbuf), list(shape), dtype))
        return Buf(t, name)

    def ps(self, name, shape, dtype, es=None):
        self.nbuf += 1
        t = (es or self.es).enter_context(self.nc.psum_tensor("%s_%d" % (name, self.nbuf), list(shape), dtype))
        return Buf(t, name, psum=True)

    def sbpool(self, name, shape, dtype, n, es=None):
        return Pool([self.sb(name, shape, dtype, es) for _ in range(n)])

    def pspool(self, name, shape, dtype, n, es=None):
        return Pool([self.ps(name, shape, dtype, es) for _ in range(n)])

    def dram(self, name, shape, dtype, kind="Internal"):
        return self.nc.dram_tensor(name, list(shape), dtype, kind=kind).ap()

    def _deps(self, reads, writes, e=None):
        deps = []
        for b in reads:
            if b.w is not None:
                deps.append(b.w)
            if b.psum:
                for kk, v in b.r.items():
                    if kk != ("c", e):
                        deps.append((kk, v))
        for b in writes:
            if b.w is not None:
                deps.append(b.w)
            for kk, v in b.r.items():
                deps.append((kk, v))
        return deps

    def _wait(self, e, deps):
        seen = self.seen[e]
        need = {}
        for kk, v in deps:
            if e == "pe" and kk == ("c", "pe"):
                continue
            if seen.get(kk, 0) >= v:
                continue
            if need.get(kk, 0) < v:
                need[kk] = v
        for kk, v in need.items():
            self.eng[e].wait_ge(self.semobj[kk], v)
            seen[kk] = v

    def _mark(self, tok, reads, writes):
        kk, v = tok
        for b in reads:
            if b.r.get(kk, 0) < v:
                b.r[kk] = v
        for b in writes:
            b.w = tok
            b.r = {}

    def op(self, e, fn, reads=(), writes=()):
        deps = self._deps(reads, writes, e)
        if e == "pe":
            self._wait(e, deps)
            ins = fn(self.eng[e])
        else:
            seen = self.seen[e]
            need = {}
            for kk, v in deps:
                if seen.get(kk, 0) >= v:
                    continue
                if need.get(kk, 0) < v:
                    need[kk] = v
            items = list(need.items())
            for kk, v in items[:-1]:
                self.eng[e].wait_ge(self.semobj[kk], v)
                seen[kk] = v
            ins = fn(self.eng[e])
            if items:
                kk, v = items[-1]
                ins._wait_ge(self.semobj[kk], v)
                seen[kk] = v
        self.cnt[e] += 1
        ins.then_inc(self.sems[e], 1)
        tok = (("c", e), self.cnt[e])
        self._mark(tok, reads, writes)
        return tok

    def dma(self, q, out, in_, reads=(), writes=(), **kw):
        i = self.dcnt[q]
        slot = i % self.NDMA
        kk = ("d", q, slot)
        deps = self._deps(reads, writes, q)
        prev = 16 * (i // self.NDMA)
        if prev > 0:
            deps.append((kk, prev))
        self._wait(q, deps)
        ins = self.eng[q].dma_start(out=out, in_=in_, **kw)
        ins.then_inc(self.dsem[q][slot], 16)
        self.dcnt[q] += 1
        tok = (kk, prev + 16)
        self._mark(tok, reads, writes)
        return tok

    def all_tokens(self):
        deps = []
        for e in self.eng:
            if self.cnt[e] > 0:
                deps.append((("c", e), self.cnt[e]))
        for q in self.dsem:
            n = self.dcnt[q]
            for slot in range(self.NDMA):
                c = (n - slot + self.NDMA - 1) // self.NDMA
                if c > 0:
                    deps.append((("d", q, slot), 16 * c))
        return deps

    def barrier(self):
        deps = self.all_tokens()
        for e in self.eng:
            self._wait(e, deps)

    def drain_all(self):
        self._wait("sp", self.all_tokens())


def build(L, stop_after="all", dbg=False):
    nc = bass.Bass("TRN2", target_bir_lowering=False)
    k = KB(nc)
    NT = (CTX + L) // 128
    NL = L // 128
    T = CTX + L

    def din(name, shape):
        return nc.dram_tensor(name, list(shape), F32, kind="ExternalInput").ap()

    x_in = din("x", [L, D])
    ctx_in = din("ctx", [CTX, D])
    c_in = din("c", [D])
    cctx_in = din("c_ctx", [D])
    ada_w = din("ada_w", [2, D, 6 * D])
    ada_b = din("ada_b", [2, 6 * D])
    norm_mix_w = din("norm_mix_w", [2, D])
    norm_ffn_w = din("norm_ffn_w", [2, D])
    even_w_in = din("even_w_in", [1, D, 3584])
    even_w_out = din("even_w_out", [1, D, D])
    gmlp_norm_w = din("gmlp_norm_w", [1, 512])
    gmlp_ws = din("gmlp_ws", [1, 4, 128, 128])
    gmlp_bs = din("gmlp_bs", [1, 4, 128])
    hgrn_lb = din("hgrn_lb_logits", [2, 3, 512])
    hgrn_norm_w = din("hgrn_norm_w", [1, 512])
    router_group_w = din("router_group_w", [2, D, 4])
    router_group_b = din("router_group_b", [2, 4])
    router_expert_w = din("router_expert_w", [2, D, 32])
    router_expert_b = din("router_expert_b", [2, 32])
    moe_w_gate = din("moe_w_gate", [2, 32, D, 512])
    moe_w_up = din("moe_w_up", [2, 32, D, 512])
    moe_w_down = din("moe_w_down", [2, 32, 512, D])
    odd_w_in = din("odd_w_in", [1, D, 2480])
    odd_w_out = din("odd_w_out", [1, D, D])
    gdn_conv_w = din("gdn_conv_w", [1, 5, 1536])
    gdn_a_log = din("gdn_a_log", [1, 2, 4])
    gdn_dt_bias = din("gdn_dt_bias", [1, 2, 4])
    gdn_norm_w = din("gdn_norm_w", [1, 128])
    mla_q_norm_w = din("mla_q_norm_w", [1, 256])
    mla_wq_up = din("mla_wq_up", [1, 256, 768])
    mla_kv_norm_w = din("mla_kv_norm_w", [1, 128])
    mla_wkv_up = din("mla_wkv_up", [1, 128, 1024])
    mla_qk_norm_q = din("mla_qk_norm_q", [1, 96])
    mla_qk_norm_k = din("mla_qk_norm_k", [1, 96])
    rope_cos = din("rope_cos", [L, 16])
    rope_sin = din("rope_sin", [L, 16])

    okind = "ExternalOutput"
    y_out = k.dram("y", [L, D], F32, okind)
    ikind = "ExternalOutput" if dbg else "Internal"
    modrow = k.dram("modrow", [2, 2, 6 * D], F32, ikind)
    of_scr = k.dram("of_scr", [T, 512], F32, ikind)
    x1 = k.dram("x1", [T, D], F32, ikind)
    x2 = k.dram("x2", [T, D], F32, ikind)
    d_modrow = Buf(None, "modrow")
    d_of = [Buf(None, "of%d" % t) for t in range(NT)]
    d_x1 = [Buf(None, "x1_%d" % t) for t in range(NT)]
    d_x2 = [Buf(None, "x2_%d" % t) for t in range(NT)]
    x3 = k.dram("x3", [T, D], F32, ikind)
    d_x3 = [Buf(None, "x3_%d" % t) for t in range(NT)]
    ob_scr = k.dram("ob_scr", [T, 512], F32, ikind)
    QT = k.dram("QT", [8, 96, L], BF16, ikind)
    KT = k.dram("KT", [8, 96, T], BF16, ikind)
    VX = k.dram("VX", [8, NT, 128, 80], BF16, ikind)
    attT = k.dram("attT", [512, L], BF16, ikind)
    d_QT = Buf(None, "QT")
    d_KT = Buf(None, "KT")
    d_VX = Buf(None, "VX")
    d_att = Buf(None, "att")

    def rows(t):
        if t < 2:
            return ctx_in[t * 128:(t + 1) * 128, :]
        return x_in[(t - 2) * 128:(t - 1) * 128, :]

    with k.es:
        ident = k.sb("ident", [128, 128], F32)
        identb = k.sb("identb", [128, 128], BF16)
        ones = k.sb("ones", [128, 128], F32)
        zeros_bf = k.sb("zeros_bf", [128, 512], BF16)
        k.op("pool", lambda e: e.memset(ones[:], 1.0), writes=[ones])
        k.op("pool", lambda e: e.memset(zeros_bf[:], 0.0), writes=[zeros_bf])
        k.op("pool", lambda e: e.affine_select(ident[:], ones[:], pattern=[[-1, 128]], compare_op=ALU.is_equal,
                                               fill=0.0, base=0, channel_multiplier=1), reads=[ones], writes=[ident])
        k.op("dve", lambda e: e.tensor_copy(out=identb[:], in_=ident[:]), reads=[ident], writes=[identb])

        def blockmask(name, lo_keep, es=None):
            m = k.sb(name, [128, 128], F32, es)
            k.op("pool", lambda e: e.memset(m[:], 0.0), writes=[m])
            for cb in range(2):
                sl = m[cb * 64:(cb + 1) * 64, cb * 64:(cb + 1) * 64]
                src = ones[cb * 64:(cb + 1) * 64, cb * 64:(cb + 1) * 64]
                if lo_keep == "all":
                    k.op("pool", lambda e: e.tensor_copy(out=sl, in_=src), reads=[ones], writes=[m])
                else:
                    cm, st = (-1, 1) if lo_keep == "le" else (1, -1)
                    k.op("pool", lambda e: e.affine_select(sl, src, pattern=[[st, 64]], compare_op=ALU.is_ge,
                                                           fill=0.0, base=0, channel_multiplier=cm),
                         reads=[ones], writes=[m])
            return m

        with ExitStack() as es:
            ccol = k.sb("ccol", [128, 8, 2], F32, es)
            scol = k.sb("scol", [128, 8, 2], F32, es)
            k.dma("sp", ccol[:, :, 0], c_in.rearrange("(kc p) -> p kc", p=128), writes=[ccol],
                  allow_slow_non_contiguous=True)
            k.dma("sp", ccol[:, :, 1], cctx_in.rearrange("(kc p) -> p kc", p=128), writes=[ccol],
                  allow_slow_non_contiguous=True)
            k.op("act", lambda e: e.activation(out=scol[:], in_=ccol[:], func=AF.Silu), reads=[ccol], writes=[scol])
            wst = k.sbpool("adaw", [128, 8, 512], F32, 2, es)
            brow = k.sbpool("adab", [2, 512], F32, 2, es)
            orow = k.sbpool("adao", [2, 512], F32, 2, es)
            pp = k.pspool("adap", [2, 512], F32, 2, es)
            for layer in range(2):
                for cb in range(12):
                    w = wst.next()
                    k.dma("sp", w[:], ada_w[layer, :, cb * 512:(cb + 1) * 512].rearrange("(kc p) n -> p kc n", p=128),
                          writes=[w])
                    b = brow.next()
                    k.dma("sp", b[:], ada_b[layer, cb * 512:(cb + 1) * 512].partition_broadcast(2), writes=[b])
                    p = pp.next()
                    for kc in range(8):
                        k.op("pe", lambda e: e.matmul(p[:], lhsT=scol[:, kc, :], rhs=w[:, kc, :], start=(kc == 0),
                                                      stop=(kc == 7)), reads=[scol, w], writes=[p])
                    o = orow.next()
                    k.op("dve", lambda e: e.tensor_tensor(out=o[:], in0=p[:], in1=b[:], op=ALU.add), reads=[p, b],
                         writes=[o])
                    k.dma("sp", modrow[layer, :, cb * 512:(cb + 1) * 512], o[:], reads=[o], writes=[d_modrow])
        k.barrier()
        if stop_after == "prologue":
            k.drain_all()
            return nc

        def rsqrt(buf, out_ap, in_ap):
            k.op("act", lambda e: e.activation(out=out_ap, in_=in_ap, func=AF.Sqrt), reads=[buf], writes=[buf])
            k.op("dve", lambda e: e.reciprocal(out=out_ap, in_=out_ap), reads=[buf], writes=[buf])

        def load_modcols(es, layer, normw_ap, which):
            mc = k.sb("mc", [128, 2, 48], F32, es)
            for s in range(2):
                k.dma("sp", mc[:, s, :], modrow[layer, s, :].rearrange("(c p) -> p c", p=128), reads=[d_modrow],
                      writes=[mc], allow_slow_non_contiguous=True)
            nw = k.sb("nw", [128, 8], F32, es)
            k.dma("sp", nw[:], normw_ap.rearrange("(c p) -> p c", p=128), writes=[nw], allow_slow_non_contiguous=True)
            A = k.sb("A", [128, 2, 8], F32, es)
            S = k.sb("S", [128, 2, 8], F32, es)
            o = which * 24
            for s in range(2):
                k.op("dve", lambda e: e.scalar_tensor_tensor(out=A[:, s, :], in0=mc[:, s, o + 8:o + 16], scalar=1.0,
                                                             in1=nw[:], op0=ALU.add, op1=ALU.mult),
                     reads=[mc, nw], writes=[A])
                k.op("dve", lambda e: e.tensor_copy(out=S[:, s, :], in_=mc[:, s, o:o + 8]), reads=[mc], writes=[S])
            return A, S

        def norm_T(xt, A, S, s, rpool, xnpool, pst, out_bf=None, out_f32=None):
            junk = xnpool["junk"].next()
            ss = rpool.next()
            k.op("act", lambda e: e.activation(out=junk[:], in_=xt[:], func=AF.Square, accum_out=ss[:, 0:1]),
                 reads=[xt], writes=[junk, ss])
            k.op("dve", lambda e: e.tensor_scalar(out=ss[:, 1:2], in0=ss[:, 0:1], scalar1=1.0 / D, scalar2=EPS,
                                                  op0=ALU.mult, op1=ALU.add), reads=[ss], writes=[ss])
            rsqrt(ss, ss[:, 2:3], ss[:, 1:2])
            if out_f32 is None:
                xn = xnpool["bf"].next()
                k.op("act", lambda e: e.activation(out=xn[:], in_=xt[:], func=AF.Copy, scale=ss[:, 2:3]),
                     reads=[xt, ss], writes=[xn])
                for fc in range(8):
                    k.op("pe", lambda e: e.transpose(pst[:, fc, :], xn[:, fc * 128:(fc + 1) * 128], identb[:]),
                         reads=[xn, identb], writes=[pst])
                k.op("dve", lambda e: e.tensor_tensor(out=out_bf[:], in0=pst[:],
                                                      in1=A[:, s, :].unsqueeze(2).to_broadcast([128, 8, 128]),
                                                      op=ALU.mult), reads=[pst, A], writes=[out_bf])
                k.op("dve", lambda e: e.tensor_tensor(out=out_bf[:], in0=out_bf[:],
                                                      in1=S[:, s, :].unsqueeze(2).to_broadcast([128, 8, 128]),
                                                      op=ALU.add), reads=[out_bf, S], writes=[out_bf])
            else:
                xn = xnpool["f32"].next()
                k.op("act", lambda e: e.activation(out=xn[:], in_=xt[:], func=AF.Copy, scale=ss[:, 2:3]),
                     reads=[xt, ss], writes=[xn])
                for half in range(2):
                    p = pst[half]
                    for f4 in range(4):
                        fc = half * 4 + f4
                        k.op("pe", lambda e: e.transpose(p[:, f4, :], xn[:, fc * 128:(fc + 1) * 128], ident[:]),
                             reads=[xn, ident], writes=[p])
                    sl = slice(half * 4, half * 4 + 4)
                    k.op("dve", lambda e: e.tensor_tensor(out=out_f32[:, sl, :], in0=p[:],
                                                          in1=A[:, s, sl].unsqueeze(2).to_broadcast([128, 4, 128]),
                                                          op=ALU.mult), reads=[p, A], writes=[out_f32])
                    k.op("dve", lambda e: e.tensor_tensor(out=out_f32[:, sl, :], in0=out_f32[:, sl, :],
                                                          in1=S[:, s, sl].unsqueeze(2).to_broadcast([128, 4, 128]),
                                                          op=ALU.add), reads=[out_f32, S], writes=[out_f32])
                k.op("act", lambda e: e.copy(out=out_bf[:], in_=out_f32[:]), reads=[out_f32], writes=[out_bf])

        def load_w_bf16(es, dst, src_ap, nkc, ncols, stage_pool, colblk=512, engs=("dve", "act")):
            i = 0
            for c0 in range(0, ncols, colblk):
                c1 = min(ncols, c0 + colblk)
                st = stage_pool.next()
                k.dma("sp", st[:, 0:nkc, 0:c1 - c0], src_ap[:, c0:c1].rearrange("(kc p) n -> p kc n", p=128),
                      writes=[st])
                eng = engs[i % len(engs)]
                i += 1
                if eng == "act":
                    k.op("act", lambda e: e.copy(out=dst[:, :, c0:c1], in_=st[:, 0:nkc, 0:c1 - c0]), reads=[st],
                         writes=[dst])
                else:
                    k.op(eng, lambda e: e.tensor_copy(out=dst[:, :, c0:c1], in_=st[:, 0:nkc, 0:c1 - c0]), reads=[st],
                         writes=[dst])

        def even_pass(direction):
            with ExitStack() as es:
                fwd = direction == 0
                A1, S1 = load_modcols(es, 0, norm_mix_w[0], 0)
                win = k.sb("win", [128, 8, 3584], BF16, es)
                rhsF = k.sb("rhsF", [128, 256], F32, es)
                lrem = k.sb("lrem", [128, 128], F32, es)
                maski = k.sb("maski", [128, 4, 128], I32, es)
                oml_bc = k.sb("oml_bc", [128, 512], F32, es)
                oml_col = k.sb("oml_col", [128, 4], F32, es)
                if not fwd:
                    wout_g = [k.sb("wout_g", [128, 8, 1024], BF16, es) for _ in range(2)]
                    gnw_bc = k.sb("gnw_bc", [128, 512], F32, es)
                    hnw_bc = k.sb("hnw_bc", [128, 512], F32, es)
                    bs_col = k.sb("bs_col", [128, 4], F32, es)
                    wsT = k.sb("wsT", [128, 4, 128], BF16, es)
                with ExitStack() as es2:
                    stage = k.sbpool("stage", [128, 8, 512], F32, 2, es2)
                    load_w_bf16(es2, win, even_w_in[0], 8, 3584, stage)
                    tri = blockmask("tri", "le" if fwd else "ge", es2)
                    allb = blockmask("allb", "all", es2)
                    mid = k.sb("mid", [128, 128], F32, es2)
                    k.op("pool", lambda e: e.memset(mid[:], 0.0), writes=[mid])
                    for cb in range(2):
                        r0 = cb * 64 + (0 if fwd else 32)
                        k.op("pool", lambda e: e.tensor_copy(out=mid[r0:r0 + 32, cb * 64:(cb + 1) * 64],
                                                             in_=ones[r0:r0 + 32, cb * 64:(cb + 1) * 64]),
                             reads=[ones], writes=[mid])
                    k.op("dve", lambda e: e.tensor_tensor(out=rhsF[:, 0:128], in0=tri[:], in1=mid[:], op=ALU.subtract),
                         reads=[tri, mid], writes=[rhsF])
                    k.op("dve", lambda e: e.tensor_copy(out=rhsF[:, 128:256], in_=tri[:]), reads=[tri], writes=[rhsF])
                    k.op("dve", lambda e: e.tensor_tensor(out=lrem[:], in0=allb[:], in1=tri[:], op=ALU.subtract),
                         reads=[allb, tri], writes=[lrem])
                    for h in range(4):
                        k.op("dve", lambda e: e.tensor_copy(out=maski[:, h, :], in_=tri[:]), reads=[tri],
                             writes=[maski])
                    lbt = k.sb("lbt", [128, 3, 512], F32, es2)
                    for r in range(3):
                        k.dma("sp", lbt[:, r, :], hgrn_lb[direction, r, :].partition_broadcast(128), writes=[lbt])
                    k.op("act", lambda e: e.activation(out=lbt[:], in_=lbt[:], func=AF.Exp), reads=[lbt], writes=[lbt])
                    tmpb = k.sb("tmpb", [128, 512], F32, es2)
                    k.op("dve", lambda e: e.tensor_tensor(out=tmpb[:], in0=lbt[:, 0, :], in1=lbt[:, 1, :], op=ALU.add),
                         reads=[lbt], writes=[tmpb])
                    k.op("dve", lambda e: e.tensor_tensor(out=tmpb[:], in0=tmpb[:], in1=lbt[:, 2, :], op=ALU.add),
                         reads=[lbt, tmpb], writes=[tmpb])
                    k.op("dve", lambda e: e.reciprocal(out=tmpb[:], in_=tmpb[:]), reads=[tmpb], writes=[tmpb])
                    k.op("dve", lambda e: e.tensor_tensor(out=oml_bc[:], in0=lbt[:, 1, :], in1=lbt[:, 2, :],
                                                          op=ALU.add), reads=[lbt], writes=[oml_bc])
                    k.op("dve", lambda e: e.tensor_tensor(out=oml_bc[:], in0=oml_bc[:], in1=tmpb[:], op=ALU.mult),
                         reads=[oml_bc, tmpb], writes=[oml_bc])
                    lbc = k.sb("lbc", [128, 3, 4], F32, es2)
                    for r in range(3):
                        k.dma("sp", lbc[:, r, :], hgrn_lb[direction, r, :].rearrange("(h p) -> p h", p=128),
                              writes=[lbc], allow_slow_non_contiguous=True)
                    k.op("act", lambda e: e.activation(out=lbc[:], in_=lbc[:], func=AF.Exp), reads=[lbc], writes=[lbc])
                    tmpc = k.sb("tmpc", [128, 4], F32, es2)
                    k.op("dve", lambda e: e.tensor_tensor(out=tmpc[:], in0=lbc[:, 0, :], in1=lbc[:, 1, :], op=ALU.add),
                         reads=[lbc], writes=[tmpc])
                    k.op("dve", lambda e: e.tensor_tensor(out=tmpc[:], in0=tmpc[:], in1=lbc[:, 2, :], op=ALU.add),
                         reads=[lbc, tmpc], writes=[tmpc])
                    k.op("dve", lambda e: e.reciprocal(out=tmpc[:], in_=tmpc[:]), reads=[tmpc], writes=[tmpc])
                    k.op("dve", lambda e: e.tensor_tensor(out=oml_col[:], in0=lbc[:, 1, :], in1=lbc[:, 2, :],
                                                          op=ALU.add), reads=[lbc], writes=[oml_col])
                    k.op("dve", lambda e: e.tensor_tensor(out=oml_col[:], in0=oml_col[:], in1=tmpc[:], op=ALU.mult),
                         reads=[oml_col, tmpc], writes=[oml_col])
                    if not fwd:
                        gbc = k.sb("gbc", [128, 2, 1024], F32, es2)
                        for s in range(2):
                            k.dma("sp", gbc[:, s, :], modrow[0, s, 2 * D:3 * D].partition_broadcast(128),
                                  reads=[d_modrow], writes=[gbc])
                        for c0 in range(0, 1024, 512):
                            st = stage.next()
                            k.dma("sp", st[:], even_w_out[0][:, c0:c0 + 512].rearrange("(kc p) n -> p kc n", p=128),
                                  writes=[st])
                            for s in range(2):
                                for kc in range(8):
                                    k.op("dve" if kc % 2 else "pool",
                                         lambda e: e.tensor_tensor(out=wout_g[s][:, kc, c0:c0 + 512], in0=st[:, kc, :],
                                                                   in1=gbc[:, s, c0:c0 + 512], op=ALU.mult),
                                         reads=[st, gbc], writes=[wout_g[s]])
                        k.dma("sp", gnw_bc[:], gmlp_norm_w[0].partition_broadcast(128), writes=[gnw_bc])
                        k.dma("sp", hnw_bc[:], hgrn_norm_w[0].partition_broadcast(128), writes=[hnw_bc])
                        k.dma("sp", bs_col[:], gmlp_bs[0].rearrange("g p -> p g"), writes=[bs_col],
                              allow_slow_non_contiguous=True)
                        wsf = k.sb("wsf", [128, 4, 128], F32, es2)
                        k.dma("sp", wsf[:], gmlp_ws[0].rearrange("g p q -> p g q"), writes=[wsf])
                        with ExitStack() as es3:
                            ptmp = k.ps("ptmp", [128, 512], F32, es3)
                            for g in range(4):
                                k.op("pe", lambda e: e.transpose(ptmp[:, g * 128:(g + 1) * 128], wsf[:, g, :], ident[:]),
                                     reads=[wsf, ident], writes=[ptmp])
                            k.op("dve", lambda e: e.tensor_copy(out=wsT[:].rearrange("p g q -> p (g q)"), in_=ptmp[:]),
                                 reads=[ptmp], writes=[wsT])
                            k.barrier()
                    k.barrier()

                Sst = k.sb("Sst", [128, 4, 128], F32, es)
                Sbf = k.sbpool("Sbf", [128, 4, 128], BF16, 3, es)
                k.op("pool", lambda e: e.memset(Sst[:], 0.0), writes=[Sst])
                xpool = k.sbpool("xt", [128, 1024], F32, 3, es)
                rpool = k.sbpool("rs", [128, 4], F32, 4, es)
                xnpool = {"junk": k.sbpool("junk", [128, 1024], BF16, 1, es),
                          "bf": k.sbpool("xnb", [128, 1024], BF16, 2, es)}
                hTp = k.sbpool("hT", [128, 8, 128], BF16, 2, es)
                pst = k.ps("pst", [128, 8, 128], BF16, es)
                proj = k.pspool("proj", [128, 512], F32, 2, es)
                big2 = k.ps("big2", [128, 1024], F32, es)
                gs = k.ps("gs", [128, 512], F32, es)
                ops_ = k.ps("ops", [128, 4, 128], F32, es)
                dSp = k.ps("dSp", [128, 4, 128], F32, es)
                t512 = k.sbpool("t512", [128, 512], F32, 10, es)
                b512 = k.sbpool("b512", [128, 512], BF16, 10, es)
                qhp = k.sbpool("qhat", [128, 2, 4, 128], BF16, 2, es)
                for qb in qhp.bufs:
                    k.op("pool", lambda e: e.memset(qb[:], 0.0), writes=[qb])
                if not fwd:
                    catp = k.sbpool("cat", [128, 1024], BF16, 2, es)
                    catTp = k.sbpool("catT", [128, 8, 128], BF16, 2, es)
                    zp = k.sbpool("z", [128, 1024], F32, 1, es)

                order = list(range(NT)) if fwd else [1, 0] + list(range(NT - 1, 1, -1))
                C_Q, C_I, C_FF, C_FB, C_G = 1024, 1536, 2048, 2560, 3072
                C_F = C_FF if fwd else C_FB
                first_rows, second_rows = (slice(0, 64), slice(64, 128)) if fwd else (slice(64, 128), slice(0, 64))
                first_idx, second_idx = (0, 1) if fwd else (1, 0)
                last_first, last_second = (63, 127) if fwd else (64, 0)

                for t in order:
                    s = 0 if t >= 2 else 1
                    xt = xpool.next()
                    k.dma("sp", xt[:], rows(t), writes=[xt])
                    hT = hTp.next()
                    norm_T(xt, A1, S1, s, rpool, xnpool, pst, out_bf=hT)

                    def proj_tok(c0, n=512):
                        p = proj.next()
                        for kc in range(8):
                            k.op("pe", lambda e: e.matmul(p[:, 0:n], lhsT=hT[:, kc, :], rhs=win[:, kc, c0:c0 + n],
                                                          start=(kc == 0), stop=(kc == 7)), reads=[hT, win], writes=[p])
                        return p

                    def proj_feat(c0):
                        p = proj.next()
                        for h in range(4):
                            for kc in range(8):
                                k.op("pe", lambda e: e.matmul(p[:, h * 128:(h + 1) * 128],
                                                              lhsT=win[:, kc, c0 + h * 128:c0 + (h + 1) * 128],
                                                              rhs=hT[:, kc, :], start=(kc == 0), stop=(kc == 7)),
                                     reads=[hT, win], writes=[p])
                        return p

                    pf = proj_tok(C_F)
                    sg = t512.next()
                    k.op("act", lambda e: e.activation(out=sg[:], in_=pf[:], func=AF.Sigmoid, scale=-1.0), reads=[pf],
                         writes=[sg])
                    ktok = t512.next()
                    k.op("dve", lambda e: e.tensor_tensor(out=ktok[:], in0=sg[:], in1=oml_bc[:], op=ALU.mult),
                         reads=[sg, oml_bc], writes=[ktok])
                    lf = t512.next()
                    k.op("act", lambda e: e.activation(out=lf[:], in_=ktok[:], func=AF.Ln, scale=-1.0, bias=1.0),
                         reads=[ktok], writes=[lf])
                    k.op("pe", lambda e: e.matmul(gs[:], lhsT=lrem[:], rhs=lf[:], start=True, stop=True),
                         reads=[lrem, lf], writes=[gs])
                    er = t512.next()
                    k.op("act", lambda e: e.activation(out=er[:], in_=gs[:], func=AF.Exp), reads=[gs], writes=[er])
                    khat = b512.next()
                    k.op("dve", lambda e: e.tensor_tensor(out=khat[:], in0=ktok[:], in1=er[:], op=ALU.mult),
                         reads=[ktok, er], writes=[khat])
                    pi = proj_tok(C_I)
                    vb = b512.next()
                    k.op("act", lambda e: e.copy(out=vb[:], in_=pi[:]), reads=[pi], writes=[vb])
                    for h in range(4):
                        k.op("pe", lambda e: e.matmul(big2[:, h * 256:(h + 1) * 256], lhsT=lf[:, h * 128:(h + 1) * 128],
                                                      rhs=rhsF[:], start=True, stop=True), reads=[lf, rhsF],
                             writes=[big2])
                    b2v = big2[:].rearrange("p (h c) -> p h c", c=256)
                    e1 = t512.next()
                    e2 = t512.next()
                    e3 = t512.next()
                    e1v = e1[:].rearrange("p (h c) -> p h c", c=128)
                    e2v = e2[:].rearrange("p (h c) -> p h c", c=128)
                    e3v = e3[:].rearrange("p (h c) -> p h c", c=128)
                    k.op("act", lambda e: e.activation(out=e1v, in_=b2v[:, :, 0:128], func=AF.Exp), reads=[big2],
                         writes=[e1])
                    k.op("act", lambda e: e.activation(out=e2v, in_=b2v[:, :, 0:128], func=AF.Exp, scale=-1.0),
                         reads=[big2], writes=[e2])
                    k.op("act", lambda e: e.activation(out=e3v, in_=b2v[:, :, 128:256], func=AF.Exp), reads=[big2],
                         writes=[e3])
                    pq = proj_feat(C_Q)
                    qT = t512.next()
                    k.op("act", lambda e: e.copy(out=qT[:], in_=pq[:]), reads=[pq], writes=[qT])
                    pfT = proj_feat(C_F)
                    kT = t512.next()
                    k.op("act", lambda e: e.activation(out=kT[:], in_=pfT[:], func=AF.Sigmoid, scale=-1.0), reads=[pfT],
                         writes=[kT])
                    kTv = kT[:].rearrange("p (h c) -> p h c", c=128)
                    k.op("dve", lambda e: e.tensor_tensor(out=kTv, in0=kTv,
                                                          in1=oml_col[:].unsqueeze(2).to_broadcast([128, 4, 128]),
                                                          op=ALU.mult), reads=[kT, oml_col], writes=[kT])
                    qtl = b512.next()
                    ktl = b512.next()
                    k.op("dve", lambda e: e.tensor_tensor(out=qtl[:], in0=qT[:], in1=e1[:], op=ALU.mult),
                         reads=[qT, e1], writes=[qtl])
                    k.op("dve", lambda e: e.tensor_tensor(out=ktl[:], in0=kT[:], in1=e2[:], op=ALU.mult),
                         reads=[kT, e2], writes=[ktl])
                    qh = qhp.next()
                    qTv = qT[:].rearrange("p (h c) -> p h c", c=128)
                    for cb in range(2):
                        cs = slice(cb * 64, (cb + 1) * 64)
                        k.op("dve", lambda e: e.tensor_tensor(out=qh[:, cb, :, cs], in0=qTv[:, :, cs],
                                                               in1=e3v[:, :, cs], op=ALU.mult),
                             reads=[qT, e3], writes=[qh])
                    for h in range(4):
                        k.op("pe", lambda e: e.matmul(gs[:, h * 128:(h + 1) * 128], lhsT=ktl[:, h * 128:(h + 1) * 128],
                                                      rhs=qtl[:, h * 128:(h + 1) * 128], start=True, stop=True),
                             reads=[ktl, qtl], writes=[gs])
                    scm = b512.next()
                    k.op("act", lambda e: e.copy(out=scm[:], in_=zeros_bf[:]), reads=[zeros_bf], writes=[scm])
                    k.op("dve", lambda e: e.copy_predicated(out=scm[:], mask=maski[:].rearrange("p h c -> p (h c)"),
                                                            data=gs[:]), reads=[gs, maski, scm], writes=[scm])
                    S0 = Sbf.next()
                    k.op("act", lambda e: e.copy(out=S0[:], in_=Sst[:]), reads=[Sst], writes=[S0])
                    for h in range(4):
                        hs = slice(h * 128, (h + 1) * 128)
                        k.op("pe", lambda e: e.matmul(dSp[:, h, :], lhsT=khat[first_rows, hs], rhs=vb[first_rows, hs],
                                                      start=True, stop=True), reads=[khat, vb], writes=[dSp])
                    for h in range(4):
                        k.op("dve", lambda e: e.scalar_tensor_tensor(out=Sst[:, h, :], in0=Sst[:, h, :],
                                                                     scalar=e3v[:, h, last_first:last_first + 1],
                                                                     in1=dSp[:, h, :], op0=ALU.mult, op1=ALU.add),
                             reads=[Sst, e3, dSp], writes=[Sst])
                    S1_ = Sbf.next()
                    k.op("act", lambda e: e.copy(out=S1_[:], in_=Sst[:]), reads=[Sst], writes=[S1_])
                    for h in range(4):
                        hs = slice(h * 128, (h + 1) * 128)
                        k.op("pe", lambda e: e.matmul(ops_[:, h, :], lhsT=scm[:, hs], rhs=vb[:, hs], start=True,
                                                      stop=False), reads=[scm, vb], writes=[ops_])
                        k.op("pe", lambda e: e.matmul(ops_[:, h, :], lhsT=qh[:, first_idx, h, :], rhs=S0[:, h, :],
                                                      start=False, stop=False), reads=[qh, S0], writes=[ops_])
                        k.op("pe", lambda e: e.matmul(ops_[:, h, :], lhsT=qh[:, second_idx, h, :], rhs=S1_[:, h, :],
                                                      start=False, stop=True), reads=[qh, S1_], writes=[ops_])
                    for h in range(4):
                        hs = slice(h * 128, (h + 1) * 128)
                        k.op("pe", lambda e: e.matmul(dSp[:, h, :], lhsT=khat[second_rows, hs], rhs=vb[second_rows, hs],
                                                      start=True, stop=True), reads=[khat, vb], writes=[dSp])
                    for h in range(4):
                        k.op("dve", lambda e: e.scalar_tensor_tensor(out=Sst[:, h, :], in0=Sst[:, h, :],
                                                                     scalar=e3v[:, h, last_second:last_second + 1],
                                                                     in1=dSp[:, h, :], op0=ALU.mult, op1=ALU.add),
                             reads=[Sst, e3, dSp], writes=[Sst])
                    osb = t512.next()
                    if fwd:
                        k.op("act", lambda e: e.copy(out=osb[:], in_=ops_[:].rearrange("p h c -> p (h c)")),
                             reads=[ops_], writes=[osb])
                        k.dma("pool", of_scr[t * 128:(t + 1) * 128, :], osb[:], reads=[osb], writes=[d_of[t]])
                        continue
                    ofl = t512.next()
                    k.dma("sp", ofl[:], of_scr[t * 128:(t + 1) * 128, :], reads=[d_of[t]], writes=[ofl])
                    k.op("dve", lambda e: e.tensor_tensor(out=osb[:], in0=ops_[:].rearrange("p h c -> p (h c)"),
                                                          in1=ofl[:], op=ALU.add), reads=[ops_, ofl], writes=[osb])
                    sq = t512.next()
                    k.op("act", lambda e: e.activation(out=sq[:], in_=osb[:], func=AF.Square), reads=[osb],
                         writes=[sq])
                    rs = rpool.next()
                    k.op("dve", lambda e: e.tensor_reduce(out=rs[:], in_=sq[:].rearrange("p (h c) -> p h c", c=128),
                                                          axis=AX.X, op=ALU.add), reads=[sq], writes=[rs])
                    k.op("dve", lambda e: e.tensor_scalar(out=rs[:], in0=rs[:], scalar1=1.0 / 128, scalar2=EPS,
                                                          op0=ALU.mult, op1=ALU.add), reads=[rs], writes=[rs])
                    rsqrt(rs, rs[:], rs[:])
                    k.op("dve", lambda e: e.tensor_tensor(out=osb[:].rearrange("p (h c) -> p h c", c=128),
                                                          in0=osb[:].rearrange("p (h c) -> p h c", c=128),
                                                          in1=rs[:].unsqueeze(2).to_broadcast([128, 4, 128]),
                                                          op=ALU.mult), reads=[osb, rs], writes=[osb])
                    k.op("dve", lambda e: e.tensor_tensor(out=osb[:], in0=osb[:], in1=hnw_bc[:], op=ALU.mult),
                         reads=[osb, hnw_bc], writes=[osb])
                    pg = proj_tok(C_G)
                    sgt = t512.next()
                    k.op("act", lambda e: e.activation(out=sgt[:], in_=pg[:], func=AF.Silu), reads=[pg], writes=[sgt])
                    cat = catp.next()
                    k.op("dve", lambda e: e.tensor_tensor(out=cat[:, 512:1024], in0=osb[:], in1=sgt[:], op=ALU.mult),
                         reads=[osb, sgt], writes=[cat])
                    z = zp.next()
                    pu = proj_tok(0)
                    k.op("act", lambda e: e.activation(out=z[:, 0:512], in_=pu[:], func=AF.Gelu), reads=[pu], writes=[z])
                    pv = proj_tok(512)
                    k.op("act", lambda e: e.activation(out=z[:, 512:1024], in_=pv[:], func=AF.Gelu), reads=[pv],
                         writes=[z])
                    rs2 = rpool.next()
                    sq2 = t512.next()
                    k.op("act", lambda e: e.activation(out=sq2[:], in_=z[:, 512:1024], func=AF.Square,
                                                       accum_out=rs2[:, 0:1]), reads=[z], writes=[sq2, rs2])
                    k.op("dve", lambda e: e.tensor_scalar(out=rs2[:, 1:2], in0=rs2[:, 0:1], scalar1=1.0 / 512,
                                                          scalar2=EPS, op0=ALU.mult, op1=ALU.add), reads=[rs2],
                         writes=[rs2])
                    rsqrt(rs2, rs2[:, 2:3], rs2[:, 1:2])
                    vn = b512.next()
                    k.op("dve", lambda e: e.scalar_tensor_tensor(out=vn[:], in0=z[:, 512:1024], scalar=rs2[:, 2:3],
                                                                 in1=gnw_bc[:], op0=ALU.mult, op1=ALU.mult),
                         reads=[z, rs2, gnw_bc], writes=[vn])
                    psg = proj.next()
                    for g in range(4):
                        gsl = slice(g * 128, (g + 1) * 128)
                        k.op("pe", lambda e: e.matmul(psg[:, gsl], lhsT=wsT[:, g, :], rhs=vn[:, gsl], start=True,
                                                      stop=True), reads=[wsT, vn], writes=[psg])
                    for g in range(4):
                        gsl = slice(g * 128, (g + 1) * 128)
                        k.op("dve", lambda e: e.scalar_tensor_tensor(out=cat[:, gsl], in0=psg[:, gsl],
                                                                     scalar=bs_col[:, g:g + 1], in1=z[:, gsl],
                                                                     op0=ALU.add, op1=ALU.mult),
                             reads=[psg, bs_col, z], writes=[cat])
                    for fc in range(8):
                        k.op("pe", lambda e: e.transpose(pst[:, fc, :], cat[:, fc * 128:(fc + 1) * 128], identb[:]),
                             reads=[cat, identb], writes=[pst])
                    catT = catTp.next()
                    k.op("act", lambda e: e.copy(out=catT[:], in_=pst[:]), reads=[pst], writes=[catT])
                    for half in range(2):
                        for kc in range(8):
                            k.op("pe", lambda e: e.matmul(big2[:, half * 512:(half + 1) * 512], lhsT=catT[:, kc, :],
                                                          rhs=wout_g[s][:, kc, half * 512:(half + 1) * 512],
                                                          start=(kc == 0), stop=(kc == 7)),
                                 reads=[catT, wout_g[s]], writes=[big2])
                    k.op("dve", lambda e: e.tensor_tensor(out=xt[:], in0=big2[:], in1=xt[:], op=ALU.add),
                         reads=[big2, xt], writes=[xt])
                    k.dma("pool", x1[t * 128:(t + 1) * 128, :], xt[:], reads=[xt], writes=[d_x1[t]])
            k.barrier()

        even_pass(0)
        even_pass(1)
        if stop_after == "mix0":
            k.drain_all()
            return nc

        def moe(layer, src, d_src, dst_fn, tiles):
            with ExitStack() as es:
                A2, S2 = load_modcols(es, layer, norm_ffn_w[layer], 1)
                wr = k.sb("wr", [128, 8, 36], F32, es)
                k.dma("sp", wr[:, :, 0:4], router_group_w[layer].rearrange("(kc p) e -> p kc e", p=128), writes=[wr],
                      allow_slow_non_contiguous=True)
                k.dma("sp", wr[:, :, 4:36], router_expert_w[layer].rearrange("(kc p) e -> p kc e", p=128), writes=[wr],
                      allow_slow_non_contiguous=True)
                rb = k.sb("rb", [128, 36], F32, es)
                k.dma("sp", rb[:, 0:4], router_group_b[layer].partition_broadcast(128), writes=[rb])
                k.dma("sp", rb[:, 4:36], router_expert_b[layer].partition_broadcast(128), writes=[rb])
                g2bc = k.sb("g2bc", [128, 2, 1024], F32, es)
                for s in range(2):
                    k.dma("sp", g2bc[:, s, :], modrow[layer, s, 5 * D:6 * D].partition_broadcast(128),
                          reads=[d_modrow], writes=[g2bc])
                n_super = -(-len(tiles) // 12)
                NTS = -(-len(tiles) // n_super)
                ST = NTS * 128
                hTs = k.sb("hTs", [128, 8, ST], BF16, es)
                hT32p = k.sbpool("hT32", [128, 8, 128], F32, 1, es)
                yacc = k.sb("yacc", [128, NTS, 1024], F32, es)
                wc = k.sb("wc", [128, NTS, 32], F32, es)
                xpool = k.sbpool("xt", [128, 1024], F32, 2, es)
                rpool = k.sbpool("rs", [128, 8], F32, 4, es)
                xnpool = {"junk": k.sbpool("junk", [128, 1024], BF16, 1, es),
                          "f32": k.sbpool("xnf", [128, 1024], F32, 1, es)}
                stg = k.sbpool("wstg", [128, 4, 512], F32, 4, es)
                wgp = k.sbpool("wg", [128, 8, 512], BF16, 2, es)
                wup = k.sbpool("wu", [128, 8, 512], BF16, 2, es)
                wdp = k.sbpool("wd", [128, 4, 1024], BF16, 2, es)
                silp = k.sbpool("sil", [128, 512], F32, 2, es)
                actp = k.sbpool("actT", [128, 4, 512], BF16, 2, es)
                r36 = k.sbpool("r36", [128, 40], F32, 12, es)
                pga = k.pspool("pga", [128, 512], F32, 2, es)
                pup = k.pspool("pup", [128, 512], F32, 2, es)
                pdn = k.pspool("pdn", [128, 1024], F32, 2, es)
                cast_i = [0]

                def cast(dst_ap, dst, st):
                    eng = ("dve", "act")[cast_i[0] % 2]
                    cast_i[0] += 1
                    if eng == "act":
                        k.op("act", lambda e: e.copy(out=dst_ap, in_=st[:]), reads=[st], writes=[dst])
                    else:
                        k.op(eng, lambda e: e.tensor_copy(out=dst_ap, in_=st[:]), reads=[st], writes=[dst])

                bounds = [round(i * len(tiles) / n_super) for i in range(n_super + 1)]
                for si_ in range(n_super):
                    stiles = tiles[bounds[si_]:bounds[si_ + 1]]
                    n_t = len(stiles)
                    for ti, t in enumerate(stiles):
                        s = 0 if t >= 2 else 1
                        xt = xpool.next()
                        k.dma("sp", xt[:], src[t * 128:(t + 1) * 128, :], reads=[d_src[t]], writes=[xt])
                        h32 = hT32p.next()
                        pstf = [pga.next(), pup.next()]
                        pst_views = [Bv(p, p[:].rearrange("p (f c) -> p f c", c=128)) for p in pstf]
                        hview = Bv(hTs, hTs[:, :, ti * 128:(ti + 1) * 128])
                        norm_T(xt, A2, S2, s, rpool, xnpool, pst_views, out_bf=hview, out_f32=h32)
                        pl = pga.next()
                        for kc in range(8):
                            k.op("pe", lambda e: e.matmul(pl[:, 0:36], lhsT=h32[:, kc, :], rhs=wr[:, kc, :],
                                                          start=(kc == 0), stop=(kc == 7)), reads=[h32, wr], writes=[pl])
                        lg = r36.next()
                        k.op("dve", lambda e: e.tensor_tensor(out=lg[:, 0:36], in0=pl[:, 0:36], in1=rb[:], op=ALU.add),
                             reads=[pl, rb], writes=[lg])
                        m = r36.next()
                        k.op("dve", lambda e: e.tensor_reduce(out=m[:, 0:1], in_=lg[:, 0:4], axis=AX.X, op=ALU.max),
                             reads=[lg], writes=[m])
                        k.op("dve", lambda e: e.tensor_scalar(out=m[:, 1:2], in0=m[:, 0:1], scalar1=-1.0, scalar2=None,
                                                              op0=ALU.mult), reads=[m], writes=[m])
                        eg = r36.next()
                        k.op("act", lambda e: e.activation(out=eg[:, 0:4], in_=lg[:, 0:4], func=AF.Exp, bias=m[:, 1:2],
                                                           accum_out=m[:, 2:3]), reads=[lg, m], writes=[eg, m])
                        k.op("dve", lambda e: e.reciprocal(out=m[:, 3:4], in_=m[:, 2:3]), reads=[m], writes=[m])
                        ohg = r36.next()
                        k.op("dve", lambda e: e.tensor_scalar(out=ohg[:, 0:4], in0=lg[:, 0:4], scalar1=m[:, 0:1],
                                                              scalar2=1e9, op0=ALU.is_lt, op1=ALU.mult),
                             reads=[lg, m], writes=[ohg])
                        lem = r36.next()
                        k.op("dve", lambda e: e.tensor_tensor(
                            out=lem[:, 0:32].rearrange("p (g e) -> p g e", e=8),
                            in0=lg[:, 4:36].rearrange("p (g e) -> p g e", e=8),
                            in1=ohg[:, 0:4].unsqueeze(2).to_broadcast([128, 4, 8]), op=ALU.subtract),
                             reads=[lg, ohg], writes=[lem])
                        k.op("dve", lambda e: e.tensor_reduce(out=m[:, 4:5], in_=lem[:, 0:32], axis=AX.X, op=ALU.max),
                             reads=[lem], writes=[m])
                        oh1 = r36.next()
                        k.op("dve", lambda e: e.tensor_scalar(out=oh1[:, 0:32], in0=lem[:, 0:32], scalar1=m[:, 4:5],
                                                              scalar2=None, op0=ALU.is_ge), reads=[lem, m], writes=[oh1])
                        lem2 = r36.next()
                        k.op("dve", lambda e: e.scalar_tensor_tensor(out=lem2[:, 0:32], in0=oh1[:, 0:32], scalar=-1e9,
                                                                     in1=lem[:, 0:32], op0=ALU.mult, op1=ALU.add),
                             reads=[oh1, lem], writes=[lem2])
                        k.op("dve", lambda e: e.tensor_reduce(out=m[:, 5:6], in_=lem2[:, 0:32], axis=AX.X, op=ALU.max),
                             reads=[lem2], writes=[m])
                        oh2 = r36.next()
                        k.op("dve", lambda e: e.tensor_scalar(out=oh2[:, 0:32], in0=lem2[:, 0:32], scalar1=m[:, 5:6],
                                                              scalar2=None, op0=ALU.is_ge), reads=[lem2, m],
                             writes=[oh2])
                        k.op("dve", lambda e: e.tensor_tensor(out=m[:, 6:7], in0=m[:, 5:6], in1=m[:, 4:5],
                                                              op=ALU.subtract), reads=[m], writes=[m])
                        k.op("act", lambda e: e.activation(out=m[:, 7:8], in_=m[:, 6:7], func=AF.Exp), reads=[m],
                             writes=[m])
                        k.op("dve", lambda e: e.tensor_scalar(out=m[:, 7:8], in0=m[:, 7:8], scalar1=1.0, scalar2=None,
                                                              op0=ALU.add), reads=[m], writes=[m])
                        k.op("dve", lambda e: e.reciprocal(out=m[:, 8:9], in_=m[:, 7:8]), reads=[m], writes=[m])
                        k.op("dve", lambda e: e.tensor_tensor(out=m[:, 9:10], in0=m[:, 8:9], in1=m[:, 3:4], op=ALU.mult),
                             reads=[m], writes=[m])
                        k.op("dve", lambda e: e.tensor_tensor(out=m[:, 10:11], in0=m[:, 3:4], in1=m[:, 9:10],
                                                              op=ALU.subtract), reads=[m], writes=[m])
                        k.op("dve", lambda e: e.tensor_scalar(out=wc[:, ti, :], in0=oh1[:, 0:32], scalar1=m[:, 9:10],
                                                              scalar2=None, op0=ALU.mult), reads=[oh1, m], writes=[wc])
                        k.op("dve", lambda e: e.scalar_tensor_tensor(out=wc[:, ti, :], in0=oh2[:, 0:32],
                                                                     scalar=m[:, 10:11], in1=wc[:, ti, :],
                                                                     op0=ALU.mult, op1=ALU.add),
                             reads=[oh2, m, wc], writes=[wc])
                    ntok = n_t * 128
                    pending = [None]
                    for ex in range(32):
                        wg = wgp.next()
                        wu = wup.next()
                        wd = wdp.next()
                        for (dst, srcw) in ((wg, moe_w_gate[layer, ex]), (wu, moe_w_up[layer, ex])):
                            for half in range(2):
                                st = stg.next()
                                k.dma("sp", st[:], srcw[half * 512:(half + 1) * 512, :].rearrange(
                                    "(kc p) n -> p kc n", p=128), writes=[st])
                                cast(dst[:, half * 4:(half + 1) * 4, :], dst, st)
                        for half in range(2):
                            st = stg.next()
                            k.dma("sp", st[:], moe_w_down[layer, ex][:, half * 512:(half + 1) * 512].rearrange(
                                "(kc p) n -> p kc n", p=128), writes=[st])
                            cast(wd[:, :, half * 512:(half + 1) * 512], wd, st)
                        for b0 in range(0, ntok, 512):
                            nb = min(512, ntok - b0)
                            aT = actp.next()
                            for ffc in range(4):
                                fsl = slice(ffc * 128, (ffc + 1) * 128)
                                pg_ = pga.next()
                                pu_ = pup.next()
                                for kc in range(8):
                                    k.op("pe", lambda e: e.matmul(pg_[:, 0:nb], lhsT=wg[:, kc, fsl],
                                                                  rhs=hTs[:, kc, b0:b0 + nb], start=(kc == 0),
                                                                  stop=(kc == 7)), reads=[wg, hTs], writes=[pg_])
                                for kc in range(8):
                                    k.op("pe", lambda e: e.matmul(pu_[:, 0:nb], lhsT=wu[:, kc, fsl],
                                                                  rhs=hTs[:, kc, b0:b0 + nb], start=(kc == 0),
                                                                  stop=(kc == 7)), reads=[wu, hTs], writes=[pu_])
                                sl_ = silp.next()
                                k.op("act", lambda e: e.activation(out=sl_[:, 0:nb], in_=pg_[:, 0:nb], func=AF.Silu),
                                     reads=[pg_], writes=[sl_])
                                k.op("dve", lambda e: e.tensor_tensor(out=aT[:, ffc, 0:nb], in0=sl_[:, 0:nb],
                                                                      in1=pu_[:, 0:nb], op=ALU.mult),
                                     reads=[sl_, pu_], writes=[aT])
                            def mk_down(aT=aT, wd=wd, ex=ex, b0=b0, nb=nb):
                              for tt in range(nb // 128):
                                ti = b0 // 128 + tt
                                pd = pdn.next()
                                for half in range(2):
                                    for ffc in range(4):
                                        k.op("pe", lambda e: e.matmul(pd[:, half * 512:(half + 1) * 512],
                                                                      lhsT=aT[:, ffc, tt * 128:(tt + 1) * 128],
                                                                      rhs=wd[:, ffc, half * 512:(half + 1) * 512],
                                                                      start=(ffc == 0), stop=(ffc == 3)),
                                             reads=[aT, wd], writes=[pd])
                                if ex == 0:
                                    k.op("dve", lambda e: e.tensor_scalar(out=yacc[:, ti, :], in0=pd[:],
                                                                          scalar1=wc[:, ti, ex:ex + 1], scalar2=None,
                                                                          op0=ALU.mult), reads=[pd, wc], writes=[yacc])
                                else:
                                    k.op("dve", lambda e: e.scalar_tensor_tensor(out=yacc[:, ti, :], in0=pd[:],
                                                                                 scalar=wc[:, ti, ex:ex + 1],
                                                                                 in1=yacc[:, ti, :], op0=ALU.mult,
                                                                                 op1=ALU.add),
                                         reads=[pd, wc, yacc], writes=[yacc])
                            if pending[0] is not None:
                                pending[0]()
                            pending[0] = mk_down
                    if pending[0] is not None:
                        pending[0]()
                        pending[0] = None
                    for ti, t in enumerate(stiles):
                        s = 0 if t >= 2 else 1
                        xt = xpool.next()
                        k.dma("sp", xt[:], src[t * 128:(t + 1) * 128, :], reads=[d_src[t]], writes=[xt])
                        k.op("dve", lambda e: e.tensor_tensor(out=yacc[:, ti, :], in0=yacc[:, ti, :], in1=g2bc[:, s, :],
                                                               op=ALU.mult), reads=[yacc, g2bc], writes=[yacc])
                        k.op("dve", lambda e: e.tensor_tensor(out=xt[:], in0=xt[:], in1=yacc[:, ti, :], op=ALU.add),
                             reads=[xt, yacc], writes=[xt])
                        dap, dbuf = dst_fn(t)
                        k.dma("pool", dap, xt[:], reads=[xt], writes=[dbuf])
            k.barrier()

        if stop_after == "moe0":
            d_y = Buf(None, "y")

            def dst0(t):
                if t >= 2:
                    return y_out[(t - 2) * 128:(t - 1) * 128, :], d_y
                return x2[t * 128:(t + 1) * 128, :], d_x2[t]
            moe(0, x1, d_x1, dst0, [0, 1] + list(range(2, NT)))
            k.drain_all()
            return nc

        moe(0, x1, d_x1, lambda t: (x2[t * 128:(t + 1) * 128, :], d_x2[t]), [0, 1] + list(range(2, NT)))
        MLA_SCALE = 96 ** -0.5
        O_Z, O_BETA = 1536, 2048

        def odd_pass(direction):
            fwd = direction == 0
            with ExitStack() as es:
                A1, S1 = load_modcols(es, 1, norm_mix_w[1], 0)
                win = k.sb("owin", [128, 8, 2480], BF16, es)
                cdiag = k.sb("cdiag", [128, 60, 128], BF16, es)
                tri = k.sb("otri", [128, 128], F32, es)
                stri = k.sb("ostri", [128, 128], F32, es)
                allb = k.sb("oallb", [128, 128], F32, es)
                nallb = k.sb("onallb", [128, 128], F32, es)
                self_ = k.sb("oself", [128, 128], F32, es)
                sels_ = k.sb("osels", [128, 128], F32, es)
                aneg_bc = k.sb("aneg", [128, 8], F32, es)
                dtb_bc = k.sb("dtb", [128, 8], F32, es)
                if fwd:
                    wq = k.sb("wq", [128, 2, 768], BF16, es)
                    wkv = k.sb("wkv", [128, 1, 1024], BF16, es)
                    qnw_bc = k.sb("qnw", [128, 256], F32, es)
                    kvnw_bc = k.sb("kvnw", [128, 128], F32, es)
                    qkq_bc = k.sb("qkq", [128, 96], F32, es)
                    qkk_bc = k.sb("qkk", [128, 96], F32, es)
                else:
                    wout_g = k.sb("owout_g", [128, 8, 1024], BF16, es)
                    gnw_bc = k.sb("ognw", [128, 128], F32, es)
                with ExitStack() as es2:
                    stage = k.sbpool("stage", [128, 8, 512], F32, 2, es2)
                    load_w_bf16(es2, win, odd_w_in[0], 8, 2480, stage)
                    cw = k.sb("cw", [128, 12, 5], F32, es2)
                    for kk in range(5):
                        k.dma("sp", cw[:, :, kk], gdn_conv_w[0, kk, :].rearrange("(c p) -> p c", p=128), writes=[cw],
                              allow_slow_non_contiguous=True)
                    for c in range(12):
                        for kk in range(5):
                            k.op("dve" if (c + kk) % 2 else "pool",
                                 lambda e: e.tensor_scalar(out=cdiag[:, c * 5 + kk, :], in0=ident[:],
                                                           scalar1=cw[:, c, kk:kk + 1], scalar2=None, op0=ALU.mult),
                                 reads=[ident, cw], writes=[cdiag])
                    tr_ = blockmask("tri_", "le" if fwd else "ge", es2)
                    al_ = blockmask("all_", "all", es2)
                    k.op("dve", lambda e: e.tensor_copy(out=tri[:], in_=tr_[:]), reads=[tr_], writes=[tri])
                    k.op("dve", lambda e: e.tensor_tensor(out=stri[:], in0=tr_[:], in1=ident[:], op=ALU.subtract),
                         reads=[tr_, ident], writes=[stri])
                    k.op("dve", lambda e: e.tensor_copy(out=allb[:], in_=al_[:]), reads=[al_], writes=[allb])
                    k.op("dve", lambda e: e.tensor_scalar(out=nallb[:], in0=al_[:], scalar1=-1.0, scalar2=None,
                                                          op0=ALU.mult), reads=[al_], writes=[nallb])
                    k.op("pool", lambda e: e.memset(self_[:], 0.0), writes=[self_])
                    k.op("pool", lambda e: e.memset(sels_[:], 0.0), writes=[sels_])
                    f0, s0_ = (0, 64) if fwd else (64, 0)
                    k.op("pool", lambda e: e.tensor_copy(out=self_[f0:f0 + 64, :], in_=ones[f0:f0 + 64, :]),
                         reads=[ones], writes=[self_])
                    k.op("pool", lambda e: e.tensor_copy(out=sels_[s0_:s0_ + 64, :], in_=ones[s0_:s0_ + 64, :]),
                         reads=[ones], writes=[sels_])
                    k.dma("sp", aneg_bc[:], gdn_a_log[0].rearrange("a b -> (a b)").partition_broadcast(128),
                          writes=[aneg_bc])
                    k.op("act", lambda e: e.activation(out=aneg_bc[:], in_=aneg_bc[:], func=AF.Exp), reads=[aneg_bc],
                         writes=[aneg_bc])
                    k.op("dve", lambda e: e.tensor_scalar(out=aneg_bc[:], in0=aneg_bc[:], scalar1=-1.0, scalar2=None,
                                                          op0=ALU.mult), reads=[aneg_bc], writes=[aneg_bc])
                    k.dma("sp", dtb_bc[:], gdn_dt_bias[0].rearrange("a b -> (a b)").partition_broadcast(128),
                          writes=[dtb_bc])
                    if fwd:
                        st = stage.next()
                        k.dma("sp", st[:, 0:2, 0:512], mla_wq_up[0][:, 0:512].rearrange("(kc p) n -> p kc n", p=128),
                              writes=[st])
                        k.op("dve", lambda e: e.tensor_copy(out=wq[:, :, 0:512], in_=st[:, 0:2, 0:512]), reads=[st],
                             writes=[wq])
                        st = stage.next()
                        k.dma("sp", st[:, 0:2, 0:256], mla_wq_up[0][:, 512:768].rearrange("(kc p) n -> p kc n", p=128),
                              writes=[st])
                        k.op("dve", lambda e: e.tensor_copy(out=wq[:, :, 512:768], in_=st[:, 0:2, 0:256]), reads=[st],
                             writes=[wq])
                        for c0 in (0, 512):
                            st = stage.next()
                            k.dma("sp", st[:, 0:1, 0:512], mla_wkv_up[0][:, c0:c0 + 512].rearrange(
                                "(kc p) n -> p kc n", p=128), writes=[st])
                            k.op("dve", lambda e: e.tensor_copy(out=wkv[:, :, c0:c0 + 512], in_=st[:, 0:1, 0:512]),
                                 reads=[st], writes=[wkv])
                        k.dma("sp", qnw_bc[:], mla_q_norm_w[0].partition_broadcast(128), writes=[qnw_bc])
                        k.dma("sp", kvnw_bc[:], mla_kv_norm_w[0].partition_broadcast(128), writes=[kvnw_bc])
                        k.dma("sp", qkq_bc[:], mla_qk_norm_q[0].partition_broadcast(128), writes=[qkq_bc])
                        k.op("dve", lambda e: e.tensor_scalar(out=qkq_bc[:], in0=qkq_bc[:], scalar1=MLA_SCALE,
                                                              scalar2=None, op0=ALU.mult), reads=[qkq_bc],
                             writes=[qkq_bc])
                        k.dma("sp", qkk_bc[:], mla_qk_norm_k[0].partition_broadcast(128), writes=[qkk_bc])
                    else:
                        gbc = k.sb("gbc", [128, 1024], F32, es2)
                        k.dma("sp", gbc[:], modrow[1, 0, 2 * D:3 * D].partition_broadcast(128), reads=[d_modrow],
                              writes=[gbc])
                        for c0 in range(0, 1024, 512):
                            st = stage.next()
                            k.dma("sp", st[:], odd_w_out[0][:, c0:c0 + 512].rearrange("(kc p) n -> p kc n", p=128),
                                  writes=[st])
                            for kc in range(8):
                                k.op("dve" if kc % 2 else "pool",
                                     lambda e: e.tensor_tensor(out=wout_g[:, kc, c0:c0 + 512], in0=st[:, kc, :],
                                                               in1=gbc[:, c0:c0 + 512], op=ALU.mult),
                                     reads=[st, gbc], writes=[wout_g])
                        k.dma("sp", gnw_bc[:], gdn_norm_w[0].partition_broadcast(128), writes=[gnw_bc])
                    k.barrier()

                Sst = k.sb("oSst", [128, 4, 128], F32, es)
                k.op("pool", lambda e: e.memset(Sst[:], 0.0), writes=[Sst])
                xpool = k.sbpool("oxt", [128, 1024], F32, 2, es)
                rpool = k.sbpool("ors", [128, 8], F32, 12, es)
                xnpool = {"junk": k.sbpool("ojunk", [128, 1024], BF16, 1, es),
                          "bf": k.sbpool("oxnb", [128, 1024], BF16, 2, es)}
                hTp = k.sbpool("ohT", [128, 8, 128], BF16, 5, es)
                hextp = k.sbpool("ohext", [128, 8, 132], BF16, 2, es)
                pTp = k.sbpool("opT", [128, 12, 132], BF16, 2, es)
                qkvTp = k.sbpool("oqkvT", [128, 12, 128], BF16, 2, es)
                qkvtokp = k.sbpool("oqkvtok", [128, 1536], F32, 2, es)
                sqp = k.sbpool("osq", [128, 1024], F32, 1, es)
                p2pool = k.sbpool("op2", [128, 432], F32, 2, es)
                qkntokp = k.sbpool("oqkntok", [128, 8, 128], BF16, 2, es)
                qknTp = k.sbpool("oqknT", [128, 8, 128], BF16, 2, es)
                hbufs = []
                for h_ in range(4):
                    d_ = {}
                    for nm in ("TL", "Gm", "t1", "t2", "u32", "oa"):
                        d_[nm] = k.sb("h" + nm, [128, 128], F32, es)
                    for nm in ("qkT", "wb", "wT", "kg", "vn", "S0", "S1"):
                        d_[nm] = k.sb("h" + nm, [128, 128], BF16, es)
                    d_["PT"] = [k.sb("hPT", [128, 128], BF16, es) for _ in range(2)]
                    d_["P"] = [k.sb("hP", [128, 128], BF16, es) for _ in range(2)]
                    d_["X32"] = k.sb("hX32", [128, 256], F32, es)
                    d_["Xb"] = k.sb("hXb", [128, 256], BF16, es)
                    hbufs.append(d_)
                s16 = k.sbpool("os16", [128, 4, 8], F32, 6, es)
                osbp = k.sbpool("oosb", [128, 4, 128], F32, 2, es)
                t512 = k.sbpool("ot512", [128, 512], F32, 5, es)
                pst = k.ps("opst", [128, 8, 128], BF16, es)
                ptb = k.ps("optb", [128, 8, 128], BF16, es)
                ptb_s = [Bv(ptb, ptb.t[:, i, :]) for i in range(8)]
                proj = k.pspool("oproj", [128, 512], F32, 2, es)
                pa = k.ps("opa", [128, 512], F32, es)
                pa_s = [Bv(pa, pa.t[:, i * 128:(i + 1) * 128]) for i in range(4)]
                pn = k.ps("opn", [128, 512], F32, es)
                pn_s = [Bv(pn, pn.t[:, i * 128:(i + 1) * 128]) for i in range(4)]
                pb = k.ps("opb", [128, 512], F32, es)
                pb_s = [Bv(pb, pb.t[:, i * 128:(i + 1) * 128]) for i in range(4)]
                pc = k.ps("opc", [128, 512], F32, es)
                pc_s = [Bv(pc, pc.t[:, i * 128:(i + 1) * 128]) for i in range(4)]
                if fwd:
                    qtp = k.sbpool("oqt", [96, 8, 128], BF16, 2, es)
                    vxp = k.sbpool("ovx", [128, 8, 80], BF16, 2, es)
                    for vb_ in vxp.bufs:
                        k.op("pool", lambda e: e.memset(vb_[:], 1.0), writes=[vb_])
                    q96p = k.sbpool("oq96", [128, 8, 96], F32, 2, es)
                    q96bp = k.sbpool("oq96b", [128, 8, 96], BF16, 2, es)
                    ropep = k.sbpool("orope", [128, 2, 16], F32, 2, es)
                    r8p = k.sbpool("or8", [128, 8, 8], F32, 8, es)
                    cqnp = k.sbpool("ocqn", [128, 256], BF16, 2, es)
                    kvsp = k.sbpool("okvs", [128, 512], F32, 2, es)
                    cqnTp = k.sbpool("ocqnT", [128, 3, 128], BF16, 2, es)
                else:
                    catp = k.sbpool("ocat", [128, 512], BF16, 2, es)
                    catTp = k.sbpool("ocatT", [128, 8, 128], BF16, 2, es)

                hcache = {}

                def get_hT(t):
                    if t in hcache:
                        return hcache[t]
                    s = 0 if t >= 2 else 1
                    xt = xpool.next()
                    k.dma("sp", xt[:], x2[t * 128:(t + 1) * 128, :], reads=[d_x2[t]], writes=[xt])
                    hT = hTp.next()
                    for kk_ in [kk_ for kk_, v_ in hcache.items() if v_ is hT]:
                        del hcache[kk_]
                    norm_T(xt, A1, S1, s, rpool, xnpool, pst, out_bf=hT)
                    hcache[t] = hT
                    return hT

                def rope(buf, tabs):
                    for part in range(2):
                        o = 64 + part * 16
                        u1 = buf[:, :, o:o + 8]
                        u2 = buf[:, :, o + 8:o + 16]
                        cs = tabs[:, 0, part * 8:(part + 1) * 8].unsqueeze(1).to_broadcast([128, 8, 8])
                        sn = tabs[:, 1, part * 8:(part + 1) * 8].unsqueeze(1).to_broadcast([128, 8, 8])
                        a = r8p.next(); b = r8p.next(); c_ = r8p.next(); d_ = r8p.next()
                        k.op("dve", lambda e: e.tensor_tensor(out=a[:], in0=u1, in1=cs, op=ALU.mult), reads=[buf, tabs],
                             writes=[a])
                        k.op("dve", lambda e: e.tensor_tensor(out=b[:], in0=u2, in1=sn, op=ALU.mult), reads=[buf, tabs],
                             writes=[b])
                        k.op("dve", lambda e: e.tensor_tensor(out=c_[:], in0=u2, in1=cs, op=ALU.mult), reads=[buf, tabs],
                             writes=[c_])
                        k.op("dve", lambda e: e.tensor_tensor(out=d_[:], in0=u1, in1=sn, op=ALU.mult), reads=[buf, tabs],
                             writes=[d_])
                        k.op("dve", lambda e: e.tensor_tensor(out=u1, in0=a[:], in1=b[:], op=ALU.subtract),
                             reads=[a, b], writes=[buf])
                        k.op("dve", lambda e: e.tensor_tensor(out=u2, in0=c_[:], in1=d_[:], op=ALU.add),
                             reads=[c_, d_], writes=[buf])

                order = list(range(NT)) if fwd else [1, 0] + list(range(NT - 1, 1, -1))
                R1, R2 = (slice(0, 64), slice(64, 128)) if fwd else (slice(64, 128), slice(0, 64))

                def tile_A(t, cx):
                    lat = t >= 2
                    has_prev = t not in (0, 2)
                    has_next = t not in (1, NT - 1)
                    hT = get_hT(t)
                    hprev = get_hT(t - 1) if has_prev else None
                    hnext = get_hT(t + 1) if has_next else None
                    hext = hextp.next()
                    k.op("act", lambda e: e.copy(out=hext[:, :, 2:130], in_=hT[:]), reads=[hT], writes=[hext])
                    yield
                    if has_prev:
                        k.op("dve", lambda e: e.tensor_copy(out=hext[:, :, 0:2], in_=hprev[:, :, 126:128]),
                             reads=[hprev], writes=[hext])
                        yield
                    else:
                        k.op("dve", lambda e: e.memset(hext[:, :, 0:2], 0.0), writes=[hext])
                        yield
                    if has_next:
                        k.op("dve", lambda e: e.tensor_copy(out=hext[:, :, 130:132], in_=hnext[:, :, 0:2]),
                             reads=[hnext], writes=[hext])
                        yield
                    else:
                        k.op("dve", lambda e: e.memset(hext[:, :, 130:132], 0.0), writes=[hext])
                        yield

                    def proj_tok(c0, n):
                        p = proj.next()
                        for kc in range(8):
                            k.op("pe", lambda e: e.matmul(p[:, 0:n], lhsT=hT[:, kc, :], rhs=win[:, kc, c0:c0 + n],
                                                          start=(kc == 0), stop=(kc == 7)), reads=[hT, win], writes=[p])
                        return p

                    yield
                    pT = pTp.next()
                    for g3 in range(4):
                        p = proj.next()
                        pv = p[:, 0:396].rearrange("p (c n) -> p c n", n=132)
                        for ci in range(3):
                            c = g3 * 3 + ci
                            for kc in range(8):
                                k.op("pe", lambda e: e.matmul(pv[:, ci, :], lhsT=win[:, kc, c * 128:(c + 1) * 128],
                                                              rhs=hext[:, kc, :], start=(kc == 0), stop=(kc == 7)),
                                     reads=[win, hext], writes=[p])
                                yield
                        k.op("act", lambda e: e.copy(out=pT[:, g3 * 3:(g3 + 1) * 3, :], in_=pv), reads=[p], writes=[pT])
                        yield
                    qkvT = qkvTp.next()
                    yield
                    for g4 in range(3):
                        p = proj.next()
                        for ci in range(4):
                            c = g4 * 4 + ci
                            for kk in range(5):
                                k.op("pe", lambda e: e.matmul(p[:, ci * 128:(ci + 1) * 128], lhsT=cdiag[:, c * 5 + kk, :],
                                                              rhs=pT[:, c, kk:kk + 128], start=(kk == 0), stop=(kk == 4)),
                                     reads=[cdiag, pT], writes=[p])
                                yield
                        k.op("act", lambda e: e.activation(out=qkvT[:, g4 * 4:(g4 + 1) * 4, :].rearrange("p c n -> p (c n)"),
                                                           in_=p[:], func=AF.Silu), reads=[p], writes=[qkvT])
                        yield
                    qkvtok = qkvtokp.next()
                    for c in range(8):
                        k.op("pe", lambda e: e.transpose(pst[:, c, :], qkvT[:, c, :], identb[:]), reads=[qkvT, identb],
                             writes=[pst])
                        yield
                    k.op("act", lambda e: e.copy(out=qkvtok[:, 0:1024], in_=pst[:].rearrange("p c n -> p (c n)")),
                         reads=[pst], writes=[qkvtok])
                    yield
                    for c in range(4):
                        k.op("pe", lambda e: e.transpose(pst[:, c, :], qkvT[:, 8 + c, :], identb[:]),
                             reads=[qkvT, identb], writes=[pst])
                        yield
                    k.op("act", lambda e: e.copy(out=qkvtok[:, 1024:1536],
                                                 in_=pst[:, 0:4, :].rearrange("p c n -> p (c n)")), reads=[pst],
                         writes=[qkvtok])
                    yield
                    yield
                    sq = sqp.next()
                    k.op("act", lambda e: e.activation(out=sq[:, 0:1024], in_=qkvtok[:, 0:1024], func=AF.Square),
                         reads=[qkvtok], writes=[sq])
                    yield
                    rn = rpool.next()
                    k.op("dve", lambda e: e.tensor_reduce(out=rn[:], in_=sq[:, 0:1024].rearrange("p (h c) -> p h c", c=128),
                                                          axis=AX.X, op=ALU.add), reads=[sq], writes=[rn])
                    yield
                    k.op("dve", lambda e: e.tensor_scalar(out=rn[:], in0=rn[:], scalar1=EPS, scalar2=None, op0=ALU.add),
                         reads=[rn], writes=[rn])
                    yield
                    rsqrt(rn, rn[:], rn[:])
                    k.op("dve", lambda e: e.tensor_scalar(out=rn[:, 0:4], in0=rn[:, 0:4], scalar1=128 ** -0.5,
                                                          scalar2=None, op0=ALU.mult), reads=[rn], writes=[rn])
                    yield
                    qkntok = qkntokp.next()
                    yield
                    k.op("dve", lambda e: e.tensor_tensor(out=qkntok[:],
                                                          in0=qkvtok[:, 0:1024].rearrange("p (h c) -> p h c", c=128),
                                                          in1=rn[:].unsqueeze(2).to_broadcast([128, 8, 128]),
                                                          op=ALU.mult), reads=[qkvtok, rn], writes=[qkntok])
                    yield
                    for c in range(8):
                        k.op("pe", lambda e: e.transpose(pst[:, c, :], qkntok[:, c, :], identb[:]),
                             reads=[qkntok, identb], writes=[pst])
                        yield
                    qknT = qknTp.next()
                    yield
                    k.op("act", lambda e: e.copy(out=qknT[:], in_=pst[:]), reads=[pst], writes=[qknT])
                    yield
                    p2p = proj_tok(O_BETA, 432)
                    yield
                    p2 = p2pool.next()
                    k.op("act", lambda e: e.copy(out=p2[:, 0:432], in_=p2p[:, 0:432]), reads=[p2p], writes=[p2])
                    yield
                    bl = s16.next()
                    blv = bl[:].rearrange("p a b -> p (a b)")
                    k.op("act", lambda e: e.activation(out=blv[:, 0:8], in_=p2[:, 0:8], func=AF.Sigmoid), reads=[p2],
                         writes=[bl])
                    yield
                    sp_ = s16.next()
                    spv = sp_[:].rearrange("p a b -> p (a b)")
                    k.op("dve", lambda e: e.tensor_tensor(out=spv[:, 0:8], in0=p2[:, 8:16], in1=dtb_bc[:], op=ALU.add),
                         reads=[p2, dtb_bc], writes=[sp_])
                    yield
                    k.op("act", lambda e: e.activation(out=spv[:, 8:16], in_=spv[:, 0:8], func=AF.Abs), reads=[sp_],
                         writes=[sp_])
                    yield
                    k.op("act", lambda e: e.activation(out=spv[:, 8:16], in_=spv[:, 8:16], func=AF.Exp, scale=-1.0),
                         reads=[sp_], writes=[sp_])
                    yield
                    k.op("act", lambda e: e.activation(out=spv[:, 8:16], in_=spv[:, 8:16], func=AF.Ln, bias=1.0),
                         reads=[sp_], writes=[sp_])
                    yield
                    k.op("dve", lambda e: e.tensor_scalar(out=spv[:, 0:8], in0=spv[:, 0:8], scalar1=0.0, scalar2=None,
                                                          op0=ALU.max), reads=[sp_], writes=[sp_])
                    yield
                    k.op("dve", lambda e: e.tensor_tensor(out=spv[:, 0:8], in0=spv[:, 0:8], in1=spv[:, 8:16],
                                                          op=ALU.add), reads=[sp_], writes=[sp_])
                    yield
                    k.op("dve", lambda e: e.tensor_tensor(out=blv[:, 8:16], in0=spv[:, 0:8], in1=aneg_bc[:],
                                                          op=ALU.mult), reads=[sp_, aneg_bc], writes=[bl])
                    yield
                    dsl = slice(direction * 4, direction * 4 + 4)
                    yield
                    la4 = bl[:, 1, dsl]
                    be4 = bl[:, 0, dsl]
                    pg = pa_s[3]
                    for i_, lh in enumerate((tri, allb, self_, sels_)):
                        k.op("pe", lambda e: e.matmul(pg[:, i_ * 4:(i_ + 1) * 4], lhsT=lh[:], rhs=la4, start=True,
                                                      stop=True), reads=[lh, bl], writes=[pg])
                        yield
                    E4 = s16.next()
                    k.op("dve", lambda e: e.tensor_copy(out=E4[:, :, 0:4], in_=pg[:, 0:16].rearrange("p (a b) -> p a b", b=4)),
                         reads=[pg], writes=[E4])
                    yield
                    k.op("dve", lambda e: e.tensor_tensor(out=E4[:, 1, 0:4], in0=E4[:, 1, 0:4], in1=E4[:, 0, 0:4],
                                                          op=ALU.subtract), reads=[E4], writes=[E4])
                    yield
                    k.op("act", lambda e: e.activation(out=E4[:, :, 0:4], in_=E4[:, :, 0:4], func=AF.Exp), reads=[E4],
                         writes=[E4])
                    yield
                    cx.update(dict(lat=lat, hT=hT, qkvtok=qkvtok, qkntok=qkntok, qknT=qknT, bl=bl, la4=la4, be4=be4,
                                   E4=E4, p2=p2))
                    yield

                def tile_B(t, cx):
                    lat = cx["lat"]; hT = cx["hT"]; qkvtok = cx["qkvtok"]; qkntok = cx["qkntok"]; qknT = cx["qknT"]
                    bl = cx["bl"]; la4 = cx["la4"]; be4 = cx["be4"]; E4 = cx["E4"]; p2 = cx["p2"]

                    def proj_tok(c0, n):
                        p = proj.next()
                        for kc in range(8):
                            k.op("pe", lambda e: e.matmul(p[:, 0:n], lhsT=hT[:, kc, :], rhs=win[:, kc, c0:c0 + n],
                                                          start=(kc == 0), stop=(kc == 7)), reads=[hT, win], writes=[p])
                        return p

                    osb = osbp.next()
                    HB = hbufs
                    yield
                    for h in range(4):
                        B_ = HB[h]
                        knT_h = qknT[:, 4 + h, :]
                        qnT_h = qknT[:, h, :]
                        la_c = la4[:, h:h + 1]
                        be_c = be4[:, h:h + 1]
                        eg_c = E4[:, 0, h:h + 1]
                        TL = B_["TL"]
                        k.op("dve", lambda e: e.tensor_scalar(out=TL[:], in0=tri[:], scalar1=la_c, scalar2=None,
                                                               op0=ALU.mult), reads=[tri, bl], writes=[TL])
                        yield
                        reg = pa_s if h % 2 == 0 else pc_s
                        GT, KK, QK = reg[0], reg[1], reg[2]
                        k.op("pe", lambda e: e.matmul(GT[:], lhsT=allb[:], rhs=TL[:], start=True, stop=False),
                             reads=[allb, TL], writes=[GT])
                        yield
                        k.op("pe", lambda e: e.matmul(GT[:], lhsT=TL[:], rhs=nallb[:], start=False, stop=True),
                             reads=[nallb, TL], writes=[GT])
                        yield
                        k.op("pe", lambda e: e.matmul(KK[:], lhsT=knT_h, rhs=knT_h, start=True, stop=True),
                             reads=[qknT], writes=[KK])
                        yield
                        k.op("pe", lambda e: e.matmul(QK[:], lhsT=knT_h, rhs=qnT_h, start=True, stop=True),
                             reads=[qknT], writes=[QK])
                        yield
                        Gm = B_["Gm"]
                        k.op("dve", lambda e: e.tensor_scalar(out=Gm[:], in0=GT[:], scalar1=0.0, scalar2=None,
                                                              op0=ALU.min), reads=[GT], writes=[Gm])
                        yield
                        k.op("act", lambda e: e.activation(out=Gm[:], in_=Gm[:], func=AF.Exp), reads=[Gm], writes=[Gm])
                        yield
                        t1 = B_["t1"]
                        k.op("dve", lambda e: e.tensor_tensor(out=t1[:], in0=KK[:], in1=Gm[:], op=ALU.mult),
                             reads=[KK, Gm], writes=[t1])
                        yield
                        PT = B_["PT"][0]
                        k.op("dve", lambda e: e.scalar_tensor_tensor(out=PT[:], in0=t1[:], scalar=be_c, in1=stri[:],
                                                                     op0=ALU.mult, op1=ALU.mult),
                             reads=[t1, bl, stri], writes=[PT])
                        yield
                        t2 = B_["t2"]
                        k.op("dve", lambda e: e.tensor_tensor(out=t2[:], in0=QK[:], in1=Gm[:], op=ALU.mult),
                             reads=[QK, Gm], writes=[t2])
                        yield
                        qkT = B_["qkT"]
                        k.op("dve", lambda e: e.tensor_tensor(out=qkT[:], in0=t2[:], in1=tri[:], op=ALU.mult),
                             reads=[t2, tri], writes=[qkT])
                        yield
                        X32 = B_["X32"]
                        Xb = B_["Xb"]
                        k.op("act", lambda e: e.copy(out=X32[:, 0:128], in_=qkvtok[:, 1024 + h * 128:1024 + (h + 1) * 128]),
                             reads=[qkvtok], writes=[X32])
                        yield
                        k.op("dve", lambda e: e.tensor_scalar(out=X32[:, 128:256], in0=qkntok[:, 4 + h, :], scalar1=eg_c,
                                                               scalar2=None, op0=ALU.mult), reads=[qkntok, E4],
                             writes=[X32])
                        yield
                        k.op("act", lambda e: e.copy(out=Xb[:], in_=X32[:]), reads=[X32], writes=[Xb])
                    yield
                    for h in range(4):
                        B_ = HB[h]
                        ptp = ptb_s[h]
                        k.op("pe", lambda e: e.transpose(ptp[:], B_["PT"][0][:], identb[:]),
                             reads=[B_["PT"][0], identb], writes=[ptp])
                        yield
                        k.op("act", lambda e: e.copy(out=B_["P"][0][:], in_=ptp[:]), reads=[ptp], writes=[B_["P"][0]])
                        yield
                    xreg = [(pn_s[0], pn_s[1]), (pn_s[2], pn_s[3]), (pb_s[0], pb_s[1]), (pb_s[2], pb_s[3])]
                    sreg = [(pa_s[0], pc_s[0]), (pa_s[1], pc_s[1]), (pa_s[2], pc_s[2]), (pa_s[3], pc_s[3])]
                    cur = [0, 0, 0, 0]

                    def x_update(h, sub):
                        B_ = HB[h]
                        PT = B_["PT"][cur[h]]
                        xa, xb_ = xreg[h]
                        k.op("pe", lambda e: e.matmul(xa[:], lhsT=PT[:], rhs=B_["Xb"][:, 0:128], start=True, stop=True),
                             reads=[PT, B_["Xb"]], writes=[xa])
                        k.op("pe", lambda e: e.matmul(xb_[:], lhsT=PT[:], rhs=B_["Xb"][:, 128:256], start=True,
                                                      stop=True), reads=[PT, B_["Xb"]], writes=[xb_])

                    def x_apply(h, sub, last):
                        B_ = HB[h]
                        xa, xb_ = xreg[h]
                        op_ = ALU.subtract if sub else ALU.add
                        k.op("dve", lambda e: e.tensor_tensor(out=B_["X32"][:, 0:128], in0=B_["X32"][:, 0:128], in1=xa[:],
                                                              op=op_), reads=[B_["X32"], xa], writes=[B_["X32"]])
                        k.op("dve", lambda e: e.tensor_tensor(out=B_["X32"][:, 128:256], in0=B_["X32"][:, 128:256],
                                                              in1=xb_[:], op=op_), reads=[B_["X32"], xb_],
                             writes=[B_["X32"]])
                        if not last:
                            k.op("act", lambda e: e.copy(out=B_["Xb"][:], in_=B_["X32"][:]), reads=[B_["X32"]],
                                 writes=[B_["Xb"]])

                    yield
                    for h in range(4):
                        x_update(h, True)
                    yield
                    for h in range(4):
                        x_apply(h, True, False)
                    for lvl in range(1, 6):
                        yield
                        for h in range(4):
                            B_ = HB[h]
                            c_ = cur[h]
                            P, PT = B_["P"][c_], B_["PT"][c_]
                            sa, sb_ = sreg[h]
                            k.op("pe", lambda e: e.matmul(sa[:], lhsT=P[:], rhs=PT[:], start=True, stop=True),
                                 reads=[P, PT], writes=[sa])
                            yield
                            if lvl < 5:
                                k.op("pe", lambda e: e.matmul(sb_[:], lhsT=PT[:], rhs=P[:], start=True, stop=True),
                                     reads=[P, PT], writes=[sb_])
                        yield
                        for h in range(4):
                            B_ = HB[h]
                            n_ = 1 - cur[h]
                            sa, sb_ = sreg[h]
                            k.op("act", lambda e: e.copy(out=B_["PT"][n_][:], in_=sa[:]), reads=[sa],
                                 writes=[B_["PT"][n_]])
                            yield
                            if lvl < 5:
                                k.op("dve", lambda e: e.tensor_copy(out=B_["P"][n_][:], in_=sb_[:]), reads=[sb_],
                                     writes=[B_["P"][n_]])
                                yield
                            cur[h] = n_
                        yield
                        for h in range(4):
                            x_update(h, False)
                        yield
                        for h in range(4):
                            x_apply(h, False, lvl == 5)
                    yield
                    for h in range(4):
                        B_ = HB[h]
                        be_c = be4[:, h:h + 1]
                        egl_c = E4[:, 1, h:h + 1]
                        k.op("dve", lambda e: e.tensor_scalar(out=B_["u32"][:], in0=B_["X32"][:, 0:128], scalar1=be_c,
                                                              scalar2=None, op0=ALU.mult), reads=[B_["X32"], bl],
                             writes=[B_["u32"]])
                        yield
                        k.op("dve", lambda e: e.tensor_scalar(out=B_["wb"][:], in0=B_["X32"][:, 128:256], scalar1=be_c,
                                                              scalar2=None, op0=ALU.mult), reads=[B_["X32"], bl],
                             writes=[B_["wb"]])
                        yield
                        k.op("dve", lambda e: e.tensor_scalar(out=B_["kg"][:], in0=qkntok[:, 4 + h, :], scalar1=egl_c,
                                                               scalar2=None, op0=ALU.mult), reads=[qkntok, E4],
                             writes=[B_["kg"]])
                        yield
                        k.op("act", lambda e: e.copy(out=B_["S0"][:], in_=Sst[:, h, :]), reads=[Sst], writes=[B_["S0"]])
                    yield
                    for h in range(4):
                        B_ = HB[h]
                        wtp = ptb_s[4 + h]
                        k.op("pe", lambda e: e.transpose(wtp[:], B_["wb"][:], identb[:]), reads=[B_["wb"], identb],
                             writes=[wtp])
                        yield
                        k.op("act", lambda e: e.copy(out=B_["wT"][:], in_=wtp[:]), reads=[wtp], writes=[B_["wT"]])
                        yield
                    banks = [pa_s, pn_s, pb_s, pc_s]
                    yield
                    for h in range(4):
                        B_ = HB[h]
                        r0, r1 = banks[h][0], banks[h][1]
                        k.op("pe", lambda e: e.matmul(r0[:], lhsT=B_["wT"][:], rhs=B_["S0"][:], start=True, stop=True),
                             reads=[B_["wT"], B_["S0"]], writes=[r0])
                    yield
                    for h in range(4):
                        B_ = HB[h]
                        r0, r1 = banks[h][0], banks[h][1]
                        k.op("dve", lambda e: e.tensor_tensor(out=B_["vn"][R1, :], in0=B_["u32"][R1, :], in1=r0[R1, :],
                                                              op=ALU.subtract), reads=[B_["u32"], r0], writes=[B_["vn"]])
                        yield
                        k.op("pe", lambda e: e.matmul(r1[:], lhsT=B_["kg"][R1, :], rhs=B_["vn"][R1, :], start=True,
                                                      stop=True), reads=[B_["kg"], B_["vn"]], writes=[r1])
                    yield
                    for h in range(4):
                        B_ = HB[h]
                        r0, r1 = banks[h][0], banks[h][1]
                        k.op("dve", lambda e: e.scalar_tensor_tensor(out=Sst[:, h, :], in0=Sst[:, h, :],
                                                                     scalar=E4[:, 2, h:h + 1], in1=r1[:],
                                                                     op0=ALU.mult, op1=ALU.add),
                             reads=[Sst, E4, r1], writes=[Sst])
                        yield
                        k.op("act", lambda e: e.copy(out=B_["S1"][:], in_=Sst[:, h, :]), reads=[Sst], writes=[B_["S1"]])
                        yield
                        k.op("pe", lambda e: e.matmul(r0[:], lhsT=B_["wT"][:], rhs=B_["S1"][:], start=True, stop=True),
                             reads=[B_["wT"], B_["S1"]], writes=[r0])
                    yield
                    for h in range(4):
                        B_ = HB[h]
                        r0, r1 = banks[h][0], banks[h][1]
                        k.op("dve", lambda e: e.tensor_tensor(out=B_["vn"][R2, :], in0=B_["u32"][R2, :], in1=r0[R2, :],
                                                              op=ALU.subtract), reads=[B_["u32"], r0], writes=[B_["vn"]])
                        yield
                        k.op("pe", lambda e: e.matmul(r1[:], lhsT=B_["kg"][R2, :], rhs=B_["vn"][R2, :], start=True,
                                                      stop=True), reads=[B_["kg"], B_["vn"]], writes=[r1])
                    yield
                    for h in range(4):
                        B_ = HB[h]
                        r0, r1, r2, r3 = banks[h]
                        qnT_h = qknT[:, h, :]
                        k.op("dve", lambda e: e.scalar_tensor_tensor(out=Sst[:, h, :], in0=Sst[:, h, :],
                                                                     scalar=E4[:, 3, h:h + 1], in1=r1[:],
                                                                     op0=ALU.mult, op1=ALU.add),
                             reads=[Sst, E4, r1], writes=[Sst])
                        yield
                        k.op("pe", lambda e: e.matmul(r0[:], lhsT=B_["qkT"][:], rhs=B_["vn"][:], start=True, stop=True),
                             reads=[B_["qkT"], B_["vn"]], writes=[r0])
                        yield
                        k.op("pe", lambda e: e.matmul(r2[:], lhsT=qnT_h, rhs=B_["S0"][:], start=True, stop=True),
                             reads=[qknT, B_["S0"]], writes=[r2])
                        yield
                        k.op("pe", lambda e: e.matmul(r3[:], lhsT=qnT_h, rhs=B_["S1"][:], start=True, stop=True),
                             reads=[qknT, B_["S1"]], writes=[r3])
                    yield
                    for h in range(4):
                        B_ = HB[h]
                        r0, r1, r2, r3 = banks[h]
                        oa = B_["oa"]
                        k.op("act", lambda e: e.copy(out=oa[:], in_=r0[:]), reads=[r0], writes=[oa])
                        yield
                        k.op("dve", lambda e: e.scalar_tensor_tensor(out=osb[R1, h, :], in0=r2[R1, :],
                                                                     scalar=E4[R1, 0, h:h + 1], in1=oa[R1, :],
                                                                     op0=ALU.mult, op1=ALU.add),
                             reads=[r2, E4, oa], writes=[osb])
                        yield
                        k.op("dve", lambda e: e.scalar_tensor_tensor(out=osb[R2, h, :], in0=r3[R2, :],
                                                                     scalar=E4[R2, 0, h:h + 1], in1=oa[R2, :],
                                                                     op0=ALU.mult, op1=ALU.add),
                             reads=[r3, E4, oa], writes=[osb])
                        yield
                    osbv = osb[:].rearrange("p h c -> p (h c)")
                    if fwd:
                        if lat:
                            k.dma("pool", of_scr[t * 128:(t + 1) * 128, :], osbv, reads=[osb], writes=[d_of[t]])
                            yield
                        if os.environ.get("SKIP_MLA"):
                            return
                        tabs = None
                        if lat:
                            tabs = ropep.next()
                            n0 = (t - 2) * 128
                            k.dma("sp", tabs[:, 0, :], rope_cos[n0:n0 + 128, :], writes=[tabs])
                            yield
                            k.dma("sp", tabs[:, 1, :], rope_sin[n0:n0 + 128, :], writes=[tabs])
                            yield
                        jk = t512.next()
                        r1 = rpool.next()
                        k.op("act", lambda e: e.activation(out=jk[:, 0:128], in_=p2[:, 272:400], func=AF.Square,
                                                           accum_out=r1[:, 0:1]), reads=[p2], writes=[jk, r1])
                        yield
                        k.op("dve", lambda e: e.tensor_scalar(out=r1[:, 1:2], in0=r1[:, 0:1], scalar1=1.0 / 128,
                                                              scalar2=EPS, op0=ALU.mult, op1=ALU.add), reads=[r1],
                             writes=[r1])
                        yield
                        rsqrt(r1, r1[:, 2:3], r1[:, 1:2])
                        cn = cqnp.next()
                        k.op("dve", lambda e: e.scalar_tensor_tensor(out=cn[:, 0:128], in0=p2[:, 272:400],
                                                                     scalar=r1[:, 2:3], in1=kvnw_bc[:], op0=ALU.mult,
                                                                     op1=ALU.mult), reads=[p2, r1, kvnw_bc], writes=[cn])
                        yield
                        cnT = cqnTp.next()
                        k.op("pe", lambda e: e.transpose(ptb_s[0][:], cn[:, 0:128], identb[:]), reads=[cn, identb],
                             writes=[ptb_s[0]])
                        yield
                        k.op("act", lambda e: e.copy(out=cnT[:, 2, :], in_=ptb_s[0][:]), reads=[ptb_s[0]], writes=[cnT])
                        yield
                        MSUB = int(os.environ.get("MLA_SUB", "9"))
                        if MSUB < 2:
                            return
                        yield
                        k96 = q96p.next()
                        vx = vxp.next()
                        for half in range(2):
                            pk = proj.next()
                            k.op("pe", lambda e: e.matmul(pk[:], lhsT=cnT[:, 2, :], rhs=wkv[:, 0, half * 512:(half + 1) * 512],
                                                          start=True, stop=True), reads=[cnT, wkv], writes=[pk])
                            yield
                            pkv = pk[:].rearrange("p (h c) -> p h c", c=128)
                            hs4 = slice(half * 4, half * 4 + 4)
                            kvs = kvsp.next()
                            k.op("act", lambda e: e.copy(out=kvs[:], in_=pk[:]), reads=[pk], writes=[kvs])
                            yield
                            kvsv = kvs[:].rearrange("p (h c) -> p h c", c=128)
                            k.op("act", lambda e: e.copy(out=k96[:, hs4, 0:64], in_=kvsv[:, :, 0:64]), reads=[kvs],
                                 writes=[k96])
                            yield
                            k.op("dve", lambda e: e.tensor_copy(out=vx[:, hs4, 0:64], in_=kvsv[:, :, 64:128]), reads=[kvs],
                                 writes=[vx])
                            yield
                        if MSUB < 3:
                            return
                        k.op("dve", lambda e: e.tensor_copy(out=k96[:, :, 64:96],
                                                            in_=p2[:, 400:432].unsqueeze(1).to_broadcast([128, 8, 32])),
                             reads=[p2], writes=[k96])
                        yield
                        if MSUB < 4:
                            return
                        k.dma("sp", VX[:, t, :, :].rearrange("h p c -> p h c"), vx[:], reads=[vx], writes=[d_VX])
                        yield

                        def headnorm(buf, wbc):
                            sqh = q96p.next()
                            k.op("act", lambda e: e.activation(out=sqh[:], in_=buf[:], func=AF.Square),
                                 reads=[buf], writes=[sqh])
                            r8 = rpool.next()
                            k.op("dve", lambda e: e.tensor_reduce(out=r8[:], in_=sqh[:], axis=AX.X, op=ALU.add),
                                 reads=[sqh], writes=[r8])
                            k.op("dve", lambda e: e.tensor_scalar(out=r8[:], in0=r8[:], scalar1=1.0 / 96, scalar2=EPS,
                                                                  op0=ALU.mult, op1=ALU.add), reads=[r8], writes=[r8])
                            rsqrt(r8, r8[:], r8[:])
                            k.op("dve", lambda e: e.tensor_tensor(out=buf[:], in0=buf[:],
                                                                  in1=r8[:].unsqueeze(2).to_broadcast([128, 8, 96]),
                                                                  op=ALU.mult), reads=[buf, r8], writes=[buf])
                            k.op("dve", lambda e: e.tensor_tensor(out=buf[:], in0=buf[:],
                                                                   in1=wbc[:].unsqueeze(1).to_broadcast([128, 8, 96]),
                                                                   op=ALU.mult), reads=[buf, wbc], writes=[buf])

                        def heads_T(buf, dram_ap, dbuf):
                            bb = q96bp.next()
                            k.op("act", lambda e: e.copy(out=bb[:], in_=buf[:]), reads=[buf], writes=[bb])
                            for hh in range(8):
                                k.op("pe", lambda e: e.transpose(pst[0:96, hh, :], bb[:, hh, :], identb[:]),
                                     reads=[bb, identb], writes=[pst])
                            qt = qtp.next()
                            k.op("act", lambda e: e.copy(out=qt[:], in_=pst[0:96, :, :]), reads=[pst], writes=[qt])
                            k.dma("sp", dram_ap, qt[:], reads=[qt], writes=[dbuf])

                        MST = int(os.environ.get("MLA_STAGE", "9"))
                        if MST < 2:
                            return
                        yield
                        headnorm(k96, qkk_bc)
                        if lat:
                            rope(k96, tabs)
                        if MST < 3:
                            return
                        heads_T(k96, KT[:, :, t * 128:(t + 1) * 128].rearrange("h d n -> d h n"), d_KT)
                        if lat and MST >= 4:
                            r2 = rpool.next()
                            k.op("act", lambda e: e.activation(out=jk[:, 0:256], in_=p2[:, 16:272], func=AF.Square,
                                                               accum_out=r2[:, 0:1]), reads=[p2], writes=[jk, r2])
                            yield
                            k.op("dve", lambda e: e.tensor_scalar(out=r2[:, 1:2], in0=r2[:, 0:1], scalar1=1.0 / 256,
                                                                  scalar2=EPS, op0=ALU.mult, op1=ALU.add), reads=[r2],
                                 writes=[r2])
                            yield
                            rsqrt(r2, r2[:, 2:3], r2[:, 1:2])
                            cq = cqnp.next()
                            k.op("dve", lambda e: e.scalar_tensor_tensor(out=cq[:], in0=p2[:, 16:272], scalar=r2[:, 2:3],
                                                                         in1=qnw_bc[:], op0=ALU.mult, op1=ALU.mult),
                                 reads=[p2, r2, qnw_bc], writes=[cq])
                            yield
                            cqT = cqnTp.next()
                            for c in range(2):
                                k.op("pe", lambda e: e.transpose(ptb_s[1 + c][:], cq[:, c * 128:(c + 1) * 128], identb[:]),
                                     reads=[cq, identb], writes=[ptb_s[1 + c]])
                                yield
                                k.op("act", lambda e: e.copy(out=cqT[:, c, :], in_=ptb_s[1 + c][:]), reads=[ptb_s[1 + c]],
                                     writes=[cqT])
                            yield
                            q96 = q96p.next()
                            q96v = q96[:].rearrange("p h c -> p (h c)")
                            for (c0, n) in ((0, 512), (512, 256)):
                                pq = proj.next()
                                for c in range(2):
                                    k.op("pe", lambda e: e.matmul(pq[:, 0:n], lhsT=cqT[:, c, :], rhs=wq[:, c, c0:c0 + n],
                                                                  start=(c == 0), stop=(c == 1)), reads=[cqT, wq],
                                         writes=[pq])
                                    yield
                                k.op("act", lambda e: e.copy(out=q96v[:, c0:c0 + n], in_=pq[:, 0:n]), reads=[pq],
                                     writes=[q96])
                                yield
                            headnorm(q96, qkq_bc)
                            rope(q96, tabs)
                            n0 = (t - 2) * 128
                            heads_T(q96, QT[:, :, n0:n0 + 128].rearrange("h d n -> d h n"), d_QT)
                        return
                    if not lat:
                        return
                    ofl = t512.next()
                    k.dma("sp", ofl[:], of_scr[t * 128:(t + 1) * 128, :], reads=[d_of[t]], writes=[ofl])
                    yield
                    k.op("dve", lambda e: e.tensor_tensor(out=osbv, in0=osbv, in1=ofl[:], op=ALU.add), reads=[osb, ofl],
                         writes=[osb])
                    yield
                    if dbg:
                        k.dma("pool", ob_scr[t * 128:(t + 1) * 128, :], osbv, reads=[osb])
                        yield
                    sq2 = t512.next()
                    k.op("act", lambda e: e.activation(out=sq2[:], in_=osbv, func=AF.Square), reads=[osb],
                         writes=[sq2])
                    yield
                    rs = rpool.next()
                    k.op("dve", lambda e: e.tensor_reduce(out=rs[:, 0:4], in_=sq2[:].rearrange("p (h c) -> p h c", c=128),
                                                          axis=AX.X, op=ALU.add), reads=[sq2], writes=[rs])
                    yield
                    k.op("dve", lambda e: e.tensor_scalar(out=rs[:, 0:4], in0=rs[:, 0:4], scalar1=1.0 / 128, scalar2=EPS,
                                                          op0=ALU.mult, op1=ALU.add), reads=[rs], writes=[rs])
                    yield
                    rsqrt(rs, rs[:, 0:4], rs[:, 0:4])
                    k.op("dve", lambda e: e.tensor_tensor(out=osb[:], in0=osb[:],
                                                          in1=rs[:, 0:4].unsqueeze(2).to_broadcast([128, 4, 128]),
                                                          op=ALU.mult), reads=[osb, rs], writes=[osb])
                    yield
                    k.op("dve", lambda e: e.tensor_tensor(out=osb[:], in0=osb[:],
                                                           in1=gnw_bc[:].unsqueeze(1).to_broadcast([128, 4, 128]),
                                                           op=ALU.mult), reads=[osb, gnw_bc], writes=[osb])
                    yield
                    pz = proj_tok(O_Z, 512)
                    sgt = t512.next()
                    k.op("act", lambda e: e.activation(out=sgt[:], in_=pz[:], func=AF.Silu), reads=[pz], writes=[sgt])
                    yield
                    cat = catp.next()
                    k.op("dve", lambda e: e.tensor_tensor(out=cat[:], in0=osbv, in1=sgt[:], op=ALU.mult),
                         reads=[osb, sgt], writes=[cat])
                    yield
                    for fc in range(4):
                        k.op("pe", lambda e: e.transpose(pst[:, fc, :], cat[:, fc * 128:(fc + 1) * 128], identb[:]),
                             reads=[cat, identb], writes=[pst])
                    yield
                    catT = catTp.next()
                    k.op("act", lambda e: e.copy(out=catT[:, 0:4, :], in_=pst[:, 0:4, :]), reads=[pst], writes=[catT])
                    yield
                    n0 = (t - 2) * 128
                    k.dma("sp", catT[:, 4:8, :], attT[:, n0:n0 + 128].rearrange("(c p) n -> p c n", p=128),
                          reads=[d_att], writes=[catT])
                    yield
                    xt = xpool.next()
                    k.dma("sp", xt[:], x2[t * 128:(t + 1) * 128, :], reads=[d_x2[t]], writes=[xt])
                    yield
                    for half in range(2):
                        py = proj.next()
                        for kc in range(8):
                            k.op("pe", lambda e: e.matmul(py[:], lhsT=catT[:, kc, :],
                                                          rhs=wout_g[:, kc, half * 512:(half + 1) * 512],
                                                          start=(kc == 0), stop=(kc == 7)), reads=[catT, wout_g],
                                 writes=[py])
                            yield
                        k.op("dve", lambda e: e.tensor_tensor(out=xt[:, half * 512:(half + 1) * 512], in0=py[:],
                                                              in1=xt[:, half * 512:(half + 1) * 512], op=ALU.add),
                             reads=[py, xt], writes=[xt])
                        yield
                    k.dma("pool", x3[t * 128:(t + 1) * 128, :], xt[:], reads=[xt], writes=[d_x3[t]])
                    yield

                def interleave(gens):
                    alive = list(gens)
                    while alive:
                        for g in list(alive):
                            try:
                                next(g)
                            except StopIteration:
                                alive.remove(g)

                ctxs = [dict() for _ in order]
                interleave([tile_A(order[0], ctxs[0])])
                for i_, t in enumerate(order):
                    gens = [tile_B(t, ctxs[i_])]
                    if i_ + 1 < len(order):
                        gens.append(tile_A(order[i_ + 1], ctxs[i_ + 1]))
                    interleave(gens)
                    ctxs[i_] = None
            k.barrier()

        def attention():
            with ExitStack() as es:
                ktp = k.sbpool("aKT", [96, T], BF16, 2, es)
                vxp = k.sbpool("aVX", [128, NT, 80], BF16, 2, es)
                qp = k.sbpool("aQ", [96, 512], BF16, 2, es)
                ptp_ = k.sbpool("aP", [128, 1024], BF16, 3, es)
                rcp = k.sbpool("arc", [65, 512], F32, 2, es)
                otp = k.sbpool("aot", [64, 512], BF16, 2, es)
                nb10 = k.sb("nb10", [128, 1], F32, es)
                k.op("pool", lambda e: e.memset(nb10[:], -10.0), writes=[nb10])
                pss = k.pspool("aS", [128, 1024], F32, 2, es)
                pso = k.pspool("aO", [65, 512], F32, 2, es)
                psb = k.pspool("aB", [64, 512], F32, 1, es)
                QB = min(512, L)
                for h in range(8):
                    kt_ = ktp.next()
                    k.dma("sp", kt_[:], KT[h], reads=[d_KT], writes=[kt_])
                    vx = vxp.next()
                    k.dma("sp", vx[:], VX[h].rearrange("t p c -> p t c"), reads=[d_VX], writes=[vx])
                    for q0 in range(0, L, QB):
                        qt = qp.next()
                        k.dma("sp", qt[:, 0:QB], QT[h, :, q0:q0 + QB], reads=[d_QT], writes=[qt])
                        po = pso.next()
                        NP = (NT + 1) // 2
                        psq = {}
                        for kp in range(NP + 1):
                            if kp < NP:
                                ps_ = pss.next()
                                for j in range(2):
                                    kt = kp * 2 + j
                                    if kt < NT:
                                        k.op("pe", lambda e: e.matmul(ps_[:, j * QB:(j + 1) * QB],
                                                                      lhsT=kt_[:, kt * 128:(kt + 1) * 128],
                                                                      rhs=qt[:, 0:QB], start=True, stop=True),
                                             reads=[kt_, qt], writes=[ps_])
                                psq[kp] = ps_
                            k2 = kp - 1
                            if k2 >= 0:
                                ps2 = psq.pop(k2)
                                nj = 2 if k2 * 2 + 1 < NT else 1
                                pt = ptp_.next()
                                k.op("act", lambda e: e.activation(out=pt[:, 0:nj * QB], in_=ps2[:, 0:nj * QB],
                                                                   func=AF.Exp, bias=nb10[:, 0:1]),
                                     reads=[ps2, nb10], writes=[pt])
                                for j in range(nj):
                                    kt = k2 * 2 + j
                                    k.op("pe", lambda e: e.matmul(po[:, 0:QB], lhsT=vx[:, kt, 0:65],
                                                                  rhs=pt[:, j * QB:(j + 1) * QB], start=(kt == 0),
                                                                  stop=(kt == NT - 1)), reads=[vx, pt], writes=[po])
                        rc = rcp.next()
                        k.op("dve", lambda e: e.reciprocal(out=rc[64:65, 0:QB], in_=po[64:65, 0:QB]), reads=[po],
                             writes=[rc])
                        pb_ = psb.next()
                        k.op("pe", lambda e: e.matmul(pb_[:, 0:QB], lhsT=ones[64:65, 0:64], rhs=rc[64:65, 0:QB],
                                                      start=True, stop=True), reads=[ones, rc], writes=[pb_])
                        k.op("act", lambda e: e.copy(out=rc[0:64, 0:QB], in_=pb_[:, 0:QB]), reads=[pb_], writes=[rc])
                        ot = otp.next()
                        k.op("dve", lambda e: e.tensor_tensor(out=ot[:, 0:QB], in0=po[0:64, 0:QB], in1=rc[0:64, 0:QB],
                                                              op=ALU.mult), reads=[po, rc], writes=[ot])
                        k.dma("pool", attT[h * 64:(h + 1) * 64, q0:q0 + QB], ot[:, 0:QB], reads=[ot], writes=[d_att])
            k.barrier()

        odd_pass(0)
        if stop_after == "odd0":
            k.drain_all()
            return nc
        attention()
        if stop_after == "att":
            k.drain_all()
            return nc
        odd_pass(1)
        d_y = Buf(None, "y")
        if stop_after == "mix1":
            k.drain_all()
            return nc
        moe(1, x3, d_x3, lambda t: (y_out[(t - 2) * 128:(t - 1) * 128, :], d_y), list(range(2, NT)))
        k.drain_all()
    return nc


_W_NAMES = ["c_ctx", "ada_w", "ada_b", "norm_mix_w", "norm_ffn_w", "even_w_in", "even_w_out", "gmlp_norm_w", "gmlp_ws",
            "gmlp_bs", "hgrn_lb_logits", "hgrn_norm_w", "odd_w_in", "odd_w_out", "gdn_conv_w", "gdn_a_log",
            "gdn_dt_bias", "gdn_norm_w", "mla_q_norm_w", "mla_wq_up", "mla_kv_norm_w", "mla_wkv_up", "mla_qk_norm_q",
            "mla_qk_norm_k", "router_group_w", "router_group_b", "router_expert_w", "router_expert_b", "moe_w_gate",
            "moe_w_up", "moe_w_down"]


def rope_tables(L):
    n = np.arange(L)
    row = (n // 64).astype(np.float32)
    col = (n % 64).astype(np.float32)
    freqs = (np.float32(10000.0) ** (-np.arange(8, dtype=np.float32) / np.float32(8))).astype(np.float32)
    ang = np.concatenate([row[:, None] * freqs[None, :], col[:, None] * freqs[None, :]], axis=1).astype(np.float32)
    return np.cos(ang).astype(np.float32), np.sin(ang).astype(np.float32)


def make_in_maps(inputs, ncores, L):
    cos, sin = rope_tables(L)
    shared = {n: np.ascontiguousarray(np.asarray(inputs[n], dtype=np.float32)) for n in _W_NAMES}
    shared["rope_cos"] = cos
    shared["rope_sin"] = sin
    maps = []
    for b in range(ncores):
        m = dict(shared)
        m["x"] = np.ascontiguousarray(np.asarray(inputs["x"][b], dtype=np.float32))
        m["ctx"] = np.ascontiguousarray(np.asarray(inputs["ctx"][b], dtype=np.float32))
        m["c"] = np.ascontiguousarray(np.asarray(inputs["c"][b], dtype=np.float32))
        maps.append(m)
    return maps


def kernel(**inputs):
    x = np.asarray(inputs["x"])
    B, L, _ = x.shape
    nc = build(L)
    maps = make_in_maps(inputs, B, L)
    res = run_bass_kernel_spmd(nc, maps, core_ids=list(range(B)))
    return np.stack([np.asarray(r["y"]) for r in res.results], axis=0).astype(np.float32)
```
